# Optimizing a Trainium2 kernel written in Bass

```python
import math
import jax
import jax.numpy as jnp
from jax import lax
import numpy as np

D_MODEL = 1024
BATCH = 2
SEQ = 16384
DEPTH = 2

GRID_W = 64
CTX_LEN = 256
N_MIX_GROUPS = 4
D_GROUP = D_MODEL // N_MIX_GROUPS
D_MIX = N_MIX_GROUPS * D_GROUP
A_HEADS = 4
A_QK = 32
A_V = D_GROUP // A_HEADS
Q_BLOCK = 128
ROPE_BASE = 10000.0
B_HEADS = 4
B_DIM = D_GROUP // B_HEADS
WIN_R = 8
WIN_C = 16
POOL_SIZES = (2, 4, 8, 16)
POOL_CH = D_GROUP // len(POOL_SIZES)
HY_CH = D_GROUP
HY_ORDER = 2
HY_SHORT = 3
HY_BANDS = 16
HY_EMB = 1 + 2 * HY_BANDS
HY_HIDDEN = 64
HY_SIN_FREQ = 1.0
HY_MIN_DECAY = math.log(1e-2) / 1.5
HY_MAX_DECAY = math.log(1e-2) / 0.3
D_IN = 2 * A_HEADS * 2 * A_QK + A_HEADS * A_V + 3 * B_HEADS * B_DIM + D_GROUP + (HY_ORDER + 1) * HY_CH
N_EXPERTS = 16
N_EXPERT_GROUPS = 4
TOPK_GROUPS = 1
TOP_K = 2
D_EXPERT = 512
EPS = 1e-6

kernel_name = 'hybrid_diffusion_parallel_heads_moe'

F32 = jnp.float32


def rmsnorm(x, g):
    xf = x.astype(F32)
    return (xf * lax.rsqrt(jnp.mean(xf * xf, axis=-1, keepdims=True) + EPS)).astype(x.dtype) * g


def split_cols(u):
    sizes = (A_HEADS * 2 * A_QK, A_HEADS * 2 * A_QK, A_HEADS * A_V,
             B_HEADS * B_DIM, B_HEADS * B_DIM, B_HEADS * B_DIM,
             D_GROUP, (HY_ORDER + 1) * HY_CH)
    offs, acc = [], 0
    for s in sizes[:-1]:
        acc += s
        offs.append(acc)
    return jnp.split(u, offs, axis=-1)


def axial_rope(x):
    L, dim = x.shape[1], x.shape[-1]
    n_freq = dim // 4
    inv = ROPE_BASE ** (-jnp.arange(n_freq, dtype=F32) / n_freq)
    t = jnp.arange(L)
    row = (t // GRID_W).astype(F32)
    col = (t % GRID_W).astype(F32)
    ang = jnp.concatenate([row[:, None] * inv, col[:, None] * inv], axis=-1)
    shape = (1, L) + (1,) * (x.ndim - 3) + (dim // 2,)
    cos, sin = jnp.cos(ang).reshape(shape), jnp.sin(ang).reshape(shape)
    x1, x2 = x[..., : dim // 2].astype(F32), x[..., dim // 2:].astype(F32)
    return jnp.concatenate([x1 * cos - x2 * sin, x2 * cos + x1 * sin], axis=-1).astype(x.dtype)


def diff_attention(uq_c, uk_c, uv_c, uq_l, uk_l, uv_l, lam_vecs, subln_g, li, need_ctx):
    B, S = uq_l.shape[:2]
    C = uq_c.shape[1]
    lam_init = 0.8 - 0.6 * math.exp(-0.3 * li)
    lv = lam_vecs.astype(F32)
    lam = jnp.exp(jnp.sum(lv[0] * lv[1])) - jnp.exp(jnp.sum(lv[2] * lv[3])) + lam_init
    qc = uq_c.reshape(B, C, A_HEADS, 2, A_QK)
    kc = uk_c.reshape(B, C, A_HEADS, 2, A_QK)
    vc = uv_c.reshape(B, C, A_HEADS, A_V)
    ql = axial_rope(uq_l.reshape(B, S, A_HEADS, 2, A_QK))
    kl = axial_rope(uk_l.reshape(B, S, A_HEADS, 2, A_QK))
    vl = uv_l.reshape(B, S, A_HEADS, A_V)
    k_all = jnp.concatenate([kc, kl], axis=1)
    v_all = jnp.concatenate([vc, vl], axis=1)

    def attend(q, k, v):
        s = jnp.einsum('bqhcd,bkhcd->bhcqk', q, k, preferred_element_type=F32) * (A_QK ** -0.5)
        p = jax.nn.softmax(s, axis=-1)
        a = p[:, :, 0] - lam * p[:, :, 1]
        return jnp.einsum('bhqk,bkhd->bqhd', a.astype(v.dtype), v)

    def post(o):
        o = rmsnorm(o, subln_g) * (1.0 - lam_init)
        return o.reshape(o.shape[0], o.shape[1], A_HEADS * A_V)

    nb = S // Q_BLOCK
    qb = jnp.moveaxis(ql.reshape(B, nb, Q_BLOCK, A_HEADS, 2, A_QK), 1, 0)
    ol = lax.map(lambda qblk: attend(qblk, k_all, v_all), qb)
    ol = jnp.moveaxis(ol, 0, 1).reshape(B, S, A_HEADS, A_V)
    out_c = post(attend(qc, kc, vc)) if need_ctx else None
    return out_c, post(ol)


def neighbourhood_attention(uq_c, uk_c, uv_c, uq_l, uk_l, uv_l, rpb, need_ctx):
    B, S = uq_l.shape[:2]
    C = uq_c.shape[1]
    R = S // GRID_W
    wr = min(WIN_R, R)
    scale = B_DIM ** -0.5
    qc = uq_c.reshape(B, C, B_HEADS, B_DIM)
    kc = uk_c.reshape(B, C, B_HEADS, B_DIM)
    vc = uv_c.reshape(B, C, B_HEADS, B_DIM)
    qg = uq_l.reshape(B, R, GRID_W, B_HEADS, B_DIM)
    kg = uk_l.reshape(B, R, GRID_W, B_HEADS, B_DIM)
    vg = uv_l.reshape(B, R, GRID_W, B_HEADS, B_DIM)
    rows = jnp.arange(R)
    r0 = jnp.clip(rows - wr // 2, 0, R - wr)
    row_idx = r0[:, None] + jnp.arange(wr)[None, :]
    k_rows = kg[:, row_idx]
    v_rows = vg[:, row_idx]
    cols = jnp.arange(GRID_W)
    c0 = jnp.clip(cols - WIN_C // 2, 0, GRID_W - WIN_C)
    in_win = (cols[None, :] >= c0[:, None]) & (cols[None, :] < c0[:, None] + WIN_C)
    dr = row_idx - rows[:, None]
    dc = jnp.clip(cols[None, :] - cols[:, None], -(WIN_C - 1), WIN_C - 1)
    bias = rpb.astype(F32)[:, dr[:, None, :, None] + (WIN_R - 1), dc[None, :, None, :] + (WIN_C - 1)]
    bias = jnp.where(in_win[:, None, :], bias, -jnp.inf)
    bias = jnp.transpose(bias, (1, 0, 2, 3, 4))[None]
    s_win = jnp.einsum('brqhd,brikhd->brhqik', qg, k_rows, preferred_element_type=F32) * scale + bias
    s_win = s_win.reshape(B, R, B_HEADS, GRID_W, wr * GRID_W)
    s_ctx = jnp.einsum('brqhd,bkhd->brhqk', qg, kc, preferred_element_type=F32) * scale
    p = jax.nn.softmax(jnp.concatenate([s_win, s_ctx], axis=-1), axis=-1)
    p_win = p[..., : wr * GRID_W].reshape(B, R, B_HEADS, GRID_W, wr, GRID_W).astype(vg.dtype)
    p_ctx = p[..., wr * GRID_W:].astype(vg.dtype)
    ol = jnp.einsum('brhqik,brikhd->brqhd', p_win, v_rows) + jnp.einsum('brhqk,bkhd->brqhd', p_ctx, vc)
    ol = ol.reshape(B, S, B_HEADS * B_DIM)
    out_c = None
    if need_ctx:
        pc = jax.nn.softmax(jnp.einsum('bqhd,bkhd->bhqk', qc, kc, preferred_element_type=F32) * scale, axis=-1)
        out_c = jnp.einsum('bhqk,bkhd->bqhd', pc.astype(vc.dtype), vc).reshape(B, C, B_HEADS * B_DIM)
    return out_c, ol


def pool_mix(u, w_pool, pool_scale):
    B, L, _ = u.shape
    uf = u.astype(F32)
    cs = jnp.concatenate([jnp.zeros((B, 1, D_GROUP), F32), lax.cumsum(uf, axis=1)], axis=1)
    t = jnp.arange(L)
    outs = []
    for g, w in enumerate(POOL_SIZES):
        lo = jnp.clip(t - w // 2, 0, L)
        hi = jnp.clip(t + w // 2, 0, L)
        sl = slice(g * POOL_CH, (g + 1) * POOL_CH)
        mean = (cs[:, hi, sl] - cs[:, lo, sl]) / (hi - lo).astype(F32)[None, :, None]
        outs.append((mean - uf[..., sl]).astype(u.dtype) @ w_pool[g])
    return jnp.concatenate(outs, axis=-1) * pool_scale


def hyena_filters(L, w1, b1, w2, b2, w3, b3):
    t = jnp.arange(L, dtype=F32)
    t_norm = t / max(L - 1, 1)
    bands = jnp.linspace(1e-4, HY_BANDS - 1, HY_BANDS, dtype=F32)
    ang = (2.0 * math.pi / L) * t[:, None] * bands[None, :]
    z = jnp.concatenate([t_norm[:, None], jnp.cos(ang), jnp.sin(ang)], axis=-1)
    h = jnp.sin(HY_SIN_FREQ * (z @ w1.astype(F32) + b1.astype(F32)))
    h = jnp.sin(HY_SIN_FREQ * (h @ w2.astype(F32) + b2.astype(F32)))
    h = (h @ w3.astype(F32) + b3.astype(F32)).reshape(L, HY_ORDER, 2, HY_CH)
    deltas = jnp.abs(jnp.linspace(HY_MIN_DECAY, HY_MAX_DECAY, HY_CH, dtype=F32))
    h = h * jnp.exp(-t_norm[:, None, None, None] * deltas)
    return h * lax.rsqrt(jnp.sum(h * h, axis=(0, 2), keepdims=True) + EPS)


def bidir_long_conv(u, h_fwd, h_bwd, d_skip):
    L = u.shape[1]
    k = jnp.concatenate([h_fwd, jnp.zeros_like(h_fwd[:1]), h_bwd[:0:-1]], axis=0)
    uf = u.astype(F32)
    y = jnp.fft.irfft(jnp.fft.rfft(uf, n=2 * L, axis=1) * jnp.fft.rfft(k, axis=0)[None], n=2 * L, axis=1)[:, :L]
    return (y + uf * d_skip.astype(F32)).astype(u.dtype)


def hyena_mix(u, w_short, b_short, filt, d_skip):
    up = jnp.pad(u, ((0, 0), (1, 1), (0, 0)))
    u = up[:, :-2] * w_short[0] + up[:, 1:-1] * w_short[1] + up[:, 2:] * w_short[2] + b_short
    x1, x2, v = jnp.split(u, 3, axis=-1)
    z = x1 * bidir_long_conv(v, filt[:, 0, 0], filt[:, 0, 1], d_skip[0])
    return x2 * bidir_long_conv(z, filt[:, 1, 0], filt[:, 1, 1], d_skip[1])


def moe(x, router_w, router_b, w1, w3, w2):
    N = x.shape[0]
    per_group = N_EXPERTS // N_EXPERT_GROUPS
    scores = jax.nn.softmax((x @ router_w).astype(F32), axis=-1)
    sel = scores + router_b.astype(F32)
    grp_score = jnp.sum(lax.top_k(sel.reshape(N, N_EXPERT_GROUPS, per_group), TOP_K)[0], axis=-1)
    _, gidx = lax.top_k(grp_score, TOPK_GROUPS)
    gmask = jnp.sum(jax.nn.one_hot(gidx, N_EXPERT_GROUPS, dtype=F32), axis=1)
    emask = jnp.repeat(gmask, per_group, axis=1) > 0
    _, eidx = lax.top_k(jnp.where(emask, sel, -jnp.inf), TOP_K)
    w = jnp.take_along_axis(scores, eidx, axis=1)
    w = w / jnp.sum(w, axis=-1, keepdims=True)
    gates = jnp.sum(jax.nn.one_hot(eidx, N_EXPERTS, dtype=F32) * w[..., None], axis=1).astype(x.dtype)
    out = jnp.zeros_like(x)
    for e in range(N_EXPERTS):
        h = jax.nn.silu(x @ w1[e]) * (x @ w3[e])
        out = out + gates[:, e:e + 1] * (h @ w2[e])
    return out


def trunk_layer(xc, xl, c, c_ctx, lp, router_w, router_b, li, need_ctx):
    B, S, D = xl.shape
    C = xc.shape[1]
    mod_l = jnp.split(jax.nn.silu(c) @ lp['ada_w'] + lp['ada_b'], 6, axis=-1)
    mod_c = jnp.split(jax.nn.silu(c_ctx) @ lp['ada_w'] + lp['ada_b'], 6, axis=-1)
    sh1_l, sc1_l, g1_l, sh2_l, sc2_l, g2_l = [m[:, None, :] for m in mod_l]
    sh1_c, sc1_c, g1_c, sh2_c, sc2_c, g2_c = mod_c
    hl = rmsnorm(xl, lp['norm1_g']) * (1.0 + sc1_l) + sh1_l
    hc = rmsnorm(xc, lp['norm1_g']) * (1.0 + sc1_c) + sh1_c
    aq_l, ak_l, av_l, bq_l, bk_l, bv_l, pool_l, hy_l = split_cols(hl @ lp['w_in'])
    aq_c, ak_c, av_c, bq_c, bk_c, bv_c, pool_c, hy_c = split_cols(hc @ lp['w_in'])
    a_c, a_l = diff_attention(aq_c, ak_c, av_c, aq_l, ak_l, av_l, lp['a_lambda'], lp['a_subln_g'], li, need_ctx)
    b_c, b_l = neighbourhood_attention(bq_c, bk_c, bv_c, bq_l, bk_l, bv_l, lp['b_rpb'], need_ctx)
    p_l = pool_mix(pool_l, lp['pool_w'], lp['pool_scale'])
    filt_l = hyena_filters(S, lp['hy_f_w1'], lp['hy_f_b1'], lp['hy_f_w2'], lp['hy_f_b2'], lp['hy_f_w3'], lp['hy_f_b3'])
    h_l = hyena_mix(hy_l, lp['hy_short_w'], lp['hy_short_b'], filt_l, lp['hy_skip'])
    xl = xl + g1_l * (jnp.concatenate([a_l, b_l, p_l, h_l], axis=-1) @ lp['w_out'])
    if need_ctx:
        p_c = pool_mix(pool_c, lp['pool_w'], lp['pool_scale'])
        filt_c = hyena_filters(C, lp['hy_f_w1'], lp['hy_f_b1'], lp['hy_f_w2'], lp['hy_f_b2'], lp['hy_f_w3'], lp['hy_f_b3'])
        h_c = hyena_mix(hy_c, lp['hy_short_w'], lp['hy_short_b'], filt_c, lp['hy_skip'])
        xc = xc + g1_c * (jnp.concatenate([a_c, b_c, p_c, h_c], axis=-1) @ lp['w_out'])
    hl2 = rmsnorm(xl, lp['norm2_g']) * (1.0 + sc2_l) + sh2_l
    if need_ctx:
        hc2 = rmsnorm(xc, lp['norm2_g']) * (1.0 + sc2_c) + sh2_c
        tok = jnp.concatenate([hc2, hl2], axis=1).reshape(B * (C + S), D)
        y = moe(tok, router_w, router_b, lp['moe_w1'], lp['moe_w3'], lp['moe_w2']).reshape(B, C + S, D)
        xc = xc + g2_c * y[:, :C]
        xl = xl + g2_l * y[:, C:]
    else:
        y = moe(hl2.reshape(B * S, D), router_w, router_b, lp['moe_w1'], lp['moe_w3'], lp['moe_w2'])
        xl = xl + g2_l * y.reshape(B, S, D)
    return xc, xl


def setup_inputs(seed: int = 0) -> dict:
    key = jax.random.key(seed)
    ks = iter(jax.random.split(key, 40))

    def nrm(shape, s):
        return s * jax.random.normal(next(ks), shape, F32)

    return {
        'x': nrm((BATCH, SEQ, D_MODEL), 1.0),
        'c': nrm((BATCH, D_MODEL), 1.0),
        'ctx': nrm((BATCH, CTX_LEN, D_MODEL), 1.0),
        'c_ctx': nrm((D_MODEL,), 1.0),
        'norm1_g': 1.0 + nrm((DEPTH, D_MODEL), 0.1),
        'norm2_g': 1.0 + nrm((DEPTH, D_MODEL), 0.1),
        'ada_w': nrm((DEPTH, D_MODEL, 6 * D_MODEL), 0.5 * D_MODEL ** -0.5),
        'ada_b': nrm((DEPTH, 6 * D_MODEL), 0.02),
        'w_in': nrm((DEPTH, D_MODEL, D_IN), D_MODEL ** -0.5),
        'w_out': nrm((DEPTH, D_MIX, D_MODEL), D_MIX ** -0.5),
        'a_lambda': nrm((DEPTH, 4, A_QK), 0.1),
        'a_subln_g': 1.0 + nrm((DEPTH, A_V), 0.1),
        'b_rpb': nrm((DEPTH, B_HEADS, 2 * WIN_R - 1, 2 * WIN_C - 1), 0.1),
        'pool_w': nrm((DEPTH, len(POOL_SIZES), POOL_CH, POOL_CH), POOL_CH ** -0.5),
        'pool_scale': 1.0 + nrm((DEPTH, D_GROUP), 0.1),
        'hy_short_w': nrm((DEPTH, HY_SHORT, (HY_ORDER + 1) * HY_CH), 0.5),
        'hy_short_b': nrm((DEPTH, (HY_ORDER + 1) * HY_CH), 0.02),
        'hy_f_w1': nrm((DEPTH, HY_EMB, HY_HIDDEN), HY_EMB ** -0.5),
        'hy_f_b1': nrm((DEPTH, HY_HIDDEN), 0.1),
        'hy_f_w2': nrm((DEPTH, HY_HIDDEN, HY_HIDDEN), HY_HIDDEN ** -0.5),
        'hy_f_b2': nrm((DEPTH, HY_HIDDEN), 0.1),
        'hy_f_w3': nrm((DEPTH, HY_HIDDEN, HY_ORDER * 2 * HY_CH), HY_HIDDEN ** -0.5),
        'hy_f_b3': nrm((DEPTH, HY_ORDER * 2 * HY_CH), 0.1),
        'hy_skip': nrm((DEPTH, HY_ORDER, HY_CH), 0.5),
        'router_w': nrm((D_MODEL, N_EXPERTS), D_MODEL ** -0.5),
        'router_b': nrm((N_EXPERTS,), 0.01),
        'moe_w1': nrm((DEPTH, N_EXPERTS, D_MODEL, D_EXPERT), D_MODEL ** -0.5),
        'moe_w3': nrm((DEPTH, N_EXPERTS, D_MODEL, D_EXPERT), D_MODEL ** -0.5),
        'moe_w2': nrm((DEPTH, N_EXPERTS, D_EXPERT, D_MODEL), D_EXPERT ** -0.5),
        'final_g': 1.0 + nrm((D_MODEL,), 0.1),
    }


def reference(x, c, ctx, c_ctx, norm1_g, norm2_g, ada_w, ada_b, w_in, w_out, a_lambda, a_subln_g,
              b_rpb, pool_w, pool_scale, hy_short_w, hy_short_b, hy_f_w1, hy_f_b1, hy_f_w2, hy_f_b2,
              hy_f_w3, hy_f_b3, hy_skip, router_w, router_b, moe_w1, moe_w3, moe_w2, final_g):
    xl, xc = x, ctx
    for li in range(DEPTH):
        lp = {
            'norm1_g': norm1_g[li], 'norm2_g': norm2_g[li], 'ada_w': ada_w[li], 'ada_b': ada_b[li],
            'w_in': w_in[li], 'w_out': w_out[li], 'a_lambda': a_lambda[li], 'a_subln_g': a_subln_g[li],
            'b_rpb': b_rpb[li], 'pool_w': pool_w[li], 'pool_scale': pool_scale[li],
            'hy_short_w': hy_short_w[li], 'hy_short_b': hy_short_b[li],
            'hy_f_w1': hy_f_w1[li], 'hy_f_b1': hy_f_b1[li], 'hy_f_w2': hy_f_w2[li], 'hy_f_b2': hy_f_b2[li],
            'hy_f_w3': hy_f_w3[li], 'hy_f_b3': hy_f_b3[li], 'hy_skip': hy_skip[li],
            'moe_w1': moe_w1[li], 'moe_w3': moe_w3[li], 'moe_w2': moe_w2[li],
        }
        xc, xl = trunk_layer(xc, xl, c, c_ctx, lp, router_w, router_b, li, need_ctx=(li < DEPTH - 1))
    return rmsnorm(xl, final_g)
```

```python
import math
from contextlib import ExitStack
import numpy as np
import concourse.bass as bass
import concourse.mybir as mybir
from concourse.bass_utils import run_bass_kernel_spmd

F32 = mybir.dt.float32
BF16 = mybir.dt.bfloat16
AF = mybir.ActivationFunctionType
ALU = mybir.AluOpType
AX = mybir.AxisListType

D = 1024
SEQ = 16384
NB = 2
CTX = 256
DIN = 2560
NCORE = 8
TPC = SEQ // 4
NAGC = 65
RSR = 208
CPC = CTX // 4
EPS = 1e-6

COMPUTE = ("pe", "dve", "act", "pool")
QUEUES = ("sp", "actq", "poolq")
ISSUER = {"sp": "sp", "actq": "act", "poolq": "pool"}
NDMASEM = 6


class Sched:
    def __init__(self, nc, same_engine_sync=True):
        self.nc = nc
        self.same = same_engine_sync
        self.streams = {e: [] for e in ("pe", "dve", "act", "pool", "sp")}
        self.cnt = {e: 0 for e in COMPUTE}
        self.sem = {}
        self.dsem = {}
        self.dnext = {q: 0 for q in QUEUES}
        self.seen = {}
        self.writer = {}
        self.readers = {}

    def alloc_sems(self, stack):
        for e in COMPUTE:
            self.sem[e] = stack.enter_context(self.nc.semaphore("s_" + e))
        for q in QUEUES:
            for i in range(NDMASEM):
                self.dsem[(q, i)] = [stack.enter_context(self.nc.semaphore(f"d_{q}{i}")), 0]

    def _deps(self, reads, writes):
        deps = []
        for b in reads:
            if b in self.writer:
                deps.append(self.writer[b])
        for b in writes:
            if b in self.writer:
                deps.append(self.writer[b])
            deps.extend(self.readers.get(b, ()))
        return deps

    def _record(self, tok, reads, writes):
        for b in reads:
            self.readers.setdefault(b, []).append(tok)
        for b in writes:
            self.writer[b] = tok
            self.readers[b] = []

    def _waits(self, stream, deps, eng_name):
        ws = []
        for (sk, val, en) in deps:
            if en == eng_name and (eng_name == "pe" or not self.same):
                continue
            key = (stream, sk)
            if self.seen.get(key, 0) >= val:
                continue
            self.seen[key] = val
            ws.append((sk, val))
        return ws

    def _semobj(self, sk):
        return self.sem[sk] if sk in self.sem else self.dsem[sk][0]

    def op(self, eng, fn, reads=(), writes=()):
        deps = self._deps(reads, writes)
        ws = self._waits(eng, deps, eng)
        self.cnt[eng] += 1
        tok = (eng, self.cnt[eng], eng)
        self.streams[eng].append((ws, fn, (eng, 1)))
        self._record(tok, reads, writes)
        return tok

    def dma(self, q, out, in_, reads=(), writes=(), **kw):
        stream = ISSUER[q]
        deps = self._deps(reads, writes)
        i = self.dnext[q]
        self.dnext[q] = (i + 1) % NDMASEM
        ent = self.dsem[(q, i)]
        if ent[1] > 0:
            deps = deps + [((q, i), ent[1], "dma")]
        ws = self._waits(stream, deps, "dma?")
        ent[1] += 16
        tok = ((q, i), ent[1], "dma")

        def fn(e, out=out, in_=in_, kw=kw):
            return e.dma_start(out=out, in_=in_, **kw)
        self.streams[stream].append((ws, fn, ((q, i), 16)))
        self._record(tok, reads, writes)
        return tok

    def _all_tokens(self):
        toks = [(e, self.cnt[e], "x") for e in COMPUTE if self.cnt[e] > 0]
        for k, ent in self.dsem.items():
            if ent[1] > 0:
                toks.append((k, ent[1], "dma"))
        return toks

    def barrier(self):
        toks = self._all_tokens()
        for s in self.streams:
            ws = self._waits(s, toks, "none")
            if ws:
                self.streams[s].append((ws, None, None))
        self.writer.clear()
        self.readers.clear()

    def emit(self):
        ws = [(sk, val) for (sk, val, _) in self._all_tokens()]
        self.streams["sp"].append((ws, None, None))
        nc = self.nc
        with nc.Block() as block:
            def mk(sname):
                def body(e):
                    for (ws, fn, inc) in self.streams[sname]:
                        for (sk, val) in ws:
                            e.wait_ge(self._semobj(sk), val)
                        if fn is not None:
                            fn(e).then_inc(self._semobj(inc[0]), inc[1])
                return body
            block.tensor(mk("pe"))
            block.vector(mk("dve"))
            block.scalar(mk("act"))
            block.gpsimd(mk("pool"))
            block.sync(mk("sp"))


class Ctx:
    def __init__(self, name="k", nc=None):
        self.nc = nc or bass.Bass("TRN2", target_bir_lowering=False)
        self.root = ExitStack()
        self.st = ExitStack()
        self.S = Sched(self.nc)
        self.S.alloc_sems(self.root)
        self.ins = {}
        self.outs = {}
        self.over = {}
        self.prefix = ""
        self.shared = set()
        self.PS = [self.root.enter_context(self.nc.psum_tensor(f"psb{i}", [128, 512], F32)) for i in range(8)]
        self._n = 0
        self._ncc = 0

    def inp(self, name, shape, dt=F32):
        if name in self.over:
            return self.over[name]
        full = name if (name in self.shared or name.startswith("c_")) else self.prefix + name
        if full in self.ins:
            return self.ins[full]
        t = self.nc.dram_tensor(full, list(shape), dt, kind="ExternalInput").ap()
        self.ins[full] = t
        return t

    def out(self, name, shape, dt=F32):
        if name in self.over:
            return self.over[name]
        t = self.nc.dram_tensor(self.prefix + name, list(shape), dt, kind="ExternalOutput").ap()
        self.outs[self.prefix + name] = t
        return t

    def scratch(self, name, shape, dt=F32):
        self._n += 1
        return self.nc.dram_tensor(f"scr{self._n}_{name}", list(shape), dt, kind="Internal").ap()

    def sb(self, name, shape, dt=F32, stack=None):
        self._n += 1
        return (stack or self.st).enter_context(self.nc.sbuf_tensor(f"sb{self._n}_{name}", list(shape), dt))

    def end_phase(self):
        self.S.barrier()
        self.st.close()
        self.st = ExitStack()

    def collective(self, kind, op, groups, pairs):
        S = self.S
        S.barrier()
        sem = self.root.enter_context(self.nc.semaphore(f"cc{self._ncc}"))
        key = ("cc", self._ncc)
        self._ncc += 1
        S.dsem[key] = [sem, len(pairs)]
        for (src, dst) in pairs:
            S.streams["pool"].append(([], lambda e, src=src, dst=dst: e.collective_compute(kind, op, replica_groups=groups, ins=[src], outs=[dst]), (key, 1)))
        S.barrier()

    def coll_group(self):
        sem = self.root.enter_context(self.nc.semaphore(f"cc{self._ncc}"))
        key = ("cc", self._ncc)
        self._ncc += 1
        self.S.dsem[key] = [sem, 0]
        return key

    def coll(self, key, kind, op, groups, src, dst, reads):
        S = self.S
        ws = S._waits("pool", S._deps(reads, []), "pool-cc")
        S.dsem[key][1] += 1
        S.streams["pool"].append((ws, lambda e: e.collective_compute(kind, op, replica_groups=groups, ins=[src], outs=[dst]), (key, 1)))

    def finish(self):
        self.S.emit()
        self.st.close()
        self.root.close()
        return self.nc


def emit_mod_rows(cx, st, scT, ada_w, ada_b, modrow, tag):
    S = cx.S
    wbuf = [cx.sb(f"adaw{tag}{i}", [128, 8, 512], F32, st) for i in range(2)]
    adab = cx.sb(f"adab{tag}", [2, 6144], F32, st)
    S.dma("sp", adab[0:1, :], ada_b, writes=["adab"])
    S.dma("sp", adab[1:2, :], ada_b, writes=["adab"])
    awv = ada_w.rearrange("(kc p) n -> p kc n", p=128)
    for cb in range(12):
        wb = wbuf[cb % 2]
        q = ("sp", "actq")[cb % 2]
        S.dma(q, wb[:, 0:4, :], awv[:, 0:4, cb * 512:(cb + 1) * 512], writes=[f"adaw{cb%2}a"])
        S.dma(q, wb[:, 4:8, :], awv[:, 4:8, cb * 512:(cb + 1) * 512], writes=[f"adaw{cb%2}b"])
        ps = cx.PS[cb % 2]
        for kc in range(8):
            S.op("pe", lambda e, ps=ps, kc=kc, wb=wb: e.matmul(ps[0:2, :], scT[:, kc, :], wb[:, kc, :],
                                                                start=(kc == 0), stop=(kc == 7)),
                 reads=["scT", f"adaw{cb%2}a", f"adaw{cb%2}b"], writes=[f"ps{cb%2}"])
        S.op("dve", lambda e, ps=ps, cb=cb: e.tensor_tensor(modrow[:, cb * 512:(cb + 1) * 512], ps[0:2, :],
                                                            adab[:, cb * 512:(cb + 1) * 512], ALU.add),
             reads=[f"ps{cb%2}", "adab"], writes=["modrow"])


def emit_bcast_row(cx, dst, row2, sel, which, ps_ids, rkey, wkey):
    S = cx.S
    for hb in range(2):
        ps = cx.PS[ps_ids[hb]]
        S.op("pe", lambda e, ps=ps, hb=hb: e.matmul(ps[:, :], sel[:, which, :], row2[:, hb * 512:(hb + 1) * 512],
                                                    start=True, stop=True),
             reads=[rkey, "sel"], writes=[f"ps{ps_ids[hb]}"])
        S.op("act", lambda e, ps=ps, hb=hb: e.copy(dst[:, hb * 512:(hb + 1) * 512], ps[:, :]),
             reads=[f"ps{ps_ids[hb]}"], writes=[wkey])


def emit_rstd(cx, xt, P, ss, rstd, junk, xkey, slot):
    S = cx.S
    S.op("pool", lambda e: e.memset(ss[0:P, :], 0.0), writes=[f"ss{slot}"])
    S.op("act", lambda e: e.activation(junk[0:P, :], xt[0:P, :], AF.Square, accum_out=ss[0:P, :]),
         reads=[xkey], writes=[f"ss{slot}", "junk"])
    S.op("dve", lambda e: e.tensor_scalar(rstd[0:P, :], ss[0:P, :], 1.0 / D, EPS, ALU.mult, ALU.add),
         reads=[f"ss{slot}"], writes=[f"rstd{slot}"])
    S.op("act", lambda e: e.sqrt(rstd[0:P, :], rstd[0:P, :]), reads=[f"rstd{slot}"], writes=[f"rstd{slot}"])
    S.op("dve", lambda e: e.reciprocal(rstd[0:P, :], rstd[0:P, :]), reads=[f"rstd{slot}"], writes=[f"rstd{slot}"])


def emit_transpose8(cx, hb, P, ident, hT, psb, hkey, tkey, pskey):
    S = cx.S
    psv = psb[:].bitcast(BF16).rearrange("p (k t) -> p k t", t=128)
    for kc in range(8):
        S.op("pe", lambda e, kc=kc: e.transpose(psv[:, kc, 0:P], hb[0:P, kc * 128:(kc + 1) * 128], ident[0:P, 0:P]),
             reads=[hkey, "ident"], writes=[pskey])
    S.op("act", lambda e: e.copy(hT[:, :, 0:P], psv[:, :, 0:P]), reads=[pskey], writes=[tkey])


def emit_A(cx, has_rope=True):
    S = cx.S
    xl = cx.inp("xl", [TPC, D])
    xc = cx.inp("xc", [CPC, D])
    cT = cx.inp("cT", [128, 8, 2])
    ada_w = cx.inp("ada_w", [D, 6 * D])
    ada_b = cx.inp("ada_b", [1, 6 * D])
    norm_g = cx.inp("norm_g", [1, D])
    w_in = cx.inp("w_in", [D, DIN])
    sel_d = cx.inp("sel", [2, 2, 128])
    ident_d = cx.inp("ident", [128, 128], BF16)
    ropec = cx.inp("ropec", [TPC, 16])
    ropes = cx.inp("ropes", [TPC, 16])
    ag_in = cx.inp("ag_in", [NAGC, DIN, 64])
    ag_out = cx.inp("ag_out", [NAGC, 4 * DIN, 64])
    cckey = cx.coll_group()
    identF_d = cx.inp("identF", [128, 128])

    sel = cx.sb("sel", [2, 2, 128])
    ident = cx.sb("ident", [128, 128], BF16)
    identF = cx.sb("identF", [128, 128])
    utT = cx.sb("utT", [128, 20, 128])
    S.dma("sp", identF[:], identF_d, writes=["identF"])
    scT = cx.sb("scT", [128, 8, 2])
    modrow = cx.sb("modrow", [2, 6 * D])
    normg2 = cx.sb("normg2", [2, D])
    grow = cx.sb("grow", [2, D])
    GL = cx.sb("GL", [128, D]); SHL = cx.sb("SHL", [128, D])
    GC = cx.sb("GC", [128, D]); SHC = cx.sb("SHC", [128, D])
    wbf = cx.sb("wbf", [128, 8, DIN], BF16)
    S.dma("sp", sel[:], sel_d, writes=["sel"])
    S.dma("sp", ident[:], ident_d, writes=["ident"])
    S.dma("sp", scT[:], cT, writes=["scT"])
    S.dma("sp", normg2[0:1, :], norm_g, writes=["normg2"])
    S.dma("sp", normg2[1:2, :], norm_g, writes=["normg2"])
    S.op("act", lambda e: e.activation(scT[:], scT[:], AF.Silu), reads=["scT"], writes=["scT"])
    with ExitStack() as st:
        emit_mod_rows(cx, st, scT, ada_w, ada_b, modrow, "A")
        wst = [cx.sb(f"wst{i}", [128, DIN], F32, st) for i in range(2)]
        wv = w_in.rearrange("(kc p) n -> p kc n", p=128)
        for kc in range(8):
            S.dma(("sp", "actq")[kc % 2], wst[kc % 2][:], wv[:, kc, :], writes=[f"wst{kc%2}"])
            eng = ("dve", "pool")[kc % 2]
            S.op(eng, lambda e, kc=kc: e.tensor_copy(wbf[:, kc, :], wst[kc % 2][:]), reads=[f"wst{kc%2}"], writes=["wbf"])
        S.barrier()
    S.op("dve", lambda e: e.scalar_tensor_tensor(grow[:], modrow[:, D:2 * D], 1.0, normg2[:], ALU.add, ALU.mult),
         reads=["modrow", "normg2"], writes=["grow"])
    emit_bcast_row(cx, GL, grow, sel, 0, (0, 1), "grow", "GL")
    emit_bcast_row(cx, SHL, modrow[:, 0:D], sel, 0, (0, 1), "modrow", "SHL")
    emit_bcast_row(cx, GC, grow, sel, 1, (0, 1), "grow", "GC")
    emit_bcast_row(cx, SHC, modrow[:, 0:D], sel, 1, (0, 1), "modrow", "SHC")

    xt = [cx.sb(f"xt{i}", [128, D]) for i in range(2)]
    junk = cx.sb("junk", [128, D], BF16)
    tmp = cx.sb("tmp", [128, D])
    hb = [cx.sb(f"hb{i}", [128, D], BF16) for i in range(2)]
    hT = [cx.sb(f"hT{i}", [128, 8, 128], BF16) for i in range(2)]
    ut = [cx.sb(f"ut{i}", [128, DIN]) for i in range(2)]
    ss = [cx.sb(f"ss{i}", [128, 1]) for i in range(2)]
    rstd = [cx.sb(f"rstd{i}", [128, 1]) for i in range(2)]
    rc = [cx.sb(f"rc{i}", [128, 16]) for i in range(2)]
    rs = [cx.sb(f"rs{i}", [128, 16]) for i in range(2)]
    rt = [cx.sb(f"rt{i}", [128, 16, 16]) for i in range(4)]

    ntl = TPC // 128
    tiles = [("l", i) for i in range(ntl)] + [("c", 0)]

    def load(ti):
        kind, i = tiles[ti]
        s = ti % 2
        if kind == "l":
            S.dma("sp", xt[s][:], xl[i * 128:(i + 1) * 128, :], writes=[f"xt{s}"])
            if has_rope:
                S.dma("sp", rc[s][:], ropec[i * 128:(i + 1) * 128, :], writes=[f"rc{s}"])
                S.dma("sp", rs[s][:], ropes[i * 128:(i + 1) * 128, :], writes=[f"rs{s}"])
        else:
            S.dma("sp", xt[s][0:CPC, :], xc, writes=[f"xt{s}"])

    load(0)
    for ti, (kind, i) in enumerate(tiles):
        s = ti % 2
        P = 128 if kind == "l" else CPC
        G, SH = (GL, SHL) if kind == "l" else (GC, SHC)
        if ti + 1 < len(tiles):
            load(ti + 1)
        emit_rstd(cx, xt[s], P, ss[s], rstd[s], junk, f"xt{s}", s)
        S.op("dve", lambda e, s=s, P=P, G=G: e.scalar_tensor_tensor(tmp[0:P, :], xt[s][0:P, :], rstd[s][0:P, :], G[0:P, :],
                                                                     ALU.mult, ALU.mult),
             reads=[f"xt{s}", f"rstd{s}", "GL", "GC"], writes=["tmp"])
        S.op("dve", lambda e, s=s, P=P, SH=SH: e.tensor_tensor(hb[s][0:P, :], tmp[0:P, :], SH[0:P, :], ALU.add),
             reads=["tmp", "SHL", "SHC"], writes=[f"hb{s}"])
        emit_transpose8(cx, hb[s], P, ident, hT[s], cx.PS[2], f"hb{s}", f"hT{s}", "ps2")
        for cb in range(5):
            ps = cx.PS[3 + cb]
            for kc in range(8):
                S.op("pe", lambda e, ps=ps, kc=kc, cb=cb, s=s, P=P: e.matmul(
                    ps[0:P, :], hT[s][:, kc, 0:P], wbf[:, kc, cb * 512:(cb + 1) * 512], start=(kc == 0), stop=(kc == 7)),
                    reads=[f"hT{s}", "wbf"], writes=[f"ps{3+cb}"])
            if cb % 2 == 0:
                S.op("act", lambda e, ps=ps, cb=cb, s=s, P=P: e.copy(ut[s][0:P, cb * 512:(cb + 1) * 512], ps[0:P, :]),
                     reads=[f"ps{3+cb}"], writes=[f"ut{s}c{cb}"])
            else:
                S.op("dve", lambda e, ps=ps, cb=cb, s=s, P=P: e.tensor_copy(ut[s][0:P, cb * 512:(cb + 1) * 512], ps[0:P, :]),
                     reads=[f"ps{3+cb}"], writes=[f"ut{s}c{cb}"])
        if kind == "l" and has_rope:
            xv = ut[s][:, 0:512].rearrange("p (g d) -> p g d", d=32)
            x1 = xv[:, :, 0:16]
            x2 = xv[:, :, 16:32]
            cb_ = rc[s][:].unsqueeze(1).to_broadcast([128, 16, 16])
            sb_ = rs[s][:].unsqueeze(1).to_broadcast([128, 16, 16])
            S.op("dve", lambda e, x1=x1, cb_=cb_: e.tensor_tensor(rt[0][:], x1, cb_, ALU.mult), reads=[f"ut{s}c0", f"rc{s}"], writes=["rt0"])
            S.op("pool", lambda e, x2=x2, sb_=sb_: e.tensor_tensor(rt[1][:], x2, sb_, ALU.mult), reads=[f"ut{s}c0", f"rs{s}"], writes=["rt1"])
            S.op("dve", lambda e, x2=x2, cb_=cb_: e.tensor_tensor(rt[2][:], x2, cb_, ALU.mult), reads=[f"ut{s}c0", f"rc{s}"], writes=["rt2"])
            S.op("pool", lambda e, x1=x1, sb_=sb_: e.tensor_tensor(rt[3][:], x1, sb_, ALU.mult), reads=[f"ut{s}c0", f"rs{s}"], writes=["rt3"])
            S.op("dve", lambda e, x1=x1: e.tensor_tensor(x1, rt[0][:], rt[1][:], ALU.subtract), reads=["rt0", "rt1"], writes=[f"ut{s}c0"])
            S.op("pool", lambda e, x2=x2: e.tensor_tensor(x2, rt[2][:], rt[3][:], ALU.add), reads=["rt2", "rt3", f"ut{s}c0"], writes=[f"ut{s}c0"])
        for q4 in range(5):
            psq = cx.PS[q4 % 2]
            for j4 in range(4):
                cc = q4 * 4 + j4
                S.op("pe", lambda e, psq=psq, j4=j4, cc=cc, s=s, P=P: e.matmul(psq[:, j4 * 128:j4 * 128 + P], ut[s][0:P, cc * 128:(cc + 1) * 128],
                                                                            identF[0:P, 0:P], start=True, stop=True),
                     reads=[f"ut{s}c{cc // 4}", "identF"], writes=[f"ps{q4 % 2}"])
            pqv = psq[:].rearrange("p (j t) -> p j t", t=128)
            S.op(("act", "dve")[q4 % 2], lambda e, pqv=pqv, q4=q4, P=P: (e.copy if hasattr(e, "copy") else e.tensor_copy)(utT[:, q4 * 4:(q4 + 1) * 4, 0:P], pqv[:, :, 0:P]),
                 reads=[f"ps{q4 % 2}"], writes=["utT"])
        for hf in range(P // 64):
            ck = (2 * i + hf) if kind == "l" else NAGC - 1
            S.dma("poolq", ag_in[ck].rearrange("(cc p) t -> p cc t", p=128), utT[:, :, hf * 64:(hf + 1) * 64], reads=["utT"], writes=[f"ag_in{ck}"])
            cx.coll(cckey, "AllGather", ALU.bypass, GROUPS, ag_in[ck], ag_out[ck], [f"ag_in{ck}"])
    cx.end_phase()


def _bf16(a):
    import ml_dtypes
    return np.asarray(a, dtype=np.float32).astype(ml_dtypes.bfloat16)


def const_sel():
    s = np.zeros((2, 2, 128), np.float32)
    s[0, 0, :] = 1.0
    s[1, 1, :] = 1.0
    return s


def const_rope():
    inv = 10000.0 ** (-np.arange(8, dtype=np.float32) / 8)
    t = np.arange(SEQ)
    row = (t // 64).astype(np.float32)
    col = (t % 64).astype(np.float32)
    ang = np.concatenate([row[:, None] * inv, col[:, None] * inv], axis=-1).astype(np.float32)
    return np.cos(ang).astype(np.float32), np.sin(ang).astype(np.float32)


def cT_layout(c_b, c_ctx):
    a = np.stack([c_b, c_ctx], axis=-1)
    return np.ascontiguousarray(a.reshape(8, 128, 2).transpose(1, 0, 2))


class Attn:
    BANKS = (0, 1, 2, 7)

    def __init__(self, cx, ident):
        self.cx = cx
        self.ident = ident
        self.PT = [cx.sb(f"PT{i}", [128, 512], BF16) for i in range(4)]
        self.it = 0

    def run(self, QT, N, chunks, pso, psokey, qkeys):
        cx, S = self.cx, self.cx.S
        n = len(chunks)

        def qk(i):
            KT, V, bias, keys = chunks[i]
            slot = (self.it + i) % 4
            bank = self.BANKS[slot]
            ps = cx.PS[bank]
            S.op("pe", lambda e: e.matmul(ps[:, 0:N], KT, QT, start=True, stop=(bias is None)),
                 reads=list(keys) + list(qkeys), writes=[f"ps{bank}"])
            if bias is not None:
                S.op("pe", lambda e: e.matmul(ps[:, 0:N], self.ident[:], bias, start=False, stop=True),
                     reads=["bias", "ident"], writes=[f"ps{bank}"])
        qk(0)
        if n > 1:
            qk(1)
        for i in range(n):
            if i + 2 < n:
                qk(i + 2)
            KT, V, bias, keys = chunks[i]
            slot = (self.it + i) % 4
            bank = self.BANKS[slot]
            ps = cx.PS[bank]
            PT = self.PT[slot]
            S.op("act", lambda e, ps=ps, PT=PT: e.activation(PT[:, 0:N], ps[:, 0:N], AF.Exp),
                 reads=[f"ps{bank}"], writes=[f"PT{slot}"])
            S.op("pe", lambda e, PT=PT, V=V, i=i: e.matmul(pso[0:65, 0:N], V, PT[:, 0:N], start=(i == 0), stop=(i == n - 1)),
                 reads=[f"PT{slot}", "vaug"], writes=[psokey])
        self.it += n


def emit_cast_rows(cx, stg, dst, src, rows, cols, key, scale=None, chunk=2048):
    S = cx.S
    nch = (cols + chunk - 1) // chunk
    for c in range(nch):
        w = min(chunk, cols - c * chunk)
        sl = slice(c * chunk, c * chunk + w)
        S.dma(("sp", "poolq")[c % 2], stg[c % 2][0:rows, 0:w], src[:, sl], writes=[f"stg{c%2}"])
        if scale is None:
            S.op("dve", lambda e, c=c, w=w, sl=sl: e.tensor_copy(dst[0:rows, sl], stg[c % 2][0:rows, 0:w]),
                 reads=[f"stg{c%2}"], writes=[key])
        else:
            S.op("dve", lambda e, c=c, w=w, sl=sl: e.tensor_scalar(dst[0:rows, sl], stg[c % 2][0:rows, 0:w], scale, None, ALU.mult),
                 reads=[f"stg{c%2}"], writes=[key])


def emit_load_v(cx, stg, Vaug, vsrc, nk, key):
    S = cx.S
    nch = nk // 128
    vv = vsrc.rearrange("(c p) d -> p c d", p=128)
    step = 26
    for i, c0 in enumerate(range(0, nch, step)):
        c1 = min(nch, c0 + step)
        sv = stg[i % 2][:, 0:(c1 - c0) * 65].rearrange("p (c d) -> p c d", d=65)
        S.dma(("sp", "poolq")[i % 2], sv, vv[:, c0:c1, :], writes=[f"stg{i%2}"])
        S.op("dve", lambda e, sv=sv, c0=c0, c1=c1: e.tensor_copy(Vaug[:, c0:c1, :], sv), reads=[f"stg{i%2}"], writes=[key])


def emit_qbound(cx, st, QT, d, N_total, kfac, ones_b, qsrc, key):
    S = cx.S
    sq = [cx.sb(f"sq_{key}{i}", [d, 512], BF16, st) for i in range(2)]
    for c in range(N_total // 512 if N_total >= 512 else 1):
        w = min(512, N_total)
        sl = slice(c * 512, c * 512 + w)
        S.op("dve", lambda e, c=c, sl=sl, w=w: e.tensor_tensor(sq[c % 2][:, 0:w], QT[0:d, sl], QT[0:d, sl], ALU.mult),
             reads=[key], writes=[f"sq{c%2}"])
        ps = cx.PS[6 + c % 2]
        S.op("pe", lambda e, ps=ps, c=c, w=w: e.matmul(ps[0:d + 1, 0:w], ones_b[0:d, 0:d + 1], sq[c % 2][:, 0:w], start=True, stop=True),
             reads=[f"sq{c%2}", "ones_b"], writes=[f"ps{6+c%2}"])
        S.op("act", lambda e, ps=ps, sl=sl, w=w: e.sqrt(QT[d:d + 1, sl], ps[d:d + 1, 0:w]),
             reads=[f"ps{6+c%2}"], writes=[key + "r"])
        S.op("dve", lambda e, sl=sl: e.tensor_scalar(QT[d:d + 1, sl], QT[d:d + 1, sl], kfac[d:d + 1, 0:1], -1.0, ALU.mult, ALU.mult),
             reads=[key + "r", "kfac"], writes=[key + "r"])


def emit_kmax(cx, st, KT, d, nk, kfac, ones_b, key):
    S = cx.S
    sq = [cx.sb(f"ksq_{key}{i}", [d, 512], BF16, st) for i in range(2)]
    kmx = cx.sb(f"kmx_{key}", [d + 1, 64], F32, st)
    S.op("pool", lambda e: e.memset(kmx[:], 0.0), writes=["kmx"])
    nch = (nk + 511) // 512
    for c in range(nch):
        w = min(512, nk - c * 512)
        sl = slice(c * 512, c * 512 + w)
        S.op("dve", lambda e, c=c, sl=sl, w=w: e.tensor_tensor(sq[c % 2][:, 0:w], KT[0:d, sl], KT[0:d, sl], ALU.mult),
             reads=[key], writes=[f"sq{c%2}"])
        ps = cx.PS[6 + c % 2]
        S.op("pe", lambda e, ps=ps, c=c, w=w: e.matmul(ps[0:d + 1, 0:w], ones_b[0:d, 0:d + 1], sq[c % 2][:, 0:w], start=True, stop=True),
             reads=[f"sq{c%2}", "ones_b"], writes=[f"ps{6+c%2}"])
        S.op("dve", lambda e, ps=ps, c=c, w=w: e.tensor_reduce(kmx[d:d + 1, c:c + 1], ps[d:d + 1, 0:w], AX.X, ALU.max),
             reads=[f"ps{6+c%2}"], writes=["kmx"])
    S.op("dve", lambda e: e.tensor_reduce(kfac[d:d + 1, 0:1], kmx[d:d + 1, 0:nch], AX.X, ALU.max), reads=["kmx"], writes=["kfac"])
    S.op("act", lambda e: e.sqrt(kfac[d:d + 1, 0:1], kfac[d:d + 1, 0:1]), reads=["kfac"], writes=["kfac"])


def emit_finalize(cx, pso, N, recrow, osb, E65, tdst, psokey, tkey):
    S = cx.S
    S.op("dve", lambda e: e.reciprocal(recrow[64:65, 0:N], pso[64:65, 0:N]), reads=[psokey], writes=["recrow"])
    S.op("pe", lambda e: e.matmul(cx.PS[5][0:64, 0:N], E65[0:65, 0:64], recrow[0:65, 0:N], start=True, stop=True),
         reads=["recrow", "E65"], writes=["ps5"])
    S.op("act", lambda e: e.copy(osb[0:64, 0:N], pso[0:64, 0:N]), reads=[psokey], writes=["osb"])
    S.op("dve", lambda e: e.tensor_tensor(tdst[0:64, 0:N], osb[0:64, 0:N], cx.PS[5][0:64, 0:N], ALU.mult),
         reads=["osb", "ps5"], writes=[tkey])


def emit_B1(cx, li):
    lam_init = 0.8 - 0.6 * math.exp(-0.3 * li)
    NK = SEQ + CTX
    S = cx.S
    aqT = cx.inp("aqT", [2, 32, SEQ]); akT = cx.inp("akT", [2, 33, NK]); av = cx.inp("av", [NK, 65])
    aqcT = cx.inp("aqcT", [2, 32, CTX])
    alam = cx.inp("alam", [1, 128]); subg = cx.inp("subg", [64, 1])
    bqT = cx.inp("bqT", [64, SEQ]); bkT = cx.inp("bkT", [65, NK]); bv = cx.inp("bv", [NK, 65])
    bqcT = cx.inp("bqcT", [64, CTX])
    bias_d = cx.inp("bias", [3, 8, 128, 512])
    ident_d = cx.inp("ident", [128, 128], BF16); onesb_d = cx.inp("onesb", [128, 128], BF16)
    E65_d = cx.inp("E65", [65, 64]); ones64_d = cx.inp("ones64", [64, 64])
    oa = cx.out("oa", [64, SEQ]); oac = cx.out("oac", [64, CTX])
    ob = cx.out("ob", [64, SEQ]); obc = cx.out("obc", [64, CTX])

    ident = cx.sb("ident", [128, 128], BF16); ones_b = cx.sb("onesb", [128, 128], BF16)
    E65 = cx.sb("E65", [65, 64]); ones64 = cx.sb("ones64", [64, 64])
    S.dma("sp", ident[:], ident_d, writes=["ident"]); S.dma("sp", ones_b[:], onesb_d, writes=["ones_b"])
    S.dma("sp", E65[:], E65_d, writes=["E65"]); S.dma("sp", ones64[:], ones64_d, writes=["ones64"])
    at = Attn(cx, ident)
    recrow = cx.sb("recrow", [65, 512]); osb = cx.sb("osb", [64, 512])
    t0 = cx.sb("t0", [64, 512]); t1 = cx.sb("t1", [64, 512]); t2 = cx.sb("t2", [64, 512])
    kfac = cx.sb("kfac", [65, 1])
    stg = [cx.sb(f"stg{i}", [128, 2048]) for i in range(2)]
    S.op("pool", lambda e: e.memset(recrow[:], 0.0), writes=["recrow"])
    lrow = cx.sb("lrow", [1, 128]); lsum = cx.sb("lsum", [1, 4]); neglam = cx.sb("neglam", [64, 1]); gsc = cx.sb("gsc", [64, 1])
    S.dma("sp", lrow[:], alam, writes=["lrow"]); S.dma("sp", gsc[:], subg, writes=["gsc"])
    S.op("pool", lambda e: e.memset(lsum[:], 0.0), writes=["lsum"])
    S.op("dve", lambda e: e.tensor_tensor(lrow[:, 0:32], lrow[:, 0:32], lrow[:, 32:64], ALU.mult), reads=["lrow"], writes=["lrow"])
    S.op("dve", lambda e: e.tensor_tensor(lrow[:, 64:96], lrow[:, 64:96], lrow[:, 96:128], ALU.mult), reads=["lrow"], writes=["lrow"])
    S.op("dve", lambda e: e.tensor_reduce(lsum[:, 0:1], lrow[:, 0:32], AX.X, ALU.add), reads=["lrow", "lsum"], writes=["lsum"])
    S.op("dve", lambda e: e.tensor_reduce(lsum[:, 1:2], lrow[:, 64:96], AX.X, ALU.add), reads=["lrow", "lsum"], writes=["lsum"])
    S.op("act", lambda e: e.activation(lsum[:, 0:2], lsum[:, 0:2], AF.Exp), reads=["lsum"], writes=["lsum"])
    S.op("dve", lambda e: e.tensor_tensor(lsum[:, 2:3], lsum[:, 1:2], lsum[:, 0:1], ALU.subtract), reads=["lsum"], writes=["lsum"])
    S.op("dve", lambda e: e.tensor_scalar(lsum[:, 2:3], lsum[:, 2:3], -lam_init, None, ALU.add), reads=["lsum"], writes=["lsum"])
    S.op("pe", lambda e: e.matmul(cx.PS[7][0:64, 0:1], ones64[0:1, 0:64], lsum[0:1, 2:3], start=True, stop=True),
         reads=["lsum", "ones64"], writes=["ps7"])
    S.op("act", lambda e: e.copy(neglam[:], cx.PS[7][0:64, 0:1]), reads=["ps7"], writes=["neglam"])
    S.op("dve", lambda e: e.tensor_scalar(gsc[:], gsc[:], 1.0 - lam_init, None, ALU.mult), reads=["gsc"], writes=["gsc"])

    with ExitStack() as st:
        QT = [cx.sb(f"aQT{c}", [33, SEQ], BF16, st) for c in range(2)]
        QTc = [cx.sb(f"aQTc{c}", [33, CTX], BF16, st) for c in range(2)]
        KT = [cx.sb(f"aKT{c}", [33, NK], BF16, st) for c in range(2)]
        Va = cx.sb("aV", [128, NK // 128, 65], BF16, st)
        sc = 32 ** -0.5
        for c in range(2):
            emit_cast_rows(cx, stg, KT[c], akT[c], 33, NK, f"akt{c}")
            emit_cast_rows(cx, stg, QT[c], aqT[c], 32, SEQ, f"aqt{c}", scale=sc)
            emit_cast_rows(cx, stg, QTc[c], aqcT[c], 32, CTX, f"aqtc{c}", scale=sc)
        emit_load_v(cx, stg, Va, av, NK, "vaug")
        for c in range(2):
            emit_kmax(cx, st, KT[c], 32, NK, kfac, ones_b, f"akt{c}")
            emit_qbound(cx, st, QT[c], 32, SEQ, kfac, ones_b, None, f"aqt{c}")
            emit_qbound(cx, st, QTc[c], 32, CTX, kfac, ones_b, None, f"aqtc{c}")

        def diff_block(qts, N, chunk_ids, odst, qkeys):
            for c in range(2):
                chunks = [(KT[c][:, k * 128:(k + 1) * 128], Va[:, k, :], None, (f"akt{c}",)) for k in chunk_ids]
                at.run(qts[c], N, chunks, cx.PS[3 + c], f"ps{3+c}", [qkeys[c], qkeys[c] + "r"])
            emit_finalize(cx, cx.PS[3], N, recrow, osb, E65, t0, "ps3", "t0")
            emit_finalize(cx, cx.PS[4], N, recrow, osb, E65, t1, "ps4", "t1")
            S.op("dve", lambda e: e.scalar_tensor_tensor(t0[:, 0:N], t1[:, 0:N], neglam[:, 0:1], t0[:, 0:N], ALU.mult, ALU.add),
                 reads=["t0", "t1", "neglam"], writes=["t0"])
            S.op("act", lambda e: e.activation(t1[:, 0:N], t0[:, 0:N], AF.Square), reads=["t0"], writes=["t1"])
            S.op("pe", lambda e: e.matmul(cx.PS[6][0:64, 0:N], ones64[:, :], t1[:, 0:N], start=True, stop=True),
                 reads=["t1", "ones64"], writes=["ps6"])
            S.op("dve", lambda e: e.tensor_scalar(t1[:, 0:N], cx.PS[6][0:64, 0:N], 1.0 / 64, EPS, ALU.mult, ALU.add),
                 reads=["ps6"], writes=["t1"])
            S.op("act", lambda e: e.sqrt(t1[:, 0:N], t1[:, 0:N]), reads=["t1"], writes=["t1"])
            S.op("dve", lambda e: e.reciprocal(t1[:, 0:N], t1[:, 0:N]), reads=["t1"], writes=["t1"])
            S.op("dve", lambda e: e.scalar_tensor_tensor(t2[:, 0:N], t0[:, 0:N], gsc[:, 0:1], t1[:, 0:N], ALU.mult, ALU.mult),
                 reads=["t0", "t1", "gsc"], writes=["t2"])
            S.dma("poolq", odst, t2[:, 0:N], reads=["t2"], writes=["oa"])

        for qb in range(SEQ // 512):
            sl = slice(qb * 512, (qb + 1) * 512)
            diff_block([QT[0][:, sl], QT[1][:, sl]], 512, range(NK // 128), oa[:, sl], ["aqt0", "aqt1"])
        diff_block([QTc[0][:, :], QTc[1][:, :]], CTX, range(SEQ // 128, NK // 128), oac[:, :], ["aqtc0", "aqtc1"])
        S.barrier()

    with ExitStack() as st:
        QT = cx.sb("bQT", [65, SEQ], BF16, st); QTc = cx.sb("bQTc", [65, CTX], BF16, st)
        KT = cx.sb("bKT", [65, NK], BF16, st); Vb = cx.sb("bV", [128, NK // 128, 65], BF16, st)
        bias = cx.sb("bias", [128, 3, 8, 512], BF16, st)
        sc = 64 ** -0.5
        emit_cast_rows(cx, stg, KT, bkT, 65, NK, "bkt")
        emit_cast_rows(cx, stg, QT, bqT, 64, SEQ, "bqt", scale=sc)
        emit_cast_rows(cx, stg, QTc, bqcT, 64, CTX, "bqtc", scale=sc)
        emit_load_v(cx, stg, Vb, bv, NK, "vaug")
        for s_ in range(3):
            for j in range(8):
                i = s_ * 8 + j
                S.dma(("sp", "poolq")[i % 2], stg[i % 2][:, 0:512], bias_d[s_, j], writes=[f"stg{i%2}"])
                S.op("dve", lambda e, i=i, s_=s_, j=j: e.tensor_copy(bias[:, s_, j, :], stg[i % 2][:, 0:512]),
                     reads=[f"stg{i%2}"], writes=["bias"])
        emit_kmax(cx, st, KT, 64, NK, kfac, ones_b, "bkt")
        emit_qbound(cx, st, QT, 64, SEQ, kfac, ones_b, None, "bqt")
        emit_qbound(cx, st, QTc, 64, CTX, kfac, ones_b, None, "bqtc")
        for qb in range(32):
            R0 = qb * 8
            if qb == 0:
                bset, kr0 = 0, 0
            elif qb == 31:
                bset, kr0 = 2, 240
            else:
                bset, kr0 = 1, R0 - 4
            sl = slice(qb * 512, (qb + 1) * 512)
            chunks = [(KT[:, (kr0 // 2 + j) * 128:(kr0 // 2 + j + 1) * 128], Vb[:, kr0 // 2 + j, :], bias[:, bset, j, :], ("bkt",))
                      for j in range(8)]
            chunks += [(KT[:, k * 128:(k + 1) * 128], Vb[:, k, :], None, ("bkt",)) for k in range(SEQ // 128, NK // 128)]
            at.run(QT[:, sl], 512, chunks, cx.PS[3], "ps3", ["bqt", "bqtr"])
            emit_finalize(cx, cx.PS[3], 512, recrow, osb, E65, t0, "ps3", "t0")
            S.dma("poolq", ob[:, sl], t0[:, 0:512], reads=["t0"], writes=["ob"])
        chunks = [(KT[:, k * 128:(k + 1) * 128], Vb[:, k, :], None, ("bkt",)) for k in range(SEQ // 128, NK // 128)]
        at.run(QTc[:, :], CTX, chunks, cx.PS[3], "ps3", ["bqtc", "bqtcr"])
        emit_finalize(cx, cx.PS[3], CTX, recrow, osb, E65, t0, "ps3", "t0")
        S.dma("poolq", obc[:, :], t0[:, 0:CTX], reads=["t0"], writes=["obc"])
        S.barrier()
    cx.end_phase()


def na_bias_sets(rpb_h):
    out = np.full((3, 8, 128, 512), -30000.0, np.float32)
    cq = np.arange(64); ck = np.arange(64)
    c0 = np.clip(cq - 8, 0, 48)
    colok = (ck[:, None] >= c0[None, :]) & (ck[:, None] < c0[None, :] + 16)
    dc = np.clip(ck[:, None] - cq[None, :], -15, 15) + 15
    for s_, (R0, kr0) in enumerate([(0, 0), (8, 4), (248, 240)]):
        for j in range(8):
            for krl in range(2):
                kr = kr0 + 2 * j + krl
                for qrl in range(8):
                    r = R0 + qrl
                    r0 = min(max(r - 4, 0), 248)
                    if not (r0 <= kr < r0 + 8):
                        continue
                    dr = kr - r + 7
                    blk = np.where(colok, rpb_h[dr][dc], np.float32(-30000.0))
                    out[s_, j, krl * 64:(krl + 1) * 64, qrl * 64:(qrl + 1) * 64] = blk
    return out


def emit_C(cx, with_ctx, final):
    S = cx.S
    xl = cx.inp("xl", [TPC, D]); xc = cx.inp("xc", [CPC, D])
    rs_out = cx.inp("rs_out", [TPC + CPC, D])
    cT = cx.inp("cT", [128, 8, 2])
    ada_w = cx.inp("ada_w", [D, 6 * D]); ada_b = cx.inp("ada_b", [1, 6 * D])
    norm_g = cx.inp("norm2_g", [1, D]); fin_g = cx.inp("fin_g", [1, D])
    router_w = cx.inp("router_w", [D, 16]); router_b = cx.inp("router_b", [1, 16])
    w1 = cx.inp("w1", [16, D, 512]); w3 = cx.inp("w3", [16, D, 512]); w2 = cx.inp("w2", [16, 512, D])
    sel_d = cx.inp("sel", [2, 2, 128]); identf_d = cx.inp("ident", [128, 128], BF16)
    ol = cx.out("ol", [TPC, D]); oc = cx.out("oc", [CPC, D])

    sel = cx.sb("sel", [2, 2, 128]); identf = cx.sb("identb", [128, 128], BF16)
    rwh = cx.sb("rwh", [128, 8, 16], BF16); rwl = cx.sb("rwl", [128, 8, 16], BF16)
    G1 = cx.sb("G1", [128, D]); G2 = cx.sb("G2", [128, D]); SH2 = cx.sb("SH2", [128, D]); GG2 = cx.sb("GG2", [128, D])
    FG = cx.sb("FG", [128, D])
    rw = cx.sb("rw", [128, 8, 16]); rb = cx.sb("rb", [128, 16])
    bcs = cx.scratch("bc_scr", [4, 128, D])
    S.dma("sp", sel[:], sel_d, writes=["sel"]); S.dma("sp", identf[:], identf_d, writes=["identf"])
    S.dma("sp", rw[:], router_w.rearrange("(kc p) n -> p kc n", p=128), writes=["rw"])
    S.dma("sp", rb[:], router_b.partition_broadcast(128), writes=["rb"])
    S.op("dve", lambda e: e.tensor_copy(rwh[:], rw[:]), reads=["rw"], writes=["rwh"])
    S.op("dve", lambda e: e.tensor_tensor(rwl[:], rw[:], rwh[:], ALU.subtract), reads=["rw", "rwh"], writes=["rwl"])
    with ExitStack() as st:
        scT = cx.sb("scT", [128, 8, 2], F32, st); modrow = cx.sb("modrow", [2, 6 * D], F32, st)
        normg2 = cx.sb("normg2", [2, D], F32, st); grow = cx.sb("grow", [2, D], F32, st); fing2 = cx.sb("fing2", [2, D], F32, st)
        S.dma("sp", scT[:], cT, writes=["scT"])
        for r in range(2):
            S.dma("sp", normg2[r:r + 1, :], norm_g, writes=["normg2"])
            S.dma("sp", fing2[r:r + 1, :], fin_g, writes=["fing2"])
        S.op("act", lambda e: e.activation(scT[:], scT[:], AF.Silu), reads=["scT"], writes=["scT"])
        with ExitStack() as st2:
            emit_mod_rows(cx, st2, scT, ada_w, ada_b, modrow, "C")
            S.barrier()
        S.op("dve", lambda e: e.scalar_tensor_tensor(grow[:], modrow[:, 4 * D:5 * D], 1.0, normg2[:], ALU.add, ALU.mult),
             reads=["modrow", "normg2"], writes=["grow"])

        def set_bcast(which):
            emit_bcast_row(cx, G1, modrow[:, 2 * D:3 * D], sel, which, (0, 1), "modrow", "G1")
            emit_bcast_row(cx, G2, grow, sel, which, (0, 1), "grow", "G2")
            emit_bcast_row(cx, SH2, modrow[:, 3 * D:4 * D], sel, which, (0, 1), "modrow", "SH2")
            emit_bcast_row(cx, GG2, modrow[:, 5 * D:6 * D], sel, which, (0, 1), "modrow", "GG2")
        set_bcast(1)
        for i_, (t_, k_) in enumerate(((G1, "G1"), (G2, "G2"), (SH2, "SH2"), (GG2, "GG2"))):
            S.dma("sp", bcs[i_], t_[:], reads=[k_], writes=["bcs"])
        set_bcast(0)
        emit_bcast_row(cx, FG, fing2, sel, 0, (0, 1), "fing2", "FG")
        S.barrier()

    def load_ctx_bcast():
        for i_, (t_, k_) in enumerate(((G1, "G1"), (G2, "G2"), (SH2, "SH2"), (GG2, "GG2"))):
            S.dma("sp", t_[:], bcs[i_], reads=["bcs"], writes=[k_])

    GT = 8
    x1 = cx.sb("x1", [128, GT, D]); yacc = cx.sb("yacc", [128, GT, D])
    hT = cx.sb("hT", [128, 8, GT * 128], BF16)
    gate = cx.sb("gate", [128, GT, 16])
    xt = [cx.sb(f"xt{i}", [128, D]) for i in range(2)]
    mpt = [cx.sb("mpt0", [128, D])] * 2
    junk = cx.sb("junk", [128, D], BF16); tmp = cx.sb("tmp", [128, D]); h2 = cx.sb("h2", [128, D])
    hTl = cx.sb("hTl", [128, 8, 128], BF16); h2hi = cx.sb("h2hi", [128, D], BF16); h2lo = cx.sb("h2lo", [128, D], BF16)
    ss = cx.sb("ss", [128, 1]); rstd = cx.sb("rstd", [128, 1])
    r_ = {k: cx.sb("r_" + k, [128, 16]) for k in ("ex", "sc", "sel", "eq", "s2", "selm", "k1", "sm2", "k2", "w")}
    q_ = {k: cx.sb("q_" + k, [128, 4]) for k in ("m1", "m2", "gs", "gmask", "pen")}
    c_ = {k: cx.sb("c_" + k, [128, 1]) for k in ("mx", "sm", "gm", "e1", "e2", "ws")}
    W13 = [cx.sb(f"W13_{i}", [128, 2, 8, 512], BF16) for i in range(2)]
    W2 = [cx.sb(f"W2_{i}", [128, 4, D], BF16) for i in range(2)]
    hs = cx.sb("hs", [128, 512]); hh = [cx.sb(f"hh{i}", [128, 4, 512], BF16) for i in range(2)]
    stage_n = [0]
    conv_st = ExitStack()
    wstg = [cx.sb(f"wstg{i}", [128, 4, 512], F32, conv_st) for i in range(2)]

    def router(P, g):
        lg = cx.PS[2]
        n_ = 0
        for (lhs, rhs) in (("hi", rwh), ("lo", rwh), ("hi", rwl)):
            for kc in range(8):
                lt = hT[:, kc, g * 128:g * 128 + P] if lhs == "hi" else hTl[:, kc, 0:P]
                S.op("pe", lambda e, lt=lt, rhs=rhs, kc=kc, n_=n_: e.matmul(lg[0:P, 0:16], lt, rhs[:, kc, :], start=(n_ == 0), stop=(n_ == 23)),
                     reads=["hT", "hTl", "rwh", "rwl"], writes=["ps2"])
                n_ += 1
        V = lambda k: r_[k][0:P, :]
        V4 = lambda k: r_[k][0:P, :].rearrange("p (g e) -> p g e", e=4)
        Q = lambda k: q_[k][0:P, :]
        Cc = lambda k: c_[k][0:P, :]
        dv = lambda fn, rd, wr: S.op("dve", fn, reads=rd, writes=wr)
        dv(lambda e: e.tensor_reduce(Cc("mx"), lg[0:P, 0:16], AX.X, ALU.max), ["ps2"], ["c_mx"])
        dv(lambda e: e.tensor_scalar(Cc("mx"), Cc("mx"), -1.0, None, ALU.mult), ["c_mx"], ["c_mx"])
        S.op("pool", lambda e: e.memset(Cc("sm"), 0.0), writes=["c_sm"])
        S.op("act", lambda e: e.activation(V("ex"), lg[0:P, 0:16], AF.Exp, bias=Cc("mx"), accum_out=Cc("sm")),
             reads=["ps2", "c_mx", "c_sm"], writes=["r_ex", "c_sm"])
        dv(lambda e: e.reciprocal(Cc("sm"), Cc("sm")), ["c_sm"], ["c_sm"])
        dv(lambda e: e.tensor_scalar(V("sc"), V("ex"), Cc("sm"), None, ALU.mult), ["r_ex", "c_sm"], ["r_sc"])
        dv(lambda e: e.tensor_tensor(V("sel"), V("sc"), rb[0:P, :], ALU.add), ["r_sc", "rb"], ["r_sel"])
        dv(lambda e: e.tensor_reduce(Q("m1"), V4("sel"), AX.X, ALU.max), ["r_sel"], ["q_m1"])
        dv(lambda e: e.tensor_tensor(V4("eq"), V4("sel"), Q("m1").unsqueeze(2).to_broadcast([P, 4, 4]), ALU.is_equal), ["r_sel", "q_m1"], ["r_eq"])
        dv(lambda e: e.scalar_tensor_tensor(V("s2"), V("eq"), -1e9, V("sel"), ALU.mult, ALU.add), ["r_eq", "r_sel"], ["r_s2"])
        dv(lambda e: e.tensor_reduce(Q("m2"), V4("s2"), AX.X, ALU.max), ["r_s2"], ["q_m2"])
        dv(lambda e: e.tensor_tensor(Q("gs"), Q("m1"), Q("m2"), ALU.add), ["q_m1", "q_m2"], ["q_gs"])
        dv(lambda e: e.tensor_reduce(Cc("gm"), Q("gs"), AX.X, ALU.max), ["q_gs"], ["c_gm"])
        dv(lambda e: e.tensor_scalar(Q("gmask"), Q("gs"), Cc("gm"), None, ALU.is_equal), ["q_gs", "c_gm"], ["q_gmask"])
        dv(lambda e: e.tensor_scalar(Q("pen"), Q("gmask"), -1.0, 1e9, ALU.add, ALU.mult), ["q_gmask"], ["q_pen"])
        dv(lambda e: e.tensor_tensor(V4("selm"), V4("sel"), Q("pen").unsqueeze(2).to_broadcast([P, 4, 4]), ALU.add), ["r_sel", "q_pen"], ["r_selm"])
        dv(lambda e: e.tensor_reduce(Cc("e1"), V("selm"), AX.X, ALU.max), ["r_selm"], ["c_e1"])
        dv(lambda e: e.tensor_scalar(V("k1"), V("selm"), Cc("e1"), None, ALU.is_equal), ["r_selm", "c_e1"], ["r_k1"])
        dv(lambda e: e.scalar_tensor_tensor(V("sm2"), V("k1"), -1e9, V("selm"), ALU.mult, ALU.add), ["r_k1", "r_selm"], ["r_sm2"])
        dv(lambda e: e.tensor_reduce(Cc("e2"), V("sm2"), AX.X, ALU.max), ["r_sm2"], ["c_e2"])
        dv(lambda e: e.tensor_scalar(V("k2"), V("sm2"), Cc("e2"), None, ALU.is_equal), ["r_sm2", "c_e2"], ["r_k2"])
        dv(lambda e: e.tensor_tensor(V("k1"), V("k1"), V("k2"), ALU.add), ["r_k1", "r_k2"], ["r_k1"])
        dv(lambda e: e.tensor_tensor(V("w"), V("sc"), V("k1"), ALU.mult), ["r_sc", "r_k1"], ["r_w"])
        dv(lambda e: e.tensor_reduce(Cc("ws"), V("w"), AX.X, ALU.add), ["r_w"], ["c_ws"])
        dv(lambda e: e.reciprocal(Cc("ws"), Cc("ws")), ["c_ws"], ["c_ws"])
        dv(lambda e: e.tensor_scalar(gate[0:P, g, :], V("w"), Cc("ws"), None, ALU.mult), ["r_w", "c_ws"], ["gate"])

    wb13 = cx.scratch("wb13", [16, 128, 2 * 8 * 512], BF16)
    wb2 = cx.scratch("wb2", [16, 128, 4 * D], BF16)

    def convert_expert(e_, slot):
        pieces = []
        for wi, wsrc in enumerate((w1, w3)):
            v = wsrc[e_].rearrange("(kc p) n -> p kc n", p=128)
            for hf in range(2):
                pieces.append((v[:, hf * 4:(hf + 1) * 4, :], W13[slot][:, wi, hf * 4:(hf + 1) * 4, :]))
        v2 = w2[e_].rearrange("(fc p) n -> p fc n", p=128)
        for hf in range(2):
            pieces.append((v2[:, :, hf * 512:(hf + 1) * 512], W2[slot][:, :, hf * 512:(hf + 1) * 512]))
        for src, dst in pieces:
            n = stage_n[0]; stage_n[0] += 1
            sg = wstg[n % 2]
            S.dma(("sp", "actq")[n % 2], sg[:], src, writes=[f"wstg{n%2}"])
            S.op(("pool", "dve")[n % 2], lambda e, sg=sg, dst=dst: e.tensor_copy(dst, sg[:]), reads=[f"wstg{n%2}"], writes=[f"W{slot}"])
        S.dma("poolq", wb13[e_], W13[slot][:].rearrange("p a b c -> p (a b c)"), reads=[f"W{slot}"], writes=["wb"])
        S.dma("poolq", wb2[e_], W2[slot][:].rearrange("p a b -> p (a b)"), reads=[f"W{slot}"], writes=["wb"])

    for e_ in range(16):
        convert_expert(e_, e_ % 2)
    S.barrier()
    conv_st.close()

    def load_expert(e_, slot):
        S.dma("sp", W13[slot][:].rearrange("p a b c -> p (a b c)"), wb13[e_], reads=["wb"], writes=[f"W{slot}"])
        S.dma("actq", W2[slot][:].rearrange("p a b -> p (a b)"), wb2[e_], reads=["wb"], writes=[f"W{slot}"])

    ntl = TPC // 128
    groups = [[("l", g * GT + t) for t in range(GT)] for g in range(ntl // GT)]
    if with_ctx:
        groups.append([("c", 0)])
    ti_glob = 0
    eload = 0
    for gi, grp in enumerate(groups):
        isctx = grp[0][0] == "c"
        if isctx:
            load_ctx_bcast()
        P = CPC if isctx else 128
        NT = len(grp) * 128 if not isctx else CPC
        for g, (kind, i) in enumerate(grp):
            s = ti_glob % 2; ti_glob += 1
            xsrc = xc if isctx else xl[i * 128:(i + 1) * 128, :]
            msrc = rs_out[TPC:TPC + CPC, :] if isctx else rs_out[i * 128:(i + 1) * 128, :]
            S.dma("sp", xt[s][0:P, :], xsrc, writes=[f"xt{s}"])
            S.dma("actq", mpt[s][0:P, :], msrc, writes=["mpt"])
            S.op("dve", lambda e, s=s, P=P: e.tensor_tensor(tmp[0:P, :], mpt[s][0:P, :], G1[0:P, :], ALU.mult),
                 reads=["mpt", "G1"], writes=["tmp"])
            S.op("dve", lambda e, s=s, g=g, P=P: e.tensor_tensor(x1[0:P, g, :], tmp[0:P, :], xt[s][0:P, :], ALU.add),
                 reads=["tmp", f"xt{s}"], writes=["x1"])
            S.op("pool", lambda e, P=P: e.memset(ss[0:P, :], 0.0), writes=["ss"])
            S.op("act", lambda e, g=g, P=P: e.activation(junk[0:P, :], x1[0:P, g, :], AF.Square, accum_out=ss[0:P, :]),
                 reads=["x1", "ss"], writes=["ss", "junk"])
            S.op("dve", lambda e, P=P: e.tensor_scalar(rstd[0:P, :], ss[0:P, :], 1.0 / D, EPS, ALU.mult, ALU.add), reads=["ss"], writes=["rstd"])
            S.op("act", lambda e, P=P: e.sqrt(rstd[0:P, :], rstd[0:P, :]), reads=["rstd"], writes=["rstd"])
            S.op("dve", lambda e, P=P: e.reciprocal(rstd[0:P, :], rstd[0:P, :]), reads=["rstd"], writes=["rstd"])
            S.op("dve", lambda e, g=g, P=P: e.scalar_tensor_tensor(tmp[0:P, :], x1[0:P, g, :], rstd[0:P, :], G2[0:P, :], ALU.mult, ALU.mult),
                 reads=["x1", "rstd", "G2"], writes=["tmp"])
            S.op("dve", lambda e, P=P: e.tensor_tensor(h2[0:P, :], tmp[0:P, :], SH2[0:P, :], ALU.add), reads=["tmp", "SH2"], writes=["h2"])
            S.op("dve", lambda e, P=P: e.tensor_copy(h2hi[0:P, :], h2[0:P, :]), reads=["h2"], writes=["h2hi"])
            S.op("dve", lambda e, P=P: e.tensor_tensor(h2lo[0:P, :], h2[0:P, :], h2hi[0:P, :], ALU.subtract), reads=["h2", "h2hi"], writes=["h2lo"])
            for (src, skey, dstv, dkey, bank) in ((h2hi, "h2hi", hT[:, :, g * 128:g * 128 + P], "hT", 3), (h2lo, "h2lo", hTl[:, :, 0:P], "hTl", 4)):
                psv = cx.PS[bank][:].bitcast(BF16).rearrange("p (k t) -> p k t", t=128)
                for kc in range(8):
                    S.op("pe", lambda e, psv=psv, kc=kc, src=src, P=P: e.transpose(psv[:, kc, 0:P], src[0:P, kc * 128:(kc + 1) * 128], identf[0:P, 0:P]),
                         reads=[skey, "identf"], writes=[f"ps{bank}"])
                S.op("act", lambda e, psv=psv, dstv=dstv, P=P: e.copy(dstv, psv[:, :, 0:P]), reads=[f"ps{bank}"], writes=[dkey])
            router(P, g)
            S.op("pool", lambda e, g=g, P=P: e.memset(yacc[0:P, g, :], 0.0), writes=["yacc"])
        for e_ in range(16):
            slot = eload % 2; eload += 1
            load_expert(e_, slot)
            for c0 in range(0, NT, 512):
                w = min(512, NT - c0)
                hsl = hh[(c0 // 512) % 2]
                for fc in range(4):
                    p1 = cx.PS[4 + (fc % 2) * 2]; p3 = cx.PS[5 + (fc % 2) * 2]
                    k1 = f"ps{4 + (fc % 2) * 2}"; k3 = f"ps{5 + (fc % 2) * 2}"
                    for wi, (pp, pk) in enumerate(((p1, k1), (p3, k3))):
                        for kc in range(8):
                            S.op("pe", lambda e, pp=pp, wi=wi, kc=kc, fc=fc, slot=slot, c0=c0, w=w: e.matmul(
                                pp[:, 0:w], W13[slot][:, wi, kc, fc * 128:(fc + 1) * 128], hT[:, kc, c0:c0 + w], start=(kc == 0), stop=(kc == 7)),
                                reads=[f"W{slot}", "hT"], writes=[pk])
                    S.op("act", lambda e, p1=p1, w=w: e.activation(hs[:, 0:w], p1[:, 0:w], AF.Silu), reads=[k1], writes=["hs"])
                    S.op("dve", lambda e, p3=p3, w=w, fc=fc, hsl=hsl: e.tensor_tensor(hsl[:, fc, 0:w], hs[:, 0:w], p3[:, 0:w], ALU.mult),
                         reads=["hs", k3], writes=[f"hh{(c0//512)%2}"])
                for t0_ in range(0, w, 128):
                    pw = min(128, w - t0_)
                    g = (c0 + t0_) // 128
                    for hb_ in range(2):
                        ps = cx.PS[hb_]
                        for fc in range(4):
                            S.op("pe", lambda e, ps=ps, fc=fc, hb_=hb_, slot=slot, t0_=t0_, pw=pw, hsl=hsl: e.matmul(
                                ps[0:pw, :], hsl[:, fc, t0_:t0_ + pw], W2[slot][:, fc, hb_ * 512:(hb_ + 1) * 512], start=(fc == 0), stop=(fc == 3)),
                                reads=[f"hh{(c0//512)%2}", f"W{slot}"], writes=[f"ps{hb_}"])
                        cs = slice(hb_ * 512, (hb_ + 1) * 512)
                        S.op("dve", lambda e, ps=ps, cs=cs, g=g, pw=pw, e_=e_: e.scalar_tensor_tensor(
                            yacc[0:pw, g, cs], ps[0:pw, :], gate[0:pw, g, e_:e_ + 1], yacc[0:pw, g, cs], ALU.mult, ALU.add),
                            reads=[f"ps{hb_}", "gate", "yacc"], writes=["yacc"])
        for g, (kind, i) in enumerate(grp):
            S.op("dve", lambda e, g=g, P=P: e.tensor_tensor(tmp[0:P, :], yacc[0:P, g, :], GG2[0:P, :], ALU.mult), reads=["yacc", "GG2"], writes=["tmp"])
            S.op("dve", lambda e, g=g, P=P: e.tensor_tensor(x1[0:P, g, :], x1[0:P, g, :], tmp[0:P, :], ALU.add), reads=["tmp", "x1"], writes=["x1"])
            dst = oc if isctx else ol[i * 128:(i + 1) * 128, :]
            if final:
                S.op("pool", lambda e, P=P: e.memset(ss[0:P, :], 0.0), writes=["ss"])
                S.op("act", lambda e, g=g, P=P: e.activation(junk[0:P, :], x1[0:P, g, :], AF.Square, accum_out=ss[0:P, :]),
                     reads=["x1", "ss"], writes=["ss", "junk"])
                S.op("dve", lambda e, P=P: e.tensor_scalar(rstd[0:P, :], ss[0:P, :], 1.0 / D, EPS, ALU.mult, ALU.add), reads=["ss"], writes=["rstd"])
                S.op("act", lambda e, P=P: e.sqrt(rstd[0:P, :], rstd[0:P, :]), reads=["rstd"], writes=["rstd"])
                S.op("dve", lambda e, P=P: e.reciprocal(rstd[0:P, :], rstd[0:P, :]), reads=["rstd"], writes=["rstd"])
                S.op("dve", lambda e, g=g, P=P: e.scalar_tensor_tensor(h2[0:P, :], x1[0:P, g, :], rstd[0:P, :], FG[0:P, :], ALU.mult, ALU.mult),
                     reads=["x1", "rstd", "FG"], writes=["h2"])
                S.dma("poolq", dst, h2[0:P, :], reads=["h2"], writes=["ol"])
            else:
                S.dma("poolq", dst, x1[0:P, g, :], reads=["x1"], writes=["ol"])
    if not with_ctx:
        S.dma("sp", xt[0][0:CPC, :], xc, writes=["xt0"])
        S.dma("sp", oc, xt[0][0:CPC, :], reads=["xt0"], writes=["oc"])
    cx.end_phase()


def fft_consts():
    N = 32768
    n1 = np.arange(64)[:, None]; k1 = np.arange(128)[None, :]
    a = 2 * np.pi * n1 * k1 / 128
    F1cat = np.concatenate([np.cos(a), -np.sin(a)], 1)
    n2 = np.arange(256)[:, None]
    t = 2 * np.pi * n2 * k1 / N
    Tr, Ti = np.cos(t), -np.sin(t)
    k2 = np.arange(256)[None, :]
    b = 2 * np.pi * n2 * k2 / 256
    F2r, F2i = np.cos(b), -np.sin(b)
    Er, Ei = np.cos(b.T), np.sin(b.T)
    IA = np.concatenate([Er, Ei], 1); IB = np.concatenate([-Ei, Er], 1)
    ITr, ITi = np.cos(t.T), np.sin(t.T)
    c = 2 * np.pi * np.arange(128)[:, None] * np.arange(64)[None, :] / 128
    G1r, G1i = np.cos(c) / N, -np.sin(c) / N
    ch2 = lambda m: np.ascontiguousarray(m.reshape(2, 128, m.shape[1]).transpose(1, 0, 2))
    psm = (np.arange(128)[:, None] % 64 == np.arange(128)[None, :] % 64).astype(np.float32)
    return {"F1cat": _bf16(F1cat), "Tr": ch2(Tr).astype(np.float32), "Ti": ch2(Ti).astype(np.float32),
            "F2r": _bf16(ch2(F2r)), "F2i": _bf16(ch2(F2i)), "F2in": _bf16(ch2(-F2i)),
            "IA": _bf16(ch2(IA)), "IB": _bf16(ch2(IB)), "ITr": ITr.astype(np.float32), "ITi": ITi.astype(np.float32),
            "G1r": _bf16(G1r), "G1i": _bf16(G1i), "PSM": psm}


def hy_pos_consts(L):
    t = np.arange(L, dtype=np.float32)
    t_norm = t / max(L - 1, 1)
    bands = np.linspace(1e-4, 15, 16, dtype=np.float32)
    ang = (np.float32(2.0 * math.pi / L) * t[:, None] * bands[None, :]).astype(np.float32)
    z = np.concatenate([t_norm[:, None], np.cos(ang), np.sin(ang)], axis=-1).astype(np.float32)
    return np.ascontiguousarray(z.T), np.ascontiguousarray(np.broadcast_to(t_norm[None, :], (128, L))).astype(np.float32)


def emit_B2(cx, with_ctx):
    L = SEQ
    PI = math.pi
    S = cx.S
    paths = [("l", SEQ)] + ([("c", CTX)] if with_ctx else [])
    I = {}
    for tag, Le in paths:
        I[tag] = dict(hy=cx.inp(f"hy_{tag}", [3, 64, Le + 2]), zT=cx.inp(f"zT_{tag}", [33, Le]), tn=cx.inp(f"tn_{tag}", [128, Le]),
                      pl=cx.inp(f"pl_{tag}", [64, Le + 24]), icnt=cx.inp(f"icnt_{tag}", [64, Le]),
                      oh=cx.out(f"oh_{tag}", [64, Le]), op=cx.out(f"op_{tag}", [64, Le]))
    shw_d = cx.inp("shw", [64, 3, 3]); shb_d = cx.inp("shb", [64, 3])
    fw1_d = cx.inp("fw1", [33, 64]); fb1_d = cx.inp("fb1", [64, 1]); fw2_d = cx.inp("fw2", [64, 64]); fb2_d = cx.inp("fb2", [64, 1])
    fw3_d = cx.inp("fw3", [64, 256]); fb3_d = cx.inp("fb3", [128, 2]); ndel_d = cx.inp("ndel", [128, 1])
    dsk_d = cx.inp("dsk", [64, 2, 64])
    psel_d = cx.inp("psel", [64, 4]); pw_d = cx.inp("pw", [64, 64]); psc_d = cx.inp("psc", [64, 1])
    FC = fft_consts()
    cd = {k: cx.inp("c_" + k, list(v.shape), BF16 if v.dtype != np.float32 else F32) for k, v in FC.items()}
    c = {k: cx.sb("c_" + k, list(v.shape), BF16 if v.dtype != np.float32 else F32) for k, v in FC.items()}
    for k in FC:
        S.dma("sp", c[k][:], cd[k], writes=["c_" + k])
    ck = ["c_" + k for k in FC]
    small = {}
    for nm, d_, shp in (("shw", shw_d, [64, 3, 3]), ("shb", shb_d, [64, 3]), ("fw1", fw1_d, [33, 64]), ("fb1", fb1_d, [64, 1]),
                        ("fw2", fw2_d, [64, 64]), ("fb2", fb2_d, [64, 1]), ("fw3", fw3_d, [64, 256]), ("fb3", fb3_d, [128, 2]),
                        ("ndel", ndel_d, [128, 1]), ("dsk", dsk_d, [64, 2, 64]), ("psel", psel_d, [64, 4]), ("pw", pw_d, [64, 64]),
                        ("psc", psc_d, [64, 1])):
        small[nm] = cx.sb("w_" + nm, shp)
        S.dma("sp", small[nm][:], d_, writes=["w_" + nm])
    pwb = cx.sb("pwb", [64, 64], BF16)
    S.op("dve", lambda e: e.tensor_copy(pwb[:], small["pw"][:]), reads=["w_pw"], writes=["pwb"])
    scx = cx.scratch("scx", [3, 64, L]); filt_s = cx.scratch("filt_s", [2, 128, L])
    zero = cx.sb("zero", [128, 2048])
    S.op("pool", lambda e: e.memset(zero[:], 0.0), writes=["zero"])

    def do_path(tag, Le):
        io = I[tag]
        CH = min(2048, Le)
        with ExitStack() as st:
            pin = cx.sb("pin", [64, CH + 24], F32, st); A2 = cx.sb("A2", [64, CH + 24], F32, st); A4 = cx.sb("A4", [64, CH + 24], F32, st)
            A8 = cx.sb("A8", [64, CH + 24], F32, st); A16 = cx.sb("A16", [64, CH + 24], F32, st)
            acc = cx.sb("pacc", [64, CH], F32, st); ic = cx.sb("pic", [64, CH], F32, st); pd = cx.sb("pd", [64, CH], BF16, st)
            po = cx.sb("po", [64, CH], F32, st)
            for c0 in range(0, Le, CH):
                S.dma("sp", pin[:], io["pl"][:, c0:c0 + CH + 24], writes=["pin"])
                S.dma("sp", ic[:], io["icnt"][:, c0:c0 + CH], writes=["pic"])
                W_ = CH + 24
                S.op("dve", lambda e: e.tensor_tensor(A2[:, 0:W_ - 1], pin[:, 0:W_ - 1], pin[:, 1:W_], ALU.add), reads=["pin"], writes=["A2"])
                S.op("dve", lambda e: e.tensor_tensor(A4[:, 0:W_ - 3], A2[:, 0:W_ - 3], A2[:, 2:W_ - 1], ALU.add), reads=["A2"], writes=["A4"])
                S.op("dve", lambda e: e.tensor_tensor(A8[:, 0:W_ - 7], A4[:, 0:W_ - 7], A4[:, 4:W_ - 3], ALU.add), reads=["A4"], writes=["A8"])
                S.op("dve", lambda e: e.tensor_tensor(A16[:, 0:W_ - 15], A8[:, 0:W_ - 15], A8[:, 8:W_ - 7], ALU.add), reads=["A8"], writes=["A16"])
                S.op("dve", lambda e: e.tensor_scalar(acc[:], A2[:, 7:7 + CH], small["psel"][:, 0:1], None, ALU.mult), reads=["A2", "w_psel"], writes=["pacc"])
                for k_, (Aw, off, key) in enumerate(((A4, 6, "A4"), (A8, 4, "A8"), (A16, 0, "A16"))):
                    S.op("dve", lambda e, Aw=Aw, off=off, k_=k_: e.scalar_tensor_tensor(acc[:], Aw[:, off:off + CH], small["psel"][:, k_ + 1:k_ + 2], acc[:],
                                                                                        ALU.mult, ALU.add), reads=[key, "w_psel", "pacc"], writes=["pacc"])
                S.op("dve", lambda e: e.tensor_tensor(acc[:], acc[:], ic[:], ALU.mult), reads=["pacc", "pic"], writes=["pacc"])
                S.op("dve", lambda e: e.tensor_tensor(pd[:], acc[:], pin[:, 8:8 + CH], ALU.subtract), reads=["pacc", "pin"], writes=["pd"])
                for s0 in range(0, CH, 512):
                    w = min(512, CH - s0)
                    S.op("pe", lambda e, s0=s0, w=w: e.matmul(cx.PS[7][0:64, 0:w], pwb[:, :], pd[:, s0:s0 + w], start=True, stop=True),
                         reads=["pd", "pwb"], writes=["ps7"])
                    S.op("dve", lambda e, s0=s0, w=w: e.tensor_scalar(po[:, s0:s0 + w], cx.PS[7][0:64, 0:w], small["psc"][:, 0:1], None, ALU.mult),
                         reads=["ps7", "w_psc"], writes=["po"])
                S.dma("poolq", io["op"][:, c0:c0 + CH], po[:], reads=["po"], writes=["op"])
            S.barrier()

        with ExitStack() as st:
            hin = cx.sb("hin", [64, CH + 2], F32, st); ho = cx.sb("ho", [64, CH], F32, st)
            if Le < L:
                for p in range(3):
                    for c0 in range(0, L, 2048):
                        S.dma("sp", scx[p][:, c0:c0 + 2048], zero[0:64, :], reads=["zero"], writes=["scx"])
                for oc in range(2):
                    for c0 in range(0, L, 2048):
                        S.dma("sp", filt_s[oc][:, c0:c0 + 2048], zero[:, :], reads=["zero"], writes=["filt_s"])
            for p in range(3):
                for c0 in range(0, Le, CH):
                    S.dma("sp", hin[:], io["hy"][p][:, c0:c0 + CH + 2], writes=["hin"])
                    S.op("dve", lambda e, p=p: e.tensor_scalar(ho[:], hin[:, 1:CH + 1], small["shw"][:, p, 1:2], small["shb"][:, p:p + 1], ALU.mult, ALU.add),
                         reads=["hin", "w_shw", "w_shb"], writes=["ho"])
                    S.op("dve", lambda e, p=p: e.scalar_tensor_tensor(ho[:], hin[:, 0:CH], small["shw"][:, p, 0:1], ho[:], ALU.mult, ALU.add),
                         reads=["hin", "w_shw", "ho"], writes=["ho"])
                    S.op("dve", lambda e, p=p: e.scalar_tensor_tensor(ho[:], hin[:, 2:CH + 2], small["shw"][:, p, 2:3], ho[:], ALU.mult, ALU.add),
                         reads=["hin", "w_shw", "ho"], writes=["ho"])
                    S.dma("poolq", scx[p][:, c0:c0 + CH], ho[:], reads=["ho"], writes=["scx"])
            S.barrier()

        with ExitStack() as st:
            FW = min(512, Le)
            nfc = Le // FW
            zt = cx.sb("zt", [33, FW], F32, st); tnt = cx.sb("tnt", [128, FW], F32, st)
            pre = cx.sb("pre", [64, FW], F32, st); msk = cx.sb("msk", [64, FW], F32, st); h1 = cx.sb("h1", [64, FW], F32, st); h2 = cx.sb("h2f", [64, FW], F32, st)
            dec = cx.sb("dec", [128, FW], F32, st); hf = [cx.sb(f"hf{i}", [128, FW], F32, st) for i in range(2)]
            junk = cx.sb("fjunk", [128, FW], F32, st)
            accsq = cx.sb("accsq", [128, 2, 32], F32, st); ssum = cx.sb("ssum", [128, 2], F32, st); rn = cx.sb("rn", [128, 2], F32, st)
            S.op("pool", lambda e: e.memset(accsq[:], 0.0), writes=["accsq"])

            def sin_layer(ps, bias, dst, key):
                S.op("dve", lambda e: e.tensor_scalar(pre[:], ps[0:64, 0:FW], bias[:, 0:1], None, ALU.add), reads=["ps0", "ps1", "w_fb1", "w_fb2"], writes=["pre"])
                for _ in range(2):
                    S.op("dve", lambda e: e.tensor_scalar(msk[:], pre[:], PI, -2 * PI, ALU.is_gt, ALU.mult), reads=["pre"], writes=["msk"])
                    S.op("dve", lambda e: e.tensor_tensor(pre[:], pre[:], msk[:], ALU.add), reads=["pre", "msk"], writes=["pre"])
                    S.op("dve", lambda e: e.tensor_scalar(msk[:], pre[:], -PI, 2 * PI, ALU.is_lt, ALU.mult), reads=["pre"], writes=["msk"])
                    S.op("dve", lambda e: e.tensor_tensor(pre[:], pre[:], msk[:], ALU.add), reads=["pre", "msk"], writes=["pre"])
                S.op("act", lambda e: e.activation(dst[:], pre[:], AF.Sin), reads=["pre"], writes=[key])

            for fc_ in range(nfc):
                sl = slice(fc_ * FW, (fc_ + 1) * FW)
                S.dma("sp", zt[:], io["zT"][:, sl], writes=["zt"]); S.dma("sp", tnt[:], io["tn"][:, sl], writes=["tnt"])
                S.op("pe", lambda e: e.matmul(cx.PS[0][0:64, 0:FW], small["fw1"][:, :], zt[:, :], start=True, stop=True), reads=["zt", "w_fw1"], writes=["ps0"])
                sin_layer(cx.PS[0], small["fb1"], h1, "h1")
                S.op("pe", lambda e: e.matmul(cx.PS[1][0:64, 0:FW], small["fw2"][:, :], h1[:, :], start=True, stop=True), reads=["h1", "w_fw2"], writes=["ps1"])
                sin_layer(cx.PS[1], small["fb2"], h2, "h2f")
                S.op("act", lambda e: e.activation(dec[:], tnt[:], AF.Exp, scale=small["ndel"][:, 0:1]), reads=["tnt", "w_ndel"], writes=["dec"])
                for oc in range(2):
                    S.op("pe", lambda e, oc=oc: e.matmul(cx.PS[2 + oc][:, 0:FW], small["fw3"][:, oc * 128:(oc + 1) * 128], h2[:, :], start=True, stop=True),
                         reads=["h2f", "w_fw3"], writes=[f"ps{2+oc}"])
                    S.op("dve", lambda e, oc=oc: e.scalar_tensor_tensor(hf[oc][:], cx.PS[2 + oc][:, 0:FW], small["fb3"][:, oc:oc + 1], dec[:], ALU.add, ALU.mult),
                         reads=[f"ps{2+oc}", "w_fb3", "dec"], writes=[f"hf{oc}"])
                    S.op("act", lambda e, oc=oc, fc_=fc_: e.activation(junk[:], hf[oc][:], AF.Square, accum_out=accsq[:, oc, fc_:fc_ + 1]),
                         reads=[f"hf{oc}", "accsq"], writes=["fjunk", "accsq"])
                    S.dma("poolq", filt_s[oc][:, sl], hf[oc][:], reads=[f"hf{oc}"], writes=["filt_s"])
            S.op("dve", lambda e: e.tensor_reduce(ssum[:], accsq[:], AX.X, ALU.add), reads=["accsq"], writes=["ssum"])
            S.op("pe", lambda e: e.matmul(cx.PS[4][:, 0:2], c["PSM"][:, :], ssum[:, :], start=True, stop=True), reads=["ssum", "c_PSM"], writes=["ps4"])
            S.op("dve", lambda e: e.tensor_scalar(rn[:], cx.PS[4][:, 0:2], EPS, None, ALU.add), reads=["ps4"], writes=["rn"])
            S.op("act", lambda e: e.sqrt(rn[:], rn[:]), reads=["rn"], writes=["rn"])
            S.op("dve", lambda e: e.reciprocal(rn[:], rn[:]), reads=["rn"], writes=["rn"])
            nb = cx.sb("nb", [128, 2048], F32, st)
            NW = min(2048, Le)
            for oc in range(2):
                for c0 in range(0, Le, NW):
                    S.dma("sp", nb[:, 0:NW], filt_s[oc][:, c0:c0 + NW], reads=["filt_s"], writes=["nb"])
                    S.op("dve", lambda e, oc=oc: e.tensor_scalar(nb[:, 0:NW], nb[:, 0:NW], rn[:, oc:oc + 1], None, ALU.mult), reads=["nb", "rn"], writes=["nb"])
                    if c0 == 0:
                        S.op("pool", lambda e: e.memset(nb[64:128, 0:1], 0.0), reads=["nb"], writes=["nb"])
                    S.dma("poolq", filt_s[oc][:, c0:c0 + NW], nb[:, 0:NW], reads=["nb"], writes=["filt_s"])
            S.barrier()

        with ExitStack() as st:
            Af = cx.sb("Af", [64, 2, 256], F32, st); Ab = cx.sb("Ab", [64, 2, 256], BF16, st)
            Bp = [cx.sb(f"Bp{i}", [128, 2, 2, 128], BF16, st) for i in range(2)]
            tw = [cx.sb(f"tw{i}", [128, 2, 128], F32, st) for i in range(4)]
            Xr = cx.sb("Xr", [128, 2, 2, 128], F32, st); Xi = cx.sb("Xi", [128, 2, 2, 128], F32, st)
            Kr = [cx.sb(f"Kr{o}", [128, 2, 2, 128], F32, st) for o in range(2)]; Ki = [cx.sb(f"Ki{o}", [128, 2, 2, 128], F32, st) for o in range(2)]
            yt_ = [cx.sb(f"yt{i}", [128, 2, 2, 128], F32, st) for i in range(4)]
            Yb = cx.sb("Yb", [128, 2, 2, 2, 128], BF16, st)
            Cp = cx.sb("Cp", [128, 2, 2, 256], BF16, st)
            it_ = [cx.sb(f"it{i}", [128, 256], F32, st) for i in range(4)]
            x1t = cx.sb("x1t", [64, 2, 256], F32, st); x2t = cx.sb("x2t", [64, 2, 256], F32, st); vt = cx.sb("vt", [64, 2, 256], F32, st)
            zt_ = cx.sb("zt_", [64, 2, 256], F32, st); e1 = cx.sb("e1", [64, 2, 256], F32, st); hout = cx.sb("hout", [64, 2, 256], F32, st)

            def tb(src2d):
                return src2d.rearrange("c (n1 n2) -> n1 c n2", n2=256)

            def fwd(Xr_, Xi_, xkey):
                for cc in range(2):
                    ps = cx.PS[cc]
                    for g in range(2):
                        S.op("pe", lambda e, ps=ps, g=g, cc=cc: e.matmul(ps[:, g * 256:(g + 1) * 256], Ab[:, g, cc * 128:(cc + 1) * 128], c["F1cat"][:, :],
                                                                      start=True, stop=True), reads=["Ab", "c_F1cat"], writes=[f"ps{cc}"])
                    psv = ps[:].rearrange("p (g r k) -> p g r k", g=2, r=2)
                    Trb = c["Tr"][:, cc, :].unsqueeze(1).to_broadcast([128, 2, 128]); Tib = c["Ti"][:, cc, :].unsqueeze(1).to_broadcast([128, 2, 128])
                    S.op("dve", lambda e, psv=psv, Trb=Trb: e.tensor_tensor(tw[0][:], psv[:, :, 0, :], Trb, ALU.mult), reads=[f"ps{cc}", "c_Tr"], writes=["tw0"])
                    S.op("dve", lambda e, psv=psv, Tib=Tib: e.tensor_tensor(tw[1][:], psv[:, :, 1, :], Tib, ALU.mult), reads=[f"ps{cc}", "c_Ti"], writes=["tw1"])
                    S.op("dve", lambda e, psv=psv, Tib=Tib: e.tensor_tensor(tw[2][:], psv[:, :, 0, :], Tib, ALU.mult), reads=[f"ps{cc}", "c_Ti"], writes=["tw2"])
                    S.op("dve", lambda e, psv=psv, Trb=Trb: e.tensor_tensor(tw[3][:], psv[:, :, 1, :], Trb, ALU.mult), reads=[f"ps{cc}", "c_Tr"], writes=["tw3"])
                    S.op("pool", lambda e, cc=cc: e.tensor_tensor(Bp[cc][:, 0, :, :], tw[0][:], tw[1][:], ALU.subtract), reads=["tw0", "tw1"], writes=[f"Bp{cc}"])
                    S.op("pool", lambda e, cc=cc: e.tensor_tensor(Bp[cc][:, 1, :, :], tw[2][:], tw[3][:], ALU.add), reads=["tw2", "tw3"], writes=[f"Bp{cc}"])
                for kc in range(2):
                    ks = slice(kc * 128, (kc + 1) * 128)
                    psr, psi = cx.PS[2 + 2 * kc], cx.PS[3 + 2 * kc]
                    seq_r = [(c["F2r"], 0), (c["F2in"], 1)]
                    seq_i = [(c["F2i"], 0), (c["F2r"], 1)]
                    for (pp, seq, pk) in ((psr, seq_r, f"ps{2+2*kc}"), (psi, seq_i, f"ps{3+2*kc}")):
                        n_ = 0
                        for cc in range(2):
                            for (M_, ri) in seq:
                                S.op("pe", lambda e, pp=pp, M_=M_, ri=ri, cc=cc, n_=n_, ks=ks: e.matmul(
                                    pp[:, 0:256], M_[:, cc, ks], Bp[cc][:, ri, :, :], start=(n_ == 0), stop=(n_ == 3)),
                                    reads=[f"Bp{cc}"] + ck, writes=[pk])
                                n_ += 1
                    S.op("act", lambda e, psr=psr, kc=kc: e.copy(Xr_[:, kc, :, :], psr[:, 0:256]), reads=[f"ps{2+2*kc}"], writes=[xkey + "r"])
                    S.op("act", lambda e, psi=psi, kc=kc: e.copy(Xi_[:, kc, :, :], psi[:, 0:256]), reads=[f"ps{3+2*kc}"], writes=[xkey + "i"])

            def conv(o, ydst_key):
                fwd(Xr, Xi, "X")
                fl = lambda t_: t_[:].rearrange("p a b c -> p (a b c)")
                S.op("dve", lambda e: e.tensor_tensor(fl(yt_[0]), fl(Xr), fl(Kr[o]), ALU.mult), reads=["Xr", f"K{o}r"], writes=["yt0"])
                S.op("pool", lambda e: e.tensor_tensor(fl(yt_[1]), fl(Xi), fl(Ki[o]), ALU.mult), reads=["Xi", f"K{o}i"], writes=["yt1"])
                S.op("dve", lambda e: e.tensor_tensor(fl(yt_[2]), fl(Xr), fl(Ki[o]), ALU.mult), reads=["Xr", f"K{o}i"], writes=["yt2"])
                S.op("pool", lambda e: e.tensor_tensor(fl(yt_[3]), fl(Xi), fl(Kr[o]), ALU.mult), reads=["Xi", f"K{o}r"], writes=["yt3"])
                S.op("dve", lambda e: e.tensor_tensor(Yb[:, :, 0, :, :], yt_[0][:], yt_[1][:], ALU.subtract), reads=["yt0", "yt1"], writes=["Yb"])
                S.op("pool", lambda e: e.tensor_tensor(Yb[:, :, 1, :, :], yt_[2][:], yt_[3][:], ALU.add), reads=["yt2", "yt3"], writes=["Yb"])
                for g in range(2):
                    ps = cx.PS[g]
                    n_ = 0
                    for kc in range(2):
                        for (ri, M_) in ((0, c["IA"]), (1, c["IB"])):
                            S.op("pe", lambda e, ps=ps, kc=kc, ri=ri, M_=M_, g=g, n_=n_: e.matmul(ps[:, :], Yb[:, kc, ri, g, :], M_[:, kc, :], start=(n_ == 0), stop=(n_ == 3)),
                                 reads=["Yb"] + ck, writes=[f"ps{g}"])
                            n_ += 1
                    S.op("dve", lambda e, ps=ps: e.tensor_tensor(it_[0][:], ps[:, 0:256], c["ITr"][:], ALU.mult), reads=[f"ps{g}", "c_ITr"], writes=["it0"])
                    S.op("dve", lambda e, ps=ps: e.tensor_tensor(it_[1][:], ps[:, 256:512], c["ITi"][:], ALU.mult), reads=[f"ps{g}", "c_ITi"], writes=["it1"])
                    S.op("dve", lambda e, ps=ps: e.tensor_tensor(it_[2][:], ps[:, 0:256], c["ITi"][:], ALU.mult), reads=[f"ps{g}", "c_ITi"], writes=["it2"])
                    S.op("dve", lambda e, ps=ps: e.tensor_tensor(it_[3][:], ps[:, 256:512], c["ITr"][:], ALU.mult), reads=[f"ps{g}", "c_ITr"], writes=["it3"])
                    S.op("pool", lambda e, g=g: e.tensor_tensor(Cp[:, 0, g, :], it_[0][:], it_[1][:], ALU.subtract), reads=["it0", "it1"], writes=["Cp"])
                    S.op("pool", lambda e, g=g: e.tensor_tensor(Cp[:, 1, g, :], it_[2][:], it_[3][:], ALU.add), reads=["it2", "it3"], writes=["Cp"])
                S.op("pe", lambda e: e.matmul(cx.PS[6][0:64, :], c["G1r"][:, :], Cp[:, 0, :, :], start=True, stop=False), reads=["Cp", "c_G1r"], writes=["ps6"])
                S.op("pe", lambda e: e.matmul(cx.PS[6][0:64, :], c["G1i"][:, :], Cp[:, 1, :, :], start=False, stop=True), reads=["Cp", "c_G1i"], writes=["ps6"])

            for pr in range(32):
                ch0 = pr * 2
                for o in range(2):
                    for d_ in range(2):
                        S.dma("sp", Af[:], tb(filt_s[o][d_ * 64 + ch0:d_ * 64 + ch0 + 2, :]), reads=["filt_s"], writes=["Af"])
                        S.op("dve", lambda e: e.tensor_copy(Ab[:], Af[:]), reads=["Af"], writes=["Ab"])
                        if d_ == 0:
                            fwd(Kr[o], Ki[o], f"K{o}")
                        else:
                            fwd(Xr, Xi, "X")
                            S.op("pool", lambda e, o=o: e.tensor_tensor(Kr[o][:], Kr[o][:], Xr[:], ALU.add), reads=[f"K{o}r", "Xr"], writes=[f"K{o}r"])
                            S.op("pool", lambda e, o=o: e.tensor_tensor(Ki[o][:], Ki[o][:], Xi[:], ALU.subtract), reads=[f"K{o}i", "Xi"], writes=[f"K{o}i"])
                S.dma("sp", x1t[:], tb(scx[0][ch0:ch0 + 2, :]), reads=["scx"], writes=["x1t"])
                S.dma("sp", x2t[:], tb(scx[1][ch0:ch0 + 2, :]), reads=["scx"], writes=["x2t"])
                S.dma("sp", vt[:], tb(scx[2][ch0:ch0 + 2, :]), reads=["scx"], writes=["vt"])
                S.op("dve", lambda e: e.tensor_copy(Ab[:], vt[:]), reads=["vt"], writes=["Ab"])
                conv(0, "y1")
                dk = lambda o: small["dsk"][:, o, ch0:ch0 + 2].unsqueeze(2).to_broadcast([64, 2, 256])
                psy = cx.PS[6][0:64, :].rearrange("p (g n) -> p g n", g=2)
                dk0 = dk(0); dk1 = dk(1)
                S.op("pool", lambda e, dk0=dk0: e.tensor_tensor(e1[:], vt[:], dk0, ALU.mult), reads=["vt", "w_dsk"], writes=["e1"])
                S.op("dve", lambda e: e.tensor_tensor(e1[:], e1[:], psy, ALU.add), reads=["e1", "ps6"], writes=["e1"])
                S.op("dve", lambda e: e.tensor_tensor(zt_[:], e1[:], x1t[:], ALU.mult), reads=["e1", "x1t"], writes=["zt_"])
                S.op("dve", lambda e: e.tensor_copy(Ab[:], zt_[:]), reads=["zt_"], writes=["Ab"])
                conv(1, "y2")
                S.op("pool", lambda e, dk1=dk1: e.tensor_tensor(e1[:], zt_[:], dk1, ALU.mult), reads=["zt_", "w_dsk"], writes=["e1"])
                S.op("dve", lambda e: e.tensor_tensor(e1[:], e1[:], psy, ALU.add), reads=["e1", "ps6"], writes=["e1"])
                S.op("dve", lambda e: e.tensor_tensor(hout[:], e1[:], x2t[:], ALU.mult), reads=["e1", "x2t"], writes=["hout"])
                if Le == L:
                    S.dma("poolq", tb(io["oh"][ch0:ch0 + 2, :]), hout[:], reads=["hout"], writes=["oh"])
                else:
                    S.dma("poolq", io["oh"][ch0:ch0 + 2, :].rearrange("(o c) n -> o c n", o=1), hout[0:1, :, 0:Le], reads=["hout"], writes=["oh"])
            S.barrier()

    for tag_, Le_ in paths:
        do_path(tag_, Le_)
    cx.end_phase()


POOL_SIZES = (2, 4, 8, 16)
NKEY = SEQ + CTX
NLOC = TPC + CPC
GROUPS = [[0, 1, 2, 3], [4, 5, 6, 7]]
SECS = {"aq": 0, "ak": 256, "av": 512, "bq": 768, "bk": 1024, "bv": 1280, "pool": 1536, "hy0": 1792, "hy1": 2048, "hy2": 2304}


def emit_R(cx, need_ctx, T):
    S = cx.S
    ag_out = T["ag_out"]
    selq_d = cx.inp("selq", [128, 2, 2, 32]); selg_d = cx.inp("selg", [128, 2, 64])
    selq = cx.sb("selq", [128, 2, 2, 32]); selg = cx.sb("selg", [128, 2, 64])
    S.dma("sp", selq[:], selq_d, writes=["selq"]); S.dma("sp", selg[:], selg_d, writes=["selg"])
    xs = [cx.sb(f"rx{i}", [128, 2, 512]) for i in range(3)]
    ev = [cx.sb(f"rev{i}", [64, 512]) for i in range(2)]
    k33 = [cx.sb(f"k33_{i}", [33, 512]) for i in range(2)]
    k65 = cx.sb("k65", [65, 512])
    v65 = [cx.sb(f"v65_{i}", [128, 65]) for i in range(2)]
    zero = cx.sb("rzero", [64, 16])
    S.op("pool", lambda e: e.memset(zero[:], 0.0), writes=["rzero"])
    for t_ in k33:
        S.op("pool", lambda e, t_=t_: e.memset(t_[32:33, :], 1.0), writes=["k33"])
    S.op("pool", lambda e: e.memset(k65[64:65, :], 1.0), writes=["k65"])
    for t_ in v65:
        S.op("pool", lambda e, t_=t_: e.memset(t_[:, 64:65], 1.0), writes=["v65"])
    for tag, Le in (("l", SEQ),) + ((("c", CTX),) if need_ctx else ()):
        for p in range(3):
            S.dma("sp", T[f"hy_{tag}"][p][:, 0:1], zero[:, 0:1], reads=["rzero"], writes=["hy"], allow_slow_non_contiguous=True)
            S.dma("sp", T[f"hy_{tag}"][p][:, Le + 1:Le + 2], zero[:, 0:1], reads=["rzero"], writes=["hy"], allow_slow_non_contiguous=True)
        S.dma("sp", T[f"pl_{tag}"][:, 0:8], zero[:, 0:8], reads=["rzero"], writes=["pl"])
        S.dma("sp", T[f"pl_{tag}"][:, Le + 8:Le + 24], zero[:, 0:16], reads=["rzero"], writes=["pl"])
    cnt = {"x": 0, "ps": 0, "ev": 0, "k": 0, "v": 0, "q": 0}

    def load(sec, pieces, w):
        i = cnt["x"] % 3; cnt["x"] += 1
        X = xs[i]
        o = 0
        for (r, c0, pw) in pieces:
            n = pw // 64
            for kc in range(2):
                r0 = r * DIN + SECS[sec] + kc * 128
                src = ag_out[c0 // 64:c0 // 64 + n, r0:r0 + 128, :].rearrange("ck p t -> p ck t")
                q = ("sp", "actq")[cnt["q"] % 2]; cnt["q"] += 1
                S.dma(q, X[:, kc, o:o + pw].rearrange("p (ck t) -> p ck t", t=64), src, writes=[f"rx{i}"])
            o += pw
        return X, f"rx{i}"

    def sel_fm(X, xk, w, SEL, M, dst, ones=None):
        pi = cnt["ps"] % 8; cnt["ps"] += 1
        ps = cx.PS[pi]
        for kc in range(2):
            S.op("pe", lambda e, ps=ps, kc=kc, SEL=SEL: e.matmul(ps[0:M, 0:w], SEL[:, kc, :], X[:, kc, 0:w], start=(kc == 0), stop=(kc == 1)),
                 reads=[xk, "selq", "selg"], writes=[f"ps{pi}"])
        if ones == "k33":
            i = cnt["k"] % 2; cnt["k"] += 1
            dt_, dk, rows = k33[i], f"k33_{i}", 33
        elif ones == "k65":
            dt_, dk, rows = k65, "k65", 65
        else:
            i = cnt["ev"] % 2; cnt["ev"] += 1
            dt_, dk, rows = ev[i], f"rev{i}", M
        S.op(("act", "dve")[cnt["ps"] % 2], lambda e, ps=ps, dt_=dt_: (e.copy if hasattr(e, "copy") else e.tensor_copy)(dt_[0:M, 0:w], ps[0:M, 0:w]),
             reads=[f"ps{pi}"], writes=[dk])
        S.dma("poolq", dst, dt_[0:rows, 0:w], reads=[dk], writes=["rdst"])

    def sel_tm(X, xk, w, dst_rows):
        for s0 in range(0, w, 128):
            pi = cnt["ps"] % 8; cnt["ps"] += 1
            ps = cx.PS[pi]
            for kc in range(2):
                S.op("pe", lambda e, ps=ps, kc=kc, s0=s0: e.matmul(ps[:, 0:64], X[:, kc, s0:s0 + 128], selg[:, kc, :], start=(kc == 0), stop=(kc == 1)),
                     reads=[xk, "selg"], writes=[f"ps{pi}"])
            i = cnt["v"] % 2; cnt["v"] += 1
            S.op(("act", "dve")[i], lambda e, ps=ps, i=i: (e.copy if hasattr(e, "copy") else e.tensor_copy)(v65[i][:, 0:64], ps[:, 0:64]),
                 reads=[f"ps{pi}"], writes=[f"v65_{i}"])
            S.dma("poolq", dst_rows[s0:s0 + 128, :], v65[i][:, :], reads=[f"v65_{i}"], writes=["rdst"])

    chunks = [("l", [(r, cp * 512, 512)], r * TPC + cp * 512, 512) for r in range(4) for cp in range(8)]
    chunks.append(("c", [(r, TPC, CPC) for r in range(4)], SEQ, CTX))
    for kind, pieces, t0, w in chunks:
        isl = kind == "l"
        if isl or need_ctx:
            X, xk = load("aq", pieces, w)
            for c_ in range(2):
                sel_fm(X, xk, w, selq[:, :, c_, :], 32, (T["aqT"][c_][:, t0:t0 + w] if isl else T["aqcT"][c_][:, :]))
            X, xk = load("bq", pieces, w)
            sel_fm(X, xk, w, selg, 64, (T["bqT"][:, t0:t0 + w] if isl else T["bqcT"][:, :]))
            tag = "l" if isl else "c"
            tt = t0 if isl else 0
            X, xk = load("pool", pieces, w)
            sel_fm(X, xk, w, selg, 64, T[f"pl_{tag}"][:, 8 + tt:8 + tt + w])
            for p in range(3):
                X, xk = load(f"hy{p}", pieces, w)
                sel_fm(X, xk, w, selg, 64, T[f"hy_{tag}"][p][:, 1 + tt:1 + tt + w])
        X, xk = load("ak", pieces, w)
        for c_ in range(2):
            sel_fm(X, xk, w, selq[:, :, c_, :], 32, T["akT"][c_][:, t0:t0 + w], ones="k33")
        X, xk = load("bk", pieces, w)
        sel_fm(X, xk, w, selg, 64, T["bkT"][:, t0:t0 + w], ones="k65")
        X, xk = load("av", pieces, w)
        sel_tm(X, xk, w, T["av"][t0:t0 + w, :])
        X, xk = load("bv", pieces, w)
        sel_tm(X, xk, w, T["bv"][t0:t0 + w, :])
    cx.end_phase()


def emit_W(cx, need_ctx, T):
    S = cx.S
    mixT = T["mixT"]; rs_in = T["rs_in"]
    wop_d = cx.inp("wo_part", [4, 64, D])
    wst = cx.sb("wwst", [64, 4, D]); wb = cx.sb("wwb", [64, 4, D], BF16)
    S.dma("sp", wst[:], wop_d.rearrange("m p n -> p m n"), writes=["wwst"])
    S.op("dve", lambda e: e.tensor_copy(wb[:], wst[:]), reads=["wwst"], writes=["wwb"])
    mt = [cx.sb(f"wmt{i}", [64, 4, 128]) for i in range(2)]
    mb = [cx.sb(f"wmb{i}", [64, 4, 128], BF16) for i in range(2)]
    ot = [cx.sb(f"wot{i}", [128, D]) for i in range(2)]
    rs_out = T["rs_out"]
    cckey = cx.coll_group()
    nlb = TPC // 128
    order = [j * nlb + lb for lb in range(nlb) for j in range(4)]
    if need_ctx:
        order += [SEQ // 128 + ci for ci in range(CTX // 128)]
    NCH = NLOC // RSR
    issued = [0]
    chunk_keys = {k: [] for k in range(NCH)}

    def issue_upto(local_done):
        while issued[0] < NCH and (issued[0] + 1) * RSR <= local_done:
            k = issued[0]
            cx.coll(cckey, "ReduceScatter", ALU.add, GROUPS, rs_in[k], rs_out[k * RSR:(k + 1) * RSR, :], sorted(set(chunk_keys[k])))
            issued[0] += 1

    for n_, i in enumerate(order):
        s = n_ % 2
        S.dma(("sp", "actq")[s], mt[s][:], mixT[:, :, i * 128:(i + 1) * 128].rearrange("m p t -> p m t"), writes=[f"wmt{s}"])
        S.op("pool", lambda e, s=s: e.tensor_copy(mb[s][:], mt[s][:]), reads=[f"wmt{s}"], writes=[f"wmb{s}"])
        for hb_ in range(2):
            pi = (2 * n_ + hb_) % 8
            ps = cx.PS[pi]
            for m in range(4):
                S.op("pe", lambda e, ps=ps, m=m, hb_=hb_, s=s: e.matmul(ps[:, :], mb[s][:, m, :], wb[:, m, hb_ * 512:(hb_ + 1) * 512], start=(m == 0), stop=(m == 3)),
                     reads=[f"wmb{s}", "wwb"], writes=[f"ps{pi}"])
            S.op(("act", "dve")[hb_], lambda e, ps=ps, hb_=hb_, s=s: (e.copy if hasattr(e, "copy") else e.tensor_copy)(ot[s][:, hb_ * 512:(hb_ + 1) * 512], ps[:, :]),
                 reads=[f"ps{pi}"], writes=[f"wot{s}h{hb_}"])

        def put(j, loc, p0, n):
            while n > 0:
                k, q0 = loc // RSR, loc % RSR
                m_ = min(n, RSR - q0)
                key = f"rs_in{k}_{j}_{q0}"
                S.dma("poolq", rs_in[k][j * RSR + q0:j * RSR + q0 + m_, :], ot[s][p0:p0 + m_, :], reads=[f"wot{s}h0", f"wot{s}h1"], writes=[key])
                chunk_keys[k].append(key)
                loc += m_; p0 += m_; n -= m_
        if i < SEQ // 128:
            j, lb = i // nlb, i % nlb
            put(j, lb * 128, 0, 128)
            if j == 3 and need_ctx:
                issue_upto((lb + 1) * 128)
            elif j == 3:
                issue_upto((lb + 1) * 128 if lb < nlb - 1 else NLOC)
        else:
            ci = i - SEQ // 128
            for hf in range(2):
                put(ci * 2 + hf, TPC, hf * 64, 64)
            if ci == CTX // 128 - 1:
                issue_upto(NLOC)
    cx.end_phase()


SHARED = {"sel", "ident", "identF", "ropec", "ropes", "onesb", "E65", "ones64", "cT", "fin_g", "router_w", "router_b",
          "zT_l", "tn_l", "zT_c", "tn_c", "icnt_l", "icnt_c", "psel", "ndel", "selq", "selg", "xl", "xc"}


def build_fused(stop=None):
    cx = Ctx("F")
    cx.shared = set(SHARED)
    sc = cx.scratch
    T = {"ag_in": sc("ag_in", [NAGC, DIN, 64]), "ag_out": sc("ag_out", [NAGC, 4 * DIN, 64]),
         "aqT": sc("aqT", [2, 32, SEQ]), "akT": sc("akT", [2, 33, NKEY]), "av": sc("av", [NKEY, 65]), "aqcT": sc("aqcT", [2, 32, CTX]),
         "bqT": sc("bqT", [64, SEQ]), "bkT": sc("bkT", [65, NKEY]), "bv": sc("bv", [NKEY, 65]), "bqcT": sc("bqcT", [64, CTX]),
         "hy_l": sc("hy_l", [3, 64, SEQ + 2]), "pl_l": sc("pl_l", [64, SEQ + 24]),
         "hy_c": sc("hy_c", [3, 64, CTX + 2]), "pl_c": sc("pl_c", [64, CTX + 24]),
         "mixT": sc("mixT", [4, 64, NKEY]), "rs_in": sc("rs_in", [NLOC // RSR, 4 * RSR, D]), "rs_out": sc("rs_out", [NLOC, D]),
         "xl_s": sc("xl_s", [TPC, D]), "xc_s": sc("xc_s", [CPC, D]), "dummy": sc("dummy_oc", [CPC, D])}
    x_l = cx.inp("xl", [TPC, D]); x_c = cx.inp("xc", [CPC, D])
    out = cx.nc.dram_tensor("out", [TPC, D], F32, kind="ExternalOutput").ap()
    mixT = T["mixT"]

    def dump(src2d):
        r, c_ = src2d.shape
        dst = out.rearrange("a d -> (a d)")[0:r * c_].rearrange("(r c) -> r c", c=c_)
        cx.S.dma("sp", dst, src2d, writes=["dbg"])
        return cx.finish(), cx

    for li in range(2):
        need_ctx = li < 1
        cx.prefix = f"L{li}_"
        cx.over = {"xl": x_l if li == 0 else T["xl_s"], "xc": x_c if li == 0 else T["xc_s"], "ag_in": T["ag_in"], "ag_out": T["ag_out"]}
        emit_A(cx)
        if stop == "A":
            return dump(T["ag_in"][0:25, 0:DIN, :].rearrange("a b c -> a (b c)"))
        if stop == "AG":
            return dump(T["ag_out"][0:25, DIN:2 * DIN, :].rearrange("a b c -> a (b c)"))
        cx.over = {}
        emit_R(cx, need_ctx, T)
        if stop == "R":
            return dump(T["bkT"][:, :])
        cx.over = {k: T[k] for k in ("aqT", "akT", "av", "aqcT", "bqT", "bkT", "bv", "bqcT")}
        cx.over.update({"oa": mixT[0][:, 0:SEQ], "oac": mixT[0][:, SEQ:NKEY], "ob": mixT[1][:, 0:SEQ], "obc": mixT[1][:, SEQ:NKEY]})
        emit_B1(cx, li)
        if stop == "B1":
            return dump(mixT[1][:, :])
        cx.over = {"hy_l": T["hy_l"], "pl_l": T["pl_l"], "hy_c": T["hy_c"], "pl_c": T["pl_c"],
                   "op_l": mixT[2][:, 0:SEQ], "op_c": mixT[2][:, SEQ:NKEY], "oh_l": mixT[3][:, 0:SEQ], "oh_c": mixT[3][:, SEQ:NKEY]}
        emit_B2(cx, need_ctx)
        cx.over = {}
        if stop == "B2":
            return dump(mixT[3][:, :])
        emit_W(cx, need_ctx, T)
        if stop == "W":
            return dump(T["rs_in"][0:4, :, :].rearrange("a b c -> (a b) c"))
        if stop == "RS":
            return dump(T["rs_out"][0:TPC, :])
        cx.over = {"xl": x_l if li == 0 else T["xl_s"], "xc": x_c if li == 0 else T["xc_s"], "rs_out": T["rs_out"],
                   "ol": T["xl_s"] if li == 0 else out, "oc": T["xc_s"] if li == 0 else T["dummy"]}
        emit_C(cx, need_ctx, li == 1)
    return cx.finish(), cx


_CACHE = {}
STOP = None


def kernel(x, c, ctx, c_ctx, norm1_g, norm2_g, ada_w, ada_b, w_in, w_out, a_lambda, a_subln_g,
           b_rpb, pool_w, pool_scale, hy_short_w, hy_short_b, hy_f_w1, hy_f_b1, hy_f_w2, hy_f_b2,
           hy_f_w3, hy_f_b3, hy_skip, router_w, router_b, moe_w1, moe_w3, moe_w2, final_g):
    f = lambda a: np.ascontiguousarray(np.asarray(a, dtype=np.float32))
    if "F" not in _CACHE:
        _CACHE["F"] = build_fused(STOP)
    nc, cx = _CACHE["F"]
    x, ctx, c, c_ctx = f(x), f(ctx), f(c), f(c_ctx)
    rc, rs = const_rope()
    FC = fft_consts()
    deltas = np.abs(np.linspace(math.log(1e-2) / 1.5, math.log(1e-2) / 0.3, 256, dtype=np.float32))
    pos = {"l": hy_pos_consts(SEQ), "c": hy_pos_consts(CTX)}
    E65 = np.zeros((65, 64), np.float32); E65[64] = 1.0
    shared_all = {"sel": const_sel(), "ident": _bf16(np.eye(128)), "identF": np.eye(128, dtype=np.float32), "onesb": _bf16(np.ones((128, 128))),
                  "E65": E65, "ones64": np.ones((64, 64), np.float32), "fin_g": f(final_g).reshape(1, -1),
                  "router_w": f(router_w), "router_b": f(router_b).reshape(1, -1),
                  "zT_l": pos["l"][0], "tn_l": pos["l"][1], "zT_c": pos["c"][0], "tn_c": pos["c"][1]}
    shared_all.update({"c_" + k: v for k, v in FC.items()})
    in_maps = []
    for core in range(NCORE):
        b, j = core // 4, core % 4
        h = g = j
        chs = slice(g * 64, (g + 1) * 64)
        m = dict(shared_all)
        m["xl"] = np.ascontiguousarray(x[b, j * TPC:(j + 1) * TPC]); m["xc"] = np.ascontiguousarray(ctx[b, j * CPC:(j + 1) * CPC])
        m["cT"] = cT_layout(c[b], c_ctx)
        m["ropec"] = np.ascontiguousarray(rc[j * TPC:(j + 1) * TPC]); m["ropes"] = np.ascontiguousarray(rs[j * TPC:(j + 1) * TPC])
        selq = np.zeros((256, 2, 32), np.float32); selg = np.zeros((256, 64), np.float32)
        for c_ in range(2):
            selq[h * 64 + c_ * 32 + np.arange(32), c_, np.arange(32)] = 1.0
        selg[h * 64 + np.arange(64), np.arange(64)] = 1.0
        m["selq"] = np.ascontiguousarray(selq.reshape(2, 128, 2, 32).transpose(1, 0, 2, 3))
        m["selg"] = np.ascontiguousarray(selg.reshape(2, 128, 64).transpose(1, 0, 2))
        for tag, Le in (("l", SEQ), ("c", CTX)):
            t = np.arange(Le); w = POOL_SIZES[g]
            cnt = (np.clip(t + w // 2, 0, Le) - np.clip(t - w // 2, 0, Le)).astype(np.float32)
            m[f"icnt_{tag}"] = np.ascontiguousarray(np.broadcast_to((1.0 / cnt)[None, :], (64, Le))).astype(np.float32)
        ps = np.zeros((64, 4), np.float32); ps[:, g] = 1.0
        m["psel"] = ps
        m["ndel"] = np.ascontiguousarray(np.tile(-deltas[chs], 2).reshape(128, 1))
        for li in range(2):
            p = f"L{li}_"
            m[p + "ada_w"] = f(ada_w[li]); m[p + "ada_b"] = f(ada_b[li]).reshape(1, -1)
            m[p + "norm_g"] = f(norm1_g[li]).reshape(1, -1); m[p + "norm2_g"] = f(norm2_g[li]).reshape(1, -1)
            m[p + "w_in"] = f(w_in[li])
            m[p + "alam"] = f(a_lambda[li]).reshape(1, 128); m[p + "subg"] = f(a_subln_g[li]).reshape(64, 1)
            m[p + "bias"] = na_bias_sets(f(b_rpb[li])[h])
            sw = f(hy_short_w[li]).reshape(3, 3, 256)[:, :, chs]
            m[p + "shw"] = np.ascontiguousarray(sw.transpose(2, 1, 0)); m[p + "shb"] = np.ascontiguousarray(f(hy_short_b[li]).reshape(3, 256)[:, chs].T)
            m[p + "fw1"] = f(hy_f_w1[li]); m[p + "fb1"] = f(hy_f_b1[li]).reshape(64, 1)
            m[p + "fw2"] = f(hy_f_w2[li]); m[p + "fb2"] = f(hy_f_b2[li]).reshape(64, 1)
            w3 = f(hy_f_w3[li]).reshape(64, 2, 2, 256)[:, :, :, chs]
            m[p + "fw3"] = np.ascontiguousarray(w3.reshape(64, 256))
            b3 = f(hy_f_b3[li]).reshape(2, 2, 256)[:, :, chs]
            m[p + "fb3"] = np.ascontiguousarray(b3.reshape(2, 128).T)
            m[p + "dsk"] = np.ascontiguousarray(np.broadcast_to(f(hy_skip[li])[:, chs][None], (64, 2, 64))).astype(np.float32)
            m[p + "pw"] = np.ascontiguousarray(f(pool_w[li])[g]); m[p + "psc"] = np.ascontiguousarray(f(pool_scale[li])[chs].reshape(64, 1))
            wo = f(w_out[li])
            m[p + "wo_part"] = np.ascontiguousarray(np.stack([wo[mm * 256 + h * 64:mm * 256 + (h + 1) * 64] for mm in range(4)]))
            m[p + "w1"] = f(moe_w1[li]); m[p + "w3"] = f(moe_w3[li]); m[p + "w2"] = f(moe_w2[li])
        in_maps.append({k: v for k, v in m.items() if k in cx.ins})
    missing = [k for k in cx.ins if k not in in_maps[0]]
    assert not missing, missing
    res = run_bass_kernel_spmd(nc, in_maps, core_ids=list(range(NCORE)))
    out = np.stack([np.concatenate([res.results[b * 4 + j]["out"] for j in range(4)], 0) for b in range(2)])
    return out.astype(np.float32)
```

```python
import math
from contextlib import ExitStack
import numpy as np
import concourse.bass as bass
import concourse.mybir as mybir
from concourse.bass_utils import run_bass_kernel_spmd

F32 = mybir.dt.float32
BF16 = mybir.dt.bfloat16
AF = mybir.ActivationFunctionType
ALU = mybir.AluOpType
AX = mybir.AxisListType

D = 1024
SEQ = 16384
NB = 2
CTX = 256
DIN = 2560
NCORE = 8
TPC = SEQ // 4
NAGC = 65
RSR = 208
CPC = CTX // 4
EPS = 1e-6

COMPUTE = ("pe", "dve", "act", "pool")
QUEUES = ("sp", "actq", "poolq")
ISSUER = {"sp": "sp", "actq": "act", "poolq": "pool"}
NDMASEM = 6


class Sched:
    def __init__(self, nc, same_engine_sync=True):
        self.nc = nc
        self.same = same_engine_sync
        self.streams = {e: [] for e in ("pe", "dve", "act", "pool", "sp")}
        self.cnt = {e: 0 for e in COMPUTE}
        self.sem = {}
        self.dsem = {}
        self.dnext = {q: 0 for q in QUEUES}
        self.seen = {}
        self.writer = {}
        self.readers = {}

    def alloc_sems(self, stack):
        for e in COMPUTE:
            self.sem[e] = stack.enter_context(self.nc.semaphore("s_" + e))
        for q in QUEUES:
            for i in range(NDMASEM):
                self.dsem[(q, i)] = [stack.enter_context(self.nc.semaphore(f"d_{q}{i}")), 0]

    def _deps(self, reads, writes):
        deps = []
        for b in reads:
            if b in self.writer:
                deps.append(self.writer[b])
        for b in writes:
            if b in self.writer:
                deps.append(self.writer[b])
            deps.extend(self.readers.get(b, ()))
        return deps

    def _record(self, tok, reads, writes):
        for b in reads:
            self.readers.setdefault(b, []).append(tok)
        for b in writes:
            self.writer[b] = tok
            self.readers[b] = []

    def _waits(self, stream, deps, eng_name):
        ws = []
        for (sk, val, en) in deps:
            if en == eng_name and (eng_name == "pe" or not self.same):
                continue
            key = (stream, sk)
            if self.seen.get(key, 0) >= val:
                continue
            self.seen[key] = val
            ws.append((sk, val))
        return ws

    def _semobj(self, sk):
        return self.sem[sk] if sk in self.sem else self.dsem[sk][0]

    def op(self, eng, fn, reads=(), writes=()):
        deps = self._deps(reads, writes)
        ws = self._waits(eng, deps, eng)
        self.cnt[eng] += 1
        tok = (eng, self.cnt[eng], eng)
        self.streams[eng].append((ws, fn, (eng, 1)))
        self._record(tok, reads, writes)
        return tok

    def dma(self, q, out, in_, reads=(), writes=(), **kw):
        stream = ISSUER[q]
        deps = self._deps(reads, writes)
        i = self.dnext[q]
        self.dnext[q] = (i + 1) % NDMASEM
        ent = self.dsem[(q, i)]
        if ent[1] > 0:
            deps = deps + [((q, i), ent[1], "dma")]
        ws = self._waits(stream, deps, "dma?")
        ent[1] += 16
        tok = ((q, i), ent[1], "dma")

        def fn(e, out=out, in_=in_, kw=kw):
            return e.dma_start(out=out, in_=in_, **kw)
        self.streams[stream].append((ws, fn, ((q, i), 16)))
        self._record(tok, reads, writes)
        return tok

    def _all_tokens(self):
        toks = [(e, self.cnt[e], "x") for e in COMPUTE if self.cnt[e] > 0]
        for k, ent in self.dsem.items():
            if ent[1] > 0:
                toks.append((k, ent[1], "dma"))
        return toks

    def barrier(self):
        toks = self._all_tokens()
        for s in self.streams:
            ws = self._waits(s, toks, "none")
            if ws:
                self.streams[s].append((ws, None, None))
        self.writer.clear()
        self.readers.clear()

    def emit(self):
        ws = [(sk, val) for (sk, val, _) in self._all_tokens()]
        self.streams["sp"].append((ws, None, None))
        nc = self.nc
        with nc.Block() as block:
            def mk(sname):
                def body(e):
                    for (ws, fn, inc) in self.streams[sname]:
                        for (sk, val) in ws:
                            e.wait_ge(self._semobj(sk), val)
                        if fn is not None:
                            fn(e).then_inc(self._semobj(inc[0]), inc[1])
                return body
            block.tensor(mk("pe"))
            block.vector(mk("dve"))
            block.scalar(mk("act"))
            block.gpsimd(mk("pool"))
            block.sync(mk("sp"))


class Ctx:
    def __init__(self, name="k", nc=None):
        self.nc = nc or bass.Bass("TRN2", target_bir_lowering=False)
        self.root = ExitStack()
        self.st = ExitStack()
        self.S = Sched(self.nc)
        self.S.alloc_sems(self.root)
        self.ins = {}
        self.outs = {}
        self.over = {}
        self.prefix = ""
        self.shared = set()
        self.PSALL = self.root.enter_context(self.nc.psum_tensor("psall", [128, 4096], F32))
        self.PS = [self.PSALL[:, i * 512:(i + 1) * 512] for i in range(8)]
        self._n = 0
        self._ncc = 0

    def inp(self, name, shape, dt=F32):
        if name in self.over:
            return self.over[name]
        full = name if (name in self.shared or name.startswith("c_")) else self.prefix + name
        if full in self.ins:
            return self.ins[full]
        t = self.nc.dram_tensor(full, list(shape), dt, kind="ExternalInput").ap()
        self.ins[full] = t
        return t

    def out(self, name, shape, dt=F32):
        if name in self.over:
            return self.over[name]
        t = self.nc.dram_tensor(self.prefix + name, list(shape), dt, kind="ExternalOutput").ap()
        self.outs[self.prefix + name] = t
        return t

    def scratch(self, name, shape, dt=F32):
        self._n += 1
        return self.nc.dram_tensor(f"scr{self._n}_{name}", list(shape), dt, kind="Internal").ap()

    def sb(self, name, shape, dt=F32, stack=None):
        self._n += 1
        return (stack or self.st).enter_context(self.nc.sbuf_tensor(f"sb{self._n}_{name}", list(shape), dt))

    def end_phase(self):
        self.S.barrier()
        self.st.close()
        self.st = ExitStack()

    def collective(self, kind, op, groups, pairs):
        S = self.S
        S.barrier()
        sem = self.root.enter_context(self.nc.semaphore(f"cc{self._ncc}"))
        key = ("cc", self._ncc)
        self._ncc += 1
        S.dsem[key] = [sem, len(pairs)]
        for (src, dst) in pairs:
            S.streams["pool"].append(([], lambda e, src=src, dst=dst: e.collective_compute(kind, op, replica_groups=groups, ins=[src], outs=[dst]), (key, 1)))
        S.barrier()

    def coll_group(self):
        sem = self.root.enter_context(self.nc.semaphore(f"cc{self._ncc}"))
        key = ("cc", self._ncc)
        self._ncc += 1
        self.S.dsem[key] = [sem, 0]
        return key

    def coll(self, key, kind, op, groups, src, dst, reads):
        S = self.S
        ws = S._waits("pool", S._deps(reads, []), "pool-cc")
        S.dsem[key][1] += 1
        S.streams["pool"].append((ws, lambda e: e.collective_compute(kind, op, replica_groups=groups, ins=[src], outs=[dst]), (key, 1)))

    def finish(self):
        self.S.emit()
        self.st.close()
        self.root.close()
        return self.nc


def emit_mod_rows(cx, st, scT, ada_w, ada_b, modrow, tag):
    S = cx.S
    wbuf = [cx.sb(f"adaw{tag}{i}", [128, 8, 512], F32, st) for i in range(2)]
    adab = cx.sb(f"adab{tag}", [2, 6144], F32, st)
    S.dma("sp", adab[0:1, :], ada_b, writes=["adab"])
    S.dma("sp", adab[1:2, :], ada_b, writes=["adab"])
    awv = ada_w.rearrange("(kc p) n -> p kc n", p=128)
    for cb in range(12):
        wb = wbuf[cb % 2]
        q = ("sp", "actq")[cb % 2]
        S.dma(q, wb[:, 0:4, :], awv[:, 0:4, cb * 512:(cb + 1) * 512], writes=[f"adaw{cb%2}a"])
        S.dma(q, wb[:, 4:8, :], awv[:, 4:8, cb * 512:(cb + 1) * 512], writes=[f"adaw{cb%2}b"])
        ps = cx.PS[cb % 2]
        for kc in range(8):
            S.op("pe", lambda e, ps=ps, kc=kc, wb=wb: e.matmul(ps[0:2, :], scT[:, kc, :], wb[:, kc, :],
                                                                start=(kc == 0), stop=(kc == 7)),
                 reads=["scT", f"adaw{cb%2}a", f"adaw{cb%2}b"], writes=[f"ps{cb%2}"])
        S.op("dve", lambda e, ps=ps, cb=cb: e.tensor_tensor(modrow[:, cb * 512:(cb + 1) * 512], ps[0:2, :],
                                                            adab[:, cb * 512:(cb + 1) * 512], ALU.add),
             reads=[f"ps{cb%2}", "adab"], writes=["modrow"])


def emit_bcast_row(cx, dst, row2, sel, which, ps_ids, rkey, wkey):
    S = cx.S
    for hb in range(2):
        ps = cx.PS[ps_ids[hb]]
        S.op("pe", lambda e, ps=ps, hb=hb: e.matmul(ps[:, :], sel[:, which, :], row2[:, hb * 512:(hb + 1) * 512],
                                                    start=True, stop=True),
             reads=[rkey, "sel"], writes=[f"ps{ps_ids[hb]}"])
        S.op("act", lambda e, ps=ps, hb=hb: e.copy(dst[:, hb * 512:(hb + 1) * 512], ps[:, :]),
             reads=[f"ps{ps_ids[hb]}"], writes=[wkey])


def emit_rstd(cx, xt, P, ss, rstd, junk, xkey, slot):
    S = cx.S
    S.op("pool", lambda e: e.memset(ss[0:P, :], 0.0), writes=[f"ss{slot}"])
    S.op("act", lambda e: e.activation(junk[0:P, :], xt[0:P, :], AF.Square, accum_out=ss[0:P, :]),
         reads=[xkey], writes=[f"ss{slot}", "junk"])
    S.op("dve", lambda e: e.tensor_scalar(rstd[0:P, :], ss[0:P, :], 1.0 / D, EPS, ALU.mult, ALU.add),
         reads=[f"ss{slot}"], writes=[f"rstd{slot}"])
    S.op("act", lambda e: e.sqrt(rstd[0:P, :], rstd[0:P, :]), reads=[f"rstd{slot}"], writes=[f"rstd{slot}"])
    S.op("dve", lambda e: e.reciprocal(rstd[0:P, :], rstd[0:P, :]), reads=[f"rstd{slot}"], writes=[f"rstd{slot}"])


def emit_transpose8(cx, hb, P, ident, hT, psb, hkey, tkey, pskey):
    S = cx.S
    psv = psb[:].bitcast(BF16).rearrange("p (k t) -> p k t", t=128)
    for kc in range(8):
        S.op("pe", lambda e, kc=kc: e.transpose(psv[:, kc, 0:P], hb[0:P, kc * 128:(kc + 1) * 128], ident[0:P, 0:P]),
             reads=[hkey, "ident"], writes=[pskey])
    S.op("act", lambda e: e.copy(hT[:, :, 0:P], psv[:, :, 0:P]), reads=[pskey], writes=[tkey])


def emit_A(cx, has_rope=True):
    S = cx.S
    xl = cx.inp("xl", [TPC, D])
    xc = cx.inp("xc", [CPC, D])
    cT = cx.inp("cT", [128, 8, 2])
    ada_w = cx.inp("ada_w", [D, 6 * D])
    ada_b = cx.inp("ada_b", [1, 6 * D])
    norm_g = cx.inp("norm_g", [1, D])
    w_in = cx.inp("w_in", [D, DIN])
    sel_d = cx.inp("sel", [2, 2, 128])
    ident_d = cx.inp("ident", [128, 128], BF16)
    ropec = cx.inp("ropec", [TPC, 16])
    ropes = cx.inp("ropes", [TPC, 16])
    ag_in = cx.inp("ag_in", [NAGC, DIN, 64])
    ag_out = cx.inp("ag_out", [NAGC, 4 * DIN, 64])
    cckey = cx.coll_group()
    identF_d = cx.inp("identF", [128, 128])

    sel = cx.sb("sel", [2, 2, 128])
    ident = cx.sb("ident", [128, 128], BF16)
    identF = cx.sb("identF", [128, 128])
    utT = cx.sb("utT", [128, 20, 128])
    S.dma("sp", identF[:], identF_d, writes=["identF"])
    scT = cx.sb("scT", [128, 8, 2])
    modrow = cx.sb("modrow", [2, 6 * D])
    normg2 = cx.sb("normg2", [2, D])
    grow = cx.sb("grow", [2, D])
    GL = cx.sb("GL", [128, D]); SHL = cx.sb("SHL", [128, D])
    GC = cx.sb("GC", [128, D]); SHC = cx.sb("SHC", [128, D])
    wbf = cx.sb("wbf", [128, 8, DIN], BF16)
    S.dma("sp", sel[:], sel_d, writes=["sel"])
    S.dma("sp", ident[:], ident_d, writes=["ident"])
    S.dma("sp", scT[:], cT, writes=["scT"])
    S.dma("sp", normg2[0:1, :], norm_g, writes=["normg2"])
    S.dma("sp", normg2[1:2, :], norm_g, writes=["normg2"])
    S.op("act", lambda e: e.activation(scT[:], scT[:], AF.Silu), reads=["scT"], writes=["scT"])
    with ExitStack() as st:
        emit_mod_rows(cx, st, scT, ada_w, ada_b, modrow, "A")
        wst = [cx.sb(f"wst{i}", [128, DIN], F32, st) for i in range(2)]
        wv = w_in.rearrange("(kc p) n -> p kc n", p=128)
        for kc in range(8):
            S.dma(("sp", "actq")[kc % 2], wst[kc % 2][:], wv[:, kc, :], writes=[f"wst{kc%2}"])
            eng = ("dve", "pool")[kc % 2]
            S.op(eng, lambda e, kc=kc: e.tensor_copy(wbf[:, kc, :], wst[kc % 2][:]), reads=[f"wst{kc%2}"], writes=["wbf"])
        S.barrier()
    S.op("dve", lambda e: e.scalar_tensor_tensor(grow[:], modrow[:, D:2 * D], 1.0, normg2[:], ALU.add, ALU.mult),
         reads=["modrow", "normg2"], writes=["grow"])
    emit_bcast_row(cx, GL, grow, sel, 0, (0, 1), "grow", "GL")
    emit_bcast_row(cx, SHL, modrow[:, 0:D], sel, 0, (0, 1), "modrow", "SHL")
    emit_bcast_row(cx, GC, grow, sel, 1, (0, 1), "grow", "GC")
    emit_bcast_row(cx, SHC, modrow[:, 0:D], sel, 1, (0, 1), "modrow", "SHC")

    xt = [cx.sb(f"xt{i}", [128, D]) for i in range(2)]
    junk = cx.sb("junk", [128, D], BF16)
    tmp = cx.sb("tmp", [128, D])
    hb = [cx.sb(f"hb{i}", [128, D], BF16) for i in range(2)]
    hT = [cx.sb(f"hT{i}", [128, 8, 128], BF16) for i in range(2)]
    ut = [cx.sb(f"ut{i}", [128, DIN]) for i in range(2)]
    ss = [cx.sb(f"ss{i}", [128, 1]) for i in range(2)]
    rstd = [cx.sb(f"rstd{i}", [128, 1]) for i in range(2)]
    rc = [cx.sb(f"rc{i}", [128, 16]) for i in range(2)]
    rs = [cx.sb(f"rs{i}", [128, 16]) for i in range(2)]
    rt = [cx.sb(f"rt{i}", [128, 16, 16]) for i in range(4)]

    ntl = TPC // 128
    tiles = [("l", i) for i in range(ntl)] + [("c", 0)]

    def load(ti):
        kind, i = tiles[ti]
        s = ti % 2
        if kind == "l":
            S.dma("sp", xt[s][:], xl[i * 128:(i + 1) * 128, :], writes=[f"xt{s}"])
            if has_rope:
                S.dma("sp", rc[s][:], ropec[i * 128:(i + 1) * 128, :], writes=[f"rc{s}"])
                S.dma("sp", rs[s][:], ropes[i * 128:(i + 1) * 128, :], writes=[f"rs{s}"])
        else:
            S.dma("sp", xt[s][0:CPC, :], xc, writes=[f"xt{s}"])

    load(0)
    for ti, (kind, i) in enumerate(tiles):
        s = ti % 2
        P = 128 if kind == "l" else CPC
        G, SH = (GL, SHL) if kind == "l" else (GC, SHC)
        if ti + 1 < len(tiles):
            load(ti + 1)
        emit_rstd(cx, xt[s], P, ss[s], rstd[s], junk, f"xt{s}", s)
        S.op("dve", lambda e, s=s, P=P, G=G: e.scalar_tensor_tensor(tmp[0:P, :], xt[s][0:P, :], rstd[s][0:P, :], G[0:P, :],
                                                                     ALU.mult, ALU.mult),
             reads=[f"xt{s}", f"rstd{s}", "GL", "GC"], writes=["tmp"])
        S.op("dve", lambda e, s=s, P=P, SH=SH: e.tensor_tensor(hb[s][0:P, :], tmp[0:P, :], SH[0:P, :], ALU.add),
             reads=["tmp", "SHL", "SHC"], writes=[f"hb{s}"])
        emit_transpose8(cx, hb[s], P, ident, hT[s], cx.PS[2], f"hb{s}", f"hT{s}", "ps2")
        for cb in range(5):
            ps = cx.PS[3 + cb]
            for kc in range(8):
                S.op("pe", lambda e, ps=ps, kc=kc, cb=cb, s=s, P=P: e.matmul(
                    ps[0:P, :], hT[s][:, kc, 0:P], wbf[:, kc, cb * 512:(cb + 1) * 512], start=(kc == 0), stop=(kc == 7)),
                    reads=[f"hT{s}", "wbf"], writes=[f"ps{3+cb}"])
            if cb % 2 == 0:
                S.op("act", lambda e, ps=ps, cb=cb, s=s, P=P: e.copy(ut[s][0:P, cb * 512:(cb + 1) * 512], ps[0:P, :]),
                     reads=[f"ps{3+cb}"], writes=[f"ut{s}c{cb}"])
            else:
                S.op("dve", lambda e, ps=ps, cb=cb, s=s, P=P: e.tensor_copy(ut[s][0:P, cb * 512:(cb + 1) * 512], ps[0:P, :]),
                     reads=[f"ps{3+cb}"], writes=[f"ut{s}c{cb}"])
        if kind == "l" and has_rope:
            xv = ut[s][:, 0:512].rearrange("p (g d) -> p g d", d=32)
            x1 = xv[:, :, 0:16]
            x2 = xv[:, :, 16:32]
            cb_ = rc[s][:].unsqueeze(1).to_broadcast([128, 16, 16])
            sb_ = rs[s][:].unsqueeze(1).to_broadcast([128, 16, 16])
            S.op("dve", lambda e, x1=x1, cb_=cb_: e.tensor_tensor(rt[0][:], x1, cb_, ALU.mult), reads=[f"ut{s}c0", f"rc{s}"], writes=["rt0"])
            S.op("pool", lambda e, x2=x2, sb_=sb_: e.tensor_tensor(rt[1][:], x2, sb_, ALU.mult), reads=[f"ut{s}c0", f"rs{s}"], writes=["rt1"])
            S.op("dve", lambda e, x2=x2, cb_=cb_: e.tensor_tensor(rt[2][:], x2, cb_, ALU.mult), reads=[f"ut{s}c0", f"rc{s}"], writes=["rt2"])
            S.op("pool", lambda e, x1=x1, sb_=sb_: e.tensor_tensor(rt[3][:], x1, sb_, ALU.mult), reads=[f"ut{s}c0", f"rs{s}"], writes=["rt3"])
            S.op("dve", lambda e, x1=x1: e.tensor_tensor(x1, rt[0][:], rt[1][:], ALU.subtract), reads=["rt0", "rt1"], writes=[f"ut{s}c0"])
            S.op("pool", lambda e, x2=x2: e.tensor_tensor(x2, rt[2][:], rt[3][:], ALU.add), reads=["rt2", "rt3", f"ut{s}c0"], writes=[f"ut{s}c0"])
        for q4 in range(5):
            psq = cx.PS[q4 % 2]
            for j4 in range(4):
                cc = q4 * 4 + j4
                S.op("pe", lambda e, psq=psq, j4=j4, cc=cc, s=s, P=P: e.matmul(psq[:, j4 * 128:j4 * 128 + P], ut[s][0:P, cc * 128:(cc + 1) * 128],
                                                                            identF[0:P, 0:P], start=True, stop=True),
                     reads=[f"ut{s}c{cc // 4}", "identF"], writes=[f"ps{q4 % 2}"])
            pqv = psq[:].rearrange("p (j t) -> p j t", t=128)
            S.op(("act", "dve")[q4 % 2], lambda e, pqv=pqv, q4=q4, P=P: (e.copy if hasattr(e, "copy") else e.tensor_copy)(utT[:, q4 * 4:(q4 + 1) * 4, 0:P], pqv[:, :, 0:P]),
                 reads=[f"ps{q4 % 2}"], writes=["utT"])
        for hf in range(P // 64):
            ck = (2 * i + hf) if kind == "l" else NAGC - 1
            S.dma("poolq", ag_in[ck].rearrange("(cc p) t -> p cc t", p=128), utT[:, :, hf * 64:(hf + 1) * 64], reads=["utT"], writes=[f"ag_in{ck}"])
            cx.coll(cckey, "AllGather", ALU.bypass, GROUPS, ag_in[ck], ag_out[ck], [f"ag_in{ck}"])
    cx.end_phase()


def _bf16(a):
    import ml_dtypes
    return np.asarray(a, dtype=np.float32).astype(ml_dtypes.bfloat16)


def const_sel():
    s = np.zeros((2, 2, 128), np.float32)
    s[0, 0, :] = 1.0
    s[1, 1, :] = 1.0
    return s


def const_rope():
    inv = 10000.0 ** (-np.arange(8, dtype=np.float32) / 8)
    t = np.arange(SEQ)
    row = (t // 64).astype(np.float32)
    col = (t % 64).astype(np.float32)
    ang = np.concatenate([row[:, None] * inv, col[:, None] * inv], axis=-1).astype(np.float32)
    return np.cos(ang).astype(np.float32), np.sin(ang).astype(np.float32)


def cT_layout(c_b, c_ctx):
    a = np.stack([c_b, c_ctx], axis=-1)
    return np.ascontiguousarray(a.reshape(8, 128, 2).transpose(1, 0, 2))


WIDE_EXP = False


class Attn:
    PAIRS = ((0, 1), (6, 7))

    def __init__(self, cx, ident):
        self.cx = cx
        self.ident = ident
        self.PT = [cx.sb(f"PT{i}", [128, 1024], BF16) for i in range(2)]
        self.it = 0

    def run(self, QT, N, chunks, pso, psokey, qkeys):
        cx, S = self.cx, self.cx.S
        n = len(chunks)
        npair = (n + 1) // 2

        def qk(p):
            slot = (self.it + p) % 2
            for h_ in range(2):
                i = 2 * p + h_
                if i >= n:
                    continue
                KT, V, bias, keys = chunks[i]
                bank = self.PAIRS[slot][h_]
                ps = cx.PS[bank]
                S.op("pe", lambda e, ps=ps, KT=KT, bias=bias: e.matmul(ps[:, 0:N], KT, QT, start=True, stop=(bias is None)),
                     reads=list(keys) + list(qkeys), writes=[f"ps{bank}"])
                if bias is not None:
                    S.op("pe", lambda e, ps=ps, bias=bias: e.matmul(ps[:, 0:N], self.ident[:], bias, start=False, stop=True),
                         reads=["bias", "ident"], writes=[f"ps{bank}"])
        qk(0)
        for p in range(npair):
            if p + 1 < npair:
                qk(p + 1)
            slot = (self.it + p) % 2
            b0, b1 = self.PAIRS[slot]
            PT = self.PT[slot]
            both = (2 * p + 1 < n)
            if both and N == 512 and WIDE_EXP:
                wide = cx.PSALL[:, b0 * 512:b0 * 512 + 1024]
                S.op("act", lambda e, wide=wide, PT=PT: e.activation(PT[:, :], wide, AF.Exp),
                     reads=[f"ps{b0}", f"ps{b1}"], writes=[f"PT{slot}"])
            else:
                for h_ in range(2 if both else 1):
                    bank = self.PAIRS[slot][h_]
                    S.op("act", lambda e, bank=bank, PT=PT, h_=h_: e.activation(PT[:, h_ * 512:h_ * 512 + N], cx.PS[bank][:, 0:N], AF.Exp),
                         reads=[f"ps{bank}"], writes=[f"PT{slot}"])
            for h_ in range(2 if both else 1):
                i = 2 * p + h_
                V = chunks[i][1]
                S.op("pe", lambda e, PT=PT, V=V, i=i, h_=h_: e.matmul(pso[0:65, 0:N], V, PT[:, h_ * 512:h_ * 512 + N], start=(i == 0), stop=(i == n - 1)),
                     reads=[f"PT{slot}", "vaug"], writes=[psokey])
        self.it += npair


def emit_cast_rows(cx, stg, dst, src, rows, cols, key, scale=None, chunk=2048):
    S = cx.S
    nch = (cols + chunk - 1) // chunk
    for c in range(nch):
        w = min(chunk, cols - c * chunk)
        sl = slice(c * chunk, c * chunk + w)
        S.dma(("sp", "poolq")[c % 2], stg[c % 2][0:rows, 0:w], src[:, sl], writes=[f"stg{c%2}"])
        if scale is None:
            S.op("dve", lambda e, c=c, w=w, sl=sl: e.tensor_copy(dst[0:rows, sl], stg[c % 2][0:rows, 0:w]),
                 reads=[f"stg{c%2}"], writes=[key])
        else:
            S.op("dve", lambda e, c=c, w=w, sl=sl: e.tensor_scalar(dst[0:rows, sl], stg[c % 2][0:rows, 0:w], scale, None, ALU.mult),
                 reads=[f"stg{c%2}"], writes=[key])


def emit_load_v(cx, stg, Vaug, vsrc, nk, key):
    S = cx.S
    nch = nk // 128
    vv = vsrc.rearrange("(c p) d -> p c d", p=128)
    step = 26
    for i, c0 in enumerate(range(0, nch, step)):
        c1 = min(nch, c0 + step)
        sv = stg[i % 2][:, 0:(c1 - c0) * 65].rearrange("p (c d) -> p c d", d=65)
        S.dma(("sp", "poolq")[i % 2], sv, vv[:, c0:c1, :], writes=[f"stg{i%2}"])
        S.op("dve", lambda e, sv=sv, c0=c0, c1=c1: e.tensor_copy(Vaug[:, c0:c1, :], sv), reads=[f"stg{i%2}"], writes=[key])


def emit_qbound(cx, st, QT, d, N_total, kfac, ones_b, qsrc, key):
    S = cx.S
    sq = [cx.sb(f"sq_{key}{i}", [d, 512], BF16, st) for i in range(2)]
    for c in range(N_total // 512 if N_total >= 512 else 1):
        w = min(512, N_total)
        sl = slice(c * 512, c * 512 + w)
        S.op("dve", lambda e, c=c, sl=sl, w=w: e.tensor_tensor(sq[c % 2][:, 0:w], QT[0:d, sl], QT[0:d, sl], ALU.mult),
             reads=[key], writes=[f"sq{c%2}"])
        ps = cx.PS[6 + c % 2]
        S.op("pe", lambda e, ps=ps, c=c, w=w: e.matmul(ps[0:d + 1, 0:w], ones_b[0:d, 0:d + 1], sq[c % 2][:, 0:w], start=True, stop=True),
             reads=[f"sq{c%2}", "ones_b"], writes=[f"ps{6+c%2}"])
        S.op("act", lambda e, ps=ps, sl=sl, w=w: e.sqrt(QT[d:d + 1, sl], ps[d:d + 1, 0:w]),
             reads=[f"ps{6+c%2}"], writes=[key + "r"])
        S.op("dve", lambda e, sl=sl: e.tensor_scalar(QT[d:d + 1, sl], QT[d:d + 1, sl], kfac[d:d + 1, 0:1], -1.0, ALU.mult, ALU.mult),
             reads=[key + "r", "kfac"], writes=[key + "r"])


def emit_kmax(cx, st, KT, d, nk, kfac, ones_b, key):
    S = cx.S
    sq = [cx.sb(f"ksq_{key}{i}", [d, 512], BF16, st) for i in range(2)]
    kmx = cx.sb(f"kmx_{key}", [d + 1, 64], F32, st)
    S.op("pool", lambda e: e.memset(kmx[:], 0.0), writes=["kmx"])
    nch = (nk + 511) // 512
    for c in range(nch):
        w = min(512, nk - c * 512)
        sl = slice(c * 512, c * 512 + w)
        S.op("dve", lambda e, c=c, sl=sl, w=w: e.tensor_tensor(sq[c % 2][:, 0:w], KT[0:d, sl], KT[0:d, sl], ALU.mult),
             reads=[key], writes=[f"sq{c%2}"])
        ps = cx.PS[6 + c % 2]
        S.op("pe", lambda e, ps=ps, c=c, w=w: e.matmul(ps[0:d + 1, 0:w], ones_b[0:d, 0:d + 1], sq[c % 2][:, 0:w], start=True, stop=True),
             reads=[f"sq{c%2}", "ones_b"], writes=[f"ps{6+c%2}"])
        S.op("dve", lambda e, ps=ps, c=c, w=w: e.tensor_reduce(kmx[d:d + 1, c:c + 1], ps[d:d + 1, 0:w], AX.X, ALU.max),
             reads=[f"ps{6+c%2}"], writes=["kmx"])
    S.op("dve", lambda e: e.tensor_reduce(kfac[d:d + 1, 0:1], kmx[d:d + 1, 0:nch], AX.X, ALU.max), reads=["kmx"], writes=["kfac"])
    S.op("act", lambda e: e.sqrt(kfac[d:d + 1, 0:1], kfac[d:d + 1, 0:1]), reads=["kfac"], writes=["kfac"])


def emit_finalize(cx, pso, N, recrow, osb, E65, tdst, psokey, tkey):
    S = cx.S
    S.op("dve", lambda e: e.reciprocal(recrow[64:65, 0:N], pso[64:65, 0:N]), reads=[psokey], writes=["recrow"])
    S.op("pe", lambda e: e.matmul(cx.PS[5][0:64, 0:N], E65[0:65, 0:64], recrow[0:65, 0:N], start=True, stop=True),
         reads=["recrow", "E65"], writes=["ps5"])
    S.op("act", lambda e: e.copy(osb[0:64, 0:N], pso[0:64, 0:N]), reads=[psokey], writes=["osb"])
    S.op("dve", lambda e: e.tensor_tensor(tdst[0:64, 0:N], osb[0:64, 0:N], cx.PS[5][0:64, 0:N], ALU.mult),
         reads=["osb", "ps5"], writes=[tkey])


def emit_B1(cx, li):
    lam_init = 0.8 - 0.6 * math.exp(-0.3 * li)
    NK = SEQ + CTX
    S = cx.S
    aqT = cx.inp("aqT", [2, 32, SEQ]); akT = cx.inp("akT", [2, 33, NK]); av = cx.inp("av", [NK, 65])
    aqcT = cx.inp("aqcT", [2, 32, CTX])
    alam = cx.inp("alam", [1, 128]); subg = cx.inp("subg", [64, 1])
    bqT = cx.inp("bqT", [64, SEQ]); bkT = cx.inp("bkT", [65, NK]); bv = cx.inp("bv", [NK, 65])
    bqcT = cx.inp("bqcT", [64, CTX])
    bias_d = cx.inp("bias", [3, 8, 128, 512])
    ident_d = cx.inp("ident", [128, 128], BF16); onesb_d = cx.inp("onesb", [128, 128], BF16)
    E65_d = cx.inp("E65", [65, 64]); ones64_d = cx.inp("ones64", [64, 64])
    oa = cx.out("oa", [64, SEQ]); oac = cx.out("oac", [64, CTX])
    ob = cx.out("ob", [64, SEQ]); obc = cx.out("obc", [64, CTX])

    ident = cx.sb("ident", [128, 128], BF16); ones_b = cx.sb("onesb", [128, 128], BF16)
    E65 = cx.sb("E65", [65, 64]); ones64 = cx.sb("ones64", [64, 64])
    S.dma("sp", ident[:], ident_d, writes=["ident"]); S.dma("sp", ones_b[:], onesb_d, writes=["ones_b"])
    S.dma("sp", E65[:], E65_d, writes=["E65"]); S.dma("sp", ones64[:], ones64_d, writes=["ones64"])
    at = Attn(cx, ident)
    recrow = cx.sb("recrow", [65, 512]); osb = cx.sb("osb", [64, 512])
    t0 = cx.sb("t0", [64, 512]); t1 = cx.sb("t1", [64, 512]); t2 = cx.sb("t2", [64, 512])
    kfac = cx.sb("kfac", [65, 1])
    stg = [cx.sb(f"stg{i}", [128, 2048]) for i in range(2)]
    S.op("pool", lambda e: e.memset(recrow[:], 0.0), writes=["recrow"])
    lrow = cx.sb("lrow", [1, 128]); lsum = cx.sb("lsum", [1, 4]); neglam = cx.sb("neglam", [64, 1]); gsc = cx.sb("gsc", [64, 1])
    S.dma("sp", lrow[:], alam, writes=["lrow"]); S.dma("sp", gsc[:], subg, writes=["gsc"])
    S.op("pool", lambda e: e.memset(lsum[:], 0.0), writes=["lsum"])
    S.op("dve", lambda e: e.tensor_tensor(lrow[:, 0:32], lrow[:, 0:32], lrow[:, 32:64], ALU.mult), reads=["lrow"], writes=["lrow"])
    S.op("dve", lambda e: e.tensor_tensor(lrow[:, 64:96], lrow[:, 64:96], lrow[:, 96:128], ALU.mult), reads=["lrow"], writes=["lrow"])
    S.op("dve", lambda e: e.tensor_reduce(lsum[:, 0:1], lrow[:, 0:32], AX.X, ALU.add), reads=["lrow", "lsum"], writes=["lsum"])
    S.op("dve", lambda e: e.tensor_reduce(lsum[:, 1:2], lrow[:, 64:96], AX.X, ALU.add), reads=["lrow", "lsum"], writes=["lsum"])
    S.op("act", lambda e: e.activation(lsum[:, 0:2], lsum[:, 0:2], AF.Exp), reads=["lsum"], writes=["lsum"])
    S.op("dve", lambda e: e.tensor_tensor(lsum[:, 2:3], lsum[:, 1:2], lsum[:, 0:1], ALU.subtract), reads=["lsum"], writes=["lsum"])
    S.op("dve", lambda e: e.tensor_scalar(lsum[:, 2:3], lsum[:, 2:3], -lam_init, None, ALU.add), reads=["lsum"], writes=["lsum"])
    S.op("pe", lambda e: e.matmul(cx.PS[7][0:64, 0:1], ones64[0:1, 0:64], lsum[0:1, 2:3], start=True, stop=True),
         reads=["lsum", "ones64"], writes=["ps7"])
    S.op("act", lambda e: e.copy(neglam[:], cx.PS[7][0:64, 0:1]), reads=["ps7"], writes=["neglam"])
    S.op("dve", lambda e: e.tensor_scalar(gsc[:], gsc[:], 1.0 - lam_init, None, ALU.mult), reads=["gsc"], writes=["gsc"])

    with ExitStack() as st:
        QT = [cx.sb(f"aQT{c}", [33, SEQ], BF16, st) for c in range(2)]
        QTc = [cx.sb(f"aQTc{c}", [33, CTX], BF16, st) for c in range(2)]
        KT = [cx.sb(f"aKT{c}", [33, NK], BF16, st) for c in range(2)]
        Va = cx.sb("aV", [128, NK // 128, 65], BF16, st)
        sc = 32 ** -0.5
        for c in range(2):
            emit_cast_rows(cx, stg, KT[c], akT[c], 33, NK, f"akt{c}")
            emit_cast_rows(cx, stg, QT[c], aqT[c], 32, SEQ, f"aqt{c}", scale=sc)
            emit_cast_rows(cx, stg, QTc[c], aqcT[c], 32, CTX, f"aqtc{c}", scale=sc)
        emit_load_v(cx, stg, Va, av, NK, "vaug")
        for c in range(2):
            emit_kmax(cx, st, KT[c], 32, NK, kfac, ones_b, f"akt{c}")
            emit_qbound(cx, st, QT[c], 32, SEQ, kfac, ones_b, None, f"aqt{c}")
            emit_qbound(cx, st, QTc[c], 32, CTX, kfac, ones_b, None, f"aqtc{c}")

        def diff_block(qts, N, chunk_ids, odst, qkeys):
            for c in range(2):
                chunks = [(KT[c][:, k * 128:(k + 1) * 128], Va[:, k, :], None, (f"akt{c}",)) for k in chunk_ids]
                at.run(qts[c], N, chunks, cx.PS[3 + c], f"ps{3+c}", [qkeys[c], qkeys[c] + "r"])
            emit_finalize(cx, cx.PS[3], N, recrow, osb, E65, t0, "ps3", "t0")
            emit_finalize(cx, cx.PS[4], N, recrow, osb, E65, t1, "ps4", "t1")
            S.op("dve", lambda e: e.scalar_tensor_tensor(t0[:, 0:N], t1[:, 0:N], neglam[:, 0:1], t0[:, 0:N], ALU.mult, ALU.add),
                 reads=["t0", "t1", "neglam"], writes=["t0"])
            S.op("act", lambda e: e.activation(t1[:, 0:N], t0[:, 0:N], AF.Square), reads=["t0"], writes=["t1"])
            S.op("pe", lambda e: e.matmul(cx.PS[2][0:64, 0:N], ones64[:, :], t1[:, 0:N], start=True, stop=True),
                 reads=["t1", "ones64"], writes=["ps2"])
            S.op("dve", lambda e: e.tensor_scalar(t1[:, 0:N], cx.PS[2][0:64, 0:N], 1.0 / 64, EPS, ALU.mult, ALU.add),
                 reads=["ps2"], writes=["t1"])
            S.op("act", lambda e: e.sqrt(t1[:, 0:N], t1[:, 0:N]), reads=["t1"], writes=["t1"])
            S.op("dve", lambda e: e.reciprocal(t1[:, 0:N], t1[:, 0:N]), reads=["t1"], writes=["t1"])
            S.op("dve", lambda e: e.scalar_tensor_tensor(t2[:, 0:N], t0[:, 0:N], gsc[:, 0:1], t1[:, 0:N], ALU.mult, ALU.mult),
                 reads=["t0", "t1", "gsc"], writes=["t2"])
            S.dma("poolq", odst, t2[:, 0:N], reads=["t2"], writes=["oa"])

        for qb in range(SEQ // 512):
            sl = slice(qb * 512, (qb + 1) * 512)
            diff_block([QT[0][:, sl], QT[1][:, sl]], 512, range(NK // 128), oa[:, sl], ["aqt0", "aqt1"])
        diff_block([QTc[0][:, :], QTc[1][:, :]], CTX, range(SEQ // 128, NK // 128), oac[:, :], ["aqtc0", "aqtc1"])
        S.barrier()

    with ExitStack() as st:
        QT = cx.sb("bQT", [65, SEQ], BF16, st); QTc = cx.sb("bQTc", [65, CTX], BF16, st)
        KT = cx.sb("bKT", [65, NK], BF16, st); Vb = cx.sb("bV", [128, NK // 128, 65], BF16, st)
        bias = cx.sb("bias", [128, 3, 8, 512], BF16, st)
        sc = 64 ** -0.5
        emit_cast_rows(cx, stg, KT, bkT, 65, NK, "bkt")
        emit_cast_rows(cx, stg, QT, bqT, 64, SEQ, "bqt", scale=sc)
        emit_cast_rows(cx, stg, QTc, bqcT, 64, CTX, "bqtc", scale=sc)
        emit_load_v(cx, stg, Vb, bv, NK, "vaug")
        for s_ in range(3):
            for j in range(8):
                i = s_ * 8 + j
                S.dma(("sp", "poolq")[i % 2], stg[i % 2][:, 0:512], bias_d[s_, j], writes=[f"stg{i%2}"])
                S.op("dve", lambda e, i=i, s_=s_, j=j: e.tensor_copy(bias[:, s_, j, :], stg[i % 2][:, 0:512]),
                     reads=[f"stg{i%2}"], writes=["bias"])
        emit_kmax(cx, st, KT, 64, NK, kfac, ones_b, "bkt")
        emit_qbound(cx, st, QT, 64, SEQ, kfac, ones_b, None, "bqt")
        emit_qbound(cx, st, QTc, 64, CTX, kfac, ones_b, None, "bqtc")
        for qb in range(32):
            R0 = qb * 8
            if qb == 0:
                bset, kr0 = 0, 0
            elif qb == 31:
                bset, kr0 = 2, 240
            else:
                bset, kr0 = 1, R0 - 4
            sl = slice(qb * 512, (qb + 1) * 512)
            chunks = [(KT[:, (kr0 // 2 + j) * 128:(kr0 // 2 + j + 1) * 128], Vb[:, kr0 // 2 + j, :], bias[:, bset, j, :], ("bkt",))
                      for j in range(8)]
            chunks += [(KT[:, k * 128:(k + 1) * 128], Vb[:, k, :], None, ("bkt",)) for k in range(SEQ // 128, NK // 128)]
            at.run(QT[:, sl], 512, chunks, cx.PS[3], "ps3", ["bqt", "bqtr"])
            emit_finalize(cx, cx.PS[3], 512, recrow, osb, E65, t0, "ps3", "t0")
            S.dma("poolq", ob[:, sl], t0[:, 0:512], reads=["t0"], writes=["ob"])
        chunks = [(KT[:, k * 128:(k + 1) * 128], Vb[:, k, :], None, ("bkt",)) for k in range(SEQ // 128, NK // 128)]
        at.run(QTc[:, :], CTX, chunks, cx.PS[3], "ps3", ["bqtc", "bqtcr"])
        emit_finalize(cx, cx.PS[3], CTX, recrow, osb, E65, t0, "ps3", "t0")
        S.dma("poolq", obc[:, :], t0[:, 0:CTX], reads=["t0"], writes=["obc"])
        S.barrier()
    cx.end_phase()


def na_bias_sets(rpb_h):
    out = np.full((3, 8, 128, 512), -30000.0, np.float32)
    cq = np.arange(64); ck = np.arange(64)
    c0 = np.clip(cq - 8, 0, 48)
    colok = (ck[:, None] >= c0[None, :]) & (ck[:, None] < c0[None, :] + 16)
    dc = np.clip(ck[:, None] - cq[None, :], -15, 15) + 15
    for s_, (R0, kr0) in enumerate([(0, 0), (8, 4), (248, 240)]):
        for j in range(8):
            for krl in range(2):
                kr = kr0 + 2 * j + krl
                for qrl in range(8):
                    r = R0 + qrl
                    r0 = min(max(r - 4, 0), 248)
                    if not (r0 <= kr < r0 + 8):
                        continue
                    dr = kr - r + 7
                    blk = np.where(colok, rpb_h[dr][dc], np.float32(-30000.0))
                    out[s_, j, krl * 64:(krl + 1) * 64, qrl * 64:(qrl + 1) * 64] = blk
    return out


def emit_C(cx, with_ctx, final):
    S = cx.S
    xl = cx.inp("xl", [TPC, D]); xc = cx.inp("xc", [CPC, D])
    rs_out = cx.inp("rs_out", [TPC + CPC, D])
    cT = cx.inp("cT", [128, 8, 2])
    ada_w = cx.inp("ada_w", [D, 6 * D]); ada_b = cx.inp("ada_b", [1, 6 * D])
    norm_g = cx.inp("norm2_g", [1, D]); fin_g = cx.inp("fin_g", [1, D])
    router_w = cx.inp("router_w", [D, 16]); router_b = cx.inp("router_b", [1, 16])
    w1 = cx.inp("w1", [16, D, 512]); w3 = cx.inp("w3", [16, D, 512]); w2 = cx.inp("w2", [16, 512, D])
    sel_d = cx.inp("sel", [2, 2, 128]); identf_d = cx.inp("ident", [128, 128], BF16)
    ol = cx.out("ol", [TPC, D]); oc = cx.out("oc", [CPC, D])

    sel = cx.sb("sel", [2, 2, 128]); identf = cx.sb("identb", [128, 128], BF16)
    rwh = cx.sb("rwh", [128, 8, 16], BF16); rwl = cx.sb("rwl", [128, 8, 16], BF16)
    G1 = cx.sb("G1", [128, D]); G2 = cx.sb("G2", [128, D]); SH2 = cx.sb("SH2", [128, D]); GG2 = cx.sb("GG2", [128, D])
    FG = cx.sb("FG", [128, D])
    rw = cx.sb("rw", [128, 8, 16]); rb = cx.sb("rb", [128, 16])
    bcs = cx.scratch("bc_scr", [4, 128, D])
    S.dma("sp", sel[:], sel_d, writes=["sel"]); S.dma("sp", identf[:], identf_d, writes=["identf"])
    S.dma("sp", rw[:], router_w.rearrange("(kc p) n -> p kc n", p=128), writes=["rw"])
    S.dma("sp", rb[:], router_b.partition_broadcast(128), writes=["rb"])
    S.op("dve", lambda e: e.tensor_copy(rwh[:], rw[:]), reads=["rw"], writes=["rwh"])
    S.op("dve", lambda e: e.tensor_tensor(rwl[:], rw[:], rwh[:], ALU.subtract), reads=["rw", "rwh"], writes=["rwl"])
    with ExitStack() as st:
        scT = cx.sb("scT", [128, 8, 2], F32, st); modrow = cx.sb("modrow", [2, 6 * D], F32, st)
        normg2 = cx.sb("normg2", [2, D], F32, st); grow = cx.sb("grow", [2, D], F32, st); fing2 = cx.sb("fing2", [2, D], F32, st)
        S.dma("sp", scT[:], cT, writes=["scT"])
        for r in range(2):
            S.dma("sp", normg2[r:r + 1, :], norm_g, writes=["normg2"])
            S.dma("sp", fing2[r:r + 1, :], fin_g, writes=["fing2"])
        S.op("act", lambda e: e.activation(scT[:], scT[:], AF.Silu), reads=["scT"], writes=["scT"])
        with ExitStack() as st2:
            emit_mod_rows(cx, st2, scT, ada_w, ada_b, modrow, "C")
            S.barrier()
        S.op("dve", lambda e: e.scalar_tensor_tensor(grow[:], modrow[:, 4 * D:5 * D], 1.0, normg2[:], ALU.add, ALU.mult),
             reads=["modrow", "normg2"], writes=["grow"])

        def set_bcast(which):
            emit_bcast_row(cx, G1, modrow[:, 2 * D:3 * D], sel, which, (0, 1), "modrow", "G1")
            emit_bcast_row(cx, G2, grow, sel, which, (0, 1), "grow", "G2")
            emit_bcast_row(cx, SH2, modrow[:, 3 * D:4 * D], sel, which, (0, 1), "modrow", "SH2")
            emit_bcast_row(cx, GG2, modrow[:, 5 * D:6 * D], sel, which, (0, 1), "modrow", "GG2")
        set_bcast(1)
        for i_, (t_, k_) in enumerate(((G1, "G1"), (G2, "G2"), (SH2, "SH2"), (GG2, "GG2"))):
            S.dma("sp", bcs[i_], t_[:], reads=[k_], writes=["bcs"])
        set_bcast(0)
        emit_bcast_row(cx, FG, fing2, sel, 0, (0, 1), "fing2", "FG")
        S.barrier()

    def load_ctx_bcast():
        for i_, (t_, k_) in enumerate(((G1, "G1"), (G2, "G2"), (SH2, "SH2"), (GG2, "GG2"))):
            S.dma("sp", t_[:], bcs[i_], reads=["bcs"], writes=[k_])

    GT = 8
    x1 = cx.sb("x1", [128, GT, D]); yacc = cx.sb("yacc", [128, GT, D])
    hT = cx.sb("hT", [128, 8, GT * 128], BF16)
    gate = cx.sb("gate", [128, GT, 16])
    xt = [cx.sb(f"xt{i}", [128, D]) for i in range(2)]
    mpt = [cx.sb("mpt0", [128, D])] * 2
    junk = cx.sb("junk", [128, D], BF16); tmp = cx.sb("tmp", [128, D]); h2 = cx.sb("h2", [128, D])
    hTl = cx.sb("hTl", [128, 8, 128], BF16); h2hi = cx.sb("h2hi", [128, D], BF16); h2lo = cx.sb("h2lo", [128, D], BF16)
    ss = cx.sb("ss", [128, 1]); rstd = cx.sb("rstd", [128, 1])
    r_ = {k: cx.sb("r_" + k, [128, 16]) for k in ("ex", "sc", "sel", "eq", "s2", "selm", "k1", "sm2", "k2", "w")}
    q_ = {k: cx.sb("q_" + k, [128, 4]) for k in ("m1", "m2", "gs", "gmask", "pen")}
    c_ = {k: cx.sb("c_" + k, [128, 1]) for k in ("mx", "sm", "gm", "e1", "e2", "ws")}
    W13 = [cx.sb(f"W13_{i}", [128, 2, 8, 512], BF16) for i in range(2)]
    W2 = [cx.sb(f"W2_{i}", [128, 4, D], BF16) for i in range(2)]
    hs = cx.sb("hs", [128, 512]); hh = [cx.sb(f"hh{i}", [128, 4, 512], BF16) for i in range(2)]
    stage_n = [0]
    conv_st = ExitStack()
    wstg = [cx.sb(f"wstg{i}", [128, 4, 512], F32, conv_st) for i in range(2)]

    def router(P, g):
        lg = cx.PS[2]
        n_ = 0
        for (lhs, rhs) in (("hi", rwh), ("lo", rwh), ("hi", rwl)):
            for kc in range(8):
                lt = hT[:, kc, g * 128:g * 128 + P] if lhs == "hi" else hTl[:, kc, 0:P]
                S.op("pe", lambda e, lt=lt, rhs=rhs, kc=kc, n_=n_: e.matmul(lg[0:P, 0:16], lt, rhs[:, kc, :], start=(n_ == 0), stop=(n_ == 23)),
                     reads=["hT", "hTl", "rwh", "rwl"], writes=["ps2"])
                n_ += 1
        V = lambda k: r_[k][0:P, :]
        V4 = lambda k: r_[k][0:P, :].rearrange("p (g e) -> p g e", e=4)
        Q = lambda k: q_[k][0:P, :]
        Cc = lambda k: c_[k][0:P, :]
        dv = lambda fn, rd, wr: S.op("dve", fn, reads=rd, writes=wr)
        dv(lambda e: e.tensor_reduce(Cc("mx"), lg[0:P, 0:16], AX.X, ALU.max), ["ps2"], ["c_mx"])
        dv(lambda e: e.tensor_scalar(Cc("mx"), Cc("mx"), -1.0, None, ALU.mult), ["c_mx"], ["c_mx"])
        S.op("pool", lambda e: e.memset(Cc("sm"), 0.0), writes=["c_sm"])
        S.op("act", lambda e: e.activation(V("ex"), lg[0:P, 0:16], AF.Exp, bias=Cc("mx"), accum_out=Cc("sm")),
             reads=["ps2", "c_mx", "c_sm"], writes=["r_ex", "c_sm"])
        dv(lambda e: e.reciprocal(Cc("sm"), Cc("sm")), ["c_sm"], ["c_sm"])
        dv(lambda e: e.tensor_scalar(V("sc"), V("ex"), Cc("sm"), None, ALU.mult), ["r_ex", "c_sm"], ["r_sc"])
        dv(lambda e: e.tensor_tensor(V("sel"), V("sc"), rb[0:P, :], ALU.add), ["r_sc", "rb"], ["r_sel"])
        dv(lambda e: e.tensor_reduce(Q("m1"), V4("sel"), AX.X, ALU.max), ["r_sel"], ["q_m1"])
        dv(lambda e: e.tensor_tensor(V4("eq"), V4("sel"), Q("m1").unsqueeze(2).to_broadcast([P, 4, 4]), ALU.is_equal), ["r_sel", "q_m1"], ["r_eq"])
        dv(lambda e: e.scalar_tensor_tensor(V("s2"), V("eq"), -1e9, V("sel"), ALU.mult, ALU.add), ["r_eq", "r_sel"], ["r_s2"])
        dv(lambda e: e.tensor_reduce(Q("m2"), V4("s2"), AX.X, ALU.max), ["r_s2"], ["q_m2"])
        dv(lambda e: e.tensor_tensor(Q("gs"), Q("m1"), Q("m2"), ALU.add), ["q_m1", "q_m2"], ["q_gs"])
        dv(lambda e: e.tensor_reduce(Cc("gm"), Q("gs"), AX.X, ALU.max), ["q_gs"], ["c_gm"])
        dv(lambda e: e.tensor_scalar(Q("gmask"), Q("gs"), Cc("gm"), None, ALU.is_equal), ["q_gs", "c_gm"], ["q_gmask"])
        dv(lambda e: e.tensor_scalar(Q("pen"), Q("gmask"), -1.0, 1e9, ALU.add, ALU.mult), ["q_gmask"], ["q_pen"])
        dv(lambda e: e.tensor_tensor(V4("selm"), V4("sel"), Q("pen").unsqueeze(2).to_broadcast([P, 4, 4]), ALU.add), ["r_sel", "q_pen"], ["r_selm"])
        dv(lambda e: e.tensor_reduce(Cc("e1"), V("selm"), AX.X, ALU.max), ["r_selm"], ["c_e1"])
        dv(lambda e: e.tensor_scalar(V("k1"), V("selm"), Cc("e1"), None, ALU.is_equal), ["r_selm", "c_e1"], ["r_k1"])
        dv(lambda e: e.scalar_tensor_tensor(V("sm2"), V("k1"), -1e9, V("selm"), ALU.mult, ALU.add), ["r_k1", "r_selm"], ["r_sm2"])
        dv(lambda e: e.tensor_reduce(Cc("e2"), V("sm2"), AX.X, ALU.max), ["r_sm2"], ["c_e2"])
        dv(lambda e: e.tensor_scalar(V("k2"), V("sm2"), Cc("e2"), None, ALU.is_equal), ["r_sm2", "c_e2"], ["r_k2"])
        dv(lambda e: e.tensor_tensor(V("k1"), V("k1"), V("k2"), ALU.add), ["r_k1", "r_k2"], ["r_k1"])
        dv(lambda e: e.tensor_tensor(V("w"), V("sc"), V("k1"), ALU.mult), ["r_sc", "r_k1"], ["r_w"])
        dv(lambda e: e.tensor_reduce(Cc("ws"), V("w"), AX.X, ALU.add), ["r_w"], ["c_ws"])
        dv(lambda e: e.reciprocal(Cc("ws"), Cc("ws")), ["c_ws"], ["c_ws"])
        dv(lambda e: e.tensor_scalar(gate[0:P, g, :], V("w"), Cc("ws"), None, ALU.mult), ["r_w", "c_ws"], ["gate"])

    wb13 = cx.scratch("wb13", [16, 128, 2 * 8 * 512], BF16)
    wb2 = cx.scratch("wb2", [16, 128, 4 * D], BF16)

    def convert_expert(e_, slot):
        pieces = []
        for wi, wsrc in enumerate((w1, w3)):
            v = wsrc[e_].rearrange("(kc p) n -> p kc n", p=128)
            for hf in range(2):
                pieces.append((v[:, hf * 4:(hf + 1) * 4, :], W13[slot][:, wi, hf * 4:(hf + 1) * 4, :]))
        v2 = w2[e_].rearrange("(fc p) n -> p fc n", p=128)
        for hf in range(2):
            pieces.append((v2[:, :, hf * 512:(hf + 1) * 512], W2[slot][:, :, hf * 512:(hf + 1) * 512]))
        for src, dst in pieces:
            n = stage_n[0]; stage_n[0] += 1
            sg = wstg[n % 2]
            S.dma(("sp", "actq")[n % 2], sg[:], src, writes=[f"wstg{n%2}"])
            S.op(("pool", "dve")[n % 2], lambda e, sg=sg, dst=dst: e.tensor_copy(dst, sg[:]), reads=[f"wstg{n%2}"], writes=[f"W{slot}"])
        S.dma("poolq", wb13[e_], W13[slot][:].rearrange("p a b c -> p (a b c)"), reads=[f"W{slot}"], writes=["wb"])
        S.dma("poolq", wb2[e_], W2[slot][:].rearrange("p a b -> p (a b)"), reads=[f"W{slot}"], writes=["wb"])

    for e_ in range(16):
        convert_expert(e_, e_ % 2)
    S.barrier()
    conv_st.close()

    def load_expert(e_, slot):
        S.dma("sp", W13[slot][:].rearrange("p a b c -> p (a b c)"), wb13[e_], reads=["wb"], writes=[f"W{slot}"])
        S.dma("actq", W2[slot][:].rearrange("p a b -> p (a b)"), wb2[e_], reads=["wb"], writes=[f"W{slot}"])

    ntl = TPC // 128
    groups = [[("l", g * GT + t) for t in range(GT)] for g in range(ntl // GT)]
    if with_ctx:
        groups.append([("c", 0)])
    ti_glob = 0
    eload = 0
    for gi, grp in enumerate(groups):
        isctx = grp[0][0] == "c"
        if isctx:
            load_ctx_bcast()
        P = CPC if isctx else 128
        NT = len(grp) * 128 if not isctx else CPC
        for g, (kind, i) in enumerate(grp):
            s = ti_glob % 2; ti_glob += 1
            xsrc = xc if isctx else xl[i * 128:(i + 1) * 128, :]
            msrc = rs_out[TPC:TPC + CPC, :] if isctx else rs_out[i * 128:(i + 1) * 128, :]
            S.dma("sp", xt[s][0:P, :], xsrc, writes=[f"xt{s}"])
            S.dma("actq", mpt[s][0:P, :], msrc, writes=["mpt"])
            S.op("dve", lambda e, s=s, P=P: e.tensor_tensor(tmp[0:P, :], mpt[s][0:P, :], G1[0:P, :], ALU.mult),
                 reads=["mpt", "G1"], writes=["tmp"])
            S.op("dve", lambda e, s=s, g=g, P=P: e.tensor_tensor(x1[0:P, g, :], tmp[0:P, :], xt[s][0:P, :], ALU.add),
                 reads=["tmp", f"xt{s}"], writes=["x1"])
            S.op("pool", lambda e, P=P: e.memset(ss[0:P, :], 0.0), writes=["ss"])
            S.op("act", lambda e, g=g, P=P: e.activation(junk[0:P, :], x1[0:P, g, :], AF.Square, accum_out=ss[0:P, :]),
                 reads=["x1", "ss"], writes=["ss", "junk"])
            S.op("dve", lambda e, P=P: e.tensor_scalar(rstd[0:P, :], ss[0:P, :], 1.0 / D, EPS, ALU.mult, ALU.add), reads=["ss"], writes=["rstd"])
            S.op("act", lambda e, P=P: e.sqrt(rstd[0:P, :], rstd[0:P, :]), reads=["rstd"], writes=["rstd"])
            S.op("dve", lambda e, P=P: e.reciprocal(rstd[0:P, :], rstd[0:P, :]), reads=["rstd"], writes=["rstd"])
            S.op("dve", lambda e, g=g, P=P: e.scalar_tensor_tensor(tmp[0:P, :], x1[0:P, g, :], rstd[0:P, :], G2[0:P, :], ALU.mult, ALU.mult),
                 reads=["x1", "rstd", "G2"], writes=["tmp"])
            S.op("dve", lambda e, P=P: e.tensor_tensor(h2[0:P, :], tmp[0:P, :], SH2[0:P, :], ALU.add), reads=["tmp", "SH2"], writes=["h2"])
            S.op("dve", lambda e, P=P: e.tensor_copy(h2hi[0:P, :], h2[0:P, :]), reads=["h2"], writes=["h2hi"])
            S.op("dve", lambda e, P=P: e.tensor_tensor(h2lo[0:P, :], h2[0:P, :], h2hi[0:P, :], ALU.subtract), reads=["h2", "h2hi"], writes=["h2lo"])
            for (src, skey, dstv, dkey, bank) in ((h2hi, "h2hi", hT[:, :, g * 128:g * 128 + P], "hT", 3), (h2lo, "h2lo", hTl[:, :, 0:P], "hTl", 4)):
                psv = cx.PS[bank][:].bitcast(BF16).rearrange("p (k t) -> p k t", t=128)
                for kc in range(8):
                    S.op("pe", lambda e, psv=psv, kc=kc, src=src, P=P: e.transpose(psv[:, kc, 0:P], src[0:P, kc * 128:(kc + 1) * 128], identf[0:P, 0:P]),
                         reads=[skey, "identf"], writes=[f"ps{bank}"])
                S.op("act", lambda e, psv=psv, dstv=dstv, P=P: e.copy(dstv, psv[:, :, 0:P]), reads=[f"ps{bank}"], writes=[dkey])
            router(P, g)
            S.op("pool", lambda e, g=g, P=P: e.memset(yacc[0:P, g, :], 0.0), writes=["yacc"])
        for e_ in range(16):
            slot = eload % 2; eload += 1
            load_expert(e_, slot)
            for c0 in range(0, NT, 512):
                w = min(512, NT - c0)
                hsl = hh[(c0 // 512) % 2]
                for fc in range(4):
                    p1 = cx.PS[4 + (fc % 2) * 2]; p3 = cx.PS[5 + (fc % 2) * 2]
                    k1 = f"ps{4 + (fc % 2) * 2}"; k3 = f"ps{5 + (fc % 2) * 2}"
                    for wi, (pp, pk) in enumerate(((p1, k1), (p3, k3))):
                        for kc in range(8):
                            S.op("pe", lambda e, pp=pp, wi=wi, kc=kc, fc=fc, slot=slot, c0=c0, w=w: e.matmul(
                                pp[:, 0:w], W13[slot][:, wi, kc, fc * 128:(fc + 1) * 128], hT[:, kc, c0:c0 + w], start=(kc == 0), stop=(kc == 7)),
                                reads=[f"W{slot}", "hT"], writes=[pk])
                    S.op("act", lambda e, p1=p1, w=w: e.activation(hs[:, 0:w], p1[:, 0:w], AF.Silu), reads=[k1], writes=["hs"])
                    S.op("dve", lambda e, p3=p3, w=w, fc=fc, hsl=hsl: e.tensor_tensor(hsl[:, fc, 0:w], hs[:, 0:w], p3[:, 0:w], ALU.mult),
                         reads=["hs", k3], writes=[f"hh{(c0//512)%2}"])
                for t0_ in range(0, w, 128):
                    pw = min(128, w - t0_)
                    g = (c0 + t0_) // 128
                    for hb_ in range(2):
                        ps = cx.PS[hb_]
                        for fc in range(4):
                            S.op("pe", lambda e, ps=ps, fc=fc, hb_=hb_, slot=slot, t0_=t0_, pw=pw, hsl=hsl: e.matmul(
                                ps[0:pw, :], hsl[:, fc, t0_:t0_ + pw], W2[slot][:, fc, hb_ * 512:(hb_ + 1) * 512], start=(fc == 0), stop=(fc == 3)),
                                reads=[f"hh{(c0//512)%2}", f"W{slot}"], writes=[f"ps{hb_}"])
                        cs = slice(hb_ * 512, (hb_ + 1) * 512)
                        S.op("dve", lambda e, ps=ps, cs=cs, g=g, pw=pw, e_=e_: e.scalar_tensor_tensor(
                            yacc[0:pw, g, cs], ps[0:pw, :], gate[0:pw, g, e_:e_ + 1], yacc[0:pw, g, cs], ALU.mult, ALU.add),
                            reads=[f"ps{hb_}", "gate", "yacc"], writes=["yacc"])
        for g, (kind, i) in enumerate(grp):
            S.op("dve", lambda e, g=g, P=P: e.tensor_tensor(tmp[0:P, :], yacc[0:P, g, :], GG2[0:P, :], ALU.mult), reads=["yacc", "GG2"], writes=["tmp"])
            S.op("dve", lambda e, g=g, P=P: e.tensor_tensor(x1[0:P, g, :], x1[0:P, g, :], tmp[0:P, :], ALU.add), reads=["tmp", "x1"], writes=["x1"])
            dst = oc if isctx else ol[i * 128:(i + 1) * 128, :]
            if final:
                S.op("pool", lambda e, P=P: e.memset(ss[0:P, :], 0.0), writes=["ss"])
                S.op("act", lambda e, g=g, P=P: e.activation(junk[0:P, :], x1[0:P, g, :], AF.Square, accum_out=ss[0:P, :]),
                     reads=["x1", "ss"], writes=["ss", "junk"])
                S.op("dve", lambda e, P=P: e.tensor_scalar(rstd[0:P, :], ss[0:P, :], 1.0 / D, EPS, ALU.mult, ALU.add), reads=["ss"], writes=["rstd"])
                S.op("act", lambda e, P=P: e.sqrt(rstd[0:P, :], rstd[0:P, :]), reads=["rstd"], writes=["rstd"])
                S.op("dve", lambda e, P=P: e.reciprocal(rstd[0:P, :], rstd[0:P, :]), reads=["rstd"], writes=["rstd"])
                S.op("dve", lambda e, g=g, P=P: e.scalar_tensor_tensor(h2[0:P, :], x1[0:P, g, :], rstd[0:P, :], FG[0:P, :], ALU.mult, ALU.mult),
                     reads=["x1", "rstd", "FG"], writes=["h2"])
                S.dma("poolq", dst, h2[0:P, :], reads=["h2"], writes=["ol"])
            else:
                S.dma("poolq", dst, x1[0:P, g, :], reads=["x1"], writes=["ol"])
    if not with_ctx:
        S.dma("sp", xt[0][0:CPC, :], xc, writes=["xt0"])
        S.dma("sp", oc, xt[0][0:CPC, :], reads=["xt0"], writes=["oc"])
    cx.end_phase()


def fft_consts():
    N = 32768
    n1 = np.arange(64)[:, None]; k1 = np.arange(128)[None, :]
    a = 2 * np.pi * n1 * k1 / 128
    F1cat = np.concatenate([np.cos(a), -np.sin(a)], 1)
    n2 = np.arange(256)[:, None]
    t = 2 * np.pi * n2 * k1 / N
    Tr, Ti = np.cos(t), -np.sin(t)
    k2 = np.arange(256)[None, :]
    b = 2 * np.pi * n2 * k2 / 256
    F2r, F2i = np.cos(b), -np.sin(b)
    Er, Ei = np.cos(b.T), np.sin(b.T)
    IA = np.concatenate([Er, Ei], 1); IB = np.concatenate([-Ei, Er], 1)
    ITr, ITi = np.cos(t.T), np.sin(t.T)
    c = 2 * np.pi * np.arange(128)[:, None] * np.arange(64)[None, :] / 128
    G1r, G1i = np.cos(c) / N, -np.sin(c) / N
    ch2 = lambda m: np.ascontiguousarray(m.reshape(2, 128, m.shape[1]).transpose(1, 0, 2))
    psm = (np.arange(128)[:, None] % 64 == np.arange(128)[None, :] % 64).astype(np.float32)
    return {"F1cat": _bf16(F1cat), "Tr": ch2(Tr).astype(np.float32), "Ti": ch2(Ti).astype(np.float32),
            "F2r": _bf16(ch2(F2r)), "F2i": _bf16(ch2(F2i)), "F2in": _bf16(ch2(-F2i)),
            "IA": _bf16(ch2(IA)), "IB": _bf16(ch2(IB)), "ITr": ITr.astype(np.float32), "ITi": ITi.astype(np.float32),
            "G1r": _bf16(G1r), "G1i": _bf16(G1i), "PSM": psm}


def hy_pos_consts(L):
    t = np.arange(L, dtype=np.float32)
    t_norm = t / max(L - 1, 1)
    bands = np.linspace(1e-4, 15, 16, dtype=np.float32)
    ang = (np.float32(2.0 * math.pi / L) * t[:, None] * bands[None, :]).astype(np.float32)
    z = np.concatenate([t_norm[:, None], np.cos(ang), np.sin(ang)], axis=-1).astype(np.float32)
    return np.ascontiguousarray(z.T), np.ascontiguousarray(np.broadcast_to(t_norm[None, :], (128, L))).astype(np.float32)


def emit_B2(cx, with_ctx):
    L = SEQ
    PI = math.pi
    S = cx.S
    paths = [("l", SEQ)] + ([("c", CTX)] if with_ctx else [])
    I = {}
    for tag, Le in paths:
        I[tag] = dict(hy=cx.inp(f"hy_{tag}", [3, 64, Le + 2]), zT=cx.inp(f"zT_{tag}", [33, Le]), tn=cx.inp(f"tn_{tag}", [128, Le]),
                      pl=cx.inp(f"pl_{tag}", [64, Le + 24]), icnt=cx.inp(f"icnt_{tag}", [64, Le]),
                      oh=cx.out(f"oh_{tag}", [64, Le]), op=cx.out(f"op_{tag}", [64, Le]))
    shw_d = cx.inp("shw", [64, 3, 3]); shb_d = cx.inp("shb", [64, 3])
    fw1_d = cx.inp("fw1", [33, 64]); fb1_d = cx.inp("fb1", [64, 1]); fw2_d = cx.inp("fw2", [64, 64]); fb2_d = cx.inp("fb2", [64, 1])
    fw3_d = cx.inp("fw3", [64, 256]); fb3_d = cx.inp("fb3", [128, 2]); ndel_d = cx.inp("ndel", [128, 1])
    dsk_d = cx.inp("dsk", [64, 2, 64])
    psel_d = cx.inp("psel", [64, 4]); pw_d = cx.inp("pw", [64, 64]); psc_d = cx.inp("psc", [64, 1])
    FC = fft_consts()
    cd = {k: cx.inp("c_" + k, list(v.shape), BF16 if v.dtype != np.float32 else F32) for k, v in FC.items()}
    c = {k: cx.sb("c_" + k, list(v.shape), BF16 if v.dtype != np.float32 else F32) for k, v in FC.items()}
    for k in FC:
        S.dma("sp", c[k][:], cd[k], writes=["c_" + k])
    ck = ["c_" + k for k in FC]
    small = {}
    for nm, d_, shp in (("shw", shw_d, [64, 3, 3]), ("shb", shb_d, [64, 3]), ("fw1", fw1_d, [33, 64]), ("fb1", fb1_d, [64, 1]),
                        ("fw2", fw2_d, [64, 64]), ("fb2", fb2_d, [64, 1]), ("fw3", fw3_d, [64, 256]), ("fb3", fb3_d, [128, 2]),
                        ("ndel", ndel_d, [128, 1]), ("dsk", dsk_d, [64, 2, 64]), ("psel", psel_d, [64, 4]), ("pw", pw_d, [64, 64]),
                        ("psc", psc_d, [64, 1])):
        small[nm] = cx.sb("w_" + nm, shp)
        S.dma("sp", small[nm][:], d_, writes=["w_" + nm])
    pwb = cx.sb("pwb", [64, 64], BF16)
    S.op("dve", lambda e: e.tensor_copy(pwb[:], small["pw"][:]), reads=["w_pw"], writes=["pwb"])
    scx = cx.scratch("scx", [3, 64, L]); filt_s = cx.scratch("filt_s", [2, 128, L])
    zero = cx.sb("zero", [128, 2048])
    S.op("pool", lambda e: e.memset(zero[:], 0.0), writes=["zero"])

    def do_path(tag, Le):
        io = I[tag]
        CH = min(2048, Le)
        with ExitStack() as st:
            pin = cx.sb("pin", [64, CH + 24], F32, st); A2 = cx.sb("A2", [64, CH + 24], F32, st); A4 = cx.sb("A4", [64, CH + 24], F32, st)
            A8 = cx.sb("A8", [64, CH + 24], F32, st); A16 = cx.sb("A16", [64, CH + 24], F32, st)
            acc = cx.sb("pacc", [64, CH], F32, st); ic = cx.sb("pic", [64, CH], F32, st); pd = cx.sb("pd", [64, CH], BF16, st)
            po = cx.sb("po", [64, CH], F32, st)
            for c0 in range(0, Le, CH):
                S.dma("sp", pin[:], io["pl"][:, c0:c0 + CH + 24], writes=["pin"])
                S.dma("sp", ic[:], io["icnt"][:, c0:c0 + CH], writes=["pic"])
                W_ = CH + 24
                S.op("dve", lambda e: e.tensor_tensor(A2[:, 0:W_ - 1], pin[:, 0:W_ - 1], pin[:, 1:W_], ALU.add), reads=["pin"], writes=["A2"])
                S.op("dve", lambda e: e.tensor_tensor(A4[:, 0:W_ - 3], A2[:, 0:W_ - 3], A2[:, 2:W_ - 1], ALU.add), reads=["A2"], writes=["A4"])
                S.op("dve", lambda e: e.tensor_tensor(A8[:, 0:W_ - 7], A4[:, 0:W_ - 7], A4[:, 4:W_ - 3], ALU.add), reads=["A4"], writes=["A8"])
                S.op("dve", lambda e: e.tensor_tensor(A16[:, 0:W_ - 15], A8[:, 0:W_ - 15], A8[:, 8:W_ - 7], ALU.add), reads=["A8"], writes=["A16"])
                S.op("dve", lambda e: e.tensor_scalar(acc[:], A2[:, 7:7 + CH], small["psel"][:, 0:1], None, ALU.mult), reads=["A2", "w_psel"], writes=["pacc"])
                for k_, (Aw, off, key) in enumerate(((A4, 6, "A4"), (A8, 4, "A8"), (A16, 0, "A16"))):
                    S.op("dve", lambda e, Aw=Aw, off=off, k_=k_: e.scalar_tensor_tensor(acc[:], Aw[:, off:off + CH], small["psel"][:, k_ + 1:k_ + 2], acc[:],
                                                                                        ALU.mult, ALU.add), reads=[key, "w_psel", "pacc"], writes=["pacc"])
                S.op("dve", lambda e: e.tensor_tensor(acc[:], acc[:], ic[:], ALU.mult), reads=["pacc", "pic"], writes=["pacc"])
                S.op("dve", lambda e: e.tensor_tensor(pd[:], acc[:], pin[:, 8:8 + CH], ALU.subtract), reads=["pacc", "pin"], writes=["pd"])
                for s0 in range(0, CH, 512):
                    w = min(512, CH - s0)
                    S.op("pe", lambda e, s0=s0, w=w: e.matmul(cx.PS[7][0:64, 0:w], pwb[:, :], pd[:, s0:s0 + w], start=True, stop=True),
                         reads=["pd", "pwb"], writes=["ps7"])
                    S.op("dve", lambda e, s0=s0, w=w: e.tensor_scalar(po[:, s0:s0 + w], cx.PS[7][0:64, 0:w], small["psc"][:, 0:1], None, ALU.mult),
                         reads=["ps7", "w_psc"], writes=["po"])
                S.dma("poolq", io["op"][:, c0:c0 + CH], po[:], reads=["po"], writes=["op"])
            S.barrier()

        with ExitStack() as st:
            hin = cx.sb("hin", [64, CH + 2], F32, st); ho = cx.sb("ho", [64, CH], F32, st)
            if Le < L:
                for p in range(3):
                    for c0 in range(0, L, 2048):
                        S.dma("sp", scx[p][:, c0:c0 + 2048], zero[0:64, :], reads=["zero"], writes=["scx"])
                for oc in range(2):
                    for c0 in range(0, L, 2048):
                        S.dma("sp", filt_s[oc][:, c0:c0 + 2048], zero[:, :], reads=["zero"], writes=["filt_s"])
            for p in range(3):
                for c0 in range(0, Le, CH):
                    S.dma("sp", hin[:], io["hy"][p][:, c0:c0 + CH + 2], writes=["hin"])
                    S.op("dve", lambda e, p=p: e.tensor_scalar(ho[:], hin[:, 1:CH + 1], small["shw"][:, p, 1:2], small["shb"][:, p:p + 1], ALU.mult, ALU.add),
                         reads=["hin", "w_shw", "w_shb"], writes=["ho"])
                    S.op("dve", lambda e, p=p: e.scalar_tensor_tensor(ho[:], hin[:, 0:CH], small["shw"][:, p, 0:1], ho[:], ALU.mult, ALU.add),
                         reads=["hin", "w_shw", "ho"], writes=["ho"])
                    S.op("dve", lambda e, p=p: e.scalar_tensor_tensor(ho[:], hin[:, 2:CH + 2], small["shw"][:, p, 2:3], ho[:], ALU.mult, ALU.add),
                         reads=["hin", "w_shw", "ho"], writes=["ho"])
                    S.dma("poolq", scx[p][:, c0:c0 + CH], ho[:], reads=["ho"], writes=["scx"])
            S.barrier()

        with ExitStack() as st:
            FW = min(512, Le)
            nfc = Le // FW
            zt = cx.sb("zt", [33, FW], F32, st); tnt = cx.sb("tnt", [128, FW], F32, st)
            pre = cx.sb("pre", [64, FW], F32, st); msk = cx.sb("msk", [64, FW], F32, st); h1 = cx.sb("h1", [64, FW], F32, st); h2 = cx.sb("h2f", [64, FW], F32, st)
            dec = cx.sb("dec", [128, FW], F32, st); hf = [cx.sb(f"hf{i}", [128, FW], F32, st) for i in range(2)]
            junk = cx.sb("fjunk", [128, FW], F32, st)
            accsq = cx.sb("accsq", [128, 2, 32], F32, st); ssum = cx.sb("ssum", [128, 2], F32, st); rn = cx.sb("rn", [128, 2], F32, st)
            S.op("pool", lambda e: e.memset(accsq[:], 0.0), writes=["accsq"])

            def sin_layer(ps, bias, dst, key):
                S.op("dve", lambda e: e.tensor_scalar(pre[:], ps[0:64, 0:FW], bias[:, 0:1], None, ALU.add), reads=["ps0", "ps1", "w_fb1", "w_fb2"], writes=["pre"])
                for _ in range(2):
                    S.op("dve", lambda e: e.tensor_scalar(msk[:], pre[:], PI, -2 * PI, ALU.is_gt, ALU.mult), reads=["pre"], writes=["msk"])
                    S.op("dve", lambda e: e.tensor_tensor(pre[:], pre[:], msk[:], ALU.add), reads=["pre", "msk"], writes=["pre"])
                    S.op("dve", lambda e: e.tensor_scalar(msk[:], pre[:], -PI, 2 * PI, ALU.is_lt, ALU.mult), reads=["pre"], writes=["msk"])
                    S.op("dve", lambda e: e.tensor_tensor(pre[:], pre[:], msk[:], ALU.add), reads=["pre", "msk"], writes=["pre"])
                S.op("act", lambda e: e.activation(dst[:], pre[:], AF.Sin), reads=["pre"], writes=[key])

            for fc_ in range(nfc):
                sl = slice(fc_ * FW, (fc_ + 1) * FW)
                S.dma("sp", zt[:], io["zT"][:, sl], writes=["zt"]); S.dma("sp", tnt[:], io["tn"][:, sl], writes=["tnt"])
                S.op("pe", lambda e: e.matmul(cx.PS[0][0:64, 0:FW], small["fw1"][:, :], zt[:, :], start=True, stop=True), reads=["zt", "w_fw1"], writes=["ps0"])
                sin_layer(cx.PS[0], small["fb1"], h1, "h1")
                S.op("pe", lambda e: e.matmul(cx.PS[1][0:64, 0:FW], small["fw2"][:, :], h1[:, :], start=True, stop=True), reads=["h1", "w_fw2"], writes=["ps1"])
                sin_layer(cx.PS[1], small["fb2"], h2, "h2f")
                S.op("act", lambda e: e.activation(dec[:], tnt[:], AF.Exp, scale=small["ndel"][:, 0:1]), reads=["tnt", "w_ndel"], writes=["dec"])
                for oc in range(2):
                    S.op("pe", lambda e, oc=oc: e.matmul(cx.PS[2 + oc][:, 0:FW], small["fw3"][:, oc * 128:(oc + 1) * 128], h2[:, :], start=True, stop=True),
                         reads=["h2f", "w_fw3"], writes=[f"ps{2+oc}"])
                    S.op("dve", lambda e, oc=oc: e.scalar_tensor_tensor(hf[oc][:], cx.PS[2 + oc][:, 0:FW], small["fb3"][:, oc:oc + 1], dec[:], ALU.add, ALU.mult),
                         reads=[f"ps{2+oc}", "w_fb3", "dec"], writes=[f"hf{oc}"])
                    S.op("act", lambda e, oc=oc, fc_=fc_: e.activation(junk[:], hf[oc][:], AF.Square, accum_out=accsq[:, oc, fc_:fc_ + 1]),
                         reads=[f"hf{oc}", "accsq"], writes=["fjunk", "accsq"])
                    S.dma("poolq", filt_s[oc][:, sl], hf[oc][:], reads=[f"hf{oc}"], writes=["filt_s"])
            S.op("dve", lambda e: e.tensor_reduce(ssum[:], accsq[:], AX.X, ALU.add), reads=["accsq"], writes=["ssum"])
            S.op("pe", lambda e: e.matmul(cx.PS[4][:, 0:2], c["PSM"][:, :], ssum[:, :], start=True, stop=True), reads=["ssum", "c_PSM"], writes=["ps4"])
            S.op("dve", lambda e: e.tensor_scalar(rn[:], cx.PS[4][:, 0:2], EPS, None, ALU.add), reads=["ps4"], writes=["rn"])
            S.op("act", lambda e: e.sqrt(rn[:], rn[:]), reads=["rn"], writes=["rn"])
            S.op("dve", lambda e: e.reciprocal(rn[:], rn[:]), reads=["rn"], writes=["rn"])
            nb = cx.sb("nb", [128, 2048], F32, st)
            NW = min(2048, Le)
            for oc in range(2):
                for c0 in range(0, Le, NW):
                    S.dma("sp", nb[:, 0:NW], filt_s[oc][:, c0:c0 + NW], reads=["filt_s"], writes=["nb"])
                    S.op("dve", lambda e, oc=oc: e.tensor_scalar(nb[:, 0:NW], nb[:, 0:NW], rn[:, oc:oc + 1], None, ALU.mult), reads=["nb", "rn"], writes=["nb"])
                    if c0 == 0:
                        S.op("pool", lambda e: e.memset(nb[64:128, 0:1], 0.0), reads=["nb"], writes=["nb"])
                    S.dma("poolq", filt_s[oc][:, c0:c0 + NW], nb[:, 0:NW], reads=["nb"], writes=["filt_s"])
            S.barrier()

        with ExitStack() as st:
            Af = cx.sb("Af", [64, 2, 256], F32, st); Ab = cx.sb("Ab", [64, 2, 256], BF16, st)
            Bp = [cx.sb(f"Bp{i}", [128, 2, 2, 128], BF16, st) for i in range(2)]
            tw = [cx.sb(f"tw{i}", [128, 2, 128], F32, st) for i in range(4)]
            Xr = cx.sb("Xr", [128, 2, 2, 128], F32, st); Xi = cx.sb("Xi", [128, 2, 2, 128], F32, st)
            Kr = [cx.sb(f"Kr{o}", [128, 2, 2, 128], F32, st) for o in range(2)]; Ki = [cx.sb(f"Ki{o}", [128, 2, 2, 128], F32, st) for o in range(2)]
            yt_ = [cx.sb(f"yt{i}", [128, 2, 2, 128], F32, st) for i in range(4)]
            Yb = cx.sb("Yb", [128, 2, 2, 2, 128], BF16, st)
            Cp = cx.sb("Cp", [128, 2, 2, 256], BF16, st)
            it_ = [cx.sb(f"it{i}", [128, 256], F32, st) for i in range(4)]
            x1t = cx.sb("x1t", [64, 2, 256], F32, st); x2t = cx.sb("x2t", [64, 2, 256], F32, st); vt = cx.sb("vt", [64, 2, 256], F32, st)
            zt_ = cx.sb("zt_", [64, 2, 256], F32, st); e1 = cx.sb("e1", [64, 2, 256], F32, st); hout = cx.sb("hout", [64, 2, 256], F32, st)

            def tb(src2d):
                return src2d.rearrange("c (n1 n2) -> n1 c n2", n2=256)

            def fwd(Xr_, Xi_, xkey):
                for cc in range(2):
                    ps = cx.PS[cc]
                    for g in range(2):
                        S.op("pe", lambda e, ps=ps, g=g, cc=cc: e.matmul(ps[:, g * 256:(g + 1) * 256], Ab[:, g, cc * 128:(cc + 1) * 128], c["F1cat"][:, :],
                                                                      start=True, stop=True), reads=["Ab", "c_F1cat"], writes=[f"ps{cc}"])
                    psv = ps[:].rearrange("p (g r k) -> p g r k", g=2, r=2)
                    Trb = c["Tr"][:, cc, :].unsqueeze(1).to_broadcast([128, 2, 128]); Tib = c["Ti"][:, cc, :].unsqueeze(1).to_broadcast([128, 2, 128])
                    S.op("dve", lambda e, psv=psv, Trb=Trb: e.tensor_tensor(tw[0][:], psv[:, :, 0, :], Trb, ALU.mult), reads=[f"ps{cc}", "c_Tr"], writes=["tw0"])
                    S.op("dve", lambda e, psv=psv, Tib=Tib: e.tensor_tensor(tw[1][:], psv[:, :, 1, :], Tib, ALU.mult), reads=[f"ps{cc}", "c_Ti"], writes=["tw1"])
                    S.op("dve", lambda e, psv=psv, Tib=Tib: e.tensor_tensor(tw[2][:], psv[:, :, 0, :], Tib, ALU.mult), reads=[f"ps{cc}", "c_Ti"], writes=["tw2"])
                    S.op("dve", lambda e, psv=psv, Trb=Trb: e.tensor_tensor(tw[3][:], psv[:, :, 1, :], Trb, ALU.mult), reads=[f"ps{cc}", "c_Tr"], writes=["tw3"])
                    S.op("pool", lambda e, cc=cc: e.tensor_tensor(Bp[cc][:, 0, :, :], tw[0][:], tw[1][:], ALU.subtract), reads=["tw0", "tw1"], writes=[f"Bp{cc}"])
                    S.op("pool", lambda e, cc=cc: e.tensor_tensor(Bp[cc][:, 1, :, :], tw[2][:], tw[3][:], ALU.add), reads=["tw2", "tw3"], writes=[f"Bp{cc}"])
                for kc in range(2):
                    ks = slice(kc * 128, (kc + 1) * 128)
                    psr, psi = cx.PS[2 + 2 * kc], cx.PS[3 + 2 * kc]
                    seq_r = [(c["F2r"], 0), (c["F2in"], 1)]
                    seq_i = [(c["F2i"], 0), (c["F2r"], 1)]
                    for (pp, seq, pk) in ((psr, seq_r, f"ps{2+2*kc}"), (psi, seq_i, f"ps{3+2*kc}")):
                        n_ = 0
                        for cc in range(2):
                            for (M_, ri) in seq:
                                S.op("pe", lambda e, pp=pp, M_=M_, ri=ri, cc=cc, n_=n_, ks=ks: e.matmul(
                                    pp[:, 0:256], M_[:, cc, ks], Bp[cc][:, ri, :, :], start=(n_ == 0), stop=(n_ == 3)),
                                    reads=[f"Bp{cc}"] + ck, writes=[pk])
                                n_ += 1
                    S.op("act", lambda e, psr=psr, kc=kc: e.copy(Xr_[:, kc, :, :], psr[:, 0:256]), reads=[f"ps{2+2*kc}"], writes=[xkey + "r"])
                    S.op("act", lambda e, psi=psi, kc=kc: e.copy(Xi_[:, kc, :, :], psi[:, 0:256]), reads=[f"ps{3+2*kc}"], writes=[xkey + "i"])

            def conv(o, ydst_key):
                fwd(Xr, Xi, "X")
                fl = lambda t_: t_[:].rearrange("p a b c -> p (a b c)")
                S.op("dve", lambda e: e.tensor_tensor(fl(yt_[0]), fl(Xr), fl(Kr[o]), ALU.mult), reads=["Xr", f"K{o}r"], writes=["yt0"])
                S.op("pool", lambda e: e.tensor_tensor(fl(yt_[1]), fl(Xi), fl(Ki[o]), ALU.mult), reads=["Xi", f"K{o}i"], writes=["yt1"])
                S.op("dve", lambda e: e.tensor_tensor(fl(yt_[2]), fl(Xr), fl(Ki[o]), ALU.mult), reads=["Xr", f"K{o}i"], writes=["yt2"])
                S.op("pool", lambda e: e.tensor_tensor(fl(yt_[3]), fl(Xi), fl(Kr[o]), ALU.mult), reads=["Xi", f"K{o}r"], writes=["yt3"])
                S.op("dve", lambda e: e.tensor_tensor(Yb[:, :, 0, :, :], yt_[0][:], yt_[1][:], ALU.subtract), reads=["yt0", "yt1"], writes=["Yb"])
                S.op("pool", lambda e: e.tensor_tensor(Yb[:, :, 1, :, :], yt_[2][:], yt_[3][:], ALU.add), reads=["yt2", "yt3"], writes=["Yb"])
                for g in range(2):
                    ps = cx.PS[g]
                    n_ = 0
                    for kc in range(2):
                        for (ri, M_) in ((0, c["IA"]), (1, c["IB"])):
                            S.op("pe", lambda e, ps=ps, kc=kc, ri=ri, M_=M_, g=g, n_=n_: e.matmul(ps[:, :], Yb[:, kc, ri, g, :], M_[:, kc, :], start=(n_ == 0), stop=(n_ == 3)),
                                 reads=["Yb"] + ck, writes=[f"ps{g}"])
                            n_ += 1
                    S.op("dve", lambda e, ps=ps: e.tensor_tensor(it_[0][:], ps[:, 0:256], c["ITr"][:], ALU.mult), reads=[f"ps{g}", "c_ITr"], writes=["it0"])
                    S.op("dve", lambda e, ps=ps: e.tensor_tensor(it_[1][:], ps[:, 256:512], c["ITi"][:], ALU.mult), reads=[f"ps{g}", "c_ITi"], writes=["it1"])
                    S.op("dve", lambda e, ps=ps: e.tensor_tensor(it_[2][:], ps[:, 0:256], c["ITi"][:], ALU.mult), reads=[f"ps{g}", "c_ITi"], writes=["it2"])
                    S.op("dve", lambda e, ps=ps: e.tensor_tensor(it_[3][:], ps[:, 256:512], c["ITr"][:], ALU.mult), reads=[f"ps{g}", "c_ITr"], writes=["it3"])
                    S.op("pool", lambda e, g=g: e.tensor_tensor(Cp[:, 0, g, :], it_[0][:], it_[1][:], ALU.subtract), reads=["it0", "it1"], writes=["Cp"])
                    S.op("pool", lambda e, g=g: e.tensor_tensor(Cp[:, 1, g, :], it_[2][:], it_[3][:], ALU.add), reads=["it2", "it3"], writes=["Cp"])
                S.op("pe", lambda e: e.matmul(cx.PS[6][0:64, :], c["G1r"][:, :], Cp[:, 0, :, :], start=True, stop=False), reads=["Cp", "c_G1r"], writes=["ps6"])
                S.op("pe", lambda e: e.matmul(cx.PS[6][0:64, :], c["G1i"][:, :], Cp[:, 1, :, :], start=False, stop=True), reads=["Cp", "c_G1i"], writes=["ps6"])

            for pr in range(32):
                ch0 = pr * 2
                for o in range(2):
                    for d_ in range(2):
                        S.dma("sp", Af[:], tb(filt_s[o][d_ * 64 + ch0:d_ * 64 + ch0 + 2, :]), reads=["filt_s"], writes=["Af"])
                        S.op("dve", lambda e: e.tensor_copy(Ab[:], Af[:]), reads=["Af"], writes=["Ab"])
                        if d_ == 0:
                            fwd(Kr[o], Ki[o], f"K{o}")
                        else:
                            fwd(Xr, Xi, "X")
                            S.op("pool", lambda e, o=o: e.tensor_tensor(Kr[o][:], Kr[o][:], Xr[:], ALU.add), reads=[f"K{o}r", "Xr"], writes=[f"K{o}r"])
                            S.op("pool", lambda e, o=o: e.tensor_tensor(Ki[o][:], Ki[o][:], Xi[:], ALU.subtract), reads=[f"K{o}i", "Xi"], writes=[f"K{o}i"])
                S.dma("sp", x1t[:], tb(scx[0][ch0:ch0 + 2, :]), reads=["scx"], writes=["x1t"])
                S.dma("sp", x2t[:], tb(scx[1][ch0:ch0 + 2, :]), reads=["scx"], writes=["x2t"])
                S.dma("sp", vt[:], tb(scx[2][ch0:ch0 + 2, :]), reads=["scx"], writes=["vt"])
                S.op("dve", lambda e: e.tensor_copy(Ab[:], vt[:]), reads=["vt"], writes=["Ab"])
                conv(0, "y1")
                dk = lambda o: small["dsk"][:, o, ch0:ch0 + 2].unsqueeze(2).to_broadcast([64, 2, 256])
                psy = cx.PS[6][0:64, :].rearrange("p (g n) -> p g n", g=2)
                dk0 = dk(0); dk1 = dk(1)
                S.op("pool", lambda e, dk0=dk0: e.tensor_tensor(e1[:], vt[:], dk0, ALU.mult), reads=["vt", "w_dsk"], writes=["e1"])
                S.op("dve", lambda e: e.tensor_tensor(e1[:], e1[:], psy, ALU.add), reads=["e1", "ps6"], writes=["e1"])
                S.op("dve", lambda e: e.tensor_tensor(zt_[:], e1[:], x1t[:], ALU.mult), reads=["e1", "x1t"], writes=["zt_"])
                S.op("dve", lambda e: e.tensor_copy(Ab[:], zt_[:]), reads=["zt_"], writes=["Ab"])
                conv(1, "y2")
                S.op("pool", lambda e, dk1=dk1: e.tensor_tensor(e1[:], zt_[:], dk1, ALU.mult), reads=["zt_", "w_dsk"], writes=["e1"])
                S.op("dve", lambda e: e.tensor_tensor(e1[:], e1[:], psy, ALU.add), reads=["e1", "ps6"], writes=["e1"])
                S.op("dve", lambda e: e.tensor_tensor(hout[:], e1[:], x2t[:], ALU.mult), reads=["e1", "x2t"], writes=["hout"])
                if Le == L:
                    S.dma("poolq", tb(io["oh"][ch0:ch0 + 2, :]), hout[:], reads=["hout"], writes=["oh"])
                else:
                    S.dma("poolq", io["oh"][ch0:ch0 + 2, :].rearrange("(o c) n -> o c n", o=1), hout[0:1, :, 0:Le], reads=["hout"], writes=["oh"])
            S.barrier()

    for tag_, Le_ in paths:
        do_path(tag_, Le_)
    cx.end_phase()


POOL_SIZES = (2, 4, 8, 16)
NKEY = SEQ + CTX
NLOC = TPC + CPC
GROUPS = [[0, 1, 2, 3], [4, 5, 6, 7]]
SECS = {"aq": 0, "ak": 256, "av": 512, "bq": 768, "bk": 1024, "bv": 1280, "pool": 1536, "hy0": 1792, "hy1": 2048, "hy2": 2304}


def emit_R(cx, need_ctx, T):
    S = cx.S
    ag_out = T["ag_out"]
    selq_d = cx.inp("selq", [128, 2, 2, 32]); selg_d = cx.inp("selg", [128, 2, 64])
    selq = cx.sb("selq", [128, 2, 2, 32]); selg = cx.sb("selg", [128, 2, 64])
    S.dma("sp", selq[:], selq_d, writes=["selq"]); S.dma("sp", selg[:], selg_d, writes=["selg"])
    xs = [cx.sb(f"rx{i}", [128, 2, 512]) for i in range(3)]
    ev = [cx.sb(f"rev{i}", [64, 512]) for i in range(2)]
    k33 = [cx.sb(f"k33_{i}", [33, 512]) for i in range(2)]
    k65 = cx.sb("k65", [65, 512])
    v65 = [cx.sb(f"v65_{i}", [128, 65]) for i in range(2)]
    zero = cx.sb("rzero", [64, 16])
    S.op("pool", lambda e: e.memset(zero[:], 0.0), writes=["rzero"])
    for t_ in k33:
        S.op("pool", lambda e, t_=t_: e.memset(t_[32:33, :], 1.0), writes=["k33"])
    S.op("pool", lambda e: e.memset(k65[64:65, :], 1.0), writes=["k65"])
    for t_ in v65:
        S.op("pool", lambda e, t_=t_: e.memset(t_[:, 64:65], 1.0), writes=["v65"])
    for tag, Le in (("l", SEQ),) + ((("c", CTX),) if need_ctx else ()):
        for p in range(3):
            S.dma("sp", T[f"hy_{tag}"][p][:, 0:1], zero[:, 0:1], reads=["rzero"], writes=["hy"], allow_slow_non_contiguous=True)
            S.dma("sp", T[f"hy_{tag}"][p][:, Le + 1:Le + 2], zero[:, 0:1], reads=["rzero"], writes=["hy"], allow_slow_non_contiguous=True)
        S.dma("sp", T[f"pl_{tag}"][:, 0:8], zero[:, 0:8], reads=["rzero"], writes=["pl"])
        S.dma("sp", T[f"pl_{tag}"][:, Le + 8:Le + 24], zero[:, 0:16], reads=["rzero"], writes=["pl"])
    cnt = {"x": 0, "ps": 0, "ev": 0, "k": 0, "v": 0, "q": 0}

    def load(sec, pieces, w):
        i = cnt["x"] % 3; cnt["x"] += 1
        X = xs[i]
        o = 0
        for (r, c0, pw) in pieces:
            n = pw // 64
            for kc in range(2):
                r0 = r * DIN + SECS[sec] + kc * 128
                src = ag_out[c0 // 64:c0 // 64 + n, r0:r0 + 128, :].rearrange("ck p t -> p ck t")
                q = ("sp", "actq")[cnt["q"] % 2]; cnt["q"] += 1
                S.dma(q, X[:, kc, o:o + pw].rearrange("p (ck t) -> p ck t", t=64), src, writes=[f"rx{i}"])
            o += pw
        return X, f"rx{i}"

    def sel_fm(X, xk, w, SEL, M, dst, ones=None):
        pi = cnt["ps"] % 8; cnt["ps"] += 1
        ps = cx.PS[pi]
        for kc in range(2):
            S.op("pe", lambda e, ps=ps, kc=kc, SEL=SEL: e.matmul(ps[0:M, 0:w], SEL[:, kc, :], X[:, kc, 0:w], start=(kc == 0), stop=(kc == 1)),
                 reads=[xk, "selq", "selg"], writes=[f"ps{pi}"])
        if ones == "k33":
            i = cnt["k"] % 2; cnt["k"] += 1
            dt_, dk, rows = k33[i], f"k33_{i}", 33
        elif ones == "k65":
            dt_, dk, rows = k65, "k65", 65
        else:
            i = cnt["ev"] % 2; cnt["ev"] += 1
            dt_, dk, rows = ev[i], f"rev{i}", M
        S.op(("act", "dve")[cnt["ps"] % 2], lambda e, ps=ps, dt_=dt_: (e.copy if hasattr(e, "copy") else e.tensor_copy)(dt_[0:M, 0:w], ps[0:M, 0:w]),
             reads=[f"ps{pi}"], writes=[dk])
        S.dma("poolq", dst, dt_[0:rows, 0:w], reads=[dk], writes=["rdst"])

    def sel_tm(X, xk, w, dst_rows):
        for s0 in range(0, w, 128):
            pi = cnt["ps"] % 8; cnt["ps"] += 1
            ps = cx.PS[pi]
            for kc in range(2):
                S.op("pe", lambda e, ps=ps, kc=kc, s0=s0: e.matmul(ps[:, 0:64], X[:, kc, s0:s0 + 128], selg[:, kc, :], start=(kc == 0), stop=(kc == 1)),
                     reads=[xk, "selg"], writes=[f"ps{pi}"])
            i = cnt["v"] % 2; cnt["v"] += 1
            S.op(("act", "dve")[i], lambda e, ps=ps, i=i: (e.copy if hasattr(e, "copy") else e.tensor_copy)(v65[i][:, 0:64], ps[:, 0:64]),
                 reads=[f"ps{pi}"], writes=[f"v65_{i}"])
            S.dma("poolq", dst_rows[s0:s0 + 128, :], v65[i][:, :], reads=[f"v65_{i}"], writes=["rdst"])

    chunks = [("l", [(r, cp * 512, 512)], r * TPC + cp * 512, 512) for r in range(4) for cp in range(8)]
    chunks.append(("c", [(r, TPC, CPC) for r in range(4)], SEQ, CTX))
    for kind, pieces, t0, w in chunks:
        isl = kind == "l"
        if isl or need_ctx:
            X, xk = load("aq", pieces, w)
            for c_ in range(2):
                sel_fm(X, xk, w, selq[:, :, c_, :], 32, (T["aqT"][c_][:, t0:t0 + w] if isl else T["aqcT"][c_][:, :]))
            X, xk = load("bq", pieces, w)
            sel_fm(X, xk, w, selg, 64, (T["bqT"][:, t0:t0 + w] if isl else T["bqcT"][:, :]))
            tag = "l" if isl else "c"
            tt = t0 if isl else 0
            X, xk = load("pool", pieces, w)
            sel_fm(X, xk, w, selg, 64, T[f"pl_{tag}"][:, 8 + tt:8 + tt + w])
            for p in range(3):
                X, xk = load(f"hy{p}", pieces, w)
                sel_fm(X, xk, w, selg, 64, T[f"hy_{tag}"][p][:, 1 + tt:1 + tt + w])
        X, xk = load("ak", pieces, w)
        for c_ in range(2):
            sel_fm(X, xk, w, selq[:, :, c_, :], 32, T["akT"][c_][:, t0:t0 + w], ones="k33")
        X, xk = load("bk", pieces, w)
        sel_fm(X, xk, w, selg, 64, T["bkT"][:, t0:t0 + w], ones="k65")
        X, xk = load("av", pieces, w)
        sel_tm(X, xk, w, T["av"][t0:t0 + w, :])
        X, xk = load("bv", pieces, w)
        sel_tm(X, xk, w, T["bv"][t0:t0 + w, :])
    cx.end_phase()


def emit_W(cx, need_ctx, T):
    S = cx.S
    mixT = T["mixT"]; rs_in = T["rs_in"]
    wop_d = cx.inp("wo_part", [4, 64, D])
    wst = cx.sb("wwst", [64, 4, D]); wb = cx.sb("wwb", [64, 4, D], BF16)
    S.dma("sp", wst[:], wop_d.rearrange("m p n -> p m n"), writes=["wwst"])
    S.op("dve", lambda e: e.tensor_copy(wb[:], wst[:]), reads=["wwst"], writes=["wwb"])
    mt = [cx.sb(f"wmt{i}", [64, 4, 128]) for i in range(2)]
    mb = [cx.sb(f"wmb{i}", [64, 4, 128], BF16) for i in range(2)]
    ot = [cx.sb(f"wot{i}", [128, D]) for i in range(2)]
    rs_out = T["rs_out"]
    cckey = cx.coll_group()
    nlb = TPC // 128
    order = [j * nlb + lb for lb in range(nlb) for j in range(4)]
    if need_ctx:
        order += [SEQ // 128 + ci for ci in range(CTX // 128)]
    NCH = NLOC // RSR
    issued = [0]
    chunk_keys = {k: [] for k in range(NCH)}

    def issue_upto(local_done):
        while issued[0] < NCH and (issued[0] + 1) * RSR <= local_done:
            k = issued[0]
            cx.coll(cckey, "ReduceScatter", ALU.add, GROUPS, rs_in[k], rs_out[k * RSR:(k + 1) * RSR, :], sorted(set(chunk_keys[k])))
            issued[0] += 1

    for n_, i in enumerate(order):
        s = n_ % 2
        S.dma(("sp", "actq")[s], mt[s][:], mixT[:, :, i * 128:(i + 1) * 128].rearrange("m p t -> p m t"), writes=[f"wmt{s}"])
        S.op("pool", lambda e, s=s: e.tensor_copy(mb[s][:], mt[s][:]), reads=[f"wmt{s}"], writes=[f"wmb{s}"])
        for hb_ in range(2):
            pi = (2 * n_ + hb_) % 8
            ps = cx.PS[pi]
            for m in range(4):
                S.op("pe", lambda e, ps=ps, m=m, hb_=hb_, s=s: e.matmul(ps[:, :], mb[s][:, m, :], wb[:, m, hb_ * 512:(hb_ + 1) * 512], start=(m == 0), stop=(m == 3)),
                     reads=[f"wmb{s}", "wwb"], writes=[f"ps{pi}"])
            S.op(("act", "dve")[hb_], lambda e, ps=ps, hb_=hb_, s=s: (e.copy if hasattr(e, "copy") else e.tensor_copy)(ot[s][:, hb_ * 512:(hb_ + 1) * 512], ps[:, :]),
                 reads=[f"ps{pi}"], writes=[f"wot{s}h{hb_}"])

        def put(j, loc, p0, n):
            while n > 0:
                k, q0 = loc // RSR, loc % RSR
                m_ = min(n, RSR - q0)
                key = f"rs_in{k}_{j}_{q0}"
                S.dma("poolq", rs_in[k][j * RSR + q0:j * RSR + q0 + m_, :], ot[s][p0:p0 + m_, :], reads=[f"wot{s}h0", f"wot{s}h1"], writes=[key])
                chunk_keys[k].append(key)
                loc += m_; p0 += m_; n -= m_
        if i < SEQ // 128:
            j, lb = i // nlb, i % nlb
            put(j, lb * 128, 0, 128)
            if j == 3 and need_ctx:
                issue_upto((lb + 1) * 128)
            elif j == 3:
                issue_upto((lb + 1) * 128 if lb < nlb - 1 else NLOC)
        else:
            ci = i - SEQ // 128
            for hf in range(2):
                put(ci * 2 + hf, TPC, hf * 64, 64)
            if ci == CTX // 128 - 1:
                issue_upto(NLOC)
    cx.end_phase()


SHARED = {"sel", "ident", "identF", "ropec", "ropes", "onesb", "E65", "ones64", "cT", "fin_g", "router_w", "router_b",
          "zT_l", "tn_l", "zT_c", "tn_c", "icnt_l", "icnt_c", "psel", "ndel", "selq", "selg", "xl", "xc"}


def build_fused(stop=None):
    cx = Ctx("F")
    cx.shared = set(SHARED)
    sc = cx.scratch
    T = {"ag_in": sc("ag_in", [NAGC, DIN, 64]), "ag_out": sc("ag_out", [NAGC, 4 * DIN, 64]),
         "aqT": sc("aqT", [2, 32, SEQ]), "akT": sc("akT", [2, 33, NKEY]), "av": sc("av", [NKEY, 65]), "aqcT": sc("aqcT", [2, 32, CTX]),
         "bqT": sc("bqT", [64, SEQ]), "bkT": sc("bkT", [65, NKEY]), "bv": sc("bv", [NKEY, 65]), "bqcT": sc("bqcT", [64, CTX]),
         "hy_l": sc("hy_l", [3, 64, SEQ + 2]), "pl_l": sc("pl_l", [64, SEQ + 24]),
         "hy_c": sc("hy_c", [3, 64, CTX + 2]), "pl_c": sc("pl_c", [64, CTX + 24]),
         "mixT": sc("mixT", [4, 64, NKEY]), "rs_in": sc("rs_in", [NLOC // RSR, 4 * RSR, D]), "rs_out": sc("rs_out", [NLOC, D]),
         "xl_s": sc("xl_s", [TPC, D]), "xc_s": sc("xc_s", [CPC, D]), "dummy": sc("dummy_oc", [CPC, D])}
    x_l = cx.inp("xl", [TPC, D]); x_c = cx.inp("xc", [CPC, D])
    out = cx.nc.dram_tensor("out", [TPC, D], F32, kind="ExternalOutput").ap()
    mixT = T["mixT"]

    def dump(src2d):
        r, c_ = src2d.shape
        dst = out.rearrange("a d -> (a d)")[0:r * c_].rearrange("(r c) -> r c", c=c_)
        cx.S.dma("sp", dst, src2d, writes=["dbg"])
        return cx.finish(), cx

    for li in range(2):
        need_ctx = li < 1
        cx.prefix = f"L{li}_"
        cx.over = {"xl": x_l if li == 0 else T["xl_s"], "xc": x_c if li == 0 else T["xc_s"], "ag_in": T["ag_in"], "ag_out": T["ag_out"]}
        emit_A(cx)
        if stop == "A":
            return dump(T["ag_in"][0:25, 0:DIN, :].rearrange("a b c -> a (b c)"))
        if stop == "AG":
            return dump(T["ag_out"][0:25, DIN:2 * DIN, :].rearrange("a b c -> a (b c)"))
        cx.over = {}
        emit_R(cx, need_ctx, T)
        if stop == "R":
            return dump(T["bkT"][:, :])
        cx.over = {k: T[k] for k in ("aqT", "akT", "av", "aqcT", "bqT", "bkT", "bv", "bqcT")}
        cx.over.update({"oa": mixT[0][:, 0:SEQ], "oac": mixT[0][:, SEQ:NKEY], "ob": mixT[1][:, 0:SEQ], "obc": mixT[1][:, SEQ:NKEY]})
        emit_B1(cx, li)
        if stop == "B1":
            return dump(mixT[1][:, :])
        cx.over = {"hy_l": T["hy_l"], "pl_l": T["pl_l"], "hy_c": T["hy_c"], "pl_c": T["pl_c"],
                   "op_l": mixT[2][:, 0:SEQ], "op_c": mixT[2][:, SEQ:NKEY], "oh_l": mixT[3][:, 0:SEQ], "oh_c": mixT[3][:, SEQ:NKEY]}
        emit_B2(cx, need_ctx)
        cx.over = {}
        if stop == "B2":
            return dump(mixT[3][:, :])
        emit_W(cx, need_ctx, T)
        if stop == "W":
            return dump(T["rs_in"][0:4, :, :].rearrange("a b c -> (a b) c"))
        if stop == "RS":
            return dump(T["rs_out"][0:TPC, :])
        cx.over = {"xl": x_l if li == 0 else T["xl_s"], "xc": x_c if li == 0 else T["xc_s"], "rs_out": T["rs_out"],
                   "ol": T["xl_s"] if li == 0 else out, "oc": T["xc_s"] if li == 0 else T["dummy"]}
        emit_C(cx, need_ctx, li == 1)
    return cx.finish(), cx


_CACHE = {}
STOP = None


def kernel(x, c, ctx, c_ctx, norm1_g, norm2_g, ada_w, ada_b, w_in, w_out, a_lambda, a_subln_g,
           b_rpb, pool_w, pool_scale, hy_short_w, hy_short_b, hy_f_w1, hy_f_b1, hy_f_w2, hy_f_b2,
           hy_f_w3, hy_f_b3, hy_skip, router_w, router_b, moe_w1, moe_w3, moe_w2, final_g):
    f = lambda a: np.ascontiguousarray(np.asarray(a, dtype=np.float32))
    if "F" not in _CACHE:
        _CACHE["F"] = build_fused(STOP)
    nc, cx = _CACHE["F"]
    x, ctx, c, c_ctx = f(x), f(ctx), f(c), f(c_ctx)
    rc, rs = const_rope()
    FC = fft_consts()
    deltas = np.abs(np.linspace(math.log(1e-2) / 1.5, math.log(1e-2) / 0.3, 256, dtype=np.float32))
    pos = {"l": hy_pos_consts(SEQ), "c": hy_pos_consts(CTX)}
    E65 = np.zeros((65, 64), np.float32); E65[64] = 1.0
    shared_all = {"sel": const_sel(), "ident": _bf16(np.eye(128)), "identF": np.eye(128, dtype=np.float32), "onesb": _bf16(np.ones((128, 128))),
                  "E65": E65, "ones64": np.ones((64, 64), np.float32), "fin_g": f(final_g).reshape(1, -1),
                  "router_w": f(router_w), "router_b": f(router_b).reshape(1, -1),
                  "zT_l": pos["l"][0], "tn_l": pos["l"][1], "zT_c": pos["c"][0], "tn_c": pos["c"][1]}
    shared_all.update({"c_" + k: v for k, v in FC.items()})
    in_maps = []
    for core in range(NCORE):
        b, j = core // 4, core % 4
        h = g = j
        chs = slice(g * 64, (g + 1) * 64)
        m = dict(shared_all)
        m["xl"] = np.ascontiguousarray(x[b, j * TPC:(j + 1) * TPC]); m["xc"] = np.ascontiguousarray(ctx[b, j * CPC:(j + 1) * CPC])
        m["cT"] = cT_layout(c[b], c_ctx)
        m["ropec"] = np.ascontiguousarray(rc[j * TPC:(j + 1) * TPC]); m["ropes"] = np.ascontiguousarray(rs[j * TPC:(j + 1) * TPC])
        selq = np.zeros((256, 2, 32), np.float32); selg = np.zeros((256, 64), np.float32)
        for c_ in range(2):
            selq[h * 64 + c_ * 32 + np.arange(32), c_, np.arange(32)] = 1.0
        selg[h * 64 + np.arange(64), np.arange(64)] = 1.0
        m["selq"] = np.ascontiguousarray(selq.reshape(2, 128, 2, 32).transpose(1, 0, 2, 3))
        m["selg"] = np.ascontiguousarray(selg.reshape(2, 128, 64).transpose(1, 0, 2))
        for tag, Le in (("l", SEQ), ("c", CTX)):
            t = np.arange(Le); w = POOL_SIZES[g]
            cnt = (np.clip(t + w // 2, 0, Le) - np.clip(t - w // 2, 0, Le)).astype(np.float32)
            m[f"icnt_{tag}"] = np.ascontiguousarray(np.broadcast_to((1.0 / cnt)[None, :], (64, Le))).astype(np.float32)
        ps = np.zeros((64, 4), np.float32); ps[:, g] = 1.0
        m["psel"] = ps
        m["ndel"] = np.ascontiguousarray(np.tile(-deltas[chs], 2).reshape(128, 1))
        for li in range(2):
            p = f"L{li}_"
            m[p + "ada_w"] = f(ada_w[li]); m[p + "ada_b"] = f(ada_b[li]).reshape(1, -1)
            m[p + "norm_g"] = f(norm1_g[li]).reshape(1, -1); m[p + "norm2_g"] = f(norm2_g[li]).reshape(1, -1)
            m[p + "w_in"] = f(w_in[li])
            m[p + "alam"] = f(a_lambda[li]).reshape(1, 128); m[p + "subg"] = f(a_subln_g[li]).reshape(64, 1)
            m[p + "bias"] = na_bias_sets(f(b_rpb[li])[h])
            sw = f(hy_short_w[li]).reshape(3, 3, 256)[:, :, chs]
            m[p + "shw"] = np.ascontiguousarray(sw.transpose(2, 1, 0)); m[p + "shb"] = np.ascontiguousarray(f(hy_short_b[li]).reshape(3, 256)[:, chs].T)
            m[p + "fw1"] = f(hy_f_w1[li]); m[p + "fb1"] = f(hy_f_b1[li]).reshape(64, 1)
            m[p + "fw2"] = f(hy_f_w2[li]); m[p + "fb2"] = f(hy_f_b2[li]).reshape(64, 1)
            w3 = f(hy_f_w3[li]).reshape(64, 2, 2, 256)[:, :, :, chs]
            m[p + "fw3"] = np.ascontiguousarray(w3.reshape(64, 256))
            b3 = f(hy_f_b3[li]).reshape(2, 2, 256)[:, :, chs]
            m[p + "fb3"] = np.ascontiguousarray(b3.reshape(2, 128).T)
            m[p + "dsk"] = np.ascontiguousarray(np.broadcast_to(f(hy_skip[li])[:, chs][None], (64, 2, 64))).astype(np.float32)
            m[p + "pw"] = np.ascontiguousarray(f(pool_w[li])[g]); m[p + "psc"] = np.ascontiguousarray(f(pool_scale[li])[chs].reshape(64, 1))
            wo = f(w_out[li])
            m[p + "wo_part"] = np.ascontiguousarray(np.stack([wo[mm * 256 + h * 64:mm * 256 + (h + 1) * 64] for mm in range(4)]))
            m[p + "w1"] = f(moe_w1[li]); m[p + "w3"] = f(moe_w3[li]); m[p + "w2"] = f(moe_w2[li])
        in_maps.append({k: v for k, v in m.items() if k in cx.ins})
    missing = [k for k in cx.ins if k not in in_maps[0]]
    assert not missing, missing
    res = run_bass_kernel_spmd(nc, in_maps, core_ids=list(range(NCORE)))
    out = np.stack([np.concatenate([res.results[b * 4 + j]["out"] for j in range(4)], 0) for b in range(2)])
    return out.astype(np.float32)
```

```python
import math
from contextlib import ExitStack
import numpy as np
import concourse.bass as bass
import concourse.mybir as mybir
from concourse.bass_utils import run_bass_kernel_spmd

F32 = mybir.dt.float32
BF16 = mybir.dt.bfloat16
AF = mybir.ActivationFunctionType
ALU = mybir.AluOpType
AX = mybir.AxisListType

D = 1024
SEQ = 16384
NB = 2
CTX = 256
DIN = 2560
NCORE = 8
TPC = SEQ // 4
NAGC = 65
RSR = 208
CPC = CTX // 4
EPS = 1e-6

COMPUTE = ("pe", "dve", "act", "pool")
QUEUES = ("sp", "actq", "poolq")
ISSUER = {"sp": "sp", "actq": "act", "poolq": "pool"}
NDMASEM = 6


class Sched:
    def __init__(self, nc, same_engine_sync=True):
        self.nc = nc
        self.same = same_engine_sync
        self.streams = {e: [] for e in ("pe", "dve", "act", "pool", "sp")}
        self.cnt = {e: 0 for e in COMPUTE}
        self.sem = {}
        self.dsem = {}
        self.dnext = {q: 0 for q in QUEUES}
        self.seen = {}
        self.writer = {}
        self.readers = {}

    def alloc_sems(self, stack):
        for e in COMPUTE:
            self.sem[e] = stack.enter_context(self.nc.semaphore("s_" + e))
        for q in QUEUES:
            for i in range(NDMASEM):
                self.dsem[(q, i)] = [stack.enter_context(self.nc.semaphore(f"d_{q}{i}")), 0]

    def _deps(self, reads, writes):
        deps = []
        for b in reads:
            if b in self.writer:
                deps.append(self.writer[b])
        for b in writes:
            if b in self.writer:
                deps.append(self.writer[b])
            deps.extend(self.readers.get(b, ()))
        return deps

    def _record(self, tok, reads, writes):
        for b in reads:
            self.readers.setdefault(b, []).append(tok)
        for b in writes:
            self.writer[b] = tok
            self.readers[b] = []

    def _waits(self, stream, deps, eng_name):
        ws = []
        for (sk, val, en) in deps:
            if en == eng_name and (eng_name == "pe" or not self.same):
                continue
            key = (stream, sk)
            if self.seen.get(key, 0) >= val:
                continue
            self.seen[key] = val
            ws.append((sk, val))
        return ws

    def _semobj(self, sk):
        return self.sem[sk] if sk in self.sem else self.dsem[sk][0]

    def op(self, eng, fn, reads=(), writes=()):
        deps = self._deps(reads, writes)
        ws = self._waits(eng, deps, eng)
        self.cnt[eng] += 1
        tok = (eng, self.cnt[eng], eng)
        self.streams[eng].append((ws, fn, (eng, 1)))
        self._record(tok, reads, writes)
        return tok

    def dma(self, q, out, in_, reads=(), writes=(), **kw):
        stream = ISSUER[q]
        deps = self._deps(reads, writes)
        i = self.dnext[q]
        self.dnext[q] = (i + 1) % NDMASEM
        ent = self.dsem[(q, i)]
        if ent[1] > 0:
            deps = deps + [((q, i), ent[1], "dma")]
        ws = self._waits(stream, deps, "dma?")
        ent[1] += 16
        tok = ((q, i), ent[1], "dma")

        def fn(e, out=out, in_=in_, kw=kw):
            return e.dma_start(out=out, in_=in_, **kw)
        self.streams[stream].append((ws, fn, ((q, i), 16)))
        self._record(tok, reads, writes)
        return tok

    def _all_tokens(self):
        toks = [(e, self.cnt[e], "x") for e in COMPUTE if self.cnt[e] > 0]
        for k, ent in self.dsem.items():
            if ent[1] > 0:
                toks.append((k, ent[1], "dma"))
        return toks

    def barrier(self):
        toks = self._all_tokens()
        for s in self.streams:
            ws = self._waits(s, toks, "none")
            if ws:
                self.streams[s].append((ws, None, None))
        self.writer.clear()
        self.readers.clear()

    def emit(self):
        ws = [(sk, val) for (sk, val, _) in self._all_tokens()]
        self.streams["sp"].append((ws, None, None))
        nc = self.nc
        with nc.Block() as block:
            def mk(sname):
                def body(e):
                    for (ws, fn, inc) in self.streams[sname]:
                        for (sk, val) in ws:
                            e.wait_ge(self._semobj(sk), val)
                        if fn is not None:
                            fn(e).then_inc(self._semobj(inc[0]), inc[1])
                return body
            block.tensor(mk("pe"))
            block.vector(mk("dve"))
            block.scalar(mk("act"))
            block.gpsimd(mk("pool"))
            block.sync(mk("sp"))


class Ctx:
    def __init__(self, name="k", nc=None):
        self.nc = nc or bass.Bass("TRN2", target_bir_lowering=False)
        self.root = ExitStack()
        self.st = ExitStack()
        self.S = Sched(self.nc)
        self.S.alloc_sems(self.root)
        self.ins = {}
        self.outs = {}
        self.over = {}
        self.prefix = ""
        self.shared = set()
        self.PSALL = self.root.enter_context(self.nc.psum_tensor("psall", [128, 4096], F32))
        self.PS = [self.PSALL[:, i * 512:(i + 1) * 512] for i in range(8)]
        self._n = 0
        self._ncc = 0

    def inp(self, name, shape, dt=F32):
        if name in self.over:
            return self.over[name]
        full = name if (name in self.shared or name.startswith("c_")) else self.prefix + name
        if full in self.ins:
            return self.ins[full]
        t = self.nc.dram_tensor(full, list(shape), dt, kind="ExternalInput").ap()
        self.ins[full] = t
        return t

    def out(self, name, shape, dt=F32):
        if name in self.over:
            return self.over[name]
        t = self.nc.dram_tensor(self.prefix + name, list(shape), dt, kind="ExternalOutput").ap()
        self.outs[self.prefix + name] = t
        return t

    def scratch(self, name, shape, dt=F32):
        self._n += 1
        return self.nc.dram_tensor(f"scr{self._n}_{name}", list(shape), dt, kind="Internal").ap()

    def sb(self, name, shape, dt=F32, stack=None):
        self._n += 1
        return (stack or self.st).enter_context(self.nc.sbuf_tensor(f"sb{self._n}_{name}", list(shape), dt))

    def end_phase(self):
        self.S.barrier()
        self.st.close()
        self.st = ExitStack()

    def collective(self, kind, op, groups, pairs):
        S = self.S
        S.barrier()
        sem = self.root.enter_context(self.nc.semaphore(f"cc{self._ncc}"))
        key = ("cc", self._ncc)
        self._ncc += 1
        S.dsem[key] = [sem, len(pairs)]
        for (src, dst) in pairs:
            S.streams["pool"].append(([], lambda e, src=src, dst=dst: e.collective_compute(kind, op, replica_groups=groups, ins=[src], outs=[dst]), (key, 1)))
        S.barrier()

    def coll_group(self):
        sem = self.root.enter_context(self.nc.semaphore(f"cc{self._ncc}"))
        key = ("cc", self._ncc)
        self._ncc += 1
        self.S.dsem[key] = [sem, 0]
        return key

    def coll(self, key, kind, op, groups, src, dst, reads):
        S = self.S
        ws = S._waits("pool", S._deps(reads, []), "pool-cc")
        S.dsem[key][1] += 1
        S.streams["pool"].append((ws, lambda e: e.collective_compute(kind, op, replica_groups=groups, ins=[src], outs=[dst]), (key, 1)))

    def finish(self):
        self.S.emit()
        self.st.close()
        self.root.close()
        return self.nc


def emit_mod_rows(cx, st, scT, ada_w, ada_b, modrow, tag):
    S = cx.S
    wbuf = [cx.sb(f"adaw{tag}{i}", [128, 8, 512], F32, st) for i in range(2)]
    adab = cx.sb(f"adab{tag}", [2, 6144], F32, st)
    S.dma("sp", adab[0:1, :], ada_b, writes=["adab"])
    S.dma("sp", adab[1:2, :], ada_b, writes=["adab"])
    awv = ada_w.rearrange("(kc p) n -> p kc n", p=128)
    for cb in range(12):
        wb = wbuf[cb % 2]
        q = ("sp", "actq")[cb % 2]
        S.dma(q, wb[:, 0:4, :], awv[:, 0:4, cb * 512:(cb + 1) * 512], writes=[f"adaw{cb%2}a"])
        S.dma(q, wb[:, 4:8, :], awv[:, 4:8, cb * 512:(cb + 1) * 512], writes=[f"adaw{cb%2}b"])
        ps = cx.PS[cb % 2]
        for kc in range(8):
            S.op("pe", lambda e, ps=ps, kc=kc, wb=wb: e.matmul(ps[0:2, :], scT[:, kc, :], wb[:, kc, :],
                                                                start=(kc == 0), stop=(kc == 7)),
                 reads=["scT", f"adaw{cb%2}a", f"adaw{cb%2}b"], writes=[f"ps{cb%2}"])
        S.op("dve", lambda e, ps=ps, cb=cb: e.tensor_tensor(modrow[:, cb * 512:(cb + 1) * 512], ps[0:2, :],
                                                            adab[:, cb * 512:(cb + 1) * 512], ALU.add),
             reads=[f"ps{cb%2}", "adab"], writes=["modrow"])


def emit_bcast_row(cx, dst, row2, sel, which, ps_ids, rkey, wkey):
    S = cx.S
    for hb in range(2):
        ps = cx.PS[ps_ids[hb]]
        S.op("pe", lambda e, ps=ps, hb=hb: e.matmul(ps[:, :], sel[:, which, :], row2[:, hb * 512:(hb + 1) * 512],
                                                    start=True, stop=True),
             reads=[rkey, "sel"], writes=[f"ps{ps_ids[hb]}"])
        S.op("act", lambda e, ps=ps, hb=hb: e.copy(dst[:, hb * 512:(hb + 1) * 512], ps[:, :]),
             reads=[f"ps{ps_ids[hb]}"], writes=[wkey])


def emit_rstd(cx, xt, P, ss, rstd, junk, xkey, slot):
    S = cx.S
    S.op("pool", lambda e: e.memset(ss[0:P, :], 0.0), writes=[f"ss{slot}"])
    S.op("act", lambda e: e.activation(junk[0:P, :], xt[0:P, :], AF.Square, accum_out=ss[0:P, :]),
         reads=[xkey], writes=[f"ss{slot}", "junk"])
    S.op("dve", lambda e: e.tensor_scalar(rstd[0:P, :], ss[0:P, :], 1.0 / D, EPS, ALU.mult, ALU.add),
         reads=[f"ss{slot}"], writes=[f"rstd{slot}"])
    S.op("act", lambda e: e.sqrt(rstd[0:P, :], rstd[0:P, :]), reads=[f"rstd{slot}"], writes=[f"rstd{slot}"])
    S.op("dve", lambda e: e.reciprocal(rstd[0:P, :], rstd[0:P, :]), reads=[f"rstd{slot}"], writes=[f"rstd{slot}"])


def emit_transpose8(cx, hb, P, ident, hT, psb, hkey, tkey, pskey):
    S = cx.S
    psv = psb[:].bitcast(BF16).rearrange("p (k t) -> p k t", t=128)
    for kc in range(8):
        S.op("pe", lambda e, kc=kc: e.transpose(psv[:, kc, 0:P], hb[0:P, kc * 128:(kc + 1) * 128], ident[0:P, 0:P]),
             reads=[hkey, "ident"], writes=[pskey])
    S.op("act", lambda e: e.copy(hT[:, :, 0:P], psv[:, :, 0:P]), reads=[pskey], writes=[tkey])


def emit_A(cx, has_rope=True):
    S = cx.S
    xl = cx.inp("xl", [TPC, D])
    xc = cx.inp("xc", [CPC, D])
    cT = cx.inp("cT", [128, 8, 2])
    ada_w = cx.inp("ada_w", [D, 6 * D])
    ada_b = cx.inp("ada_b", [1, 6 * D])
    norm_g = cx.inp("norm_g", [1, D])
    w_in = cx.inp("w_in", [D, DIN])
    sel_d = cx.inp("sel", [2, 2, 128])
    ident_d = cx.inp("ident", [128, 128], BF16)
    ropec = cx.inp("ropec", [TPC, 16])
    ropes = cx.inp("ropes", [TPC, 16])
    ag_in = cx.inp("ag_in", [NAGC, DIN, 64])
    ag_out = cx.inp("ag_out", [NAGC, 4 * DIN, 64])
    cckey = cx.coll_group()
    identF_d = cx.inp("identF", [128, 128])

    sel = cx.sb("sel", [2, 2, 128])
    ident = cx.sb("ident", [128, 128], BF16)
    identF = cx.sb("identF", [128, 128])
    utT = cx.sb("utT", [128, 20, 128])
    S.dma("sp", identF[:], identF_d, writes=["identF"])
    scT = cx.sb("scT", [128, 8, 2])
    modrow = cx.sb("modrow", [2, 6 * D])
    normg2 = cx.sb("normg2", [2, D])
    grow = cx.sb("grow", [2, D])
    GL = cx.sb("GL", [128, D]); SHL = cx.sb("SHL", [128, D])
    GC = cx.sb("GC", [128, D]); SHC = cx.sb("SHC", [128, D])
    wbf = cx.sb("wbf", [128, 8, DIN], BF16)
    S.dma("sp", sel[:], sel_d, writes=["sel"])
    S.dma("sp", ident[:], ident_d, writes=["ident"])
    S.dma("sp", scT[:], cT, writes=["scT"])
    S.dma("sp", normg2[0:1, :], norm_g, writes=["normg2"])
    S.dma("sp", normg2[1:2, :], norm_g, writes=["normg2"])
    S.op("act", lambda e: e.activation(scT[:], scT[:], AF.Silu), reads=["scT"], writes=["scT"])
    with ExitStack() as st:
        emit_mod_rows(cx, st, scT, ada_w, ada_b, modrow, "A")
        wst = [cx.sb(f"wst{i}", [128, DIN], F32, st) for i in range(2)]
        wv = w_in.rearrange("(kc p) n -> p kc n", p=128)
        for kc in range(8):
            S.dma(("sp", "actq")[kc % 2], wst[kc % 2][:], wv[:, kc, :], writes=[f"wst{kc%2}"])
            eng = ("dve", "pool")[kc % 2]
            S.op(eng, lambda e, kc=kc: e.tensor_copy(wbf[:, kc, :], wst[kc % 2][:]), reads=[f"wst{kc%2}"], writes=["wbf"])
        S.barrier()
    S.op("dve", lambda e: e.scalar_tensor_tensor(grow[:], modrow[:, D:2 * D], 1.0, normg2[:], ALU.add, ALU.mult),
         reads=["modrow", "normg2"], writes=["grow"])
    emit_bcast_row(cx, GL, grow, sel, 0, (0, 1), "grow", "GL")
    emit_bcast_row(cx, SHL, modrow[:, 0:D], sel, 0, (0, 1), "modrow", "SHL")
    emit_bcast_row(cx, GC, grow, sel, 1, (0, 1), "grow", "GC")
    emit_bcast_row(cx, SHC, modrow[:, 0:D], sel, 1, (0, 1), "modrow", "SHC")

    xt = [cx.sb(f"xt{i}", [128, D]) for i in range(2)]
    junk = cx.sb("junk", [128, D], BF16)
    tmp = cx.sb("tmp", [128, D])
    hb = [cx.sb(f"hb{i}", [128, D], BF16) for i in range(2)]
    hT = [cx.sb(f"hT{i}", [128, 8, 128], BF16) for i in range(2)]
    ut = [cx.sb(f"ut{i}", [128, DIN]) for i in range(2)]
    ss = [cx.sb(f"ss{i}", [128, 1]) for i in range(2)]
    rstd = [cx.sb(f"rstd{i}", [128, 1]) for i in range(2)]
    rc = [cx.sb(f"rc{i}", [128, 16]) for i in range(2)]
    rs = [cx.sb(f"rs{i}", [128, 16]) for i in range(2)]
    rt = [cx.sb(f"rt{i}", [128, 16, 16]) for i in range(4)]

    ntl = TPC // 128
    tiles = [("l", i) for i in range(ntl)] + [("c", 0)]

    def load(ti):
        kind, i = tiles[ti]
        s = ti % 2
        if kind == "l":
            S.dma("sp", xt[s][:], xl[i * 128:(i + 1) * 128, :], writes=[f"xt{s}"])
            if has_rope:
                S.dma("sp", rc[s][:], ropec[i * 128:(i + 1) * 128, :], writes=[f"rc{s}"])
                S.dma("sp", rs[s][:], ropes[i * 128:(i + 1) * 128, :], writes=[f"rs{s}"])
        else:
            S.dma("sp", xt[s][0:CPC, :], xc, writes=[f"xt{s}"])

    load(0)
    for ti, (kind, i) in enumerate(tiles):
        s = ti % 2
        P = 128 if kind == "l" else CPC
        G, SH = (GL, SHL) if kind == "l" else (GC, SHC)
        if ti + 1 < len(tiles):
            load(ti + 1)
        emit_rstd(cx, xt[s], P, ss[s], rstd[s], junk, f"xt{s}", s)
        S.op("dve", lambda e, s=s, P=P, G=G: e.scalar_tensor_tensor(tmp[0:P, :], xt[s][0:P, :], rstd[s][0:P, :], G[0:P, :],
                                                                     ALU.mult, ALU.mult),
             reads=[f"xt{s}", f"rstd{s}", "GL", "GC"], writes=["tmp"])
        S.op("dve", lambda e, s=s, P=P, SH=SH: e.tensor_tensor(hb[s][0:P, :], tmp[0:P, :], SH[0:P, :], ALU.add),
             reads=["tmp", "SHL", "SHC"], writes=[f"hb{s}"])
        emit_transpose8(cx, hb[s], P, ident, hT[s], cx.PS[2], f"hb{s}", f"hT{s}", "ps2")
        for cb in range(5):
            ps = cx.PS[3 + cb]
            for kc in range(8):
                S.op("pe", lambda e, ps=ps, kc=kc, cb=cb, s=s, P=P: e.matmul(
                    ps[0:P, :], hT[s][:, kc, 0:P], wbf[:, kc, cb * 512:(cb + 1) * 512], start=(kc == 0), stop=(kc == 7)),
                    reads=[f"hT{s}", "wbf"], writes=[f"ps{3+cb}"])
            if cb % 2 == 0:
                S.op("act", lambda e, ps=ps, cb=cb, s=s, P=P: e.copy(ut[s][0:P, cb * 512:(cb + 1) * 512], ps[0:P, :]),
                     reads=[f"ps{3+cb}"], writes=[f"ut{s}c{cb}"])
            else:
                S.op("dve", lambda e, ps=ps, cb=cb, s=s, P=P: e.tensor_copy(ut[s][0:P, cb * 512:(cb + 1) * 512], ps[0:P, :]),
                     reads=[f"ps{3+cb}"], writes=[f"ut{s}c{cb}"])
        if kind == "l" and has_rope:
            xv = ut[s][:, 0:512].rearrange("p (g d) -> p g d", d=32)
            x1 = xv[:, :, 0:16]
            x2 = xv[:, :, 16:32]
            cb_ = rc[s][:].unsqueeze(1).to_broadcast([128, 16, 16])
            sb_ = rs[s][:].unsqueeze(1).to_broadcast([128, 16, 16])
            S.op("dve", lambda e, x1=x1, cb_=cb_: e.tensor_tensor(rt[0][:], x1, cb_, ALU.mult), reads=[f"ut{s}c0", f"rc{s}"], writes=["rt0"])
            S.op("pool", lambda e, x2=x2, sb_=sb_: e.tensor_tensor(rt[1][:], x2, sb_, ALU.mult), reads=[f"ut{s}c0", f"rs{s}"], writes=["rt1"])
            S.op("dve", lambda e, x2=x2, cb_=cb_: e.tensor_tensor(rt[2][:], x2, cb_, ALU.mult), reads=[f"ut{s}c0", f"rc{s}"], writes=["rt2"])
            S.op("pool", lambda e, x1=x1, sb_=sb_: e.tensor_tensor(rt[3][:], x1, sb_, ALU.mult), reads=[f"ut{s}c0", f"rs{s}"], writes=["rt3"])
            S.op("dve", lambda e, x1=x1: e.tensor_tensor(x1, rt[0][:], rt[1][:], ALU.subtract), reads=["rt0", "rt1"], writes=[f"ut{s}c0"])
            S.op("pool", lambda e, x2=x2: e.tensor_tensor(x2, rt[2][:], rt[3][:], ALU.add), reads=["rt2", "rt3", f"ut{s}c0"], writes=[f"ut{s}c0"])
        for q4 in range(5):
            psq = cx.PS[q4 % 2]
            for j4 in range(4):
                cc = q4 * 4 + j4
                S.op("pe", lambda e, psq=psq, j4=j4, cc=cc, s=s, P=P: e.matmul(psq[:, j4 * 128:j4 * 128 + P], ut[s][0:P, cc * 128:(cc + 1) * 128],
                                                                            identF[0:P, 0:P], start=True, stop=True),
                     reads=[f"ut{s}c{cc // 4}", "identF"], writes=[f"ps{q4 % 2}"])
            pqv = psq[:].rearrange("p (j t) -> p j t", t=128)
            S.op(("act", "dve")[q4 % 2], lambda e, pqv=pqv, q4=q4, P=P: (e.copy if hasattr(e, "copy") else e.tensor_copy)(utT[:, q4 * 4:(q4 + 1) * 4, 0:P], pqv[:, :, 0:P]),
                 reads=[f"ps{q4 % 2}"], writes=["utT"])
        for hf in range(P // 64):
            ck = (2 * i + hf) if kind == "l" else NAGC - 1
            S.dma("poolq", ag_in[ck].rearrange("(cc p) t -> p cc t", p=128), utT[:, :, hf * 64:(hf + 1) * 64], reads=["utT"], writes=[f"ag_in{ck}"])
            cx.coll(cckey, "AllGather", ALU.bypass, GROUPS, ag_in[ck], ag_out[ck], [f"ag_in{ck}"])
    cx.end_phase()


def _bf16(a):
    import ml_dtypes
    return np.asarray(a, dtype=np.float32).astype(ml_dtypes.bfloat16)


def const_sel():
    s = np.zeros((2, 2, 128), np.float32)
    s[0, 0, :] = 1.0
    s[1, 1, :] = 1.0
    return s


def const_rope():
    inv = 10000.0 ** (-np.arange(8, dtype=np.float32) / 8)
    t = np.arange(SEQ)
    row = (t // 64).astype(np.float32)
    col = (t % 64).astype(np.float32)
    ang = np.concatenate([row[:, None] * inv, col[:, None] * inv], axis=-1).astype(np.float32)
    return np.cos(ang).astype(np.float32), np.sin(ang).astype(np.float32)


def cT_layout(c_b, c_ctx):
    a = np.stack([c_b, c_ctx], axis=-1)
    return np.ascontiguousarray(a.reshape(8, 128, 2).transpose(1, 0, 2))


WIDE_EXP = False


class Attn:
    GROUPS_ = ((0, 1, 2), (5, 6, 7))
    G = 3

    def __init__(self, cx, ident):
        self.cx = cx
        self.ident = ident
        self.PT = [cx.sb(f"PT{i}", [128, self.G * 512], BF16) for i in range(2)]
        self.it = 0

    def run(self, QT, N, chunks, pso, psokey, qkeys):
        cx, S = self.cx, self.cx.S
        n = len(chunks)
        G = self.G
        ngrp = (n + G - 1) // G

        def qk(p):
            slot = (self.it + p) % 2
            for h_ in range(G):
                i = G * p + h_
                if i >= n:
                    continue
                KT, V, bias, keys = chunks[i]
                bank = self.GROUPS_[slot][h_]
                ps = cx.PS[bank]
                S.op("pe", lambda e, ps=ps, KT=KT, bias=bias: e.matmul(ps[:, 0:N], KT, QT, start=True, stop=(bias is None)),
                     reads=list(keys) + list(qkeys), writes=[f"ps{bank}"])
                if bias is not None:
                    S.op("pe", lambda e, ps=ps, bias=bias: e.matmul(ps[:, 0:N], self.ident[:], bias, start=False, stop=True),
                         reads=["bias", "ident"], writes=[f"ps{bank}"])
        qk(0)
        for p in range(ngrp):
            if p + 1 < ngrp:
                qk(p + 1)
            slot = (self.it + p) % 2
            PT = self.PT[slot]
            m = min(G, n - G * p)
            for h_ in range(m):
                bank = self.GROUPS_[slot][h_]
                S.op("act", lambda e, bank=bank, PT=PT, h_=h_: e.activation(PT[:, h_ * 512:h_ * 512 + N], cx.PS[bank][:, 0:N], AF.Exp),
                     reads=[f"ps{bank}"], writes=[f"PT{slot}_{h_}"])
            for h_ in range(m):
                i = G * p + h_
                V = chunks[i][1]
                S.op("pe", lambda e, PT=PT, V=V, i=i, h_=h_: e.matmul(pso[0:65, 0:N], V, PT[:, h_ * 512:h_ * 512 + N], start=(i == 0), stop=(i == n - 1)),
                     reads=[f"PT{slot}_{h_}", "vaug"], writes=[psokey])
        self.it += ngrp


def emit_cast_rows(cx, stg, dst, src, rows, cols, key, scale=None, chunk=2048):
    S = cx.S
    nch = (cols + chunk - 1) // chunk
    for c in range(nch):
        w = min(chunk, cols - c * chunk)
        sl = slice(c * chunk, c * chunk + w)
        S.dma(("sp", "poolq")[c % 2], stg[c % 2][0:rows, 0:w], src[:, sl], writes=[f"stg{c%2}"])
        if scale is None:
            S.op("dve", lambda e, c=c, w=w, sl=sl: e.tensor_copy(dst[0:rows, sl], stg[c % 2][0:rows, 0:w]),
                 reads=[f"stg{c%2}"], writes=[key])
        else:
            S.op("dve", lambda e, c=c, w=w, sl=sl: e.tensor_scalar(dst[0:rows, sl], stg[c % 2][0:rows, 0:w], scale, None, ALU.mult),
                 reads=[f"stg{c%2}"], writes=[key])


def emit_load_v(cx, stg, Vaug, vsrc, nk, key):
    S = cx.S
    nch = nk // 128
    vv = vsrc.rearrange("(c p) d -> p c d", p=128)
    step = 26
    for i, c0 in enumerate(range(0, nch, step)):
        c1 = min(nch, c0 + step)
        sv = stg[i % 2][:, 0:(c1 - c0) * 65].rearrange("p (c d) -> p c d", d=65)
        S.dma(("sp", "poolq")[i % 2], sv, vv[:, c0:c1, :], writes=[f"stg{i%2}"])
        S.op("dve", lambda e, sv=sv, c0=c0, c1=c1: e.tensor_copy(Vaug[:, c0:c1, :], sv), reads=[f"stg{i%2}"], writes=[key])


def emit_qbound(cx, st, QT, d, N_total, kfac, ones_b, qsrc, key):
    S = cx.S
    sq = [cx.sb(f"sq_{key}{i}", [d, 512], BF16, st) for i in range(2)]
    for c in range(N_total // 512 if N_total >= 512 else 1):
        w = min(512, N_total)
        sl = slice(c * 512, c * 512 + w)
        S.op("dve", lambda e, c=c, sl=sl, w=w: e.tensor_tensor(sq[c % 2][:, 0:w], QT[0:d, sl], QT[0:d, sl], ALU.mult),
             reads=[key], writes=[f"sq{c%2}"])
        ps = cx.PS[6 + c % 2]
        S.op("pe", lambda e, ps=ps, c=c, w=w: e.matmul(ps[0:d + 1, 0:w], ones_b[0:d, 0:d + 1], sq[c % 2][:, 0:w], start=True, stop=True),
             reads=[f"sq{c%2}", "ones_b"], writes=[f"ps{6+c%2}"])
        S.op("act", lambda e, ps=ps, sl=sl, w=w: e.sqrt(QT[d:d + 1, sl], ps[d:d + 1, 0:w]),
             reads=[f"ps{6+c%2}"], writes=[key + "r"])
        S.op("dve", lambda e, sl=sl: e.tensor_scalar(QT[d:d + 1, sl], QT[d:d + 1, sl], kfac[d:d + 1, 0:1], -1.0, ALU.mult, ALU.mult),
             reads=[key + "r", "kfac"], writes=[key + "r"])


def emit_kmax(cx, st, KT, d, nk, kfac, ones_b, key):
    S = cx.S
    sq = [cx.sb(f"ksq_{key}{i}", [d, 512], BF16, st) for i in range(2)]
    kmx = cx.sb(f"kmx_{key}", [d + 1, 64], F32, st)
    S.op("pool", lambda e: e.memset(kmx[:], 0.0), writes=["kmx"])
    nch = (nk + 511) // 512
    for c in range(nch):
        w = min(512, nk - c * 512)
        sl = slice(c * 512, c * 512 + w)
        S.op("dve", lambda e, c=c, sl=sl, w=w: e.tensor_tensor(sq[c % 2][:, 0:w], KT[0:d, sl], KT[0:d, sl], ALU.mult),
             reads=[key], writes=[f"sq{c%2}"])
        ps = cx.PS[6 + c % 2]
        S.op("pe", lambda e, ps=ps, c=c, w=w: e.matmul(ps[0:d + 1, 0:w], ones_b[0:d, 0:d + 1], sq[c % 2][:, 0:w], start=True, stop=True),
             reads=[f"sq{c%2}", "ones_b"], writes=[f"ps{6+c%2}"])
        S.op("dve", lambda e, ps=ps, c=c, w=w: e.tensor_reduce(kmx[d:d + 1, c:c + 1], ps[d:d + 1, 0:w], AX.X, ALU.max),
             reads=[f"ps{6+c%2}"], writes=["kmx"])
    S.op("dve", lambda e: e.tensor_reduce(kfac[d:d + 1, 0:1], kmx[d:d + 1, 0:nch], AX.X, ALU.max), reads=["kmx"], writes=["kfac"])
    S.op("act", lambda e: e.sqrt(kfac[d:d + 1, 0:1], kfac[d:d + 1, 0:1]), reads=["kfac"], writes=["kfac"])


def emit_finalize(cx, pso, N, recrow, osb, E65, tdst, psokey, tkey):
    S = cx.S
    S.op("dve", lambda e: e.reciprocal(recrow[64:65, 0:N], pso[64:65, 0:N]), reads=[psokey], writes=["recrow"])
    S.op("pe", lambda e: e.matmul(cx.PS[5][0:64, 0:N], E65[0:65, 0:64], recrow[0:65, 0:N], start=True, stop=True),
         reads=["recrow", "E65"], writes=["ps5"])
    S.op("act", lambda e: e.copy(osb[0:64, 0:N], pso[0:64, 0:N]), reads=[psokey], writes=["osb"])
    S.op("dve", lambda e: e.tensor_tensor(tdst[0:64, 0:N], osb[0:64, 0:N], cx.PS[5][0:64, 0:N], ALU.mult),
         reads=["osb", "ps5"], writes=[tkey])


def emit_B1(cx, li):
    lam_init = 0.8 - 0.6 * math.exp(-0.3 * li)
    NK = SEQ + CTX
    S = cx.S
    aqT = cx.inp("aqT", [2, 32, SEQ]); akT = cx.inp("akT", [2, 33, NK]); av = cx.inp("av", [NK, 65])
    aqcT = cx.inp("aqcT", [2, 32, CTX])
    alam = cx.inp("alam", [1, 128]); subg = cx.inp("subg", [64, 1])
    bqT = cx.inp("bqT", [64, SEQ]); bkT = cx.inp("bkT", [65, NK]); bv = cx.inp("bv", [NK, 65])
    bqcT = cx.inp("bqcT", [64, CTX])
    bias_d = cx.inp("bias", [3, 8, 128, 512])
    ident_d = cx.inp("ident", [128, 128], BF16); onesb_d = cx.inp("onesb", [128, 128], BF16)
    E65_d = cx.inp("E65", [65, 64]); ones64_d = cx.inp("ones64", [64, 64])
    oa = cx.out("oa", [64, SEQ]); oac = cx.out("oac", [64, CTX])
    ob = cx.out("ob", [64, SEQ]); obc = cx.out("obc", [64, CTX])

    ident = cx.sb("ident", [128, 128], BF16); ones_b = cx.sb("onesb", [128, 128], BF16)
    E65 = cx.sb("E65", [65, 64]); ones64 = cx.sb("ones64", [64, 64])
    S.dma("sp", ident[:], ident_d, writes=["ident"]); S.dma("sp", ones_b[:], onesb_d, writes=["ones_b"])
    S.dma("sp", E65[:], E65_d, writes=["E65"]); S.dma("sp", ones64[:], ones64_d, writes=["ones64"])
    at = Attn(cx, ident)
    recrow = cx.sb("recrow", [65, 512]); osb = cx.sb("osb", [64, 512])
    t0 = cx.sb("t0", [64, 512]); t1 = cx.sb("t1", [64, 512]); t2 = cx.sb("t2", [64, 512])
    kfac = cx.sb("kfac", [65, 1])
    stg = [cx.sb(f"stg{i}", [128, 2048]) for i in range(2)]
    S.op("pool", lambda e: e.memset(recrow[:], 0.0), writes=["recrow"])
    lrow = cx.sb("lrow", [1, 128]); lsum = cx.sb("lsum", [1, 4]); neglam = cx.sb("neglam", [64, 1]); gsc = cx.sb("gsc", [64, 1])
    S.dma("sp", lrow[:], alam, writes=["lrow"]); S.dma("sp", gsc[:], subg, writes=["gsc"])
    S.op("pool", lambda e: e.memset(lsum[:], 0.0), writes=["lsum"])
    S.op("dve", lambda e: e.tensor_tensor(lrow[:, 0:32], lrow[:, 0:32], lrow[:, 32:64], ALU.mult), reads=["lrow"], writes=["lrow"])
    S.op("dve", lambda e: e.tensor_tensor(lrow[:, 64:96], lrow[:, 64:96], lrow[:, 96:128], ALU.mult), reads=["lrow"], writes=["lrow"])
    S.op("dve", lambda e: e.tensor_reduce(lsum[:, 0:1], lrow[:, 0:32], AX.X, ALU.add), reads=["lrow", "lsum"], writes=["lsum"])
    S.op("dve", lambda e: e.tensor_reduce(lsum[:, 1:2], lrow[:, 64:96], AX.X, ALU.add), reads=["lrow", "lsum"], writes=["lsum"])
    S.op("act", lambda e: e.activation(lsum[:, 0:2], lsum[:, 0:2], AF.Exp), reads=["lsum"], writes=["lsum"])
    S.op("dve", lambda e: e.tensor_tensor(lsum[:, 2:3], lsum[:, 1:2], lsum[:, 0:1], ALU.subtract), reads=["lsum"], writes=["lsum"])
    S.op("dve", lambda e: e.tensor_scalar(lsum[:, 2:3], lsum[:, 2:3], -lam_init, None, ALU.add), reads=["lsum"], writes=["lsum"])
    S.op("pe", lambda e: e.matmul(cx.PS[7][0:64, 0:1], ones64[0:1, 0:64], lsum[0:1, 2:3], start=True, stop=True),
         reads=["lsum", "ones64"], writes=["ps7"])
    S.op("act", lambda e: e.copy(neglam[:], cx.PS[7][0:64, 0:1]), reads=["ps7"], writes=["neglam"])
    S.op("dve", lambda e: e.tensor_scalar(gsc[:], gsc[:], 1.0 - lam_init, None, ALU.mult), reads=["gsc"], writes=["gsc"])

    with ExitStack() as st:
        QT = [cx.sb(f"aQT{c}", [33, SEQ], BF16, st) for c in range(2)]
        QTc = [cx.sb(f"aQTc{c}", [33, CTX], BF16, st) for c in range(2)]
        KT = [cx.sb(f"aKT{c}", [33, NK], BF16, st) for c in range(2)]
        Va = cx.sb("aV", [128, NK // 128, 65], BF16, st)
        sc = 32 ** -0.5
        for c in range(2):
            emit_cast_rows(cx, stg, KT[c], akT[c], 33, NK, f"akt{c}")
            emit_cast_rows(cx, stg, QT[c], aqT[c], 32, SEQ, f"aqt{c}", scale=sc)
            emit_cast_rows(cx, stg, QTc[c], aqcT[c], 32, CTX, f"aqtc{c}", scale=sc)
        emit_load_v(cx, stg, Va, av, NK, "vaug")
        for c in range(2):
            emit_kmax(cx, st, KT[c], 32, NK, kfac, ones_b, f"akt{c}")
            emit_qbound(cx, st, QT[c], 32, SEQ, kfac, ones_b, None, f"aqt{c}")
            emit_qbound(cx, st, QTc[c], 32, CTX, kfac, ones_b, None, f"aqtc{c}")

        def diff_block(qts, N, chunk_ids, odst, qkeys):
            for c in range(2):
                chunks = [(KT[c][:, k * 128:(k + 1) * 128], Va[:, k, :], None, (f"akt{c}",)) for k in chunk_ids]
                at.run(qts[c], N, chunks, cx.PS[3 + c], f"ps{3+c}", [qkeys[c], qkeys[c] + "r"])
            emit_finalize(cx, cx.PS[3], N, recrow, osb, E65, t0, "ps3", "t0")
            emit_finalize(cx, cx.PS[4], N, recrow, osb, E65, t1, "ps4", "t1")
            S.op("dve", lambda e: e.scalar_tensor_tensor(t0[:, 0:N], t1[:, 0:N], neglam[:, 0:1], t0[:, 0:N], ALU.mult, ALU.add),
                 reads=["t0", "t1", "neglam"], writes=["t0"])
            S.op("act", lambda e: e.activation(t1[:, 0:N], t0[:, 0:N], AF.Square), reads=["t0"], writes=["t1"])
            S.op("pe", lambda e: e.matmul(cx.PS[2][0:64, 0:N], ones64[:, :], t1[:, 0:N], start=True, stop=True),
                 reads=["t1", "ones64"], writes=["ps2"])
            S.op("dve", lambda e: e.tensor_scalar(t1[:, 0:N], cx.PS[2][0:64, 0:N], 1.0 / 64, EPS, ALU.mult, ALU.add),
                 reads=["ps2"], writes=["t1"])
            S.op("act", lambda e: e.sqrt(t1[:, 0:N], t1[:, 0:N]), reads=["t1"], writes=["t1"])
            S.op("dve", lambda e: e.reciprocal(t1[:, 0:N], t1[:, 0:N]), reads=["t1"], writes=["t1"])
            S.op("dve", lambda e: e.scalar_tensor_tensor(t2[:, 0:N], t0[:, 0:N], gsc[:, 0:1], t1[:, 0:N], ALU.mult, ALU.mult),
                 reads=["t0", "t1", "gsc"], writes=["t2"])
            S.dma("poolq", odst, t2[:, 0:N], reads=["t2"], writes=["oa"])

        for qb in range(SEQ // 512):
            sl = slice(qb * 512, (qb + 1) * 512)
            diff_block([QT[0][:, sl], QT[1][:, sl]], 512, range(NK // 128), oa[:, sl], ["aqt0", "aqt1"])
        diff_block([QTc[0][:, :], QTc[1][:, :]], CTX, range(SEQ // 128, NK // 128), oac[:, :], ["aqtc0", "aqtc1"])
        S.barrier()

    with ExitStack() as st:
        QT = cx.sb("bQT", [65, SEQ], BF16, st); QTc = cx.sb("bQTc", [65, CTX], BF16, st)
        KT = cx.sb("bKT", [65, NK], BF16, st); Vb = cx.sb("bV", [128, NK // 128, 65], BF16, st)
        bias = cx.sb("bias", [128, 3, 8, 512], BF16, st)
        sc = 64 ** -0.5
        emit_cast_rows(cx, stg, KT, bkT, 65, NK, "bkt")
        emit_cast_rows(cx, stg, QT, bqT, 64, SEQ, "bqt", scale=sc)
        emit_cast_rows(cx, stg, QTc, bqcT, 64, CTX, "bqtc", scale=sc)
        emit_load_v(cx, stg, Vb, bv, NK, "vaug")
        for s_ in range(3):
            for j in range(8):
                i = s_ * 8 + j
                S.dma(("sp", "poolq")[i % 2], stg[i % 2][:, 0:512], bias_d[s_, j], writes=[f"stg{i%2}"])
                S.op("dve", lambda e, i=i, s_=s_, j=j: e.tensor_copy(bias[:, s_, j, :], stg[i % 2][:, 0:512]),
                     reads=[f"stg{i%2}"], writes=["bias"])
        emit_kmax(cx, st, KT, 64, NK, kfac, ones_b, "bkt")
        emit_qbound(cx, st, QT, 64, SEQ, kfac, ones_b, None, "bqt")
        emit_qbound(cx, st, QTc, 64, CTX, kfac, ones_b, None, "bqtc")
        for qb in range(32):
            R0 = qb * 8
            if qb == 0:
                bset, kr0 = 0, 0
            elif qb == 31:
                bset, kr0 = 2, 240
            else:
                bset, kr0 = 1, R0 - 4
            sl = slice(qb * 512, (qb + 1) * 512)
            chunks = [(KT[:, (kr0 // 2 + j) * 128:(kr0 // 2 + j + 1) * 128], Vb[:, kr0 // 2 + j, :], bias[:, bset, j, :], ("bkt",))
                      for j in range(8)]
            chunks += [(KT[:, k * 128:(k + 1) * 128], Vb[:, k, :], None, ("bkt",)) for k in range(SEQ // 128, NK // 128)]
            at.run(QT[:, sl], 512, chunks, cx.PS[3], "ps3", ["bqt", "bqtr"])
            emit_finalize(cx, cx.PS[3], 512, recrow, osb, E65, t0, "ps3", "t0")
            S.dma("poolq", ob[:, sl], t0[:, 0:512], reads=["t0"], writes=["ob"])
        chunks = [(KT[:, k * 128:(k + 1) * 128], Vb[:, k, :], None, ("bkt",)) for k in range(SEQ // 128, NK // 128)]
        at.run(QTc[:, :], CTX, chunks, cx.PS[3], "ps3", ["bqtc", "bqtcr"])
        emit_finalize(cx, cx.PS[3], CTX, recrow, osb, E65, t0, "ps3", "t0")
        S.dma("poolq", obc[:, :], t0[:, 0:CTX], reads=["t0"], writes=["obc"])
        S.barrier()
    cx.end_phase()


def na_bias_sets(rpb_h):
    out = np.full((3, 8, 128, 512), -30000.0, np.float32)
    cq = np.arange(64); ck = np.arange(64)
    c0 = np.clip(cq - 8, 0, 48)
    colok = (ck[:, None] >= c0[None, :]) & (ck[:, None] < c0[None, :] + 16)
    dc = np.clip(ck[:, None] - cq[None, :], -15, 15) + 15
    for s_, (R0, kr0) in enumerate([(0, 0), (8, 4), (248, 240)]):
        for j in range(8):
            for krl in range(2):
                kr = kr0 + 2 * j + krl
                for qrl in range(8):
                    r = R0 + qrl
                    r0 = min(max(r - 4, 0), 248)
                    if not (r0 <= kr < r0 + 8):
                        continue
                    dr = kr - r + 7
                    blk = np.where(colok, rpb_h[dr][dc], np.float32(-30000.0))
                    out[s_, j, krl * 64:(krl + 1) * 64, qrl * 64:(qrl + 1) * 64] = blk
    return out


def emit_C(cx, with_ctx, final):
    S = cx.S
    xl = cx.inp("xl", [TPC, D]); xc = cx.inp("xc", [CPC, D])
    rs_out = cx.inp("rs_out", [TPC + CPC, D])
    cT = cx.inp("cT", [128, 8, 2])
    ada_w = cx.inp("ada_w", [D, 6 * D]); ada_b = cx.inp("ada_b", [1, 6 * D])
    norm_g = cx.inp("norm2_g", [1, D]); fin_g = cx.inp("fin_g", [1, D])
    router_w = cx.inp("router_w", [D, 16]); router_b = cx.inp("router_b", [1, 16])
    w1 = cx.inp("w1", [16, D, 512]); w3 = cx.inp("w3", [16, D, 512]); w2 = cx.inp("w2", [16, 512, D])
    sel_d = cx.inp("sel", [2, 2, 128]); identf_d = cx.inp("ident", [128, 128], BF16)
    ol = cx.out("ol", [TPC, D]); oc = cx.out("oc", [CPC, D])

    sel = cx.sb("sel", [2, 2, 128]); identf = cx.sb("identb", [128, 128], BF16)
    rwh = cx.sb("rwh", [128, 8, 16], BF16); rwl = cx.sb("rwl", [128, 8, 16], BF16)
    G1 = cx.sb("G1", [128, D]); G2 = cx.sb("G2", [128, D]); SH2 = cx.sb("SH2", [128, D]); GG2 = cx.sb("GG2", [128, D])
    FG = cx.sb("FG", [128, D])
    rw = cx.sb("rw", [128, 8, 16]); rb = cx.sb("rb", [128, 16])
    bcs = cx.scratch("bc_scr", [4, 128, D])
    S.dma("sp", sel[:], sel_d, writes=["sel"]); S.dma("sp", identf[:], identf_d, writes=["identf"])
    S.dma("sp", rw[:], router_w.rearrange("(kc p) n -> p kc n", p=128), writes=["rw"])
    S.dma("sp", rb[:], router_b.partition_broadcast(128), writes=["rb"])
    S.op("dve", lambda e: e.tensor_copy(rwh[:], rw[:]), reads=["rw"], writes=["rwh"])
    S.op("dve", lambda e: e.tensor_tensor(rwl[:], rw[:], rwh[:], ALU.subtract), reads=["rw", "rwh"], writes=["rwl"])
    with ExitStack() as st:
        scT = cx.sb("scT", [128, 8, 2], F32, st); modrow = cx.sb("modrow", [2, 6 * D], F32, st)
        normg2 = cx.sb("normg2", [2, D], F32, st); grow = cx.sb("grow", [2, D], F32, st); fing2 = cx.sb("fing2", [2, D], F32, st)
        S.dma("sp", scT[:], cT, writes=["scT"])
        for r in range(2):
            S.dma("sp", normg2[r:r + 1, :], norm_g, writes=["normg2"])
            S.dma("sp", fing2[r:r + 1, :], fin_g, writes=["fing2"])
        S.op("act", lambda e: e.activation(scT[:], scT[:], AF.Silu), reads=["scT"], writes=["scT"])
        with ExitStack() as st2:
            emit_mod_rows(cx, st2, scT, ada_w, ada_b, modrow, "C")
            S.barrier()
        S.op("dve", lambda e: e.scalar_tensor_tensor(grow[:], modrow[:, 4 * D:5 * D], 1.0, normg2[:], ALU.add, ALU.mult),
             reads=["modrow", "normg2"], writes=["grow"])

        def set_bcast(which):
            emit_bcast_row(cx, G1, modrow[:, 2 * D:3 * D], sel, which, (0, 1), "modrow", "G1")
            emit_bcast_row(cx, G2, grow, sel, which, (0, 1), "grow", "G2")
            emit_bcast_row(cx, SH2, modrow[:, 3 * D:4 * D], sel, which, (0, 1), "modrow", "SH2")
            emit_bcast_row(cx, GG2, modrow[:, 5 * D:6 * D], sel, which, (0, 1), "modrow", "GG2")
        set_bcast(1)
        for i_, (t_, k_) in enumerate(((G1, "G1"), (G2, "G2"), (SH2, "SH2"), (GG2, "GG2"))):
            S.dma("sp", bcs[i_], t_[:], reads=[k_], writes=["bcs"])
        set_bcast(0)
        emit_bcast_row(cx, FG, fing2, sel, 0, (0, 1), "fing2", "FG")
        S.barrier()

    def load_ctx_bcast():
        for i_, (t_, k_) in enumerate(((G1, "G1"), (G2, "G2"), (SH2, "SH2"), (GG2, "GG2"))):
            S.dma("sp", t_[:], bcs[i_], reads=["bcs"], writes=[k_])

    GT = 8
    x1 = cx.sb("x1", [128, GT, D]); yacc = cx.sb("yacc", [128, GT, D])
    hT = cx.sb("hT", [128, 8, GT * 128], BF16)
    gate = cx.sb("gate", [128, GT, 16])
    xt = [cx.sb(f"xt{i}", [128, D]) for i in range(2)]
    mpt = [cx.sb("mpt0", [128, D])] * 2
    junk = cx.sb("junk", [128, D], BF16); tmp = cx.sb("tmp", [128, D]); h2 = cx.sb("h2", [128, D])
    hTl = cx.sb("hTl", [128, 8, 128], BF16); h2hi = cx.sb("h2hi", [128, D], BF16); h2lo = cx.sb("h2lo", [128, D], BF16)
    ss = cx.sb("ss", [128, 1]); rstd = cx.sb("rstd", [128, 1])
    r_ = {k: cx.sb("r_" + k, [128, 16]) for k in ("ex", "sc", "sel", "eq", "s2", "selm", "k1", "sm2", "k2", "w")}
    q_ = {k: cx.sb("q_" + k, [128, 4]) for k in ("m1", "m2", "gs", "gmask", "pen")}
    c_ = {k: cx.sb("c_" + k, [128, 1]) for k in ("mx", "sm", "gm", "e1", "e2", "ws")}
    W13 = [cx.sb(f"W13_{i}", [128, 2, 8, 512], BF16) for i in range(2)]
    W2 = [cx.sb(f"W2_{i}", [128, 4, D], BF16) for i in range(2)]
    hs = cx.sb("hs", [128, 512]); hh = [cx.sb(f"hh{i}", [128, 4, 512], BF16) for i in range(2)]
    stage_n = [0]
    conv_st = ExitStack()
    wstg = [cx.sb(f"wstg{i}", [128, 4, 512], F32, conv_st) for i in range(2)]

    def router(P, g):
        lg = cx.PS[2]
        n_ = 0
        for (lhs, rhs) in (("hi", rwh), ("lo", rwh), ("hi", rwl)):
            for kc in range(8):
                lt = hT[:, kc, g * 128:g * 128 + P] if lhs == "hi" else hTl[:, kc, 0:P]
                S.op("pe", lambda e, lt=lt, rhs=rhs, kc=kc, n_=n_: e.matmul(lg[0:P, 0:16], lt, rhs[:, kc, :], start=(n_ == 0), stop=(n_ == 23)),
                     reads=["hT", "hTl", "rwh", "rwl"], writes=["ps2"])
                n_ += 1
        V = lambda k: r_[k][0:P, :]
        V4 = lambda k: r_[k][0:P, :].rearrange("p (g e) -> p g e", e=4)
        Q = lambda k: q_[k][0:P, :]
        Cc = lambda k: c_[k][0:P, :]
        dv = lambda fn, rd, wr: S.op("dve", fn, reads=rd, writes=wr)
        dv(lambda e: e.tensor_reduce(Cc("mx"), lg[0:P, 0:16], AX.X, ALU.max), ["ps2"], ["c_mx"])
        dv(lambda e: e.tensor_scalar(Cc("mx"), Cc("mx"), -1.0, None, ALU.mult), ["c_mx"], ["c_mx"])
        S.op("pool", lambda e: e.memset(Cc("sm"), 0.0), writes=["c_sm"])
        S.op("act", lambda e: e.activation(V("ex"), lg[0:P, 0:16], AF.Exp, bias=Cc("mx"), accum_out=Cc("sm")),
             reads=["ps2", "c_mx", "c_sm"], writes=["r_ex", "c_sm"])
        dv(lambda e: e.reciprocal(Cc("sm"), Cc("sm")), ["c_sm"], ["c_sm"])
        dv(lambda e: e.tensor_scalar(V("sc"), V("ex"), Cc("sm"), None, ALU.mult), ["r_ex", "c_sm"], ["r_sc"])
        dv(lambda e: e.tensor_tensor(V("sel"), V("sc"), rb[0:P, :], ALU.add), ["r_sc", "rb"], ["r_sel"])
        dv(lambda e: e.tensor_reduce(Q("m1"), V4("sel"), AX.X, ALU.max), ["r_sel"], ["q_m1"])
        dv(lambda e: e.tensor_tensor(V4("eq"), V4("sel"), Q("m1").unsqueeze(2).to_broadcast([P, 4, 4]), ALU.is_equal), ["r_sel", "q_m1"], ["r_eq"])
        dv(lambda e: e.scalar_tensor_tensor(V("s2"), V("eq"), -1e9, V("sel"), ALU.mult, ALU.add), ["r_eq", "r_sel"], ["r_s2"])
        dv(lambda e: e.tensor_reduce(Q("m2"), V4("s2"), AX.X, ALU.max), ["r_s2"], ["q_m2"])
        dv(lambda e: e.tensor_tensor(Q("gs"), Q("m1"), Q("m2"), ALU.add), ["q_m1", "q_m2"], ["q_gs"])
        dv(lambda e: e.tensor_reduce(Cc("gm"), Q("gs"), AX.X, ALU.max), ["q_gs"], ["c_gm"])
        dv(lambda e: e.tensor_scalar(Q("gmask"), Q("gs"), Cc("gm"), None, ALU.is_equal), ["q_gs", "c_gm"], ["q_gmask"])
        dv(lambda e: e.tensor_scalar(Q("pen"), Q("gmask"), -1.0, 1e9, ALU.add, ALU.mult), ["q_gmask"], ["q_pen"])
        dv(lambda e: e.tensor_tensor(V4("selm"), V4("sel"), Q("pen").unsqueeze(2).to_broadcast([P, 4, 4]), ALU.add), ["r_sel", "q_pen"], ["r_selm"])
        dv(lambda e: e.tensor_reduce(Cc("e1"), V("selm"), AX.X, ALU.max), ["r_selm"], ["c_e1"])
        dv(lambda e: e.tensor_scalar(V("k1"), V("selm"), Cc("e1"), None, ALU.is_equal), ["r_selm", "c_e1"], ["r_k1"])
        dv(lambda e: e.scalar_tensor_tensor(V("sm2"), V("k1"), -1e9, V("selm"), ALU.mult, ALU.add), ["r_k1", "r_selm"], ["r_sm2"])
        dv(lambda e: e.tensor_reduce(Cc("e2"), V("sm2"), AX.X, ALU.max), ["r_sm2"], ["c_e2"])
        dv(lambda e: e.tensor_scalar(V("k2"), V("sm2"), Cc("e2"), None, ALU.is_equal), ["r_sm2", "c_e2"], ["r_k2"])
        dv(lambda e: e.tensor_tensor(V("k1"), V("k1"), V("k2"), ALU.add), ["r_k1", "r_k2"], ["r_k1"])
        dv(lambda e: e.tensor_tensor(V("w"), V("sc"), V("k1"), ALU.mult), ["r_sc", "r_k1"], ["r_w"])
        dv(lambda e: e.tensor_reduce(Cc("ws"), V("w"), AX.X, ALU.add), ["r_w"], ["c_ws"])
        dv(lambda e: e.reciprocal(Cc("ws"), Cc("ws")), ["c_ws"], ["c_ws"])
        dv(lambda e: e.tensor_scalar(gate[0:P, g, :], V("w"), Cc("ws"), None, ALU.mult), ["r_w", "c_ws"], ["gate"])

    wb13 = cx.scratch("wb13", [16, 128, 2 * 8 * 512], BF16)
    wb2 = cx.scratch("wb2", [16, 128, 4 * D], BF16)

    def convert_expert(e_, slot):
        pieces = []
        for wi, wsrc in enumerate((w1, w3)):
            v = wsrc[e_].rearrange("(kc p) n -> p kc n", p=128)
            for hf in range(2):
                pieces.append((v[:, hf * 4:(hf + 1) * 4, :], W13[slot][:, wi, hf * 4:(hf + 1) * 4, :]))
        v2 = w2[e_].rearrange("(fc p) n -> p fc n", p=128)
        for hf in range(2):
            pieces.append((v2[:, :, hf * 512:(hf + 1) * 512], W2[slot][:, :, hf * 512:(hf + 1) * 512]))
        for src, dst in pieces:
            n = stage_n[0]; stage_n[0] += 1
            sg = wstg[n % 2]
            S.dma(("sp", "actq")[n % 2], sg[:], src, writes=[f"wstg{n%2}"])
            S.op(("pool", "dve")[n % 2], lambda e, sg=sg, dst=dst: e.tensor_copy(dst, sg[:]), reads=[f"wstg{n%2}"], writes=[f"W{slot}"])
        S.dma("poolq", wb13[e_], W13[slot][:].rearrange("p a b c -> p (a b c)"), reads=[f"W{slot}"], writes=["wb"])
        S.dma("poolq", wb2[e_], W2[slot][:].rearrange("p a b -> p (a b)"), reads=[f"W{slot}"], writes=["wb"])

    for e_ in range(16):
        convert_expert(e_, e_ % 2)
    S.barrier()
    conv_st.close()

    def load_expert(e_, slot):
        S.dma("sp", W13[slot][:].rearrange("p a b c -> p (a b c)"), wb13[e_], reads=["wb"], writes=[f"W{slot}"])
        S.dma("actq", W2[slot][:].rearrange("p a b -> p (a b)"), wb2[e_], reads=["wb"], writes=[f"W{slot}"])

    ntl = TPC // 128
    groups = [[("l", g * GT + t) for t in range(GT)] for g in range(ntl // GT)]
    if with_ctx:
        groups.append([("c", 0)])
    ti_glob = 0
    eload = 0
    for gi, grp in enumerate(groups):
        isctx = grp[0][0] == "c"
        if isctx:
            load_ctx_bcast()
        P = CPC if isctx else 128
        NT = len(grp) * 128 if not isctx else CPC
        for g, (kind, i) in enumerate(grp):
            s = ti_glob % 2; ti_glob += 1
            xsrc = xc if isctx else xl[i * 128:(i + 1) * 128, :]
            msrc = rs_out[TPC:TPC + CPC, :] if isctx else rs_out[i * 128:(i + 1) * 128, :]
            S.dma("sp", xt[s][0:P, :], xsrc, writes=[f"xt{s}"])
            S.dma("actq", mpt[s][0:P, :], msrc, writes=["mpt"])
            S.op("dve", lambda e, s=s, P=P: e.tensor_tensor(tmp[0:P, :], mpt[s][0:P, :], G1[0:P, :], ALU.mult),
                 reads=["mpt", "G1"], writes=["tmp"])
            S.op("dve", lambda e, s=s, g=g, P=P: e.tensor_tensor(x1[0:P, g, :], tmp[0:P, :], xt[s][0:P, :], ALU.add),
                 reads=["tmp", f"xt{s}"], writes=["x1"])
            S.op("pool", lambda e, P=P: e.memset(ss[0:P, :], 0.0), writes=["ss"])
            S.op("act", lambda e, g=g, P=P: e.activation(junk[0:P, :], x1[0:P, g, :], AF.Square, accum_out=ss[0:P, :]),
                 reads=["x1", "ss"], writes=["ss", "junk"])
            S.op("dve", lambda e, P=P: e.tensor_scalar(rstd[0:P, :], ss[0:P, :], 1.0 / D, EPS, ALU.mult, ALU.add), reads=["ss"], writes=["rstd"])
            S.op("act", lambda e, P=P: e.sqrt(rstd[0:P, :], rstd[0:P, :]), reads=["rstd"], writes=["rstd"])
            S.op("dve", lambda e, P=P: e.reciprocal(rstd[0:P, :], rstd[0:P, :]), reads=["rstd"], writes=["rstd"])
            S.op("dve", lambda e, g=g, P=P: e.scalar_tensor_tensor(tmp[0:P, :], x1[0:P, g, :], rstd[0:P, :], G2[0:P, :], ALU.mult, ALU.mult),
                 reads=["x1", "rstd", "G2"], writes=["tmp"])
            S.op("dve", lambda e, P=P: e.tensor_tensor(h2[0:P, :], tmp[0:P, :], SH2[0:P, :], ALU.add), reads=["tmp", "SH2"], writes=["h2"])
            S.op("dve", lambda e, P=P: e.tensor_copy(h2hi[0:P, :], h2[0:P, :]), reads=["h2"], writes=["h2hi"])
            S.op("dve", lambda e, P=P: e.tensor_tensor(h2lo[0:P, :], h2[0:P, :], h2hi[0:P, :], ALU.subtract), reads=["h2", "h2hi"], writes=["h2lo"])
            for (src, skey, dstv, dkey, bank) in ((h2hi, "h2hi", hT[:, :, g * 128:g * 128 + P], "hT", 3), (h2lo, "h2lo", hTl[:, :, 0:P], "hTl", 4)):
                psv = cx.PS[bank][:].bitcast(BF16).rearrange("p (k t) -> p k t", t=128)
                for kc in range(8):
                    S.op("pe", lambda e, psv=psv, kc=kc, src=src, P=P: e.transpose(psv[:, kc, 0:P], src[0:P, kc * 128:(kc + 1) * 128], identf[0:P, 0:P]),
                         reads=[skey, "identf"], writes=[f"ps{bank}"])
                S.op("act", lambda e, psv=psv, dstv=dstv, P=P: e.copy(dstv, psv[:, :, 0:P]), reads=[f"ps{bank}"], writes=[dkey])
            router(P, g)
            S.op("pool", lambda e, g=g, P=P: e.memset(yacc[0:P, g, :], 0.0), writes=["yacc"])
        for e_ in range(16):
            slot = eload % 2; eload += 1
            load_expert(e_, slot)
            for c0 in range(0, NT, 512):
                w = min(512, NT - c0)
                hsl = hh[(c0 // 512) % 2]
                for fc in range(4):
                    p1 = cx.PS[4 + (fc % 2) * 2]; p3 = cx.PS[5 + (fc % 2) * 2]
                    k1 = f"ps{4 + (fc % 2) * 2}"; k3 = f"ps{5 + (fc % 2) * 2}"
                    for wi, (pp, pk) in enumerate(((p1, k1), (p3, k3))):
                        for kc in range(8):
                            S.op("pe", lambda e, pp=pp, wi=wi, kc=kc, fc=fc, slot=slot, c0=c0, w=w: e.matmul(
                                pp[:, 0:w], W13[slot][:, wi, kc, fc * 128:(fc + 1) * 128], hT[:, kc, c0:c0 + w], start=(kc == 0), stop=(kc == 7)),
                                reads=[f"W{slot}", "hT"], writes=[pk])
                    S.op("act", lambda e, p1=p1, w=w: e.activation(hs[:, 0:w], p1[:, 0:w], AF.Silu), reads=[k1], writes=["hs"])
                    S.op("dve", lambda e, p3=p3, w=w, fc=fc, hsl=hsl: e.tensor_tensor(hsl[:, fc, 0:w], hs[:, 0:w], p3[:, 0:w], ALU.mult),
                         reads=["hs", k3], writes=[f"hh{(c0//512)%2}"])
                for t0_ in range(0, w, 128):
                    pw = min(128, w - t0_)
                    g = (c0 + t0_) // 128
                    for hb_ in range(2):
                        ps = cx.PS[hb_]
                        for fc in range(4):
                            S.op("pe", lambda e, ps=ps, fc=fc, hb_=hb_, slot=slot, t0_=t0_, pw=pw, hsl=hsl: e.matmul(
                                ps[0:pw, :], hsl[:, fc, t0_:t0_ + pw], W2[slot][:, fc, hb_ * 512:(hb_ + 1) * 512], start=(fc == 0), stop=(fc == 3)),
                                reads=[f"hh{(c0//512)%2}", f"W{slot}"], writes=[f"ps{hb_}"])
                        cs = slice(hb_ * 512, (hb_ + 1) * 512)
                        S.op("dve", lambda e, ps=ps, cs=cs, g=g, pw=pw, e_=e_: e.scalar_tensor_tensor(
                            yacc[0:pw, g, cs], ps[0:pw, :], gate[0:pw, g, e_:e_ + 1], yacc[0:pw, g, cs], ALU.mult, ALU.add),
                            reads=[f"ps{hb_}", "gate", "yacc"], writes=["yacc"])
        for g, (kind, i) in enumerate(grp):
            S.op("dve", lambda e, g=g, P=P: e.tensor_tensor(tmp[0:P, :], yacc[0:P, g, :], GG2[0:P, :], ALU.mult), reads=["yacc", "GG2"], writes=["tmp"])
            S.op("dve", lambda e, g=g, P=P: e.tensor_tensor(x1[0:P, g, :], x1[0:P, g, :], tmp[0:P, :], ALU.add), reads=["tmp", "x1"], writes=["x1"])
            dst = oc if isctx else ol[i * 128:(i + 1) * 128, :]
            if final:
                S.op("pool", lambda e, P=P: e.memset(ss[0:P, :], 0.0), writes=["ss"])
                S.op("act", lambda e, g=g, P=P: e.activation(junk[0:P, :], x1[0:P, g, :], AF.Square, accum_out=ss[0:P, :]),
                     reads=["x1", "ss"], writes=["ss", "junk"])
                S.op("dve", lambda e, P=P: e.tensor_scalar(rstd[0:P, :], ss[0:P, :], 1.0 / D, EPS, ALU.mult, ALU.add), reads=["ss"], writes=["rstd"])
                S.op("act", lambda e, P=P: e.sqrt(rstd[0:P, :], rstd[0:P, :]), reads=["rstd"], writes=["rstd"])
                S.op("dve", lambda e, P=P: e.reciprocal(rstd[0:P, :], rstd[0:P, :]), reads=["rstd"], writes=["rstd"])
                S.op("dve", lambda e, g=g, P=P: e.scalar_tensor_tensor(h2[0:P, :], x1[0:P, g, :], rstd[0:P, :], FG[0:P, :], ALU.mult, ALU.mult),
                     reads=["x1", "rstd", "FG"], writes=["h2"])
                S.dma("poolq", dst, h2[0:P, :], reads=["h2"], writes=["ol"])
            else:
                S.dma("poolq", dst, x1[0:P, g, :], reads=["x1"], writes=["ol"])
    if not with_ctx:
        S.dma("sp", xt[0][0:CPC, :], xc, writes=["xt0"])
        S.dma("sp", oc, xt[0][0:CPC, :], reads=["xt0"], writes=["oc"])
    cx.end_phase()


def fft_consts():
    N = 32768
    n1 = np.arange(64)[:, None]; k1 = np.arange(128)[None, :]
    a = 2 * np.pi * n1 * k1 / 128
    F1cat = np.concatenate([np.cos(a), -np.sin(a)], 1)
    n2 = np.arange(256)[:, None]
    t = 2 * np.pi * n2 * k1 / N
    Tr, Ti = np.cos(t), -np.sin(t)
    k2 = np.arange(256)[None, :]
    b = 2 * np.pi * n2 * k2 / 256
    F2r, F2i = np.cos(b), -np.sin(b)
    Er, Ei = np.cos(b.T), np.sin(b.T)
    IA = np.concatenate([Er, Ei], 1); IB = np.concatenate([-Ei, Er], 1)
    ITr, ITi = np.cos(t.T), np.sin(t.T)
    c = 2 * np.pi * np.arange(128)[:, None] * np.arange(64)[None, :] / 128
    G1r, G1i = np.cos(c) / N, -np.sin(c) / N
    ch2 = lambda m: np.ascontiguousarray(m.reshape(2, 128, m.shape[1]).transpose(1, 0, 2))
    psm = (np.arange(128)[:, None] % 64 == np.arange(128)[None, :] % 64).astype(np.float32)
    return {"F1cat": _bf16(F1cat), "Tr": ch2(Tr).astype(np.float32), "Ti": ch2(Ti).astype(np.float32),
            "F2r": _bf16(ch2(F2r)), "F2i": _bf16(ch2(F2i)), "F2in": _bf16(ch2(-F2i)),
            "IA": _bf16(ch2(IA)), "IB": _bf16(ch2(IB)), "ITr": ITr.astype(np.float32), "ITi": ITi.astype(np.float32),
            "G1r": _bf16(G1r), "G1i": _bf16(G1i), "PSM": psm}


def hy_pos_consts(L):
    t = np.arange(L, dtype=np.float32)
    t_norm = t / max(L - 1, 1)
    bands = np.linspace(1e-4, 15, 16, dtype=np.float32)
    ang = (np.float32(2.0 * math.pi / L) * t[:, None] * bands[None, :]).astype(np.float32)
    z = np.concatenate([t_norm[:, None], np.cos(ang), np.sin(ang)], axis=-1).astype(np.float32)
    return np.ascontiguousarray(z.T), np.ascontiguousarray(np.broadcast_to(t_norm[None, :], (128, L))).astype(np.float32)


def emit_B2(cx, with_ctx):
    L = SEQ
    PI = math.pi
    S = cx.S
    paths = [("l", SEQ)] + ([("c", CTX)] if with_ctx else [])
    I = {}
    for tag, Le in paths:
        I[tag] = dict(hy=cx.inp(f"hy_{tag}", [3, 64, Le + 2]), zT=cx.inp(f"zT_{tag}", [33, Le]), tn=cx.inp(f"tn_{tag}", [128, Le]),
                      pl=cx.inp(f"pl_{tag}", [64, Le + 24]), icnt=cx.inp(f"icnt_{tag}", [64, Le]),
                      oh=cx.out(f"oh_{tag}", [64, Le]), op=cx.out(f"op_{tag}", [64, Le]))
    shw_d = cx.inp("shw", [64, 3, 3]); shb_d = cx.inp("shb", [64, 3])
    fw1_d = cx.inp("fw1", [33, 64]); fb1_d = cx.inp("fb1", [64, 1]); fw2_d = cx.inp("fw2", [64, 64]); fb2_d = cx.inp("fb2", [64, 1])
    fw3_d = cx.inp("fw3", [64, 256]); fb3_d = cx.inp("fb3", [128, 2]); ndel_d = cx.inp("ndel", [128, 1])
    dsk_d = cx.inp("dsk", [64, 2, 64])
    psel_d = cx.inp("psel", [64, 4]); pw_d = cx.inp("pw", [64, 64]); psc_d = cx.inp("psc", [64, 1])
    FC = fft_consts()
    cd = {k: cx.inp("c_" + k, list(v.shape), BF16 if v.dtype != np.float32 else F32) for k, v in FC.items()}
    c = {k: cx.sb("c_" + k, list(v.shape), BF16 if v.dtype != np.float32 else F32) for k, v in FC.items()}
    for k in FC:
        S.dma("sp", c[k][:], cd[k], writes=["c_" + k])
    ck = ["c_" + k for k in FC]
    small = {}
    for nm, d_, shp in (("shw", shw_d, [64, 3, 3]), ("shb", shb_d, [64, 3]), ("fw1", fw1_d, [33, 64]), ("fb1", fb1_d, [64, 1]),
                        ("fw2", fw2_d, [64, 64]), ("fb2", fb2_d, [64, 1]), ("fw3", fw3_d, [64, 256]), ("fb3", fb3_d, [128, 2]),
                        ("ndel", ndel_d, [128, 1]), ("dsk", dsk_d, [64, 2, 64]), ("psel", psel_d, [64, 4]), ("pw", pw_d, [64, 64]),
                        ("psc", psc_d, [64, 1])):
        small[nm] = cx.sb("w_" + nm, shp)
        S.dma("sp", small[nm][:], d_, writes=["w_" + nm])
    pwb = cx.sb("pwb", [64, 64], BF16)
    S.op("dve", lambda e: e.tensor_copy(pwb[:], small["pw"][:]), reads=["w_pw"], writes=["pwb"])
    scx = cx.scratch("scx", [3, 64, L]); filt_s = cx.scratch("filt_s", [2, 128, L])
    zero = cx.sb("zero", [128, 2048])
    S.op("pool", lambda e: e.memset(zero[:], 0.0), writes=["zero"])

    def do_path(tag, Le):
        io = I[tag]
        CH = min(2048, Le)
        with ExitStack() as st:
            pin = cx.sb("pin", [64, CH + 24], F32, st); A2 = cx.sb("A2", [64, CH + 24], F32, st); A4 = cx.sb("A4", [64, CH + 24], F32, st)
            A8 = cx.sb("A8", [64, CH + 24], F32, st); A16 = cx.sb("A16", [64, CH + 24], F32, st)
            acc = cx.sb("pacc", [64, CH], F32, st); ic = cx.sb("pic", [64, CH], F32, st); pd = cx.sb("pd", [64, CH], BF16, st)
            po = cx.sb("po", [64, CH], F32, st)
            for c0 in range(0, Le, CH):
                S.dma("sp", pin[:], io["pl"][:, c0:c0 + CH + 24], writes=["pin"])
                S.dma("sp", ic[:], io["icnt"][:, c0:c0 + CH], writes=["pic"])
                W_ = CH + 24
                S.op("dve", lambda e: e.tensor_tensor(A2[:, 0:W_ - 1], pin[:, 0:W_ - 1], pin[:, 1:W_], ALU.add), reads=["pin"], writes=["A2"])
                S.op("dve", lambda e: e.tensor_tensor(A4[:, 0:W_ - 3], A2[:, 0:W_ - 3], A2[:, 2:W_ - 1], ALU.add), reads=["A2"], writes=["A4"])
                S.op("dve", lambda e: e.tensor_tensor(A8[:, 0:W_ - 7], A4[:, 0:W_ - 7], A4[:, 4:W_ - 3], ALU.add), reads=["A4"], writes=["A8"])
                S.op("dve", lambda e: e.tensor_tensor(A16[:, 0:W_ - 15], A8[:, 0:W_ - 15], A8[:, 8:W_ - 7], ALU.add), reads=["A8"], writes=["A16"])
                S.op("dve", lambda e: e.tensor_scalar(acc[:], A2[:, 7:7 + CH], small["psel"][:, 0:1], None, ALU.mult), reads=["A2", "w_psel"], writes=["pacc"])
                for k_, (Aw, off, key) in enumerate(((A4, 6, "A4"), (A8, 4, "A8"), (A16, 0, "A16"))):
                    S.op("dve", lambda e, Aw=Aw, off=off, k_=k_: e.scalar_tensor_tensor(acc[:], Aw[:, off:off + CH], small["psel"][:, k_ + 1:k_ + 2], acc[:],
                                                                                        ALU.mult, ALU.add), reads=[key, "w_psel", "pacc"], writes=["pacc"])
                S.op("dve", lambda e: e.tensor_tensor(acc[:], acc[:], ic[:], ALU.mult), reads=["pacc", "pic"], writes=["pacc"])
                S.op("dve", lambda e: e.tensor_tensor(pd[:], acc[:], pin[:, 8:8 + CH], ALU.subtract), reads=["pacc", "pin"], writes=["pd"])
                for s0 in range(0, CH, 512):
                    w = min(512, CH - s0)
                    S.op("pe", lambda e, s0=s0, w=w: e.matmul(cx.PS[7][0:64, 0:w], pwb[:, :], pd[:, s0:s0 + w], start=True, stop=True),
                         reads=["pd", "pwb"], writes=["ps7"])
                    S.op("dve", lambda e, s0=s0, w=w: e.tensor_scalar(po[:, s0:s0 + w], cx.PS[7][0:64, 0:w], small["psc"][:, 0:1], None, ALU.mult),
                         reads=["ps7", "w_psc"], writes=["po"])
                S.dma("poolq", io["op"][:, c0:c0 + CH], po[:], reads=["po"], writes=["op"])
            S.barrier()

        with ExitStack() as st:
            hin = cx.sb("hin", [64, CH + 2], F32, st); ho = cx.sb("ho", [64, CH], F32, st)
            if Le < L:
                for p in range(3):
                    for c0 in range(0, L, 2048):
                        S.dma("sp", scx[p][:, c0:c0 + 2048], zero[0:64, :], reads=["zero"], writes=["scx"])
                for oc in range(2):
                    for c0 in range(0, L, 2048):
                        S.dma("sp", filt_s[oc][:, c0:c0 + 2048], zero[:, :], reads=["zero"], writes=["filt_s"])
            for p in range(3):
                for c0 in range(0, Le, CH):
                    S.dma("sp", hin[:], io["hy"][p][:, c0:c0 + CH + 2], writes=["hin"])
                    S.op("dve", lambda e, p=p: e.tensor_scalar(ho[:], hin[:, 1:CH + 1], small["shw"][:, p, 1:2], small["shb"][:, p:p + 1], ALU.mult, ALU.add),
                         reads=["hin", "w_shw", "w_shb"], writes=["ho"])
                    S.op("dve", lambda e, p=p: e.scalar_tensor_tensor(ho[:], hin[:, 0:CH], small["shw"][:, p, 0:1], ho[:], ALU.mult, ALU.add),
                         reads=["hin", "w_shw", "ho"], writes=["ho"])
                    S.op("dve", lambda e, p=p: e.scalar_tensor_tensor(ho[:], hin[:, 2:CH + 2], small["shw"][:, p, 2:3], ho[:], ALU.mult, ALU.add),
                         reads=["hin", "w_shw", "ho"], writes=["ho"])
                    S.dma("poolq", scx[p][:, c0:c0 + CH], ho[:], reads=["ho"], writes=["scx"])
            S.barrier()

        with ExitStack() as st:
            FW = min(512, Le)
            nfc = Le // FW
            zt = cx.sb("zt", [33, FW], F32, st); tnt = cx.sb("tnt", [128, FW], F32, st)
            pre = cx.sb("pre", [64, FW], F32, st); msk = cx.sb("msk", [64, FW], F32, st); h1 = cx.sb("h1", [64, FW], F32, st); h2 = cx.sb("h2f", [64, FW], F32, st)
            dec = cx.sb("dec", [128, FW], F32, st); hf = [cx.sb(f"hf{i}", [128, FW], F32, st) for i in range(2)]
            junk = cx.sb("fjunk", [128, FW], F32, st)
            accsq = cx.sb("accsq", [128, 2, 32], F32, st); ssum = cx.sb("ssum", [128, 2], F32, st); rn = cx.sb("rn", [128, 2], F32, st)
            S.op("pool", lambda e: e.memset(accsq[:], 0.0), writes=["accsq"])

            def sin_layer(ps, bias, dst, key):
                S.op("dve", lambda e: e.tensor_scalar(pre[:], ps[0:64, 0:FW], bias[:, 0:1], None, ALU.add), reads=["ps0", "ps1", "w_fb1", "w_fb2"], writes=["pre"])
                for _ in range(2):
                    S.op("dve", lambda e: e.tensor_scalar(msk[:], pre[:], PI, -2 * PI, ALU.is_gt, ALU.mult), reads=["pre"], writes=["msk"])
                    S.op("dve", lambda e: e.tensor_tensor(pre[:], pre[:], msk[:], ALU.add), reads=["pre", "msk"], writes=["pre"])
                    S.op("dve", lambda e: e.tensor_scalar(msk[:], pre[:], -PI, 2 * PI, ALU.is_lt, ALU.mult), reads=["pre"], writes=["msk"])
                    S.op("dve", lambda e: e.tensor_tensor(pre[:], pre[:], msk[:], ALU.add), reads=["pre", "msk"], writes=["pre"])
                S.op("act", lambda e: e.activation(dst[:], pre[:], AF.Sin), reads=["pre"], writes=[key])

            for fc_ in range(nfc):
                sl = slice(fc_ * FW, (fc_ + 1) * FW)
                S.dma("sp", zt[:], io["zT"][:, sl], writes=["zt"]); S.dma("sp", tnt[:], io["tn"][:, sl], writes=["tnt"])
                S.op("pe", lambda e: e.matmul(cx.PS[0][0:64, 0:FW], small["fw1"][:, :], zt[:, :], start=True, stop=True), reads=["zt", "w_fw1"], writes=["ps0"])
                sin_layer(cx.PS[0], small["fb1"], h1, "h1")
                S.op("pe", lambda e: e.matmul(cx.PS[1][0:64, 0:FW], small["fw2"][:, :], h1[:, :], start=True, stop=True), reads=["h1", "w_fw2"], writes=["ps1"])
                sin_layer(cx.PS[1], small["fb2"], h2, "h2f")
                S.op("act", lambda e: e.activation(dec[:], tnt[:], AF.Exp, scale=small["ndel"][:, 0:1]), reads=["tnt", "w_ndel"], writes=["dec"])
                for oc in range(2):
                    S.op("pe", lambda e, oc=oc: e.matmul(cx.PS[2 + oc][:, 0:FW], small["fw3"][:, oc * 128:(oc + 1) * 128], h2[:, :], start=True, stop=True),
                         reads=["h2f", "w_fw3"], writes=[f"ps{2+oc}"])
                    S.op("dve", lambda e, oc=oc: e.scalar_tensor_tensor(hf[oc][:], cx.PS[2 + oc][:, 0:FW], small["fb3"][:, oc:oc + 1], dec[:], ALU.add, ALU.mult),
                         reads=[f"ps{2+oc}", "w_fb3", "dec"], writes=[f"hf{oc}"])
                    S.op("act", lambda e, oc=oc, fc_=fc_: e.activation(junk[:], hf[oc][:], AF.Square, accum_out=accsq[:, oc, fc_:fc_ + 1]),
                         reads=[f"hf{oc}", "accsq"], writes=["fjunk", "accsq"])
                    S.dma("poolq", filt_s[oc][:, sl], hf[oc][:], reads=[f"hf{oc}"], writes=["filt_s"])
            S.op("dve", lambda e: e.tensor_reduce(ssum[:], accsq[:], AX.X, ALU.add), reads=["accsq"], writes=["ssum"])
            S.op("pe", lambda e: e.matmul(cx.PS[4][:, 0:2], c["PSM"][:, :], ssum[:, :], start=True, stop=True), reads=["ssum", "c_PSM"], writes=["ps4"])
            S.op("dve", lambda e: e.tensor_scalar(rn[:], cx.PS[4][:, 0:2], EPS, None, ALU.add), reads=["ps4"], writes=["rn"])
            S.op("act", lambda e: e.sqrt(rn[:], rn[:]), reads=["rn"], writes=["rn"])
            S.op("dve", lambda e: e.reciprocal(rn[:], rn[:]), reads=["rn"], writes=["rn"])
            nb = cx.sb("nb", [128, 2048], F32, st)
            NW = min(2048, Le)
            for oc in range(2):
                for c0 in range(0, Le, NW):
                    S.dma("sp", nb[:, 0:NW], filt_s[oc][:, c0:c0 + NW], reads=["filt_s"], writes=["nb"])
                    S.op("dve", lambda e, oc=oc: e.tensor_scalar(nb[:, 0:NW], nb[:, 0:NW], rn[:, oc:oc + 1], None, ALU.mult), reads=["nb", "rn"], writes=["nb"])
                    if c0 == 0:
                        S.op("pool", lambda e: e.memset(nb[64:128, 0:1], 0.0), reads=["nb"], writes=["nb"])
                    S.dma("poolq", filt_s[oc][:, c0:c0 + NW], nb[:, 0:NW], reads=["nb"], writes=["filt_s"])
            S.barrier()

        with ExitStack() as st:
            Af = cx.sb("Af", [64, 2, 256], F32, st); Ab = cx.sb("Ab", [64, 2, 256], BF16, st)
            Bp = [cx.sb(f"Bp{i}", [128, 2, 2, 128], BF16, st) for i in range(2)]
            tw = [cx.sb(f"tw{i}", [128, 2, 128], F32, st) for i in range(4)]
            Xr = cx.sb("Xr", [128, 2, 2, 128], F32, st); Xi = cx.sb("Xi", [128, 2, 2, 128], F32, st)
            Kr = [cx.sb(f"Kr{o}", [128, 2, 2, 128], F32, st) for o in range(2)]; Ki = [cx.sb(f"Ki{o}", [128, 2, 2, 128], F32, st) for o in range(2)]
            yt_ = [cx.sb(f"yt{i}", [128, 2, 2, 128], F32, st) for i in range(4)]
            Yb = cx.sb("Yb", [128, 2, 2, 2, 128], BF16, st)
            Cp = cx.sb("Cp", [128, 2, 2, 256], BF16, st)
            it_ = [cx.sb(f"it{i}", [128, 256], F32, st) for i in range(4)]
            x1t = cx.sb("x1t", [64, 2, 256], F32, st); x2t = cx.sb("x2t", [64, 2, 256], F32, st); vt = cx.sb("vt", [64, 2, 256], F32, st)
            zt_ = cx.sb("zt_", [64, 2, 256], F32, st); e1 = cx.sb("e1", [64, 2, 256], F32, st); hout = cx.sb("hout", [64, 2, 256], F32, st)

            def tb(src2d):
                return src2d.rearrange("c (n1 n2) -> n1 c n2", n2=256)

            def fwd(Xr_, Xi_, xkey):
                for cc in range(2):
                    ps = cx.PS[cc]
                    for g in range(2):
                        S.op("pe", lambda e, ps=ps, g=g, cc=cc: e.matmul(ps[:, g * 256:(g + 1) * 256], Ab[:, g, cc * 128:(cc + 1) * 128], c["F1cat"][:, :],
                                                                      start=True, stop=True), reads=["Ab", "c_F1cat"], writes=[f"ps{cc}"])
                    psv = ps[:].rearrange("p (g r k) -> p g r k", g=2, r=2)
                    Trb = c["Tr"][:, cc, :].unsqueeze(1).to_broadcast([128, 2, 128]); Tib = c["Ti"][:, cc, :].unsqueeze(1).to_broadcast([128, 2, 128])
                    S.op("dve", lambda e, psv=psv, Trb=Trb: e.tensor_tensor(tw[0][:], psv[:, :, 0, :], Trb, ALU.mult), reads=[f"ps{cc}", "c_Tr"], writes=["tw0"])
                    S.op("dve", lambda e, psv=psv, Tib=Tib: e.tensor_tensor(tw[1][:], psv[:, :, 1, :], Tib, ALU.mult), reads=[f"ps{cc}", "c_Ti"], writes=["tw1"])
                    S.op("dve", lambda e, psv=psv, Tib=Tib: e.tensor_tensor(tw[2][:], psv[:, :, 0, :], Tib, ALU.mult), reads=[f"ps{cc}", "c_Ti"], writes=["tw2"])
                    S.op("dve", lambda e, psv=psv, Trb=Trb: e.tensor_tensor(tw[3][:], psv[:, :, 1, :], Trb, ALU.mult), reads=[f"ps{cc}", "c_Tr"], writes=["tw3"])
                    S.op("pool", lambda e, cc=cc: e.tensor_tensor(Bp[cc][:, 0, :, :], tw[0][:], tw[1][:], ALU.subtract), reads=["tw0", "tw1"], writes=[f"Bp{cc}"])
                    S.op("pool", lambda e, cc=cc: e.tensor_tensor(Bp[cc][:, 1, :, :], tw[2][:], tw[3][:], ALU.add), reads=["tw2", "tw3"], writes=[f"Bp{cc}"])
                for kc in range(2):
                    ks = slice(kc * 128, (kc + 1) * 128)
                    psr, psi = cx.PS[2 + 2 * kc], cx.PS[3 + 2 * kc]
                    seq_r = [(c["F2r"], 0), (c["F2in"], 1)]
                    seq_i = [(c["F2i"], 0), (c["F2r"], 1)]
                    for (pp, seq, pk) in ((psr, seq_r, f"ps{2+2*kc}"), (psi, seq_i, f"ps{3+2*kc}")):
                        n_ = 0
                        for cc in range(2):
                            for (M_, ri) in seq:
                                S.op("pe", lambda e, pp=pp, M_=M_, ri=ri, cc=cc, n_=n_, ks=ks: e.matmul(
                                    pp[:, 0:256], M_[:, cc, ks], Bp[cc][:, ri, :, :], start=(n_ == 0), stop=(n_ == 3)),
                                    reads=[f"Bp{cc}"] + ck, writes=[pk])
                                n_ += 1
                    S.op("act", lambda e, psr=psr, kc=kc: e.copy(Xr_[:, kc, :, :], psr[:, 0:256]), reads=[f"ps{2+2*kc}"], writes=[xkey + "r"])
                    S.op("act", lambda e, psi=psi, kc=kc: e.copy(Xi_[:, kc, :, :], psi[:, 0:256]), reads=[f"ps{3+2*kc}"], writes=[xkey + "i"])

            def conv(o, ydst_key):
                fwd(Xr, Xi, "X")
                fl = lambda t_: t_[:].rearrange("p a b c -> p (a b c)")
                S.op("dve", lambda e: e.tensor_tensor(fl(yt_[0]), fl(Xr), fl(Kr[o]), ALU.mult), reads=["Xr", f"K{o}r"], writes=["yt0"])
                S.op("pool", lambda e: e.tensor_tensor(fl(yt_[1]), fl(Xi), fl(Ki[o]), ALU.mult), reads=["Xi", f"K{o}i"], writes=["yt1"])
                S.op("dve", lambda e: e.tensor_tensor(fl(yt_[2]), fl(Xr), fl(Ki[o]), ALU.mult), reads=["Xr", f"K{o}i"], writes=["yt2"])
                S.op("pool", lambda e: e.tensor_tensor(fl(yt_[3]), fl(Xi), fl(Kr[o]), ALU.mult), reads=["Xi", f"K{o}r"], writes=["yt3"])
                S.op("dve", lambda e: e.tensor_tensor(Yb[:, :, 0, :, :], yt_[0][:], yt_[1][:], ALU.subtract), reads=["yt0", "yt1"], writes=["Yb"])
                S.op("pool", lambda e: e.tensor_tensor(Yb[:, :, 1, :, :], yt_[2][:], yt_[3][:], ALU.add), reads=["yt2", "yt3"], writes=["Yb"])
                for g in range(2):
                    ps = cx.PS[g]
                    n_ = 0
                    for kc in range(2):
                        for (ri, M_) in ((0, c["IA"]), (1, c["IB"])):
                            S.op("pe", lambda e, ps=ps, kc=kc, ri=ri, M_=M_, g=g, n_=n_: e.matmul(ps[:, :], Yb[:, kc, ri, g, :], M_[:, kc, :], start=(n_ == 0), stop=(n_ == 3)),
                                 reads=["Yb"] + ck, writes=[f"ps{g}"])
                            n_ += 1
                    S.op("dve", lambda e, ps=ps: e.tensor_tensor(it_[0][:], ps[:, 0:256], c["ITr"][:], ALU.mult), reads=[f"ps{g}", "c_ITr"], writes=["it0"])
                    S.op("dve", lambda e, ps=ps: e.tensor_tensor(it_[1][:], ps[:, 256:512], c["ITi"][:], ALU.mult), reads=[f"ps{g}", "c_ITi"], writes=["it1"])
                    S.op("dve", lambda e, ps=ps: e.tensor_tensor(it_[2][:], ps[:, 0:256], c["ITi"][:], ALU.mult), reads=[f"ps{g}", "c_ITi"], writes=["it2"])
                    S.op("dve", lambda e, ps=ps: e.tensor_tensor(it_[3][:], ps[:, 256:512], c["ITr"][:], ALU.mult), reads=[f"ps{g}", "c_ITr"], writes=["it3"])
                    S.op("pool", lambda e, g=g: e.tensor_tensor(Cp[:, 0, g, :], it_[0][:], it_[1][:], ALU.subtract), reads=["it0", "it1"], writes=["Cp"])
                    S.op("pool", lambda e, g=g: e.tensor_tensor(Cp[:, 1, g, :], it_[2][:], it_[3][:], ALU.add), reads=["it2", "it3"], writes=["Cp"])
                S.op("pe", lambda e: e.matmul(cx.PS[6][0:64, :], c["G1r"][:, :], Cp[:, 0, :, :], start=True, stop=False), reads=["Cp", "c_G1r"], writes=["ps6"])
                S.op("pe", lambda e: e.matmul(cx.PS[6][0:64, :], c["G1i"][:, :], Cp[:, 1, :, :], start=False, stop=True), reads=["Cp", "c_G1i"], writes=["ps6"])

            for pr in range(32):
                ch0 = pr * 2
                for o in range(2):
                    for d_ in range(2):
                        S.dma("sp", Af[:], tb(filt_s[o][d_ * 64 + ch0:d_ * 64 + ch0 + 2, :]), reads=["filt_s"], writes=["Af"])
                        S.op("dve", lambda e: e.tensor_copy(Ab[:], Af[:]), reads=["Af"], writes=["Ab"])
                        if d_ == 0:
                            fwd(Kr[o], Ki[o], f"K{o}")
                        else:
                            fwd(Xr, Xi, "X")
                            S.op("pool", lambda e, o=o: e.tensor_tensor(Kr[o][:], Kr[o][:], Xr[:], ALU.add), reads=[f"K{o}r", "Xr"], writes=[f"K{o}r"])
                            S.op("pool", lambda e, o=o: e.tensor_tensor(Ki[o][:], Ki[o][:], Xi[:], ALU.subtract), reads=[f"K{o}i", "Xi"], writes=[f"K{o}i"])
                S.dma("sp", x1t[:], tb(scx[0][ch0:ch0 + 2, :]), reads=["scx"], writes=["x1t"])
                S.dma("sp", x2t[:], tb(scx[1][ch0:ch0 + 2, :]), reads=["scx"], writes=["x2t"])
                S.dma("sp", vt[:], tb(scx[2][ch0:ch0 + 2, :]), reads=["scx"], writes=["vt"])
                S.op("dve", lambda e: e.tensor_copy(Ab[:], vt[:]), reads=["vt"], writes=["Ab"])
                conv(0, "y1")
                dk = lambda o: small["dsk"][:, o, ch0:ch0 + 2].unsqueeze(2).to_broadcast([64, 2, 256])
                psy = cx.PS[6][0:64, :].rearrange("p (g n) -> p g n", g=2)
                dk0 = dk(0); dk1 = dk(1)
                S.op("pool", lambda e, dk0=dk0: e.tensor_tensor(e1[:], vt[:], dk0, ALU.mult), reads=["vt", "w_dsk"], writes=["e1"])
                S.op("dve", lambda e: e.tensor_tensor(e1[:], e1[:], psy, ALU.add), reads=["e1", "ps6"], writes=["e1"])
                S.op("dve", lambda e: e.tensor_tensor(zt_[:], e1[:], x1t[:], ALU.mult), reads=["e1", "x1t"], writes=["zt_"])
                S.op("dve", lambda e: e.tensor_copy(Ab[:], zt_[:]), reads=["zt_"], writes=["Ab"])
                conv(1, "y2")
                S.op("pool", lambda e, dk1=dk1: e.tensor_tensor(e1[:], zt_[:], dk1, ALU.mult), reads=["zt_", "w_dsk"], writes=["e1"])
                S.op("dve", lambda e: e.tensor_tensor(e1[:], e1[:], psy, ALU.add), reads=["e1", "ps6"], writes=["e1"])
                S.op("dve", lambda e: e.tensor_tensor(hout[:], e1[:], x2t[:], ALU.mult), reads=["e1", "x2t"], writes=["hout"])
                if Le == L:
                    S.dma("poolq", tb(io["oh"][ch0:ch0 + 2, :]), hout[:], reads=["hout"], writes=["oh"])
                else:
                    S.dma("poolq", io["oh"][ch0:ch0 + 2, :].rearrange("(o c) n -> o c n", o=1), hout[0:1, :, 0:Le], reads=["hout"], writes=["oh"])
            S.barrier()

    for tag_, Le_ in paths:
        do_path(tag_, Le_)
    cx.end_phase()


POOL_SIZES = (2, 4, 8, 16)
NKEY = SEQ + CTX
NLOC = TPC + CPC
GROUPS = [[0, 1, 2, 3], [4, 5, 6, 7]]
SECS = {"aq": 0, "ak": 256, "av": 512, "bq": 768, "bk": 1024, "bv": 1280, "pool": 1536, "hy0": 1792, "hy1": 2048, "hy2": 2304}


def emit_R(cx, need_ctx, T):
    S = cx.S
    ag_out = T["ag_out"]
    selq_d = cx.inp("selq", [128, 2, 2, 32]); selg_d = cx.inp("selg", [128, 2, 64])
    selq = cx.sb("selq", [128, 2, 2, 32]); selg = cx.sb("selg", [128, 2, 64])
    S.dma("sp", selq[:], selq_d, writes=["selq"]); S.dma("sp", selg[:], selg_d, writes=["selg"])
    xs = [cx.sb(f"rx{i}", [128, 2, 512]) for i in range(3)]
    ev = [cx.sb(f"rev{i}", [64, 512]) for i in range(2)]
    k33 = [cx.sb(f"k33_{i}", [33, 512]) for i in range(2)]
    k65 = cx.sb("k65", [65, 512])
    v65 = [cx.sb(f"v65_{i}", [128, 65]) for i in range(2)]
    zero = cx.sb("rzero", [64, 16])
    S.op("pool", lambda e: e.memset(zero[:], 0.0), writes=["rzero"])
    for t_ in k33:
        S.op("pool", lambda e, t_=t_: e.memset(t_[32:33, :], 1.0), writes=["k33"])
    S.op("pool", lambda e: e.memset(k65[64:65, :], 1.0), writes=["k65"])
    for t_ in v65:
        S.op("pool", lambda e, t_=t_: e.memset(t_[:, 64:65], 1.0), writes=["v65"])
    for tag, Le in (("l", SEQ),) + ((("c", CTX),) if need_ctx else ()):
        for p in range(3):
            S.dma("sp", T[f"hy_{tag}"][p][:, 0:1], zero[:, 0:1], reads=["rzero"], writes=["hy"], allow_slow_non_contiguous=True)
            S.dma("sp", T[f"hy_{tag}"][p][:, Le + 1:Le + 2], zero[:, 0:1], reads=["rzero"], writes=["hy"], allow_slow_non_contiguous=True)
        S.dma("sp", T[f"pl_{tag}"][:, 0:8], zero[:, 0:8], reads=["rzero"], writes=["pl"])
        S.dma("sp", T[f"pl_{tag}"][:, Le + 8:Le + 24], zero[:, 0:16], reads=["rzero"], writes=["pl"])
    cnt = {"x": 0, "ps": 0, "ev": 0, "k": 0, "v": 0, "q": 0}

    def load(sec, pieces, w):
        i = cnt["x"] % 3; cnt["x"] += 1
        X = xs[i]
        o = 0
        for (r, c0, pw) in pieces:
            n = pw // 64
            for kc in range(2):
                r0 = r * DIN + SECS[sec] + kc * 128
                src = ag_out[c0 // 64:c0 // 64 + n, r0:r0 + 128, :].rearrange("ck p t -> p ck t")
                q = ("sp", "actq")[cnt["q"] % 2]; cnt["q"] += 1
                S.dma(q, X[:, kc, o:o + pw].rearrange("p (ck t) -> p ck t", t=64), src, writes=[f"rx{i}"])
            o += pw
        return X, f"rx{i}"

    def sel_fm(X, xk, w, SEL, M, dst, ones=None):
        pi = cnt["ps"] % 8; cnt["ps"] += 1
        ps = cx.PS[pi]
        for kc in range(2):
            S.op("pe", lambda e, ps=ps, kc=kc, SEL=SEL: e.matmul(ps[0:M, 0:w], SEL[:, kc, :], X[:, kc, 0:w], start=(kc == 0), stop=(kc == 1)),
                 reads=[xk, "selq", "selg"], writes=[f"ps{pi}"])
        if ones == "k33":
            i = cnt["k"] % 2; cnt["k"] += 1
            dt_, dk, rows = k33[i], f"k33_{i}", 33
        elif ones == "k65":
            dt_, dk, rows = k65, "k65", 65
        else:
            i = cnt["ev"] % 2; cnt["ev"] += 1
            dt_, dk, rows = ev[i], f"rev{i}", M
        S.op(("act", "dve")[cnt["ps"] % 2], lambda e, ps=ps, dt_=dt_: (e.copy if hasattr(e, "copy") else e.tensor_copy)(dt_[0:M, 0:w], ps[0:M, 0:w]),
             reads=[f"ps{pi}"], writes=[dk])
        S.dma("poolq", dst, dt_[0:rows, 0:w], reads=[dk], writes=["rdst"])

    def sel_tm(X, xk, w, dst_rows):
        for s0 in range(0, w, 128):
            pi = cnt["ps"] % 8; cnt["ps"] += 1
            ps = cx.PS[pi]
            for kc in range(2):
                S.op("pe", lambda e, ps=ps, kc=kc, s0=s0: e.matmul(ps[:, 0:64], X[:, kc, s0:s0 + 128], selg[:, kc, :], start=(kc == 0), stop=(kc == 1)),
                     reads=[xk, "selg"], writes=[f"ps{pi}"])
            i = cnt["v"] % 2; cnt["v"] += 1
            S.op(("act", "dve")[i], lambda e, ps=ps, i=i: (e.copy if hasattr(e, "copy") else e.tensor_copy)(v65[i][:, 0:64], ps[:, 0:64]),
                 reads=[f"ps{pi}"], writes=[f"v65_{i}"])
            S.dma("poolq", dst_rows[s0:s0 + 128, :], v65[i][:, :], reads=[f"v65_{i}"], writes=["rdst"])

    chunks = [("l", [(r, cp * 512, 512)], r * TPC + cp * 512, 512) for r in range(4) for cp in range(8)]
    chunks.append(("c", [(r, TPC, CPC) for r in range(4)], SEQ, CTX))
    for kind, pieces, t0, w in chunks:
        isl = kind == "l"
        if isl or need_ctx:
            X, xk = load("aq", pieces, w)
            for c_ in range(2):
                sel_fm(X, xk, w, selq[:, :, c_, :], 32, (T["aqT"][c_][:, t0:t0 + w] if isl else T["aqcT"][c_][:, :]))
            X, xk = load("bq", pieces, w)
            sel_fm(X, xk, w, selg, 64, (T["bqT"][:, t0:t0 + w] if isl else T["bqcT"][:, :]))
            tag = "l" if isl else "c"
            tt = t0 if isl else 0
            X, xk = load("pool", pieces, w)
            sel_fm(X, xk, w, selg, 64, T[f"pl_{tag}"][:, 8 + tt:8 + tt + w])
            for p in range(3):
                X, xk = load(f"hy{p}", pieces, w)
                sel_fm(X, xk, w, selg, 64, T[f"hy_{tag}"][p][:, 1 + tt:1 + tt + w])
        X, xk = load("ak", pieces, w)
        for c_ in range(2):
            sel_fm(X, xk, w, selq[:, :, c_, :], 32, T["akT"][c_][:, t0:t0 + w], ones="k33")
        X, xk = load("bk", pieces, w)
        sel_fm(X, xk, w, selg, 64, T["bkT"][:, t0:t0 + w], ones="k65")
        X, xk = load("av", pieces, w)
        sel_tm(X, xk, w, T["av"][t0:t0 + w, :])
        X, xk = load("bv", pieces, w)
        sel_tm(X, xk, w, T["bv"][t0:t0 + w, :])
    cx.end_phase()


def emit_W(cx, need_ctx, T):
    S = cx.S
    mixT = T["mixT"]; rs_in = T["rs_in"]
    wop_d = cx.inp("wo_part", [4, 64, D])
    wst = cx.sb("wwst", [64, 4, D]); wb = cx.sb("wwb", [64, 4, D], BF16)
    S.dma("sp", wst[:], wop_d.rearrange("m p n -> p m n"), writes=["wwst"])
    S.op("dve", lambda e: e.tensor_copy(wb[:], wst[:]), reads=["wwst"], writes=["wwb"])
    mt = [cx.sb(f"wmt{i}", [64, 4, 128]) for i in range(2)]
    mb = [cx.sb(f"wmb{i}", [64, 4, 128], BF16) for i in range(2)]
    ot = [cx.sb(f"wot{i}", [128, D]) for i in range(2)]
    rs_out = T["rs_out"]
    cckey = cx.coll_group()
    nlb = TPC // 128
    order = [j * nlb + lb for lb in range(nlb) for j in range(4)]
    if need_ctx:
        order += [SEQ // 128 + ci for ci in range(CTX // 128)]
    NCH = NLOC // RSR
    issued = [0]
    chunk_keys = {k: [] for k in range(NCH)}

    def issue_upto(local_done):
        while issued[0] < NCH and (issued[0] + 1) * RSR <= local_done:
            k = issued[0]
            cx.coll(cckey, "ReduceScatter", ALU.add, GROUPS, rs_in[k], rs_out[k * RSR:(k + 1) * RSR, :], sorted(set(chunk_keys[k])))
            issued[0] += 1

    for n_, i in enumerate(order):
        s = n_ % 2
        S.dma(("sp", "actq")[s], mt[s][:], mixT[:, :, i * 128:(i + 1) * 128].rearrange("m p t -> p m t"), writes=[f"wmt{s}"])
        S.op("pool", lambda e, s=s: e.tensor_copy(mb[s][:], mt[s][:]), reads=[f"wmt{s}"], writes=[f"wmb{s}"])
        for hb_ in range(2):
            pi = (2 * n_ + hb_) % 8
            ps = cx.PS[pi]
            for m in range(4):
                S.op("pe", lambda e, ps=ps, m=m, hb_=hb_, s=s: e.matmul(ps[:, :], mb[s][:, m, :], wb[:, m, hb_ * 512:(hb_ + 1) * 512], start=(m == 0), stop=(m == 3)),
                     reads=[f"wmb{s}", "wwb"], writes=[f"ps{pi}"])
            S.op(("act", "dve")[hb_], lambda e, ps=ps, hb_=hb_, s=s: (e.copy if hasattr(e, "copy") else e.tensor_copy)(ot[s][:, hb_ * 512:(hb_ + 1) * 512], ps[:, :]),
                 reads=[f"ps{pi}"], writes=[f"wot{s}h{hb_}"])

        def put(j, loc, p0, n):
            while n > 0:
                k, q0 = loc // RSR, loc % RSR
                m_ = min(n, RSR - q0)
                key = f"rs_in{k}_{j}_{q0}"
                S.dma("poolq", rs_in[k][j * RSR + q0:j * RSR + q0 + m_, :], ot[s][p0:p0 + m_, :], reads=[f"wot{s}h0", f"wot{s}h1"], writes=[key])
                chunk_keys[k].append(key)
                loc += m_; p0 += m_; n -= m_
        if i < SEQ // 128:
            j, lb = i // nlb, i % nlb
            put(j, lb * 128, 0, 128)
            if j == 3 and need_ctx:
                issue_upto((lb + 1) * 128)
            elif j == 3:
                issue_upto((lb + 1) * 128 if lb < nlb - 1 else NLOC)
        else:
            ci = i - SEQ // 128
            for hf in range(2):
                put(ci * 2 + hf, TPC, hf * 64, 64)
            if ci == CTX // 128 - 1:
                issue_upto(NLOC)
    cx.end_phase()


SHARED = {"sel", "ident", "identF", "ropec", "ropes", "onesb", "E65", "ones64", "cT", "fin_g", "router_w", "router_b",
          "zT_l", "tn_l", "zT_c", "tn_c", "icnt_l", "icnt_c", "psel", "ndel", "selq", "selg", "xl", "xc"}


def build_fused(stop=None):
    cx = Ctx("F")
    cx.shared = set(SHARED)
    sc = cx.scratch
    T = {"ag_in": sc("ag_in", [NAGC, DIN, 64]), "ag_out": sc("ag_out", [NAGC, 4 * DIN, 64]),
         "aqT": sc("aqT", [2, 32, SEQ]), "akT": sc("akT", [2, 33, NKEY]), "av": sc("av", [NKEY, 65]), "aqcT": sc("aqcT", [2, 32, CTX]),
         "bqT": sc("bqT", [64, SEQ]), "bkT": sc("bkT", [65, NKEY]), "bv": sc("bv", [NKEY, 65]), "bqcT": sc("bqcT", [64, CTX]),
         "hy_l": sc("hy_l", [3, 64, SEQ + 2]), "pl_l": sc("pl_l", [64, SEQ + 24]),
         "hy_c": sc("hy_c", [3, 64, CTX + 2]), "pl_c": sc("pl_c", [64, CTX + 24]),
         "mixT": sc("mixT", [4, 64, NKEY]), "rs_in": sc("rs_in", [NLOC // RSR, 4 * RSR, D]), "rs_out": sc("rs_out", [NLOC, D]),
         "xl_s": sc("xl_s", [TPC, D]), "xc_s": sc("xc_s", [CPC, D]), "dummy": sc("dummy_oc", [CPC, D])}
    x_l = cx.inp("xl", [TPC, D]); x_c = cx.inp("xc", [CPC, D])
    out = cx.nc.dram_tensor("out", [TPC, D], F32, kind="ExternalOutput").ap()
    mixT = T["mixT"]

    def dump(src2d):
        r, c_ = src2d.shape
        dst = out.rearrange("a d -> (a d)")[0:r * c_].rearrange("(r c) -> r c", c=c_)
        cx.S.dma("sp", dst, src2d, writes=["dbg"])
        return cx.finish(), cx

    for li in range(2):
        need_ctx = li < 1
        cx.prefix = f"L{li}_"
        cx.over = {"xl": x_l if li == 0 else T["xl_s"], "xc": x_c if li == 0 else T["xc_s"], "ag_in": T["ag_in"], "ag_out": T["ag_out"]}
        emit_A(cx)
        if stop == "A":
            return dump(T["ag_in"][0:25, 0:DIN, :].rearrange("a b c -> a (b c)"))
        if stop == "AG":
            return dump(T["ag_out"][0:25, DIN:2 * DIN, :].rearrange("a b c -> a (b c)"))
        cx.over = {}
        emit_R(cx, need_ctx, T)
        if stop == "R":
            return dump(T["bkT"][:, :])
        cx.over = {k: T[k] for k in ("aqT", "akT", "av", "aqcT", "bqT", "bkT", "bv", "bqcT")}
        cx.over.update({"oa": mixT[0][:, 0:SEQ], "oac": mixT[0][:, SEQ:NKEY], "ob": mixT[1][:, 0:SEQ], "obc": mixT[1][:, SEQ:NKEY]})
        emit_B1(cx, li)
        if stop == "B1":
            return dump(mixT[1][:, :])
        cx.over = {"hy_l": T["hy_l"], "pl_l": T["pl_l"], "hy_c": T["hy_c"], "pl_c": T["pl_c"],
                   "op_l": mixT[2][:, 0:SEQ], "op_c": mixT[2][:, SEQ:NKEY], "oh_l": mixT[3][:, 0:SEQ], "oh_c": mixT[3][:, SEQ:NKEY]}
        emit_B2(cx, need_ctx)
        cx.over = {}
        if stop == "B2":
            return dump(mixT[3][:, :])
        emit_W(cx, need_ctx, T)
        if stop == "W":
            return dump(T["rs_in"][0:4, :, :].rearrange("a b c -> (a b) c"))
        if stop == "RS":
            return dump(T["rs_out"][0:TPC, :])
        cx.over = {"xl": x_l if li == 0 else T["xl_s"], "xc": x_c if li == 0 else T["xc_s"], "rs_out": T["rs_out"],
                   "ol": T["xl_s"] if li == 0 else out, "oc": T["xc_s"] if li == 0 else T["dummy"]}
        emit_C(cx, need_ctx, li == 1)
    return cx.finish(), cx


_CACHE = {}
STOP = None


def kernel(x, c, ctx, c_ctx, norm1_g, norm2_g, ada_w, ada_b, w_in, w_out, a_lambda, a_subln_g,
           b_rpb, pool_w, pool_scale, hy_short_w, hy_short_b, hy_f_w1, hy_f_b1, hy_f_w2, hy_f_b2,
           hy_f_w3, hy_f_b3, hy_skip, router_w, router_b, moe_w1, moe_w3, moe_w2, final_g):
    f = lambda a: np.ascontiguousarray(np.asarray(a, dtype=np.float32))
    if "F" not in _CACHE:
        _CACHE["F"] = build_fused(STOP)
    nc, cx = _CACHE["F"]
    x, ctx, c, c_ctx = f(x), f(ctx), f(c), f(c_ctx)
    rc, rs = const_rope()
    FC = fft_consts()
    deltas = np.abs(np.linspace(math.log(1e-2) / 1.5, math.log(1e-2) / 0.3, 256, dtype=np.float32))
    pos = {"l": hy_pos_consts(SEQ), "c": hy_pos_consts(CTX)}
    E65 = np.zeros((65, 64), np.float32); E65[64] = 1.0
    shared_all = {"sel": const_sel(), "ident": _bf16(np.eye(128)), "identF": np.eye(128, dtype=np.float32), "onesb": _bf16(np.ones((128, 128))),
                  "E65": E65, "ones64": np.ones((64, 64), np.float32), "fin_g": f(final_g).reshape(1, -1),
                  "router_w": f(router_w), "router_b": f(router_b).reshape(1, -1),
                  "zT_l": pos["l"][0], "tn_l": pos["l"][1], "zT_c": pos["c"][0], "tn_c": pos["c"][1]}
    shared_all.update({"c_" + k: v for k, v in FC.items()})
    in_maps = []
    for core in range(NCORE):
        b, j = core // 4, core % 4
        h = g = j
        chs = slice(g * 64, (g + 1) * 64)
        m = dict(shared_all)
        m["xl"] = np.ascontiguousarray(x[b, j * TPC:(j + 1) * TPC]); m["xc"] = np.ascontiguousarray(ctx[b, j * CPC:(j + 1) * CPC])
        m["cT"] = cT_layout(c[b], c_ctx)
        m["ropec"] = np.ascontiguousarray(rc[j * TPC:(j + 1) * TPC]); m["ropes"] = np.ascontiguousarray(rs[j * TPC:(j + 1) * TPC])
        selq = np.zeros((256, 2, 32), np.float32); selg = np.zeros((256, 64), np.float32)
        for c_ in range(2):
            selq[h * 64 + c_ * 32 + np.arange(32), c_, np.arange(32)] = 1.0
        selg[h * 64 + np.arange(64), np.arange(64)] = 1.0
        m["selq"] = np.ascontiguousarray(selq.reshape(2, 128, 2, 32).transpose(1, 0, 2, 3))
        m["selg"] = np.ascontiguousarray(selg.reshape(2, 128, 64).transpose(1, 0, 2))
        for tag, Le in (("l", SEQ), ("c", CTX)):
            t = np.arange(Le); w = POOL_SIZES[g]
            cnt = (np.clip(t + w // 2, 0, Le) - np.clip(t - w // 2, 0, Le)).astype(np.float32)
            m[f"icnt_{tag}"] = np.ascontiguousarray(np.broadcast_to((1.0 / cnt)[None, :], (64, Le))).astype(np.float32)
        ps = np.zeros((64, 4), np.float32); ps[:, g] = 1.0
        m["psel"] = ps
        m["ndel"] = np.ascontiguousarray(np.tile(-deltas[chs], 2).reshape(128, 1))
        for li in range(2):
            p = f"L{li}_"
            m[p + "ada_w"] = f(ada_w[li]); m[p + "ada_b"] = f(ada_b[li]).reshape(1, -1)
            m[p + "norm_g"] = f(norm1_g[li]).reshape(1, -1); m[p + "norm2_g"] = f(norm2_g[li]).reshape(1, -1)
            m[p + "w_in"] = f(w_in[li])
            m[p + "alam"] = f(a_lambda[li]).reshape(1, 128); m[p + "subg"] = f(a_subln_g[li]).reshape(64, 1)
            m[p + "bias"] = na_bias_sets(f(b_rpb[li])[h])
            sw = f(hy_short_w[li]).reshape(3, 3, 256)[:, :, chs]
            m[p + "shw"] = np.ascontiguousarray(sw.transpose(2, 1, 0)); m[p + "shb"] = np.ascontiguousarray(f(hy_short_b[li]).reshape(3, 256)[:, chs].T)
            m[p + "fw1"] = f(hy_f_w1[li]); m[p + "fb1"] = f(hy_f_b1[li]).reshape(64, 1)
            m[p + "fw2"] = f(hy_f_w2[li]); m[p + "fb2"] = f(hy_f_b2[li]).reshape(64, 1)
            w3 = f(hy_f_w3[li]).reshape(64, 2, 2, 256)[:, :, :, chs]
            m[p + "fw3"] = np.ascontiguousarray(w3.reshape(64, 256))
            b3 = f(hy_f_b3[li]).reshape(2, 2, 256)[:, :, chs]
            m[p + "fb3"] = np.ascontiguousarray(b3.reshape(2, 128).T)
            m[p + "dsk"] = np.ascontiguousarray(np.broadcast_to(f(hy_skip[li])[:, chs][None], (64, 2, 64))).astype(np.float32)
            m[p + "pw"] = np.ascontiguousarray(f(pool_w[li])[g]); m[p + "psc"] = np.ascontiguousarray(f(pool_scale[li])[chs].reshape(64, 1))
            wo = f(w_out[li])
            m[p + "wo_part"] = np.ascontiguousarray(np.stack([wo[mm * 256 + h * 64:mm * 256 + (h + 1) * 64] for mm in range(4)]))
            m[p + "w1"] = f(moe_w1[li]); m[p + "w3"] = f(moe_w3[li]); m[p + "w2"] = f(moe_w2[li])
        in_maps.append({k: v for k, v in m.items() if k in cx.ins})
    missing = [k for k in cx.ins if k not in in_maps[0]]
    assert not missing, missing
    res = run_bass_kernel_spmd(nc, in_maps, core_ids=list(range(NCORE)))
    out = np.stack([np.concatenate([res.results[b * 4 + j]["out"] for j in range(4)], 0) for b in range(2)])
    return out.astype(np.float32)
```

```python
import math
from contextlib import ExitStack
import numpy as np
import concourse.bass as bass
import concourse.mybir as mybir
from concourse.bass_utils import run_bass_kernel_spmd

F32 = mybir.dt.float32
BF16 = mybir.dt.bfloat16
AF = mybir.ActivationFunctionType
ALU = mybir.AluOpType
AX = mybir.AxisListType

D = 1024
SEQ = 16384
NB = 2
CTX = 256
DIN = 2560
NCORE = 8
TPC = SEQ // 4
NAGC = 65
RSR = 208
CPC = CTX // 4
EPS = 1e-6

COMPUTE = ("pe", "dve", "act", "pool")
QUEUES = ("sp", "actq", "poolq")
ISSUER = {"sp": "sp", "actq": "act", "poolq": "pool"}
NDMASEM = 12


class Sched:
    def __init__(self, nc, same_engine_sync=True):
        self.nc = nc
        self.same = same_engine_sync
        self.streams = {e: [] for e in ("pe", "dve", "act", "pool", "sp")}
        self.cnt = {e: 0 for e in COMPUTE}
        self.sem = {}
        self.dsem = {}
        self.dnext = {q: 0 for q in QUEUES}
        self.seen = {}
        self.writer = {}
        self.readers = {}

    def alloc_sems(self, stack):
        for e in COMPUTE:
            self.sem[e] = stack.enter_context(self.nc.semaphore("s_" + e))
        for q in QUEUES:
            for i in range(NDMASEM):
                self.dsem[(q, i)] = [stack.enter_context(self.nc.semaphore(f"d_{q}{i}")), 0]

    def _deps(self, reads, writes):
        deps = []
        for b in reads:
            if b in self.writer:
                deps.append(self.writer[b])
        for b in writes:
            if b in self.writer:
                deps.append(self.writer[b])
            deps.extend(self.readers.get(b, ()))
        return deps

    def _record(self, tok, reads, writes):
        for b in reads:
            self.readers.setdefault(b, []).append(tok)
        for b in writes:
            self.writer[b] = tok
            self.readers[b] = []

    def _waits(self, stream, deps, eng_name):
        ws = []
        for (sk, val, en) in deps:
            if en == eng_name and (eng_name == "pe" or not self.same):
                continue
            key = (stream, sk)
            if self.seen.get(key, 0) >= val:
                continue
            self.seen[key] = val
            ws.append((sk, val))
        return ws

    def _semobj(self, sk):
        return self.sem[sk] if sk in self.sem else self.dsem[sk][0]

    def op(self, eng, fn, reads=(), writes=()):
        deps = self._deps(reads, writes)
        ws = self._waits(eng, deps, eng)
        self.cnt[eng] += 1
        tok = (eng, self.cnt[eng], eng)
        self.streams[eng].append((ws, fn, (eng, 1)))
        self._record(tok, reads, writes)
        return tok

    def dma(self, q, out, in_, reads=(), writes=(), **kw):
        stream = ISSUER[q]
        deps = self._deps(reads, writes)
        i = self.dnext[q]
        self.dnext[q] = (i + 1) % NDMASEM
        ent = self.dsem[(q, i)]
        if ent[1] > 0:
            deps = deps + [((q, i), ent[1], "dma")]
        ws = self._waits(stream, deps, "dma?")
        ent[1] += 16
        tok = ((q, i), ent[1], "dma")

        def fn(e, out=out, in_=in_, kw=kw):
            return e.dma_start(out=out, in_=in_, **kw)
        self.streams[stream].append((ws, fn, ((q, i), 16)))
        self._record(tok, reads, writes)
        return tok

    def _all_tokens(self):
        toks = [(e, self.cnt[e], "x") for e in COMPUTE if self.cnt[e] > 0]
        for k, ent in self.dsem.items():
            if ent[1] > 0:
                toks.append((k, ent[1], "dma"))
        return toks

    def barrier(self):
        toks = self._all_tokens()
        for s in self.streams:
            ws = self._waits(s, toks, "none")
            if ws:
                self.streams[s].append((ws, None, None))
        self.writer.clear()
        self.readers.clear()

    def emit(self):
        ws = [(sk, val) for (sk, val, _) in self._all_tokens()]
        self.streams["sp"].append((ws, None, None))
        nc = self.nc
        with nc.Block() as block:
            def mk(sname):
                def body(e):
                    for (ws, fn, inc) in self.streams[sname]:
                        for (sk, val) in ws:
                            e.wait_ge(self._semobj(sk), val)
                        if fn is not None:
                            fn(e).then_inc(self._semobj(inc[0]), inc[1])
                return body
            block.tensor(mk("pe"))
            block.vector(mk("dve"))
            block.scalar(mk("act"))
            block.gpsimd(mk("pool"))
            block.sync(mk("sp"))


class Ctx:
    def __init__(self, name="k", nc=None):
        self.nc = nc or bass.Bass("TRN2", target_bir_lowering=False)
        self.root = ExitStack()
        self.st = ExitStack()
        self.S = Sched(self.nc)
        self.S.alloc_sems(self.root)
        self.ins = {}
        self.outs = {}
        self.over = {}
        self.prefix = ""
        self.shared = set()
        self.PSALL = self.root.enter_context(self.nc.psum_tensor("psall", [128, 4096], F32))
        self.PS = [self.PSALL[:, i * 512:(i + 1) * 512] for i in range(8)]
        self._n = 0
        self._ncc = 0

    def inp(self, name, shape, dt=F32):
        if name in self.over:
            return self.over[name]
        full = name if (name in self.shared or name.startswith("c_")) else self.prefix + name
        if full in self.ins:
            return self.ins[full]
        t = self.nc.dram_tensor(full, list(shape), dt, kind="ExternalInput").ap()
        self.ins[full] = t
        return t

    def out(self, name, shape, dt=F32):
        if name in self.over:
            return self.over[name]
        t = self.nc.dram_tensor(self.prefix + name, list(shape), dt, kind="ExternalOutput").ap()
        self.outs[self.prefix + name] = t
        return t

    def scratch(self, name, shape, dt=F32):
        self._n += 1
        return self.nc.dram_tensor(f"scr{self._n}_{name}", list(shape), dt, kind="Internal").ap()

    def sb(self, name, shape, dt=F32, stack=None):
        self._n += 1
        return (stack or self.st).enter_context(self.nc.sbuf_tensor(f"sb{self._n}_{name}", list(shape), dt))

    def end_phase(self):
        self.S.barrier()
        self.st.close()
        self.st = ExitStack()

    def collective(self, kind, op, groups, pairs):
        S = self.S
        S.barrier()
        sem = self.root.enter_context(self.nc.semaphore(f"cc{self._ncc}"))
        key = ("cc", self._ncc)
        self._ncc += 1
        S.dsem[key] = [sem, len(pairs)]
        for (src, dst) in pairs:
            S.streams["pool"].append(([], lambda e, src=src, dst=dst: e.collective_compute(kind, op, replica_groups=groups, ins=[src], outs=[dst]), (key, 1)))
        S.barrier()

    def coll_group(self):
        sem = self.root.enter_context(self.nc.semaphore(f"cc{self._ncc}"))
        key = ("cc", self._ncc)
        self._ncc += 1
        self.S.dsem[key] = [sem, 0]
        return key

    def coll(self, key, kind, op, groups, src, dst, reads):
        S = self.S
        ws = S._waits("pool", S._deps(reads, []), "pool-cc")
        S.dsem[key][1] += 1
        S.streams["pool"].append((ws, lambda e: e.collective_compute(kind, op, replica_groups=groups, ins=[src], outs=[dst]), (key, 1)))

    def finish(self):
        self.S.emit()
        self.st.close()
        self.root.close()
        return self.nc


def emit_mod_rows(cx, st, scT, ada_w, ada_b, modrow, tag):
    S = cx.S
    wbuf = [cx.sb(f"adaw{tag}{i}", [128, 8, 512], F32, st) for i in range(2)]
    adab = cx.sb(f"adab{tag}", [2, 6144], F32, st)
    S.dma("sp", adab[0:1, :], ada_b, writes=["adab"])
    S.dma("sp", adab[1:2, :], ada_b, writes=["adab"])
    awv = ada_w.rearrange("(kc p) n -> p kc n", p=128)
    for cb in range(12):
        wb = wbuf[cb % 2]
        q = ("sp", "actq")[cb % 2]
        S.dma(q, wb[:, 0:4, :], awv[:, 0:4, cb * 512:(cb + 1) * 512], writes=[f"adaw{cb%2}a"])
        S.dma(q, wb[:, 4:8, :], awv[:, 4:8, cb * 512:(cb + 1) * 512], writes=[f"adaw{cb%2}b"])
        ps = cx.PS[cb % 2]
        for kc in range(8):
            S.op("pe", lambda e, ps=ps, kc=kc, wb=wb: e.matmul(ps[0:2, :], scT[:, kc, :], wb[:, kc, :],
                                                                start=(kc == 0), stop=(kc == 7)),
                 reads=["scT", f"adaw{cb%2}a", f"adaw{cb%2}b"], writes=[f"ps{cb%2}"])
        S.op("dve", lambda e, ps=ps, cb=cb: e.tensor_tensor(modrow[:, cb * 512:(cb + 1) * 512], ps[0:2, :],
                                                            adab[:, cb * 512:(cb + 1) * 512], ALU.add),
             reads=[f"ps{cb%2}", "adab"], writes=["modrow"])


def emit_bcast_row(cx, dst, row2, sel, which, ps_ids, rkey, wkey):
    S = cx.S
    for hb in range(2):
        ps = cx.PS[ps_ids[hb]]
        S.op("pe", lambda e, ps=ps, hb=hb: e.matmul(ps[:, :], sel[:, which, :], row2[:, hb * 512:(hb + 1) * 512],
                                                    start=True, stop=True),
             reads=[rkey, "sel"], writes=[f"ps{ps_ids[hb]}"])
        S.op("act", lambda e, ps=ps, hb=hb: e.copy(dst[:, hb * 512:(hb + 1) * 512], ps[:, :]),
             reads=[f"ps{ps_ids[hb]}"], writes=[wkey])


def emit_rstd(cx, xt, P, ss, rstd, junk, xkey, slot):
    S = cx.S
    S.op("pool", lambda e: e.memset(ss[0:P, :], 0.0), writes=[f"ss{slot}"])
    S.op("act", lambda e: e.activation(junk[0:P, :], xt[0:P, :], AF.Square, accum_out=ss[0:P, :]),
         reads=[xkey], writes=[f"ss{slot}", "junk"])
    S.op("dve", lambda e: e.tensor_scalar(rstd[0:P, :], ss[0:P, :], 1.0 / D, EPS, ALU.mult, ALU.add),
         reads=[f"ss{slot}"], writes=[f"rstd{slot}"])
    S.op("act", lambda e: e.sqrt(rstd[0:P, :], rstd[0:P, :]), reads=[f"rstd{slot}"], writes=[f"rstd{slot}"])
    S.op("dve", lambda e: e.reciprocal(rstd[0:P, :], rstd[0:P, :]), reads=[f"rstd{slot}"], writes=[f"rstd{slot}"])


def emit_transpose8(cx, hb, P, ident, hT, psb, hkey, tkey, pskey):
    S = cx.S
    psv = psb[:].bitcast(BF16).rearrange("p (k t) -> p k t", t=128)
    for kc in range(8):
        S.op("pe", lambda e, kc=kc: e.transpose(psv[:, kc, 0:P], hb[0:P, kc * 128:(kc + 1) * 128], ident[0:P, 0:P]),
             reads=[hkey, "ident"], writes=[pskey])
    S.op("act", lambda e: e.copy(hT[:, :, 0:P], psv[:, :, 0:P]), reads=[pskey], writes=[tkey])


def emit_A(cx, has_rope=True):
    S = cx.S
    xl = cx.inp("xl", [TPC, D])
    xc = cx.inp("xc", [CPC, D])
    cT = cx.inp("cT", [128, 8, 2])
    ada_w = cx.inp("ada_w", [D, 6 * D])
    ada_b = cx.inp("ada_b", [1, 6 * D])
    norm_g = cx.inp("norm_g", [1, D])
    w_in = cx.inp("w_in", [D, DIN])
    sel_d = cx.inp("sel", [2, 2, 128])
    ident_d = cx.inp("ident", [128, 128], BF16)
    ropec = cx.inp("ropec", [TPC, 16])
    ropes = cx.inp("ropes", [TPC, 16])
    ag_in = cx.inp("ag_in", [NAGC, DIN, 64])
    ag_out = cx.inp("ag_out", [NAGC, 4 * DIN, 64])
    cckey = cx.coll_group()
    identF_d = cx.inp("identF", [128, 128])

    sel = cx.sb("sel", [2, 2, 128])
    ident = cx.sb("ident", [128, 128], BF16)
    identF = cx.sb("identF", [128, 128])
    utT = cx.sb("utT", [128, 20, 128])
    S.dma("sp", identF[:], identF_d, writes=["identF"])
    scT = cx.sb("scT", [128, 8, 2])
    modrow = cx.sb("modrow", [2, 6 * D])
    normg2 = cx.sb("normg2", [2, D])
    grow = cx.sb("grow", [2, D])
    GL = cx.sb("GL", [128, D]); SHL = cx.sb("SHL", [128, D])
    GC = cx.sb("GC", [128, D]); SHC = cx.sb("SHC", [128, D])
    wbf = cx.sb("wbf", [128, 8, DIN], BF16)
    S.dma("sp", sel[:], sel_d, writes=["sel"])
    S.dma("sp", ident[:], ident_d, writes=["ident"])
    S.dma("sp", scT[:], cT, writes=["scT"])
    S.dma("sp", normg2[0:1, :], norm_g, writes=["normg2"])
    S.dma("sp", normg2[1:2, :], norm_g, writes=["normg2"])
    S.op("act", lambda e: e.activation(scT[:], scT[:], AF.Silu), reads=["scT"], writes=["scT"])
    with ExitStack() as st:
        emit_mod_rows(cx, st, scT, ada_w, ada_b, modrow, "A")
        wst = [cx.sb(f"wst{i}", [128, DIN], F32, st) for i in range(2)]
        wv = w_in.rearrange("(kc p) n -> p kc n", p=128)
        for kc in range(8):
            S.dma(("sp", "actq")[kc % 2], wst[kc % 2][:], wv[:, kc, :], writes=[f"wst{kc%2}"])
            eng = ("dve", "pool")[kc % 2]
            S.op(eng, lambda e, kc=kc: e.tensor_copy(wbf[:, kc, :], wst[kc % 2][:]), reads=[f"wst{kc%2}"], writes=["wbf"])
        S.barrier()
    S.op("dve", lambda e: e.scalar_tensor_tensor(grow[:], modrow[:, D:2 * D], 1.0, normg2[:], ALU.add, ALU.mult),
         reads=["modrow", "normg2"], writes=["grow"])
    emit_bcast_row(cx, GL, grow, sel, 0, (0, 1), "grow", "GL")
    emit_bcast_row(cx, SHL, modrow[:, 0:D], sel, 0, (0, 1), "modrow", "SHL")
    emit_bcast_row(cx, GC, grow, sel, 1, (0, 1), "grow", "GC")
    emit_bcast_row(cx, SHC, modrow[:, 0:D], sel, 1, (0, 1), "modrow", "SHC")

    xt = [cx.sb(f"xt{i}", [128, D]) for i in range(2)]
    junk = cx.sb("junk", [128, D], BF16)
    tmp = cx.sb("tmp", [128, D])
    hb = [cx.sb(f"hb{i}", [128, D], BF16) for i in range(2)]
    hT = [cx.sb(f"hT{i}", [128, 8, 128], BF16) for i in range(2)]
    ut = [cx.sb(f"ut{i}", [128, DIN]) for i in range(2)]
    ss = [cx.sb(f"ss{i}", [128, 1]) for i in range(2)]
    rstd = [cx.sb(f"rstd{i}", [128, 1]) for i in range(2)]
    rc = [cx.sb(f"rc{i}", [128, 16]) for i in range(2)]
    rs = [cx.sb(f"rs{i}", [128, 16]) for i in range(2)]
    rt = [cx.sb(f"rt{i}", [128, 16, 16]) for i in range(4)]

    ntl = TPC // 128
    tiles = [("l", i) for i in range(ntl)] + [("c", 0)]

    def load(ti):
        kind, i = tiles[ti]
        s = ti % 2
        if kind == "l":
            S.dma("sp", xt[s][:], xl[i * 128:(i + 1) * 128, :], writes=[f"xt{s}"])
            if has_rope:
                S.dma("sp", rc[s][:], ropec[i * 128:(i + 1) * 128, :], writes=[f"rc{s}"])
                S.dma("sp", rs[s][:], ropes[i * 128:(i + 1) * 128, :], writes=[f"rs{s}"])
        else:
            S.dma("sp", xt[s][0:CPC, :], xc, writes=[f"xt{s}"])

    load(0)
    for ti, (kind, i) in enumerate(tiles):
        s = ti % 2
        P = 128 if kind == "l" else CPC
        G, SH = (GL, SHL) if kind == "l" else (GC, SHC)
        if ti + 1 < len(tiles):
            load(ti + 1)
        emit_rstd(cx, xt[s], P, ss[s], rstd[s], junk, f"xt{s}", s)
        S.op("dve", lambda e, s=s, P=P, G=G: e.scalar_tensor_tensor(tmp[0:P, :], xt[s][0:P, :], rstd[s][0:P, :], G[0:P, :],
                                                                     ALU.mult, ALU.mult),
             reads=[f"xt{s}", f"rstd{s}", "GL", "GC"], writes=["tmp"])
        S.op("dve", lambda e, s=s, P=P, SH=SH: e.tensor_tensor(hb[s][0:P, :], tmp[0:P, :], SH[0:P, :], ALU.add),
             reads=["tmp", "SHL", "SHC"], writes=[f"hb{s}"])
        emit_transpose8(cx, hb[s], P, ident, hT[s], cx.PS[2], f"hb{s}", f"hT{s}", "ps2")
        for cb in range(5):
            ps = cx.PS[3 + cb]
            for kc in range(8):
                S.op("pe", lambda e, ps=ps, kc=kc, cb=cb, s=s, P=P: e.matmul(
                    ps[0:P, :], hT[s][:, kc, 0:P], wbf[:, kc, cb * 512:(cb + 1) * 512], start=(kc == 0), stop=(kc == 7)),
                    reads=[f"hT{s}", "wbf"], writes=[f"ps{3+cb}"])
            if cb % 2 == 0:
                S.op("act", lambda e, ps=ps, cb=cb, s=s, P=P: e.copy(ut[s][0:P, cb * 512:(cb + 1) * 512], ps[0:P, :]),
                     reads=[f"ps{3+cb}"], writes=[f"ut{s}c{cb}"])
            else:
                S.op("dve", lambda e, ps=ps, cb=cb, s=s, P=P: e.tensor_copy(ut[s][0:P, cb * 512:(cb + 1) * 512], ps[0:P, :]),
                     reads=[f"ps{3+cb}"], writes=[f"ut{s}c{cb}"])
        if kind == "l" and has_rope:
            xv = ut[s][:, 0:512].rearrange("p (g d) -> p g d", d=32)
            x1 = xv[:, :, 0:16]
            x2 = xv[:, :, 16:32]
            cb_ = rc[s][:].unsqueeze(1).to_broadcast([128, 16, 16])
            sb_ = rs[s][:].unsqueeze(1).to_broadcast([128, 16, 16])
            S.op("dve", lambda e, x1=x1, cb_=cb_: e.tensor_tensor(rt[0][:], x1, cb_, ALU.mult), reads=[f"ut{s}c0", f"rc{s}"], writes=["rt0"])
            S.op("pool", lambda e, x2=x2, sb_=sb_: e.tensor_tensor(rt[1][:], x2, sb_, ALU.mult), reads=[f"ut{s}c0", f"rs{s}"], writes=["rt1"])
            S.op("dve", lambda e, x2=x2, cb_=cb_: e.tensor_tensor(rt[2][:], x2, cb_, ALU.mult), reads=[f"ut{s}c0", f"rc{s}"], writes=["rt2"])
            S.op("pool", lambda e, x1=x1, sb_=sb_: e.tensor_tensor(rt[3][:], x1, sb_, ALU.mult), reads=[f"ut{s}c0", f"rs{s}"], writes=["rt3"])
            S.op("dve", lambda e, x1=x1: e.tensor_tensor(x1, rt[0][:], rt[1][:], ALU.subtract), reads=["rt0", "rt1"], writes=[f"ut{s}c0"])
            S.op("pool", lambda e, x2=x2: e.tensor_tensor(x2, rt[2][:], rt[3][:], ALU.add), reads=["rt2", "rt3", f"ut{s}c0"], writes=[f"ut{s}c0"])
        for q4 in range(5):
            psq = cx.PS[q4 % 2]
            for j4 in range(4):
                cc = q4 * 4 + j4
                S.op("pe", lambda e, psq=psq, j4=j4, cc=cc, s=s, P=P: e.matmul(psq[:, j4 * 128:j4 * 128 + P], ut[s][0:P, cc * 128:(cc + 1) * 128],
                                                                            identF[0:P, 0:P], start=True, stop=True),
                     reads=[f"ut{s}c{cc // 4}", "identF"], writes=[f"ps{q4 % 2}"])
            pqv = psq[:].rearrange("p (j t) -> p j t", t=128)
            S.op(("act", "dve")[q4 % 2], lambda e, pqv=pqv, q4=q4, P=P: (e.copy if hasattr(e, "copy") else e.tensor_copy)(utT[:, q4 * 4:(q4 + 1) * 4, 0:P], pqv[:, :, 0:P]),
                 reads=[f"ps{q4 % 2}"], writes=["utT"])
        for hf in range(P // 64):
            ck = (2 * i + hf) if kind == "l" else NAGC - 1
            S.dma("poolq", ag_in[ck].rearrange("(cc p) t -> p cc t", p=128), utT[:, :, hf * 64:(hf + 1) * 64], reads=["utT"], writes=[f"ag_in{ck}"])
            cx.coll(cckey, "AllGather", ALU.bypass, GROUPS, ag_in[ck], ag_out[ck], [f"ag_in{ck}"])
    cx.end_phase()


def _bf16(a):
    import ml_dtypes
    return np.asarray(a, dtype=np.float32).astype(ml_dtypes.bfloat16)


def const_sel():
    s = np.zeros((2, 2, 128), np.float32)
    s[0, 0, :] = 1.0
    s[1, 1, :] = 1.0
    return s


def const_rope():
    inv = 10000.0 ** (-np.arange(8, dtype=np.float32) / 8)
    t = np.arange(SEQ)
    row = (t // 64).astype(np.float32)
    col = (t % 64).astype(np.float32)
    ang = np.concatenate([row[:, None] * inv, col[:, None] * inv], axis=-1).astype(np.float32)
    return np.cos(ang).astype(np.float32), np.sin(ang).astype(np.float32)


def cT_layout(c_b, c_ctx):
    a = np.stack([c_b, c_ctx], axis=-1)
    return np.ascontiguousarray(a.reshape(8, 128, 2).transpose(1, 0, 2))


WIDE_EXP = False


class Attn:
    GROUPS_ = ((0, 1, 2), (5, 6, 7))
    G = 3

    def __init__(self, cx, ident):
        self.cx = cx
        self.ident = ident
        self.PT = [cx.sb(f"PT{i}", [128, self.G * 512], BF16) for i in range(2)]
        self.it = 0

    def run(self, QT, N, chunks, pso, psokey, qkeys):
        cx, S = self.cx, self.cx.S
        n = len(chunks)
        G = self.G
        ngrp = (n + G - 1) // G

        def qk(p):
            slot = (self.it + p) % 2
            for h_ in range(G):
                i = G * p + h_
                if i >= n:
                    continue
                KT, V, bias, keys = chunks[i]
                bank = self.GROUPS_[slot][h_]
                ps = cx.PS[bank]
                S.op("pe", lambda e, ps=ps, KT=KT, bias=bias: e.matmul(ps[:, 0:N], KT, QT, start=True, stop=(bias is None)),
                     reads=list(keys) + list(qkeys), writes=[f"ps{bank}"])
                if bias is not None:
                    S.op("pe", lambda e, ps=ps, bias=bias: e.matmul(ps[:, 0:N], self.ident[:], bias, start=False, stop=True),
                         reads=["bias", "ident"], writes=[f"ps{bank}"])
        qk(0)
        for p in range(ngrp):
            if p + 1 < ngrp:
                qk(p + 1)
            slot = (self.it + p) % 2
            PT = self.PT[slot]
            m = min(G, n - G * p)
            for h_ in range(m):
                bank = self.GROUPS_[slot][h_]
                S.op("act", lambda e, bank=bank, PT=PT, h_=h_: e.activation(PT[:, h_ * 512:h_ * 512 + N], cx.PS[bank][:, 0:N], AF.Exp),
                     reads=[f"ps{bank}"], writes=[f"PT{slot}_{h_}"])
            for h_ in range(m):
                i = G * p + h_
                V = chunks[i][1]
                S.op("pe", lambda e, PT=PT, V=V, i=i, h_=h_: e.matmul(pso[0:65, 0:N], V, PT[:, h_ * 512:h_ * 512 + N], start=(i == 0), stop=(i == n - 1)),
                     reads=[f"PT{slot}_{h_}", "vaug"], writes=[psokey])
        self.it += ngrp


def emit_cast_rows(cx, stg, dst, src, rows, cols, key, scale=None, chunk=2048):
    S = cx.S
    nch = (cols + chunk - 1) // chunk
    for c in range(nch):
        w = min(chunk, cols - c * chunk)
        sl = slice(c * chunk, c * chunk + w)
        S.dma(("sp", "poolq")[c % 2], stg[c % 2][0:rows, 0:w], src[:, sl], writes=[f"stg{c%2}"])
        if scale is None:
            S.op("dve", lambda e, c=c, w=w, sl=sl: e.tensor_copy(dst[0:rows, sl], stg[c % 2][0:rows, 0:w]),
                 reads=[f"stg{c%2}"], writes=[key])
        else:
            S.op("dve", lambda e, c=c, w=w, sl=sl: e.tensor_scalar(dst[0:rows, sl], stg[c % 2][0:rows, 0:w], scale, None, ALU.mult),
                 reads=[f"stg{c%2}"], writes=[key])


def emit_load_v(cx, stg, Vaug, vsrc, nk, key):
    S = cx.S
    nch = nk // 128
    vv = vsrc.rearrange("(c p) d -> p c d", p=128)
    step = 26
    for i, c0 in enumerate(range(0, nch, step)):
        c1 = min(nch, c0 + step)
        sv = stg[i % 2][:, 0:(c1 - c0) * 65].rearrange("p (c d) -> p c d", d=65)
        S.dma(("sp", "poolq")[i % 2], sv, vv[:, c0:c1, :], writes=[f"stg{i%2}"])
        S.op("dve", lambda e, sv=sv, c0=c0, c1=c1: e.tensor_copy(Vaug[:, c0:c1, :], sv), reads=[f"stg{i%2}"], writes=[key])


def emit_qbound(cx, st, QT, d, N_total, kfac, ones_b, qsrc, key):
    S = cx.S
    sq = [cx.sb(f"sq_{key}{i}", [d, 512], BF16, st) for i in range(2)]
    for c in range(N_total // 512 if N_total >= 512 else 1):
        w = min(512, N_total)
        sl = slice(c * 512, c * 512 + w)
        S.op("dve", lambda e, c=c, sl=sl, w=w: e.tensor_tensor(sq[c % 2][:, 0:w], QT[0:d, sl], QT[0:d, sl], ALU.mult),
             reads=[key], writes=[f"sq{c%2}"])
        ps = cx.PS[6 + c % 2]
        S.op("pe", lambda e, ps=ps, c=c, w=w: e.matmul(ps[0:d + 1, 0:w], ones_b[0:d, 0:d + 1], sq[c % 2][:, 0:w], start=True, stop=True),
             reads=[f"sq{c%2}", "ones_b"], writes=[f"ps{6+c%2}"])
        S.op("act", lambda e, ps=ps, sl=sl, w=w: e.sqrt(QT[d:d + 1, sl], ps[d:d + 1, 0:w]),
             reads=[f"ps{6+c%2}"], writes=[key + "r"])
        S.op("dve", lambda e, sl=sl: e.tensor_scalar(QT[d:d + 1, sl], QT[d:d + 1, sl], kfac[d:d + 1, 0:1], -1.0, ALU.mult, ALU.mult),
             reads=[key + "r", "kfac"], writes=[key + "r"])


def emit_kmax(cx, st, KT, d, nk, kfac, ones_b, key):
    S = cx.S
    sq = [cx.sb(f"ksq_{key}{i}", [d, 512], BF16, st) for i in range(2)]
    kmx = cx.sb(f"kmx_{key}", [d + 1, 64], F32, st)
    S.op("pool", lambda e: e.memset(kmx[:], 0.0), writes=["kmx"])
    nch = (nk + 511) // 512
    for c in range(nch):
        w = min(512, nk - c * 512)
        sl = slice(c * 512, c * 512 + w)
        S.op("dve", lambda e, c=c, sl=sl, w=w: e.tensor_tensor(sq[c % 2][:, 0:w], KT[0:d, sl], KT[0:d, sl], ALU.mult),
             reads=[key], writes=[f"sq{c%2}"])
        ps = cx.PS[6 + c % 2]
        S.op("pe", lambda e, ps=ps, c=c, w=w: e.matmul(ps[0:d + 1, 0:w], ones_b[0:d, 0:d + 1], sq[c % 2][:, 0:w], start=True, stop=True),
             reads=[f"sq{c%2}", "ones_b"], writes=[f"ps{6+c%2}"])
        S.op("dve", lambda e, ps=ps, c=c, w=w: e.tensor_reduce(kmx[d:d + 1, c:c + 1], ps[d:d + 1, 0:w], AX.X, ALU.max),
             reads=[f"ps{6+c%2}"], writes=["kmx"])
    S.op("dve", lambda e: e.tensor_reduce(kfac[d:d + 1, 0:1], kmx[d:d + 1, 0:nch], AX.X, ALU.max), reads=["kmx"], writes=["kfac"])
    S.op("act", lambda e: e.sqrt(kfac[d:d + 1, 0:1], kfac[d:d + 1, 0:1]), reads=["kfac"], writes=["kfac"])


def emit_finalize(cx, pso, N, recrow, osb, E65, tdst, psokey, tkey):
    S = cx.S
    S.op("dve", lambda e: e.reciprocal(recrow[64:65, 0:N], pso[64:65, 0:N]), reads=[psokey], writes=["recrow"])
    S.op("pe", lambda e: e.matmul(cx.PS[5][0:64, 0:N], E65[0:65, 0:64], recrow[0:65, 0:N], start=True, stop=True),
         reads=["recrow", "E65"], writes=["ps5"])
    S.op("act", lambda e: e.copy(osb[0:64, 0:N], pso[0:64, 0:N]), reads=[psokey], writes=["osb"])
    S.op("dve", lambda e: e.tensor_tensor(tdst[0:64, 0:N], osb[0:64, 0:N], cx.PS[5][0:64, 0:N], ALU.mult),
         reads=["osb", "ps5"], writes=[tkey])


def emit_B1(cx, li):
    lam_init = 0.8 - 0.6 * math.exp(-0.3 * li)
    NK = SEQ + CTX
    S = cx.S
    aqT = cx.inp("aqT", [2, 32, SEQ]); akT = cx.inp("akT", [2, 33, NK]); av = cx.inp("av", [NK, 65])
    aqcT = cx.inp("aqcT", [2, 32, CTX])
    alam = cx.inp("alam", [1, 128]); subg = cx.inp("subg", [64, 1])
    bqT = cx.inp("bqT", [64, SEQ]); bkT = cx.inp("bkT", [65, NK]); bv = cx.inp("bv", [NK, 65])
    bqcT = cx.inp("bqcT", [64, CTX])
    bias_d = cx.inp("bias", [3, 8, 128, 512])
    ident_d = cx.inp("ident", [128, 128], BF16); onesb_d = cx.inp("onesb", [128, 128], BF16)
    E65_d = cx.inp("E65", [65, 64]); ones64_d = cx.inp("ones64", [64, 64])
    oa = cx.out("oa", [64, SEQ]); oac = cx.out("oac", [64, CTX])
    ob = cx.out("ob", [64, SEQ]); obc = cx.out("obc", [64, CTX])

    ident = cx.sb("ident", [128, 128], BF16); ones_b = cx.sb("onesb", [128, 128], BF16)
    E65 = cx.sb("E65", [65, 64]); ones64 = cx.sb("ones64", [64, 64])
    S.dma("sp", ident[:], ident_d, writes=["ident"]); S.dma("sp", ones_b[:], onesb_d, writes=["ones_b"])
    S.dma("sp", E65[:], E65_d, writes=["E65"]); S.dma("sp", ones64[:], ones64_d, writes=["ones64"])
    at = Attn(cx, ident)
    recrow = cx.sb("recrow", [65, 512]); osb = cx.sb("osb", [64, 512])
    t0 = cx.sb("t0", [64, 512]); t1 = cx.sb("t1", [64, 512]); t2 = cx.sb("t2", [64, 512])
    kfac = cx.sb("kfac", [65, 1])
    stg = [cx.sb(f"stg{i}", [128, 2048]) for i in range(2)]
    S.op("pool", lambda e: e.memset(recrow[:], 0.0), writes=["recrow"])
    lrow = cx.sb("lrow", [1, 128]); lsum = cx.sb("lsum", [1, 4]); neglam = cx.sb("neglam", [64, 1]); gsc = cx.sb("gsc", [64, 1])
    S.dma("sp", lrow[:], alam, writes=["lrow"]); S.dma("sp", gsc[:], subg, writes=["gsc"])
    S.op("pool", lambda e: e.memset(lsum[:], 0.0), writes=["lsum"])
    S.op("dve", lambda e: e.tensor_tensor(lrow[:, 0:32], lrow[:, 0:32], lrow[:, 32:64], ALU.mult), reads=["lrow"], writes=["lrow"])
    S.op("dve", lambda e: e.tensor_tensor(lrow[:, 64:96], lrow[:, 64:96], lrow[:, 96:128], ALU.mult), reads=["lrow"], writes=["lrow"])
    S.op("dve", lambda e: e.tensor_reduce(lsum[:, 0:1], lrow[:, 0:32], AX.X, ALU.add), reads=["lrow", "lsum"], writes=["lsum"])
    S.op("dve", lambda e: e.tensor_reduce(lsum[:, 1:2], lrow[:, 64:96], AX.X, ALU.add), reads=["lrow", "lsum"], writes=["lsum"])
    S.op("act", lambda e: e.activation(lsum[:, 0:2], lsum[:, 0:2], AF.Exp), reads=["lsum"], writes=["lsum"])
    S.op("dve", lambda e: e.tensor_tensor(lsum[:, 2:3], lsum[:, 1:2], lsum[:, 0:1], ALU.subtract), reads=["lsum"], writes=["lsum"])
    S.op("dve", lambda e: e.tensor_scalar(lsum[:, 2:3], lsum[:, 2:3], -lam_init, None, ALU.add), reads=["lsum"], writes=["lsum"])
    S.op("pe", lambda e: e.matmul(cx.PS[7][0:64, 0:1], ones64[0:1, 0:64], lsum[0:1, 2:3], start=True, stop=True),
         reads=["lsum", "ones64"], writes=["ps7"])
    S.op("act", lambda e: e.copy(neglam[:], cx.PS[7][0:64, 0:1]), reads=["ps7"], writes=["neglam"])
    S.op("dve", lambda e: e.tensor_scalar(gsc[:], gsc[:], 1.0 - lam_init, None, ALU.mult), reads=["gsc"], writes=["gsc"])

    with ExitStack() as st:
        QT = [cx.sb(f"aQT{c}", [33, SEQ], BF16, st) for c in range(2)]
        QTc = [cx.sb(f"aQTc{c}", [33, CTX], BF16, st) for c in range(2)]
        KT = [cx.sb(f"aKT{c}", [33, NK], BF16, st) for c in range(2)]
        Va = cx.sb("aV", [128, NK // 128, 65], BF16, st)
        sc = 32 ** -0.5
        for c in range(2):
            emit_cast_rows(cx, stg, KT[c], akT[c], 33, NK, f"akt{c}")
            emit_cast_rows(cx, stg, QT[c], aqT[c], 32, SEQ, f"aqt{c}", scale=sc)
            emit_cast_rows(cx, stg, QTc[c], aqcT[c], 32, CTX, f"aqtc{c}", scale=sc)
        emit_load_v(cx, stg, Va, av, NK, "vaug")
        for c in range(2):
            emit_kmax(cx, st, KT[c], 32, NK, kfac, ones_b, f"akt{c}")
            emit_qbound(cx, st, QT[c], 32, SEQ, kfac, ones_b, None, f"aqt{c}")
            emit_qbound(cx, st, QTc[c], 32, CTX, kfac, ones_b, None, f"aqtc{c}")

        def diff_block(qts, N, chunk_ids, odst, qkeys):
            for c in range(2):
                chunks = [(KT[c][:, k * 128:(k + 1) * 128], Va[:, k, :], None, (f"akt{c}",)) for k in chunk_ids]
                at.run(qts[c], N, chunks, cx.PS[3 + c], f"ps{3+c}", [qkeys[c], qkeys[c] + "r"])
            emit_finalize(cx, cx.PS[3], N, recrow, osb, E65, t0, "ps3", "t0")
            emit_finalize(cx, cx.PS[4], N, recrow, osb, E65, t1, "ps4", "t1")
            S.op("dve", lambda e: e.scalar_tensor_tensor(t0[:, 0:N], t1[:, 0:N], neglam[:, 0:1], t0[:, 0:N], ALU.mult, ALU.add),
                 reads=["t0", "t1", "neglam"], writes=["t0"])
            S.op("act", lambda e: e.activation(t1[:, 0:N], t0[:, 0:N], AF.Square), reads=["t0"], writes=["t1"])
            S.op("pe", lambda e: e.matmul(cx.PS[2][0:64, 0:N], ones64[:, :], t1[:, 0:N], start=True, stop=True),
                 reads=["t1", "ones64"], writes=["ps2"])
            S.op("dve", lambda e: e.tensor_scalar(t1[:, 0:N], cx.PS[2][0:64, 0:N], 1.0 / 64, EPS, ALU.mult, ALU.add),
                 reads=["ps2"], writes=["t1"])
            S.op("act", lambda e: e.sqrt(t1[:, 0:N], t1[:, 0:N]), reads=["t1"], writes=["t1"])
            S.op("dve", lambda e: e.reciprocal(t1[:, 0:N], t1[:, 0:N]), reads=["t1"], writes=["t1"])
            S.op("dve", lambda e: e.scalar_tensor_tensor(t2[:, 0:N], t0[:, 0:N], gsc[:, 0:1], t1[:, 0:N], ALU.mult, ALU.mult),
                 reads=["t0", "t1", "gsc"], writes=["t2"])
            S.dma("poolq", odst, t2[:, 0:N], reads=["t2"], writes=["oa"])

        for qb in range(SEQ // 512):
            sl = slice(qb * 512, (qb + 1) * 512)
            diff_block([QT[0][:, sl], QT[1][:, sl]], 512, range(NK // 128), oa[:, sl], ["aqt0", "aqt1"])
        diff_block([QTc[0][:, :], QTc[1][:, :]], CTX, range(SEQ // 128, NK // 128), oac[:, :], ["aqtc0", "aqtc1"])
        S.barrier()

    with ExitStack() as st:
        QT = cx.sb("bQT", [65, SEQ], BF16, st); QTc = cx.sb("bQTc", [65, CTX], BF16, st)
        KT = cx.sb("bKT", [65, NK], BF16, st); Vb = cx.sb("bV", [128, NK // 128, 65], BF16, st)
        bias = cx.sb("bias", [128, 3, 8, 512], BF16, st)
        sc = 64 ** -0.5
        emit_cast_rows(cx, stg, KT, bkT, 65, NK, "bkt")
        emit_cast_rows(cx, stg, QT, bqT, 64, SEQ, "bqt", scale=sc)
        emit_cast_rows(cx, stg, QTc, bqcT, 64, CTX, "bqtc", scale=sc)
        emit_load_v(cx, stg, Vb, bv, NK, "vaug")
        for s_ in range(3):
            for j in range(8):
                i = s_ * 8 + j
                S.dma(("sp", "poolq")[i % 2], stg[i % 2][:, 0:512], bias_d[s_, j], writes=[f"stg{i%2}"])
                S.op("dve", lambda e, i=i, s_=s_, j=j: e.tensor_copy(bias[:, s_, j, :], stg[i % 2][:, 0:512]),
                     reads=[f"stg{i%2}"], writes=["bias"])
        emit_kmax(cx, st, KT, 64, NK, kfac, ones_b, "bkt")
        emit_qbound(cx, st, QT, 64, SEQ, kfac, ones_b, None, "bqt")
        emit_qbound(cx, st, QTc, 64, CTX, kfac, ones_b, None, "bqtc")
        for qb in range(32):
            R0 = qb * 8
            if qb == 0:
                bset, kr0 = 0, 0
            elif qb == 31:
                bset, kr0 = 2, 240
            else:
                bset, kr0 = 1, R0 - 4
            sl = slice(qb * 512, (qb + 1) * 512)
            chunks = [(KT[:, (kr0 // 2 + j) * 128:(kr0 // 2 + j + 1) * 128], Vb[:, kr0 // 2 + j, :], bias[:, bset, j, :], ("bkt",))
                      for j in range(8)]
            chunks += [(KT[:, k * 128:(k + 1) * 128], Vb[:, k, :], None, ("bkt",)) for k in range(SEQ // 128, NK // 128)]
            at.run(QT[:, sl], 512, chunks, cx.PS[3], "ps3", ["bqt", "bqtr"])
            emit_finalize(cx, cx.PS[3], 512, recrow, osb, E65, t0, "ps3", "t0")
            S.dma("poolq", ob[:, sl], t0[:, 0:512], reads=["t0"], writes=["ob"])
        chunks = [(KT[:, k * 128:(k + 1) * 128], Vb[:, k, :], None, ("bkt",)) for k in range(SEQ // 128, NK // 128)]
        at.run(QTc[:, :], CTX, chunks, cx.PS[3], "ps3", ["bqtc", "bqtcr"])
        emit_finalize(cx, cx.PS[3], CTX, recrow, osb, E65, t0, "ps3", "t0")
        S.dma("poolq", obc[:, :], t0[:, 0:CTX], reads=["t0"], writes=["obc"])
        S.barrier()
    cx.end_phase()


def na_bias_sets(rpb_h):
    out = np.full((3, 8, 128, 512), -30000.0, np.float32)
    cq = np.arange(64); ck = np.arange(64)
    c0 = np.clip(cq - 8, 0, 48)
    colok = (ck[:, None] >= c0[None, :]) & (ck[:, None] < c0[None, :] + 16)
    dc = np.clip(ck[:, None] - cq[None, :], -15, 15) + 15
    for s_, (R0, kr0) in enumerate([(0, 0), (8, 4), (248, 240)]):
        for j in range(8):
            for krl in range(2):
                kr = kr0 + 2 * j + krl
                for qrl in range(8):
                    r = R0 + qrl
                    r0 = min(max(r - 4, 0), 248)
                    if not (r0 <= kr < r0 + 8):
                        continue
                    dr = kr - r + 7
                    blk = np.where(colok, rpb_h[dr][dc], np.float32(-30000.0))
                    out[s_, j, krl * 64:(krl + 1) * 64, qrl * 64:(qrl + 1) * 64] = blk
    return out


def emit_C(cx, with_ctx, final):
    S = cx.S
    xl = cx.inp("xl", [TPC, D]); xc = cx.inp("xc", [CPC, D])
    rs_out = cx.inp("rs_out", [TPC + CPC, D])
    cT = cx.inp("cT", [128, 8, 2])
    ada_w = cx.inp("ada_w", [D, 6 * D]); ada_b = cx.inp("ada_b", [1, 6 * D])
    norm_g = cx.inp("norm2_g", [1, D]); fin_g = cx.inp("fin_g", [1, D])
    router_w = cx.inp("router_w", [D, 16]); router_b = cx.inp("router_b", [1, 16])
    w1 = cx.inp("w1", [16, D, 512]); w3 = cx.inp("w3", [16, D, 512]); w2 = cx.inp("w2", [16, 512, D])
    sel_d = cx.inp("sel", [2, 2, 128]); identf_d = cx.inp("ident", [128, 128], BF16)
    ol = cx.out("ol", [TPC, D]); oc = cx.out("oc", [CPC, D])

    sel = cx.sb("sel", [2, 2, 128]); identf = cx.sb("identb", [128, 128], BF16)
    rwh = cx.sb("rwh", [128, 8, 16], BF16); rwl = cx.sb("rwl", [128, 8, 16], BF16)
    G1 = cx.sb("G1", [128, D]); G2 = cx.sb("G2", [128, D]); SH2 = cx.sb("SH2", [128, D]); GG2 = cx.sb("GG2", [128, D])
    FG = cx.sb("FG", [128, D])
    rw = cx.sb("rw", [128, 8, 16]); rb = cx.sb("rb", [128, 16])
    bcs = cx.scratch("bc_scr", [4, 128, D])
    S.dma("sp", sel[:], sel_d, writes=["sel"]); S.dma("sp", identf[:], identf_d, writes=["identf"])
    S.dma("sp", rw[:], router_w.rearrange("(kc p) n -> p kc n", p=128), writes=["rw"])
    S.dma("sp", rb[:], router_b.partition_broadcast(128), writes=["rb"])
    S.op("dve", lambda e: e.tensor_copy(rwh[:], rw[:]), reads=["rw"], writes=["rwh"])
    S.op("dve", lambda e: e.tensor_tensor(rwl[:], rw[:], rwh[:], ALU.subtract), reads=["rw", "rwh"], writes=["rwl"])
    with ExitStack() as st:
        scT = cx.sb("scT", [128, 8, 2], F32, st); modrow = cx.sb("modrow", [2, 6 * D], F32, st)
        normg2 = cx.sb("normg2", [2, D], F32, st); grow = cx.sb("grow", [2, D], F32, st); fing2 = cx.sb("fing2", [2, D], F32, st)
        S.dma("sp", scT[:], cT, writes=["scT"])
        for r in range(2):
            S.dma("sp", normg2[r:r + 1, :], norm_g, writes=["normg2"])
            S.dma("sp", fing2[r:r + 1, :], fin_g, writes=["fing2"])
        S.op("act", lambda e: e.activation(scT[:], scT[:], AF.Silu), reads=["scT"], writes=["scT"])
        with ExitStack() as st2:
            emit_mod_rows(cx, st2, scT, ada_w, ada_b, modrow, "C")
            S.barrier()
        S.op("dve", lambda e: e.scalar_tensor_tensor(grow[:], modrow[:, 4 * D:5 * D], 1.0, normg2[:], ALU.add, ALU.mult),
             reads=["modrow", "normg2"], writes=["grow"])

        def set_bcast(which):
            emit_bcast_row(cx, G1, modrow[:, 2 * D:3 * D], sel, which, (0, 1), "modrow", "G1")
            emit_bcast_row(cx, G2, grow, sel, which, (0, 1), "grow", "G2")
            emit_bcast_row(cx, SH2, modrow[:, 3 * D:4 * D], sel, which, (0, 1), "modrow", "SH2")
            emit_bcast_row(cx, GG2, modrow[:, 5 * D:6 * D], sel, which, (0, 1), "modrow", "GG2")
        set_bcast(1)
        for i_, (t_, k_) in enumerate(((G1, "G1"), (G2, "G2"), (SH2, "SH2"), (GG2, "GG2"))):
            S.dma("sp", bcs[i_], t_[:], reads=[k_], writes=["bcs"])
        set_bcast(0)
        emit_bcast_row(cx, FG, fing2, sel, 0, (0, 1), "fing2", "FG")
        S.barrier()

    def load_ctx_bcast():
        for i_, (t_, k_) in enumerate(((G1, "G1"), (G2, "G2"), (SH2, "SH2"), (GG2, "GG2"))):
            S.dma("sp", t_[:], bcs[i_], reads=["bcs"], writes=[k_])

    GT = 8
    x1 = cx.sb("x1", [128, GT, D]); yacc = cx.sb("yacc", [128, GT, D])
    hT = cx.sb("hT", [128, 8, GT * 128], BF16)
    gate = cx.sb("gate", [128, GT, 16])
    xt = [cx.sb(f"xt{i}", [128, D]) for i in range(2)]
    mpt = [cx.sb("mpt0", [128, D])] * 2
    junk = cx.sb("junk", [128, D], BF16); tmp = cx.sb("tmp", [128, D]); h2 = cx.sb("h2", [128, D])
    hTl = cx.sb("hTl", [128, 8, 128], BF16); h2hi = cx.sb("h2hi", [128, D], BF16); h2lo = cx.sb("h2lo", [128, D], BF16)
    ss = cx.sb("ss", [128, 1]); rstd = cx.sb("rstd", [128, 1])
    r_ = {k: cx.sb("r_" + k, [128, 16]) for k in ("ex", "sc", "sel", "eq", "s2", "selm", "k1", "sm2", "k2", "w")}
    q_ = {k: cx.sb("q_" + k, [128, 4]) for k in ("m1", "m2", "gs", "gmask", "pen")}
    c_ = {k: cx.sb("c_" + k, [128, 1]) for k in ("mx", "sm", "gm", "e1", "e2", "ws")}
    W13 = [cx.sb(f"W13_{i}", [128, 2, 8, 512], BF16) for i in range(2)]
    W2 = [cx.sb(f"W2_{i}", [128, 4, D], BF16) for i in range(2)]
    hs = cx.sb("hs", [128, 512]); hh = [cx.sb(f"hh{i}", [128, 4, 512], BF16) for i in range(2)]
    stage_n = [0]
    conv_st = ExitStack()
    wstg = [cx.sb(f"wstg{i}", [128, 4, 512], F32, conv_st) for i in range(2)]

    def router(P, g):
        lg = cx.PS[2]
        n_ = 0
        for (lhs, rhs) in (("hi", rwh), ("lo", rwh), ("hi", rwl)):
            for kc in range(8):
                lt = hT[:, kc, g * 128:g * 128 + P] if lhs == "hi" else hTl[:, kc, 0:P]
                S.op("pe", lambda e, lt=lt, rhs=rhs, kc=kc, n_=n_: e.matmul(lg[0:P, 0:16], lt, rhs[:, kc, :], start=(n_ == 0), stop=(n_ == 23)),
                     reads=["hT", "hTl", "rwh", "rwl"], writes=["ps2"])
                n_ += 1
        V = lambda k: r_[k][0:P, :]
        V4 = lambda k: r_[k][0:P, :].rearrange("p (g e) -> p g e", e=4)
        Q = lambda k: q_[k][0:P, :]
        Cc = lambda k: c_[k][0:P, :]
        dv = lambda fn, rd, wr: S.op("dve", fn, reads=rd, writes=wr)
        dv(lambda e: e.tensor_reduce(Cc("mx"), lg[0:P, 0:16], AX.X, ALU.max), ["ps2"], ["c_mx"])
        dv(lambda e: e.tensor_scalar(Cc("mx"), Cc("mx"), -1.0, None, ALU.mult), ["c_mx"], ["c_mx"])
        S.op("pool", lambda e: e.memset(Cc("sm"), 0.0), writes=["c_sm"])
        S.op("act", lambda e: e.activation(V("ex"), lg[0:P, 0:16], AF.Exp, bias=Cc("mx"), accum_out=Cc("sm")),
             reads=["ps2", "c_mx", "c_sm"], writes=["r_ex", "c_sm"])
        dv(lambda e: e.reciprocal(Cc("sm"), Cc("sm")), ["c_sm"], ["c_sm"])
        dv(lambda e: e.tensor_scalar(V("sc"), V("ex"), Cc("sm"), None, ALU.mult), ["r_ex", "c_sm"], ["r_sc"])
        dv(lambda e: e.tensor_tensor(V("sel"), V("sc"), rb[0:P, :], ALU.add), ["r_sc", "rb"], ["r_sel"])
        dv(lambda e: e.tensor_reduce(Q("m1"), V4("sel"), AX.X, ALU.max), ["r_sel"], ["q_m1"])
        dv(lambda e: e.tensor_tensor(V4("eq"), V4("sel"), Q("m1").unsqueeze(2).to_broadcast([P, 4, 4]), ALU.is_equal), ["r_sel", "q_m1"], ["r_eq"])
        dv(lambda e: e.scalar_tensor_tensor(V("s2"), V("eq"), -1e9, V("sel"), ALU.mult, ALU.add), ["r_eq", "r_sel"], ["r_s2"])
        dv(lambda e: e.tensor_reduce(Q("m2"), V4("s2"), AX.X, ALU.max), ["r_s2"], ["q_m2"])
        dv(lambda e: e.tensor_tensor(Q("gs"), Q("m1"), Q("m2"), ALU.add), ["q_m1", "q_m2"], ["q_gs"])
        dv(lambda e: e.tensor_reduce(Cc("gm"), Q("gs"), AX.X, ALU.max), ["q_gs"], ["c_gm"])
        dv(lambda e: e.tensor_scalar(Q("gmask"), Q("gs"), Cc("gm"), None, ALU.is_equal), ["q_gs", "c_gm"], ["q_gmask"])
        dv(lambda e: e.tensor_scalar(Q("pen"), Q("gmask"), -1.0, 1e9, ALU.add, ALU.mult), ["q_gmask"], ["q_pen"])
        dv(lambda e: e.tensor_tensor(V4("selm"), V4("sel"), Q("pen").unsqueeze(2).to_broadcast([P, 4, 4]), ALU.add), ["r_sel", "q_pen"], ["r_selm"])
        dv(lambda e: e.tensor_reduce(Cc("e1"), V("selm"), AX.X, ALU.max), ["r_selm"], ["c_e1"])
        dv(lambda e: e.tensor_scalar(V("k1"), V("selm"), Cc("e1"), None, ALU.is_equal), ["r_selm", "c_e1"], ["r_k1"])
        dv(lambda e: e.scalar_tensor_tensor(V("sm2"), V("k1"), -1e9, V("selm"), ALU.mult, ALU.add), ["r_k1", "r_selm"], ["r_sm2"])
        dv(lambda e: e.tensor_reduce(Cc("e2"), V("sm2"), AX.X, ALU.max), ["r_sm2"], ["c_e2"])
        dv(lambda e: e.tensor_scalar(V("k2"), V("sm2"), Cc("e2"), None, ALU.is_equal), ["r_sm2", "c_e2"], ["r_k2"])
        dv(lambda e: e.tensor_tensor(V("k1"), V("k1"), V("k2"), ALU.add), ["r_k1", "r_k2"], ["r_k1"])
        dv(lambda e: e.tensor_tensor(V("w"), V("sc"), V("k1"), ALU.mult), ["r_sc", "r_k1"], ["r_w"])
        dv(lambda e: e.tensor_reduce(Cc("ws"), V("w"), AX.X, ALU.add), ["r_w"], ["c_ws"])
        dv(lambda e: e.reciprocal(Cc("ws"), Cc("ws")), ["c_ws"], ["c_ws"])
        dv(lambda e: e.tensor_scalar(gate[0:P, g, :], V("w"), Cc("ws"), None, ALU.mult), ["r_w", "c_ws"], ["gate"])

    wb13 = cx.scratch("wb13", [16, 128, 2 * 8 * 512], BF16)
    wb2 = cx.scratch("wb2", [16, 128, 4 * D], BF16)

    def convert_expert(e_, slot):
        pieces = []
        for wi, wsrc in enumerate((w1, w3)):
            v = wsrc[e_].rearrange("(kc p) n -> p kc n", p=128)
            for hf in range(2):
                pieces.append((v[:, hf * 4:(hf + 1) * 4, :], W13[slot][:, wi, hf * 4:(hf + 1) * 4, :]))
        v2 = w2[e_].rearrange("(fc p) n -> p fc n", p=128)
        for hf in range(2):
            pieces.append((v2[:, :, hf * 512:(hf + 1) * 512], W2[slot][:, :, hf * 512:(hf + 1) * 512]))
        for src, dst in pieces:
            n = stage_n[0]; stage_n[0] += 1
            sg = wstg[n % 2]
            S.dma(("sp", "actq")[n % 2], sg[:], src, writes=[f"wstg{n%2}"])
            S.op(("pool", "dve")[n % 2], lambda e, sg=sg, dst=dst: e.tensor_copy(dst, sg[:]), reads=[f"wstg{n%2}"], writes=[f"W{slot}"])
        S.dma("poolq", wb13[e_], W13[slot][:].rearrange("p a b c -> p (a b c)"), reads=[f"W{slot}"], writes=["wb"])
        S.dma("poolq", wb2[e_], W2[slot][:].rearrange("p a b -> p (a b)"), reads=[f"W{slot}"], writes=["wb"])

    for e_ in range(16):
        convert_expert(e_, e_ % 2)
    S.barrier()
    conv_st.close()

    def load_expert(e_, slot):
        S.dma("sp", W13[slot][:].rearrange("p a b c -> p (a b c)"), wb13[e_], reads=["wb"], writes=[f"W{slot}"])
        S.dma("actq", W2[slot][:].rearrange("p a b -> p (a b)"), wb2[e_], reads=["wb"], writes=[f"W{slot}"])

    ntl = TPC // 128
    groups = [[("l", g * GT + t) for t in range(GT)] for g in range(ntl // GT)]
    if with_ctx:
        groups.append([("c", 0)])
    ti_glob = 0
    eload = 0
    for gi, grp in enumerate(groups):
        isctx = grp[0][0] == "c"
        if isctx:
            load_ctx_bcast()
        P = CPC if isctx else 128
        NT = len(grp) * 128 if not isctx else CPC
        for g, (kind, i) in enumerate(grp):
            s = ti_glob % 2; ti_glob += 1
            xsrc = xc if isctx else xl[i * 128:(i + 1) * 128, :]
            msrc = rs_out[TPC:TPC + CPC, :] if isctx else rs_out[i * 128:(i + 1) * 128, :]
            S.dma("sp", xt[s][0:P, :], xsrc, writes=[f"xt{s}"])
            S.dma("actq", mpt[s][0:P, :], msrc, writes=["mpt"])
            S.op("dve", lambda e, s=s, P=P: e.tensor_tensor(tmp[0:P, :], mpt[s][0:P, :], G1[0:P, :], ALU.mult),
                 reads=["mpt", "G1"], writes=["tmp"])
            S.op("dve", lambda e, s=s, g=g, P=P: e.tensor_tensor(x1[0:P, g, :], tmp[0:P, :], xt[s][0:P, :], ALU.add),
                 reads=["tmp", f"xt{s}"], writes=["x1"])
            S.op("pool", lambda e, P=P: e.memset(ss[0:P, :], 0.0), writes=["ss"])
            S.op("act", lambda e, g=g, P=P: e.activation(junk[0:P, :], x1[0:P, g, :], AF.Square, accum_out=ss[0:P, :]),
                 reads=["x1", "ss"], writes=["ss", "junk"])
            S.op("dve", lambda e, P=P: e.tensor_scalar(rstd[0:P, :], ss[0:P, :], 1.0 / D, EPS, ALU.mult, ALU.add), reads=["ss"], writes=["rstd"])
            S.op("act", lambda e, P=P: e.sqrt(rstd[0:P, :], rstd[0:P, :]), reads=["rstd"], writes=["rstd"])
            S.op("dve", lambda e, P=P: e.reciprocal(rstd[0:P, :], rstd[0:P, :]), reads=["rstd"], writes=["rstd"])
            S.op("dve", lambda e, g=g, P=P: e.scalar_tensor_tensor(tmp[0:P, :], x1[0:P, g, :], rstd[0:P, :], G2[0:P, :], ALU.mult, ALU.mult),
                 reads=["x1", "rstd", "G2"], writes=["tmp"])
            S.op("dve", lambda e, P=P: e.tensor_tensor(h2[0:P, :], tmp[0:P, :], SH2[0:P, :], ALU.add), reads=["tmp", "SH2"], writes=["h2"])
            S.op("dve", lambda e, P=P: e.tensor_copy(h2hi[0:P, :], h2[0:P, :]), reads=["h2"], writes=["h2hi"])
            S.op("dve", lambda e, P=P: e.tensor_tensor(h2lo[0:P, :], h2[0:P, :], h2hi[0:P, :], ALU.subtract), reads=["h2", "h2hi"], writes=["h2lo"])
            for (src, skey, dstv, dkey, bank) in ((h2hi, "h2hi", hT[:, :, g * 128:g * 128 + P], "hT", 3), (h2lo, "h2lo", hTl[:, :, 0:P], "hTl", 4)):
                psv = cx.PS[bank][:].bitcast(BF16).rearrange("p (k t) -> p k t", t=128)
                for kc in range(8):
                    S.op("pe", lambda e, psv=psv, kc=kc, src=src, P=P: e.transpose(psv[:, kc, 0:P], src[0:P, kc * 128:(kc + 1) * 128], identf[0:P, 0:P]),
                         reads=[skey, "identf"], writes=[f"ps{bank}"])
                S.op("act", lambda e, psv=psv, dstv=dstv, P=P: e.copy(dstv, psv[:, :, 0:P]), reads=[f"ps{bank}"], writes=[dkey])
            router(P, g)
            S.op("pool", lambda e, g=g, P=P: e.memset(yacc[0:P, g, :], 0.0), writes=["yacc"])
        for e_ in range(16):
            slot = eload % 2; eload += 1
            load_expert(e_, slot)
            for c0 in range(0, NT, 512):
                w = min(512, NT - c0)
                hsl = hh[(c0 // 512) % 2]
                for fc in range(4):
                    p1 = cx.PS[4 + (fc % 2) * 2]; p3 = cx.PS[5 + (fc % 2) * 2]
                    k1 = f"ps{4 + (fc % 2) * 2}"; k3 = f"ps{5 + (fc % 2) * 2}"
                    for wi, (pp, pk) in enumerate(((p1, k1), (p3, k3))):
                        for kc in range(8):
                            S.op("pe", lambda e, pp=pp, wi=wi, kc=kc, fc=fc, slot=slot, c0=c0, w=w: e.matmul(
                                pp[:, 0:w], W13[slot][:, wi, kc, fc * 128:(fc + 1) * 128], hT[:, kc, c0:c0 + w], start=(kc == 0), stop=(kc == 7)),
                                reads=[f"W{slot}", "hT"], writes=[pk])
                    S.op("act", lambda e, p1=p1, w=w: e.activation(hs[:, 0:w], p1[:, 0:w], AF.Silu), reads=[k1], writes=["hs"])
                    S.op("dve", lambda e, p3=p3, w=w, fc=fc, hsl=hsl: e.tensor_tensor(hsl[:, fc, 0:w], hs[:, 0:w], p3[:, 0:w], ALU.mult),
                         reads=["hs", k3], writes=[f"hh{(c0//512)%2}"])
                for t0_ in range(0, w, 128):
                    pw = min(128, w - t0_)
                    g = (c0 + t0_) // 128
                    for hb_ in range(2):
                        ps = cx.PS[hb_]
                        for fc in range(4):
                            S.op("pe", lambda e, ps=ps, fc=fc, hb_=hb_, slot=slot, t0_=t0_, pw=pw, hsl=hsl: e.matmul(
                                ps[0:pw, :], hsl[:, fc, t0_:t0_ + pw], W2[slot][:, fc, hb_ * 512:(hb_ + 1) * 512], start=(fc == 0), stop=(fc == 3)),
                                reads=[f"hh{(c0//512)%2}", f"W{slot}"], writes=[f"ps{hb_}"])
                        cs = slice(hb_ * 512, (hb_ + 1) * 512)
                        S.op("dve", lambda e, ps=ps, cs=cs, g=g, pw=pw, e_=e_: e.scalar_tensor_tensor(
                            yacc[0:pw, g, cs], ps[0:pw, :], gate[0:pw, g, e_:e_ + 1], yacc[0:pw, g, cs], ALU.mult, ALU.add),
                            reads=[f"ps{hb_}", "gate", "yacc"], writes=["yacc"])
        for g, (kind, i) in enumerate(grp):
            S.op("dve", lambda e, g=g, P=P: e.tensor_tensor(tmp[0:P, :], yacc[0:P, g, :], GG2[0:P, :], ALU.mult), reads=["yacc", "GG2"], writes=["tmp"])
            S.op("dve", lambda e, g=g, P=P: e.tensor_tensor(x1[0:P, g, :], x1[0:P, g, :], tmp[0:P, :], ALU.add), reads=["tmp", "x1"], writes=["x1"])
            dst = oc if isctx else ol[i * 128:(i + 1) * 128, :]
            if final:
                S.op("pool", lambda e, P=P: e.memset(ss[0:P, :], 0.0), writes=["ss"])
                S.op("act", lambda e, g=g, P=P: e.activation(junk[0:P, :], x1[0:P, g, :], AF.Square, accum_out=ss[0:P, :]),
                     reads=["x1", "ss"], writes=["ss", "junk"])
                S.op("dve", lambda e, P=P: e.tensor_scalar(rstd[0:P, :], ss[0:P, :], 1.0 / D, EPS, ALU.mult, ALU.add), reads=["ss"], writes=["rstd"])
                S.op("act", lambda e, P=P: e.sqrt(rstd[0:P, :], rstd[0:P, :]), reads=["rstd"], writes=["rstd"])
                S.op("dve", lambda e, P=P: e.reciprocal(rstd[0:P, :], rstd[0:P, :]), reads=["rstd"], writes=["rstd"])
                S.op("dve", lambda e, g=g, P=P: e.scalar_tensor_tensor(h2[0:P, :], x1[0:P, g, :], rstd[0:P, :], FG[0:P, :], ALU.mult, ALU.mult),
                     reads=["x1", "rstd", "FG"], writes=["h2"])
                S.dma("poolq", dst, h2[0:P, :], reads=["h2"], writes=["ol"])
            else:
                S.dma("poolq", dst, x1[0:P, g, :], reads=["x1"], writes=["ol"])
    if not with_ctx:
        S.dma("sp", xt[0][0:CPC, :], xc, writes=["xt0"])
        S.dma("sp", oc, xt[0][0:CPC, :], reads=["xt0"], writes=["oc"])
    cx.end_phase()


def fft_consts():
    N = 32768
    n1 = np.arange(64)[:, None]; k1 = np.arange(128)[None, :]
    a = 2 * np.pi * n1 * k1 / 128
    F1cat = np.concatenate([np.cos(a), -np.sin(a)], 1)
    n2 = np.arange(256)[:, None]
    t = 2 * np.pi * n2 * k1 / N
    Tr, Ti = np.cos(t), -np.sin(t)
    k2 = np.arange(256)[None, :]
    b = 2 * np.pi * n2 * k2 / 256
    F2r, F2i = np.cos(b), -np.sin(b)
    Er, Ei = np.cos(b.T), np.sin(b.T)
    IA = np.concatenate([Er, Ei], 1); IB = np.concatenate([-Ei, Er], 1)
    ITr, ITi = np.cos(t.T), np.sin(t.T)
    c = 2 * np.pi * np.arange(128)[:, None] * np.arange(64)[None, :] / 128
    G1r, G1i = np.cos(c) / N, -np.sin(c) / N
    ch2 = lambda m: np.ascontiguousarray(m.reshape(2, 128, m.shape[1]).transpose(1, 0, 2))
    psm = (np.arange(128)[:, None] % 64 == np.arange(128)[None, :] % 64).astype(np.float32)
    return {"F1cat": _bf16(F1cat), "Tr": ch2(Tr).astype(np.float32), "Ti": ch2(Ti).astype(np.float32),
            "F2r": _bf16(ch2(F2r)), "F2i": _bf16(ch2(F2i)), "F2in": _bf16(ch2(-F2i)),
            "IA": _bf16(ch2(IA)), "IB": _bf16(ch2(IB)), "ITr": ITr.astype(np.float32), "ITi": ITi.astype(np.float32),
            "G1r": _bf16(G1r), "G1i": _bf16(G1i), "PSM": psm}


def hy_pos_consts(L):
    t = np.arange(L, dtype=np.float32)
    t_norm = t / max(L - 1, 1)
    bands = np.linspace(1e-4, 15, 16, dtype=np.float32)
    ang = (np.float32(2.0 * math.pi / L) * t[:, None] * bands[None, :]).astype(np.float32)
    z = np.concatenate([t_norm[:, None], np.cos(ang), np.sin(ang)], axis=-1).astype(np.float32)
    return np.ascontiguousarray(z.T), np.ascontiguousarray(np.broadcast_to(t_norm[None, :], (128, L))).astype(np.float32)


def emit_B2(cx, with_ctx):
    L = SEQ
    PI = math.pi
    S = cx.S
    paths = [("l", SEQ)] + ([("c", CTX)] if with_ctx else [])
    I = {}
    for tag, Le in paths:
        I[tag] = dict(hy=cx.inp(f"hy_{tag}", [3, 64, Le + 2]), zT=cx.inp(f"zT_{tag}", [33, Le]), tn=cx.inp(f"tn_{tag}", [128, Le]),
                      pl=cx.inp(f"pl_{tag}", [64, Le + 24]), icnt=cx.inp(f"icnt_{tag}", [64, Le]),
                      oh=cx.out(f"oh_{tag}", [64, Le]), op=cx.out(f"op_{tag}", [64, Le]))
    shw_d = cx.inp("shw", [64, 3, 3]); shb_d = cx.inp("shb", [64, 3])
    fw1_d = cx.inp("fw1", [33, 64]); fb1_d = cx.inp("fb1", [64, 1]); fw2_d = cx.inp("fw2", [64, 64]); fb2_d = cx.inp("fb2", [64, 1])
    fw3_d = cx.inp("fw3", [64, 256]); fb3_d = cx.inp("fb3", [128, 2]); ndel_d = cx.inp("ndel", [128, 1])
    dsk_d = cx.inp("dsk", [64, 2, 64])
    psel_d = cx.inp("psel", [64, 4]); pw_d = cx.inp("pw", [64, 64]); psc_d = cx.inp("psc", [64, 1])
    FC = fft_consts()
    cd = {k: cx.inp("c_" + k, list(v.shape), BF16 if v.dtype != np.float32 else F32) for k, v in FC.items()}
    c = {k: cx.sb("c_" + k, list(v.shape), BF16 if v.dtype != np.float32 else F32) for k, v in FC.items()}
    for k in FC:
        S.dma("sp", c[k][:], cd[k], writes=["c_" + k])
    ck = ["c_" + k for k in FC]
    small = {}
    for nm, d_, shp in (("shw", shw_d, [64, 3, 3]), ("shb", shb_d, [64, 3]), ("fw1", fw1_d, [33, 64]), ("fb1", fb1_d, [64, 1]),
                        ("fw2", fw2_d, [64, 64]), ("fb2", fb2_d, [64, 1]), ("fw3", fw3_d, [64, 256]), ("fb3", fb3_d, [128, 2]),
                        ("ndel", ndel_d, [128, 1]), ("dsk", dsk_d, [64, 2, 64]), ("psel", psel_d, [64, 4]), ("pw", pw_d, [64, 64]),
                        ("psc", psc_d, [64, 1])):
        small[nm] = cx.sb("w_" + nm, shp)
        S.dma("sp", small[nm][:], d_, writes=["w_" + nm])
    pwb = cx.sb("pwb", [64, 64], BF16)
    S.op("dve", lambda e: e.tensor_copy(pwb[:], small["pw"][:]), reads=["w_pw"], writes=["pwb"])
    scx = cx.scratch("scx", [3, 64, L]); filt_s = cx.scratch("filt_s", [2, 128, L])
    zero = cx.sb("zero", [128, 2048])
    S.op("pool", lambda e: e.memset(zero[:], 0.0), writes=["zero"])

    def do_path(tag, Le):
        io = I[tag]
        CH = min(2048, Le)
        with ExitStack() as st:
            pin = cx.sb("pin", [64, CH + 24], F32, st); A2 = cx.sb("A2", [64, CH + 24], F32, st); A4 = cx.sb("A4", [64, CH + 24], F32, st)
            A8 = cx.sb("A8", [64, CH + 24], F32, st); A16 = cx.sb("A16", [64, CH + 24], F32, st)
            acc = cx.sb("pacc", [64, CH], F32, st); ic = cx.sb("pic", [64, CH], F32, st); pd = cx.sb("pd", [64, CH], BF16, st)
            po = cx.sb("po", [64, CH], F32, st)
            for c0 in range(0, Le, CH):
                S.dma("sp", pin[:], io["pl"][:, c0:c0 + CH + 24], writes=["pin"])
                S.dma("sp", ic[:], io["icnt"][:, c0:c0 + CH], writes=["pic"])
                W_ = CH + 24
                S.op("dve", lambda e: e.tensor_tensor(A2[:, 0:W_ - 1], pin[:, 0:W_ - 1], pin[:, 1:W_], ALU.add), reads=["pin"], writes=["A2"])
                S.op("dve", lambda e: e.tensor_tensor(A4[:, 0:W_ - 3], A2[:, 0:W_ - 3], A2[:, 2:W_ - 1], ALU.add), reads=["A2"], writes=["A4"])
                S.op("dve", lambda e: e.tensor_tensor(A8[:, 0:W_ - 7], A4[:, 0:W_ - 7], A4[:, 4:W_ - 3], ALU.add), reads=["A4"], writes=["A8"])
                S.op("dve", lambda e: e.tensor_tensor(A16[:, 0:W_ - 15], A8[:, 0:W_ - 15], A8[:, 8:W_ - 7], ALU.add), reads=["A8"], writes=["A16"])
                S.op("dve", lambda e: e.tensor_scalar(acc[:], A2[:, 7:7 + CH], small["psel"][:, 0:1], None, ALU.mult), reads=["A2", "w_psel"], writes=["pacc"])
                for k_, (Aw, off, key) in enumerate(((A4, 6, "A4"), (A8, 4, "A8"), (A16, 0, "A16"))):
                    S.op("dve", lambda e, Aw=Aw, off=off, k_=k_: e.scalar_tensor_tensor(acc[:], Aw[:, off:off + CH], small["psel"][:, k_ + 1:k_ + 2], acc[:],
                                                                                        ALU.mult, ALU.add), reads=[key, "w_psel", "pacc"], writes=["pacc"])
                S.op("dve", lambda e: e.tensor_tensor(acc[:], acc[:], ic[:], ALU.mult), reads=["pacc", "pic"], writes=["pacc"])
                S.op("dve", lambda e: e.tensor_tensor(pd[:], acc[:], pin[:, 8:8 + CH], ALU.subtract), reads=["pacc", "pin"], writes=["pd"])
                for s0 in range(0, CH, 512):
                    w = min(512, CH - s0)
                    S.op("pe", lambda e, s0=s0, w=w: e.matmul(cx.PS[7][0:64, 0:w], pwb[:, :], pd[:, s0:s0 + w], start=True, stop=True),
                         reads=["pd", "pwb"], writes=["ps7"])
                    S.op("dve", lambda e, s0=s0, w=w: e.tensor_scalar(po[:, s0:s0 + w], cx.PS[7][0:64, 0:w], small["psc"][:, 0:1], None, ALU.mult),
                         reads=["ps7", "w_psc"], writes=["po"])
                S.dma("poolq", io["op"][:, c0:c0 + CH], po[:], reads=["po"], writes=["op"])
            S.barrier()

        with ExitStack() as st:
            hin = cx.sb("hin", [64, CH + 2], F32, st); ho = cx.sb("ho", [64, CH], F32, st)
            if Le < L:
                for p in range(3):
                    for c0 in range(0, L, 2048):
                        S.dma("sp", scx[p][:, c0:c0 + 2048], zero[0:64, :], reads=["zero"], writes=["scx"])
                for oc in range(2):
                    for c0 in range(0, L, 2048):
                        S.dma("sp", filt_s[oc][:, c0:c0 + 2048], zero[:, :], reads=["zero"], writes=["filt_s"])
            for p in range(3):
                for c0 in range(0, Le, CH):
                    S.dma("sp", hin[:], io["hy"][p][:, c0:c0 + CH + 2], writes=["hin"])
                    S.op("dve", lambda e, p=p: e.tensor_scalar(ho[:], hin[:, 1:CH + 1], small["shw"][:, p, 1:2], small["shb"][:, p:p + 1], ALU.mult, ALU.add),
                         reads=["hin", "w_shw", "w_shb"], writes=["ho"])
                    S.op("dve", lambda e, p=p: e.scalar_tensor_tensor(ho[:], hin[:, 0:CH], small["shw"][:, p, 0:1], ho[:], ALU.mult, ALU.add),
                         reads=["hin", "w_shw", "ho"], writes=["ho"])
                    S.op("dve", lambda e, p=p: e.scalar_tensor_tensor(ho[:], hin[:, 2:CH + 2], small["shw"][:, p, 2:3], ho[:], ALU.mult, ALU.add),
                         reads=["hin", "w_shw", "ho"], writes=["ho"])
                    S.dma("poolq", scx[p][:, c0:c0 + CH], ho[:], reads=["ho"], writes=["scx"])
            S.barrier()

        with ExitStack() as st:
            FW = min(512, Le)
            nfc = Le // FW
            zt = cx.sb("zt", [33, FW], F32, st); tnt = cx.sb("tnt", [128, FW], F32, st)
            pre = cx.sb("pre", [64, FW], F32, st); msk = cx.sb("msk", [64, FW], F32, st); h1 = cx.sb("h1", [64, FW], F32, st); h2 = cx.sb("h2f", [64, FW], F32, st)
            dec = cx.sb("dec", [128, FW], F32, st); hf = [cx.sb(f"hf{i}", [128, FW], F32, st) for i in range(2)]
            junk = cx.sb("fjunk", [128, FW], F32, st)
            accsq = cx.sb("accsq", [128, 2, 32], F32, st); ssum = cx.sb("ssum", [128, 2], F32, st); rn = cx.sb("rn", [128, 2], F32, st)
            S.op("pool", lambda e: e.memset(accsq[:], 0.0), writes=["accsq"])

            def sin_layer(ps, bias, dst, key):
                S.op("dve", lambda e: e.tensor_scalar(pre[:], ps[0:64, 0:FW], bias[:, 0:1], None, ALU.add), reads=["ps0", "ps1", "w_fb1", "w_fb2"], writes=["pre"])
                for _ in range(2):
                    S.op("dve", lambda e: e.tensor_scalar(msk[:], pre[:], PI, -2 * PI, ALU.is_gt, ALU.mult), reads=["pre"], writes=["msk"])
                    S.op("dve", lambda e: e.tensor_tensor(pre[:], pre[:], msk[:], ALU.add), reads=["pre", "msk"], writes=["pre"])
                    S.op("dve", lambda e: e.tensor_scalar(msk[:], pre[:], -PI, 2 * PI, ALU.is_lt, ALU.mult), reads=["pre"], writes=["msk"])
                    S.op("dve", lambda e: e.tensor_tensor(pre[:], pre[:], msk[:], ALU.add), reads=["pre", "msk"], writes=["pre"])
                S.op("act", lambda e: e.activation(dst[:], pre[:], AF.Sin), reads=["pre"], writes=[key])

            for fc_ in range(nfc):
                sl = slice(fc_ * FW, (fc_ + 1) * FW)
                S.dma("sp", zt[:], io["zT"][:, sl], writes=["zt"]); S.dma("sp", tnt[:], io["tn"][:, sl], writes=["tnt"])
                S.op("pe", lambda e: e.matmul(cx.PS[0][0:64, 0:FW], small["fw1"][:, :], zt[:, :], start=True, stop=True), reads=["zt", "w_fw1"], writes=["ps0"])
                sin_layer(cx.PS[0], small["fb1"], h1, "h1")
                S.op("pe", lambda e: e.matmul(cx.PS[1][0:64, 0:FW], small["fw2"][:, :], h1[:, :], start=True, stop=True), reads=["h1", "w_fw2"], writes=["ps1"])
                sin_layer(cx.PS[1], small["fb2"], h2, "h2f")
                S.op("act", lambda e: e.activation(dec[:], tnt[:], AF.Exp, scale=small["ndel"][:, 0:1]), reads=["tnt", "w_ndel"], writes=["dec"])
                for oc in range(2):
                    S.op("pe", lambda e, oc=oc: e.matmul(cx.PS[2 + oc][:, 0:FW], small["fw3"][:, oc * 128:(oc + 1) * 128], h2[:, :], start=True, stop=True),
                         reads=["h2f", "w_fw3"], writes=[f"ps{2+oc}"])
                    S.op("dve", lambda e, oc=oc: e.scalar_tensor_tensor(hf[oc][:], cx.PS[2 + oc][:, 0:FW], small["fb3"][:, oc:oc + 1], dec[:], ALU.add, ALU.mult),
                         reads=[f"ps{2+oc}", "w_fb3", "dec"], writes=[f"hf{oc}"])
                    S.op("act", lambda e, oc=oc, fc_=fc_: e.activation(junk[:], hf[oc][:], AF.Square, accum_out=accsq[:, oc, fc_:fc_ + 1]),
                         reads=[f"hf{oc}", "accsq"], writes=["fjunk", "accsq"])
                    S.dma("poolq", filt_s[oc][:, sl], hf[oc][:], reads=[f"hf{oc}"], writes=["filt_s"])
            S.op("dve", lambda e: e.tensor_reduce(ssum[:], accsq[:], AX.X, ALU.add), reads=["accsq"], writes=["ssum"])
            S.op("pe", lambda e: e.matmul(cx.PS[4][:, 0:2], c["PSM"][:, :], ssum[:, :], start=True, stop=True), reads=["ssum", "c_PSM"], writes=["ps4"])
            S.op("dve", lambda e: e.tensor_scalar(rn[:], cx.PS[4][:, 0:2], EPS, None, ALU.add), reads=["ps4"], writes=["rn"])
            S.op("act", lambda e: e.sqrt(rn[:], rn[:]), reads=["rn"], writes=["rn"])
            S.op("dve", lambda e: e.reciprocal(rn[:], rn[:]), reads=["rn"], writes=["rn"])
            nb = cx.sb("nb", [128, 2048], F32, st)
            NW = min(2048, Le)
            for oc in range(2):
                for c0 in range(0, Le, NW):
                    S.dma("sp", nb[:, 0:NW], filt_s[oc][:, c0:c0 + NW], reads=["filt_s"], writes=["nb"])
                    S.op("dve", lambda e, oc=oc: e.tensor_scalar(nb[:, 0:NW], nb[:, 0:NW], rn[:, oc:oc + 1], None, ALU.mult), reads=["nb", "rn"], writes=["nb"])
                    if c0 == 0:
                        S.op("pool", lambda e: e.memset(nb[64:128, 0:1], 0.0), reads=["nb"], writes=["nb"])
                    S.dma("poolq", filt_s[oc][:, c0:c0 + NW], nb[:, 0:NW], reads=["nb"], writes=["filt_s"])
            S.barrier()

        with ExitStack() as st:
            Af = cx.sb("Af", [64, 2, 256], F32, st); Ab = cx.sb("Ab", [64, 2, 256], BF16, st)
            Bp = [cx.sb(f"Bp{i}", [128, 2, 2, 128], BF16, st) for i in range(2)]
            tw = [cx.sb(f"tw{i}", [128, 2, 128], F32, st) for i in range(4)]
            Xr = cx.sb("Xr", [128, 2, 2, 128], F32, st); Xi = cx.sb("Xi", [128, 2, 2, 128], F32, st)
            Kr = [cx.sb(f"Kr{o}", [128, 2, 2, 128], F32, st) for o in range(2)]; Ki = [cx.sb(f"Ki{o}", [128, 2, 2, 128], F32, st) for o in range(2)]
            yt_ = [cx.sb(f"yt{i}", [128, 2, 2, 128], F32, st) for i in range(4)]
            Yb = cx.sb("Yb", [128, 2, 2, 2, 128], BF16, st)
            Cp = cx.sb("Cp", [128, 2, 2, 256], BF16, st)
            it_ = [cx.sb(f"it{i}", [128, 256], F32, st) for i in range(4)]
            x1t = cx.sb("x1t", [64, 2, 256], F32, st); x2t = cx.sb("x2t", [64, 2, 256], F32, st); vt = cx.sb("vt", [64, 2, 256], F32, st)
            zt_ = cx.sb("zt_", [64, 2, 256], F32, st); e1 = cx.sb("e1", [64, 2, 256], F32, st); hout = cx.sb("hout", [64, 2, 256], F32, st)

            def tb(src2d):
                return src2d.rearrange("c (n1 n2) -> n1 c n2", n2=256)

            def fwd(Xr_, Xi_, xkey):
                for cc in range(2):
                    ps = cx.PS[cc]
                    for g in range(2):
                        S.op("pe", lambda e, ps=ps, g=g, cc=cc: e.matmul(ps[:, g * 256:(g + 1) * 256], Ab[:, g, cc * 128:(cc + 1) * 128], c["F1cat"][:, :],
                                                                      start=True, stop=True), reads=["Ab", "c_F1cat"], writes=[f"ps{cc}"])
                    psv = ps[:].rearrange("p (g r k) -> p g r k", g=2, r=2)
                    Trb = c["Tr"][:, cc, :].unsqueeze(1).to_broadcast([128, 2, 128]); Tib = c["Ti"][:, cc, :].unsqueeze(1).to_broadcast([128, 2, 128])
                    S.op("dve", lambda e, psv=psv, Trb=Trb: e.tensor_tensor(tw[0][:], psv[:, :, 0, :], Trb, ALU.mult), reads=[f"ps{cc}", "c_Tr"], writes=["tw0"])
                    S.op("dve", lambda e, psv=psv, Tib=Tib: e.tensor_tensor(tw[1][:], psv[:, :, 1, :], Tib, ALU.mult), reads=[f"ps{cc}", "c_Ti"], writes=["tw1"])
                    S.op("dve", lambda e, psv=psv, Tib=Tib: e.tensor_tensor(tw[2][:], psv[:, :, 0, :], Tib, ALU.mult), reads=[f"ps{cc}", "c_Ti"], writes=["tw2"])
                    S.op("dve", lambda e, psv=psv, Trb=Trb: e.tensor_tensor(tw[3][:], psv[:, :, 1, :], Trb, ALU.mult), reads=[f"ps{cc}", "c_Tr"], writes=["tw3"])
                    S.op("pool", lambda e, cc=cc: e.tensor_tensor(Bp[cc][:, 0, :, :], tw[0][:], tw[1][:], ALU.subtract), reads=["tw0", "tw1"], writes=[f"Bp{cc}"])
                    S.op("pool", lambda e, cc=cc: e.tensor_tensor(Bp[cc][:, 1, :, :], tw[2][:], tw[3][:], ALU.add), reads=["tw2", "tw3"], writes=[f"Bp{cc}"])
                for kc in range(2):
                    ks = slice(kc * 128, (kc + 1) * 128)
                    psr, psi = cx.PS[2 + 2 * kc], cx.PS[3 + 2 * kc]
                    seq_r = [(c["F2r"], 0), (c["F2in"], 1)]
                    seq_i = [(c["F2i"], 0), (c["F2r"], 1)]
                    for (pp, seq, pk) in ((psr, seq_r, f"ps{2+2*kc}"), (psi, seq_i, f"ps{3+2*kc}")):
                        n_ = 0
                        for cc in range(2):
                            for (M_, ri) in seq:
                                S.op("pe", lambda e, pp=pp, M_=M_, ri=ri, cc=cc, n_=n_, ks=ks: e.matmul(
                                    pp[:, 0:256], M_[:, cc, ks], Bp[cc][:, ri, :, :], start=(n_ == 0), stop=(n_ == 3)),
                                    reads=[f"Bp{cc}"] + ck, writes=[pk])
                                n_ += 1
                    S.op("act", lambda e, psr=psr, kc=kc: e.copy(Xr_[:, kc, :, :], psr[:, 0:256]), reads=[f"ps{2+2*kc}"], writes=[xkey + "r"])
                    S.op("act", lambda e, psi=psi, kc=kc: e.copy(Xi_[:, kc, :, :], psi[:, 0:256]), reads=[f"ps{3+2*kc}"], writes=[xkey + "i"])

            def conv(o, ydst_key):
                fwd(Xr, Xi, "X")
                fl = lambda t_: t_[:].rearrange("p a b c -> p (a b c)")
                S.op("dve", lambda e: e.tensor_tensor(fl(yt_[0]), fl(Xr), fl(Kr[o]), ALU.mult), reads=["Xr", f"K{o}r"], writes=["yt0"])
                S.op("pool", lambda e: e.tensor_tensor(fl(yt_[1]), fl(Xi), fl(Ki[o]), ALU.mult), reads=["Xi", f"K{o}i"], writes=["yt1"])
                S.op("dve", lambda e: e.tensor_tensor(fl(yt_[2]), fl(Xr), fl(Ki[o]), ALU.mult), reads=["Xr", f"K{o}i"], writes=["yt2"])
                S.op("pool", lambda e: e.tensor_tensor(fl(yt_[3]), fl(Xi), fl(Kr[o]), ALU.mult), reads=["Xi", f"K{o}r"], writes=["yt3"])
                S.op("dve", lambda e: e.tensor_tensor(Yb[:, :, 0, :, :], yt_[0][:], yt_[1][:], ALU.subtract), reads=["yt0", "yt1"], writes=["Yb"])
                S.op("pool", lambda e: e.tensor_tensor(Yb[:, :, 1, :, :], yt_[2][:], yt_[3][:], ALU.add), reads=["yt2", "yt3"], writes=["Yb"])
                for g in range(2):
                    ps = cx.PS[g]
                    n_ = 0
                    for kc in range(2):
                        for (ri, M_) in ((0, c["IA"]), (1, c["IB"])):
                            S.op("pe", lambda e, ps=ps, kc=kc, ri=ri, M_=M_, g=g, n_=n_: e.matmul(ps[:, :], Yb[:, kc, ri, g, :], M_[:, kc, :], start=(n_ == 0), stop=(n_ == 3)),
                                 reads=["Yb"] + ck, writes=[f"ps{g}"])
                            n_ += 1
                    S.op("dve", lambda e, ps=ps: e.tensor_tensor(it_[0][:], ps[:, 0:256], c["ITr"][:], ALU.mult), reads=[f"ps{g}", "c_ITr"], writes=["it0"])
                    S.op("dve", lambda e, ps=ps: e.tensor_tensor(it_[1][:], ps[:, 256:512], c["ITi"][:], ALU.mult), reads=[f"ps{g}", "c_ITi"], writes=["it1"])
                    S.op("dve", lambda e, ps=ps: e.tensor_tensor(it_[2][:], ps[:, 0:256], c["ITi"][:], ALU.mult), reads=[f"ps{g}", "c_ITi"], writes=["it2"])
                    S.op("dve", lambda e, ps=ps: e.tensor_tensor(it_[3][:], ps[:, 256:512], c["ITr"][:], ALU.mult), reads=[f"ps{g}", "c_ITr"], writes=["it3"])
                    S.op("pool", lambda e, g=g: e.tensor_tensor(Cp[:, 0, g, :], it_[0][:], it_[1][:], ALU.subtract), reads=["it0", "it1"], writes=["Cp"])
                    S.op("pool", lambda e, g=g: e.tensor_tensor(Cp[:, 1, g, :], it_[2][:], it_[3][:], ALU.add), reads=["it2", "it3"], writes=["Cp"])
                S.op("pe", lambda e: e.matmul(cx.PS[6][0:64, :], c["G1r"][:, :], Cp[:, 0, :, :], start=True, stop=False), reads=["Cp", "c_G1r"], writes=["ps6"])
                S.op("pe", lambda e: e.matmul(cx.PS[6][0:64, :], c["G1i"][:, :], Cp[:, 1, :, :], start=False, stop=True), reads=["Cp", "c_G1i"], writes=["ps6"])

            for pr in range(32):
                ch0 = pr * 2
                for o in range(2):
                    for d_ in range(2):
                        S.dma("sp", Af[:], tb(filt_s[o][d_ * 64 + ch0:d_ * 64 + ch0 + 2, :]), reads=["filt_s"], writes=["Af"])
                        S.op("dve", lambda e: e.tensor_copy(Ab[:], Af[:]), reads=["Af"], writes=["Ab"])
                        if d_ == 0:
                            fwd(Kr[o], Ki[o], f"K{o}")
                        else:
                            fwd(Xr, Xi, "X")
                            S.op("pool", lambda e, o=o: e.tensor_tensor(Kr[o][:], Kr[o][:], Xr[:], ALU.add), reads=[f"K{o}r", "Xr"], writes=[f"K{o}r"])
                            S.op("pool", lambda e, o=o: e.tensor_tensor(Ki[o][:], Ki[o][:], Xi[:], ALU.subtract), reads=[f"K{o}i", "Xi"], writes=[f"K{o}i"])
                S.dma("sp", x1t[:], tb(scx[0][ch0:ch0 + 2, :]), reads=["scx"], writes=["x1t"])
                S.dma("sp", x2t[:], tb(scx[1][ch0:ch0 + 2, :]), reads=["scx"], writes=["x2t"])
                S.dma("sp", vt[:], tb(scx[2][ch0:ch0 + 2, :]), reads=["scx"], writes=["vt"])
                S.op("dve", lambda e: e.tensor_copy(Ab[:], vt[:]), reads=["vt"], writes=["Ab"])
                conv(0, "y1")
                dk = lambda o: small["dsk"][:, o, ch0:ch0 + 2].unsqueeze(2).to_broadcast([64, 2, 256])
                psy = cx.PS[6][0:64, :].rearrange("p (g n) -> p g n", g=2)
                dk0 = dk(0); dk1 = dk(1)
                S.op("pool", lambda e, dk0=dk0: e.tensor_tensor(e1[:], vt[:], dk0, ALU.mult), reads=["vt", "w_dsk"], writes=["e1"])
                S.op("dve", lambda e: e.tensor_tensor(e1[:], e1[:], psy, ALU.add), reads=["e1", "ps6"], writes=["e1"])
                S.op("dve", lambda e: e.tensor_tensor(zt_[:], e1[:], x1t[:], ALU.mult), reads=["e1", "x1t"], writes=["zt_"])
                S.op("dve", lambda e: e.tensor_copy(Ab[:], zt_[:]), reads=["zt_"], writes=["Ab"])
                conv(1, "y2")
                S.op("pool", lambda e, dk1=dk1: e.tensor_tensor(e1[:], zt_[:], dk1, ALU.mult), reads=["zt_", "w_dsk"], writes=["e1"])
                S.op("dve", lambda e: e.tensor_tensor(e1[:], e1[:], psy, ALU.add), reads=["e1", "ps6"], writes=["e1"])
                S.op("dve", lambda e: e.tensor_tensor(hout[:], e1[:], x2t[:], ALU.mult), reads=["e1", "x2t"], writes=["hout"])
                if Le == L:
                    S.dma("poolq", tb(io["oh"][ch0:ch0 + 2, :]), hout[:], reads=["hout"], writes=["oh"])
                else:
                    S.dma("poolq", io["oh"][ch0:ch0 + 2, :].rearrange("(o c) n -> o c n", o=1), hout[0:1, :, 0:Le], reads=["hout"], writes=["oh"])
            S.barrier()

    for tag_, Le_ in paths:
        do_path(tag_, Le_)
    cx.end_phase()


POOL_SIZES = (2, 4, 8, 16)
NKEY = SEQ + CTX
NLOC = TPC + CPC
GROUPS = [[0, 1, 2, 3], [4, 5, 6, 7]]
SECS = {"aq": 0, "ak": 256, "av": 512, "bq": 768, "bk": 1024, "bv": 1280, "pool": 1536, "hy0": 1792, "hy1": 2048, "hy2": 2304}


def emit_R(cx, need_ctx, T):
    S = cx.S
    ag_out = T["ag_out"]
    selq_d = cx.inp("selq", [128, 2, 2, 32]); selg_d = cx.inp("selg", [128, 2, 64])
    selq = cx.sb("selq", [128, 2, 2, 32]); selg = cx.sb("selg", [128, 2, 64])
    S.dma("sp", selq[:], selq_d, writes=["selq"]); S.dma("sp", selg[:], selg_d, writes=["selg"])
    xs = [cx.sb(f"rx{i}", [128, 2, 512]) for i in range(6)]
    ev = [cx.sb(f"rev{i}", [64, 512]) for i in range(2)]
    k33 = [cx.sb(f"k33_{i}", [33, 512]) for i in range(2)]
    k65 = cx.sb("k65", [65, 512])
    v65 = [cx.sb(f"v65_{i}", [128, 65]) for i in range(2)]
    zero = cx.sb("rzero", [64, 16])
    S.op("pool", lambda e: e.memset(zero[:], 0.0), writes=["rzero"])
    for t_ in k33:
        S.op("pool", lambda e, t_=t_: e.memset(t_[32:33, :], 1.0), writes=["k33"])
    S.op("pool", lambda e: e.memset(k65[64:65, :], 1.0), writes=["k65"])
    for t_ in v65:
        S.op("pool", lambda e, t_=t_: e.memset(t_[:, 64:65], 1.0), writes=["v65"])
    for tag, Le in (("l", SEQ),) + ((("c", CTX),) if need_ctx else ()):
        for p in range(3):
            S.dma("sp", T[f"hy_{tag}"][p][:, 0:1], zero[:, 0:1], reads=["rzero"], writes=["hy"], allow_slow_non_contiguous=True)
            S.dma("sp", T[f"hy_{tag}"][p][:, Le + 1:Le + 2], zero[:, 0:1], reads=["rzero"], writes=["hy"], allow_slow_non_contiguous=True)
        S.dma("sp", T[f"pl_{tag}"][:, 0:8], zero[:, 0:8], reads=["rzero"], writes=["pl"])
        S.dma("sp", T[f"pl_{tag}"][:, Le + 8:Le + 24], zero[:, 0:16], reads=["rzero"], writes=["pl"])
    cnt = {"x": 0, "ps": 0, "ev": 0, "k": 0, "v": 0, "q": 0}

    def load(sec, pieces, w):
        i = cnt["x"] % 6; cnt["x"] += 1
        X = xs[i]
        o = 0
        for (r, c0, pw) in pieces:
            n = pw // 64
            for kc in range(2):
                r0 = r * DIN + SECS[sec] + kc * 128
                src = ag_out[c0 // 64:c0 // 64 + n, r0:r0 + 128, :].rearrange("ck p t -> p ck t")
                q = ("sp", "actq")[cnt["q"] % 2]; cnt["q"] += 1
                S.dma(q, X[:, kc, o:o + pw].rearrange("p (ck t) -> p ck t", t=64), src, writes=[f"rx{i}"])
            o += pw
        return X, f"rx{i}"

    def sel_fm(X, xk, w, SEL, M, dst, ones=None):
        pi = cnt["ps"] % 8; cnt["ps"] += 1
        ps = cx.PS[pi]
        for kc in range(2):
            S.op("pe", lambda e, ps=ps, kc=kc, SEL=SEL: e.matmul(ps[0:M, 0:w], SEL[:, kc, :], X[:, kc, 0:w], start=(kc == 0), stop=(kc == 1)),
                 reads=[xk, "selq", "selg"], writes=[f"ps{pi}"])
        if ones == "k33":
            i = cnt["k"] % 2; cnt["k"] += 1
            dt_, dk, rows = k33[i], f"k33_{i}", 33
        elif ones == "k65":
            dt_, dk, rows = k65, "k65", 65
        else:
            i = cnt["ev"] % 2; cnt["ev"] += 1
            dt_, dk, rows = ev[i], f"rev{i}", M
        S.op(("act", "dve")[cnt["ps"] % 2], lambda e, ps=ps, dt_=dt_: (e.copy if hasattr(e, "copy") else e.tensor_copy)(dt_[0:M, 0:w], ps[0:M, 0:w]),
             reads=[f"ps{pi}"], writes=[dk])
        S.dma("poolq", dst, dt_[0:rows, 0:w], reads=[dk], writes=["rdst"])

    def sel_tm(X, xk, w, dst_rows):
        for s0 in range(0, w, 128):
            pi = cnt["ps"] % 8; cnt["ps"] += 1
            ps = cx.PS[pi]
            for kc in range(2):
                S.op("pe", lambda e, ps=ps, kc=kc, s0=s0: e.matmul(ps[:, 0:64], X[:, kc, s0:s0 + 128], selg[:, kc, :], start=(kc == 0), stop=(kc == 1)),
                     reads=[xk, "selg"], writes=[f"ps{pi}"])
            i = cnt["v"] % 2; cnt["v"] += 1
            S.op(("act", "dve")[i], lambda e, ps=ps, i=i: (e.copy if hasattr(e, "copy") else e.tensor_copy)(v65[i][:, 0:64], ps[:, 0:64]),
                 reads=[f"ps{pi}"], writes=[f"v65_{i}"])
            S.dma("poolq", dst_rows[s0:s0 + 128, :], v65[i][:, :], reads=[f"v65_{i}"], writes=["rdst"])

    chunks = [("l", [(r, cp * 512, 512)], r * TPC + cp * 512, 512) for r in range(4) for cp in range(8)]
    chunks.append(("c", [(r, TPC, CPC) for r in range(4)], SEQ, CTX))
    for kind, pieces, t0, w in chunks:
        isl = kind == "l"
        if isl or need_ctx:
            X, xk = load("aq", pieces, w)
            for c_ in range(2):
                sel_fm(X, xk, w, selq[:, :, c_, :], 32, (T["aqT"][c_][:, t0:t0 + w] if isl else T["aqcT"][c_][:, :]))
            X, xk = load("bq", pieces, w)
            sel_fm(X, xk, w, selg, 64, (T["bqT"][:, t0:t0 + w] if isl else T["bqcT"][:, :]))
            tag = "l" if isl else "c"
            tt = t0 if isl else 0
            X, xk = load("pool", pieces, w)
            sel_fm(X, xk, w, selg, 64, T[f"pl_{tag}"][:, 8 + tt:8 + tt + w])
            for p in range(3):
                X, xk = load(f"hy{p}", pieces, w)
                sel_fm(X, xk, w, selg, 64, T[f"hy_{tag}"][p][:, 1 + tt:1 + tt + w])
        X, xk = load("ak", pieces, w)
        for c_ in range(2):
            sel_fm(X, xk, w, selq[:, :, c_, :], 32, T["akT"][c_][:, t0:t0 + w], ones="k33")
        X, xk = load("bk", pieces, w)
        sel_fm(X, xk, w, selg, 64, T["bkT"][:, t0:t0 + w], ones="k65")
        X, xk = load("av", pieces, w)
        sel_tm(X, xk, w, T["av"][t0:t0 + w, :])
        X, xk = load("bv", pieces, w)
        sel_tm(X, xk, w, T["bv"][t0:t0 + w, :])
    cx.end_phase()


def emit_W(cx, need_ctx, T):
    S = cx.S
    mixT = T["mixT"]; rs_in = T["rs_in"]
    wop_d = cx.inp("wo_part", [4, 64, D])
    wst = cx.sb("wwst", [64, 4, D]); wb = cx.sb("wwb", [64, 4, D], BF16)
    S.dma("sp", wst[:], wop_d.rearrange("m p n -> p m n"), writes=["wwst"])
    S.op("dve", lambda e: e.tensor_copy(wb[:], wst[:]), reads=["wwst"], writes=["wwb"])
    mt = [cx.sb(f"wmt{i}", [64, 4, 128]) for i in range(2)]
    mb = [cx.sb(f"wmb{i}", [64, 4, 128], BF16) for i in range(2)]
    ot = [cx.sb(f"wot{i}", [128, D]) for i in range(2)]
    rs_out = T["rs_out"]
    cckey = cx.coll_group()
    nlb = TPC // 128
    order = [j * nlb + lb for lb in range(nlb) for j in range(4)]
    if need_ctx:
        order += [SEQ // 128 + ci for ci in range(CTX // 128)]
    NCH = NLOC // RSR
    issued = [0]
    chunk_keys = {k: [] for k in range(NCH)}

    def issue_upto(local_done):
        while issued[0] < NCH and (issued[0] + 1) * RSR <= local_done:
            k = issued[0]
            cx.coll(cckey, "ReduceScatter", ALU.add, GROUPS, rs_in[k], rs_out[k * RSR:(k + 1) * RSR, :], sorted(set(chunk_keys[k])))
            issued[0] += 1

    for n_, i in enumerate(order):
        s = n_ % 2
        S.dma(("sp", "actq")[s], mt[s][:], mixT[:, :, i * 128:(i + 1) * 128].rearrange("m p t -> p m t"), writes=[f"wmt{s}"])
        S.op("pool", lambda e, s=s: e.tensor_copy(mb[s][:], mt[s][:]), reads=[f"wmt{s}"], writes=[f"wmb{s}"])
        for hb_ in range(2):
            pi = (2 * n_ + hb_) % 8
            ps = cx.PS[pi]
            for m in range(4):
                S.op("pe", lambda e, ps=ps, m=m, hb_=hb_, s=s: e.matmul(ps[:, :], mb[s][:, m, :], wb[:, m, hb_ * 512:(hb_ + 1) * 512], start=(m == 0), stop=(m == 3)),
                     reads=[f"wmb{s}", "wwb"], writes=[f"ps{pi}"])
            S.op(("act", "dve")[hb_], lambda e, ps=ps, hb_=hb_, s=s: (e.copy if hasattr(e, "copy") else e.tensor_copy)(ot[s][:, hb_ * 512:(hb_ + 1) * 512], ps[:, :]),
                 reads=[f"ps{pi}"], writes=[f"wot{s}h{hb_}"])

        def put(j, loc, p0, n):
            while n > 0:
                k, q0 = loc // RSR, loc % RSR
                m_ = min(n, RSR - q0)
                key = f"rs_in{k}_{j}_{q0}"
                S.dma("poolq", rs_in[k][j * RSR + q0:j * RSR + q0 + m_, :], ot[s][p0:p0 + m_, :], reads=[f"wot{s}h0", f"wot{s}h1"], writes=[key])
                chunk_keys[k].append(key)
                loc += m_; p0 += m_; n -= m_
        if i < SEQ // 128:
            j, lb = i // nlb, i % nlb
            put(j, lb * 128, 0, 128)
            if j == 3 and need_ctx:
                issue_upto((lb + 1) * 128)
            elif j == 3:
                issue_upto((lb + 1) * 128 if lb < nlb - 1 else NLOC)
        else:
            ci = i - SEQ // 128
            for hf in range(2):
                put(ci * 2 + hf, TPC, hf * 64, 64)
            if ci == CTX // 128 - 1:
                issue_upto(NLOC)
    cx.end_phase()


SHARED = {"sel", "ident", "identF", "ropec", "ropes", "onesb", "E65", "ones64", "cT", "fin_g", "router_w", "router_b",
          "zT_l", "tn_l", "zT_c", "tn_c", "icnt_l", "icnt_c", "psel", "ndel", "selq", "selg", "xl", "xc"}


def build_fused(stop=None):
    cx = Ctx("F")
    cx.shared = set(SHARED)
    sc = cx.scratch
    T = {"ag_in": sc("ag_in", [NAGC, DIN, 64]), "ag_out": sc("ag_out", [NAGC, 4 * DIN, 64]),
         "aqT": sc("aqT", [2, 32, SEQ]), "akT": sc("akT", [2, 33, NKEY]), "av": sc("av", [NKEY, 65]), "aqcT": sc("aqcT", [2, 32, CTX]),
         "bqT": sc("bqT", [64, SEQ]), "bkT": sc("bkT", [65, NKEY]), "bv": sc("bv", [NKEY, 65]), "bqcT": sc("bqcT", [64, CTX]),
         "hy_l": sc("hy_l", [3, 64, SEQ + 2]), "pl_l": sc("pl_l", [64, SEQ + 24]),
         "hy_c": sc("hy_c", [3, 64, CTX + 2]), "pl_c": sc("pl_c", [64, CTX + 24]),
         "mixT": sc("mixT", [4, 64, NKEY]), "rs_in": sc("rs_in", [NLOC // RSR, 4 * RSR, D]), "rs_out": sc("rs_out", [NLOC, D]),
         "xl_s": sc("xl_s", [TPC, D]), "xc_s": sc("xc_s", [CPC, D]), "dummy": sc("dummy_oc", [CPC, D])}
    x_l = cx.inp("xl", [TPC, D]); x_c = cx.inp("xc", [CPC, D])
    out = cx.nc.dram_tensor("out", [TPC, D], F32, kind="ExternalOutput").ap()
    mixT = T["mixT"]

    def dump(src2d):
        r, c_ = src2d.shape
        dst = out.rearrange("a d -> (a d)")[0:r * c_].rearrange("(r c) -> r c", c=c_)
        cx.S.dma("sp", dst, src2d, writes=["dbg"])
        return cx.finish(), cx

    for li in range(2):
        need_ctx = li < 1
        cx.prefix = f"L{li}_"
        cx.over = {"xl": x_l if li == 0 else T["xl_s"], "xc": x_c if li == 0 else T["xc_s"], "ag_in": T["ag_in"], "ag_out": T["ag_out"]}
        emit_A(cx)
        if stop == "A":
            return dump(T["ag_in"][0:25, 0:DIN, :].rearrange("a b c -> a (b c)"))
        if stop == "AG":
            return dump(T["ag_out"][0:25, DIN:2 * DIN, :].rearrange("a b c -> a (b c)"))
        cx.over = {}
        emit_R(cx, need_ctx, T)
        if stop == "R":
            return dump(T["bkT"][:, :])
        cx.over = {k: T[k] for k in ("aqT", "akT", "av", "aqcT", "bqT", "bkT", "bv", "bqcT")}
        cx.over.update({"oa": mixT[0][:, 0:SEQ], "oac": mixT[0][:, SEQ:NKEY], "ob": mixT[1][:, 0:SEQ], "obc": mixT[1][:, SEQ:NKEY]})
        emit_B1(cx, li)
        if stop == "B1":
            return dump(mixT[1][:, :])
        cx.over = {"hy_l": T["hy_l"], "pl_l": T["pl_l"], "hy_c": T["hy_c"], "pl_c": T["pl_c"],
                   "op_l": mixT[2][:, 0:SEQ], "op_c": mixT[2][:, SEQ:NKEY], "oh_l": mixT[3][:, 0:SEQ], "oh_c": mixT[3][:, SEQ:NKEY]}
        emit_B2(cx, need_ctx)
        cx.over = {}
        if stop == "B2":
            return dump(mixT[3][:, :])
        emit_W(cx, need_ctx, T)
        if stop == "W":
            return dump(T["rs_in"][0:4, :, :].rearrange("a b c -> (a b) c"))
        if stop == "RS":
            return dump(T["rs_out"][0:TPC, :])
        cx.over = {"xl": x_l if li == 0 else T["xl_s"], "xc": x_c if li == 0 else T["xc_s"], "rs_out": T["rs_out"],
                   "ol": T["xl_s"] if li == 0 else out, "oc": T["xc_s"] if li == 0 else T["dummy"]}
        emit_C(cx, need_ctx, li == 1)
    return cx.finish(), cx


_CACHE = {}
STOP = None


def kernel(x, c, ctx, c_ctx, norm1_g, norm2_g, ada_w, ada_b, w_in, w_out, a_lambda, a_subln_g,
           b_rpb, pool_w, pool_scale, hy_short_w, hy_short_b, hy_f_w1, hy_f_b1, hy_f_w2, hy_f_b2,
           hy_f_w3, hy_f_b3, hy_skip, router_w, router_b, moe_w1, moe_w3, moe_w2, final_g):
    f = lambda a: np.ascontiguousarray(np.asarray(a, dtype=np.float32))
    if "F" not in _CACHE:
        _CACHE["F"] = build_fused(STOP)
    nc, cx = _CACHE["F"]
    x, ctx, c, c_ctx = f(x), f(ctx), f(c), f(c_ctx)
    rc, rs = const_rope()
    FC = fft_consts()
    deltas = np.abs(np.linspace(math.log(1e-2) / 1.5, math.log(1e-2) / 0.3, 256, dtype=np.float32))
    pos = {"l": hy_pos_consts(SEQ), "c": hy_pos_consts(CTX)}
    E65 = np.zeros((65, 64), np.float32); E65[64] = 1.0
    shared_all = {"sel": const_sel(), "ident": _bf16(np.eye(128)), "identF": np.eye(128, dtype=np.float32), "onesb": _bf16(np.ones((128, 128))),
                  "E65": E65, "ones64": np.ones((64, 64), np.float32), "fin_g": f(final_g).reshape(1, -1),
                  "router_w": f(router_w), "router_b": f(router_b).reshape(1, -1),
                  "zT_l": pos["l"][0], "tn_l": pos["l"][1], "zT_c": pos["c"][0], "tn_c": pos["c"][1]}
    shared_all.update({"c_" + k: v for k, v in FC.items()})
    in_maps = []
    for core in range(NCORE):
        b, j = core // 4, core % 4
        h = g = j
        chs = slice(g * 64, (g + 1) * 64)
        m = dict(shared_all)
        m["xl"] = np.ascontiguousarray(x[b, j * TPC:(j + 1) * TPC]); m["xc"] = np.ascontiguousarray(ctx[b, j * CPC:(j + 1) * CPC])
        m["cT"] = cT_layout(c[b], c_ctx)
        m["ropec"] = np.ascontiguousarray(rc[j * TPC:(j + 1) * TPC]); m["ropes"] = np.ascontiguousarray(rs[j * TPC:(j + 1) * TPC])
        selq = np.zeros((256, 2, 32), np.float32); selg = np.zeros((256, 64), np.float32)
        for c_ in range(2):
            selq[h * 64 + c_ * 32 + np.arange(32), c_, np.arange(32)] = 1.0
        selg[h * 64 + np.arange(64), np.arange(64)] = 1.0
        m["selq"] = np.ascontiguousarray(selq.reshape(2, 128, 2, 32).transpose(1, 0, 2, 3))
        m["selg"] = np.ascontiguousarray(selg.reshape(2, 128, 64).transpose(1, 0, 2))
        for tag, Le in (("l", SEQ), ("c", CTX)):
            t = np.arange(Le); w = POOL_SIZES[g]
            cnt = (np.clip(t + w // 2, 0, Le) - np.clip(t - w // 2, 0, Le)).astype(np.float32)
            m[f"icnt_{tag}"] = np.ascontiguousarray(np.broadcast_to((1.0 / cnt)[None, :], (64, Le))).astype(np.float32)
        ps = np.zeros((64, 4), np.float32); ps[:, g] = 1.0
        m["psel"] = ps
        m["ndel"] = np.ascontiguousarray(np.tile(-deltas[chs], 2).reshape(128, 1))
        for li in range(2):
            p = f"L{li}_"
            m[p + "ada_w"] = f(ada_w[li]); m[p + "ada_b"] = f(ada_b[li]).reshape(1, -1)
            m[p + "norm_g"] = f(norm1_g[li]).reshape(1, -1); m[p + "norm2_g"] = f(norm2_g[li]).reshape(1, -1)
            m[p + "w_in"] = f(w_in[li])
            m[p + "alam"] = f(a_lambda[li]).reshape(1, 128); m[p + "subg"] = f(a_subln_g[li]).reshape(64, 1)
            m[p + "bias"] = na_bias_sets(f(b_rpb[li])[h])
            sw = f(hy_short_w[li]).reshape(3, 3, 256)[:, :, chs]
            m[p + "shw"] = np.ascontiguousarray(sw.transpose(2, 1, 0)); m[p + "shb"] = np.ascontiguousarray(f(hy_short_b[li]).reshape(3, 256)[:, chs].T)
            m[p + "fw1"] = f(hy_f_w1[li]); m[p + "fb1"] = f(hy_f_b1[li]).reshape(64, 1)
            m[p + "fw2"] = f(hy_f_w2[li]); m[p + "fb2"] = f(hy_f_b2[li]).reshape(64, 1)
            w3 = f(hy_f_w3[li]).reshape(64, 2, 2, 256)[:, :, :, chs]
            m[p + "fw3"] = np.ascontiguousarray(w3.reshape(64, 256))
            b3 = f(hy_f_b3[li]).reshape(2, 2, 256)[:, :, chs]
            m[p + "fb3"] = np.ascontiguousarray(b3.reshape(2, 128).T)
            m[p + "dsk"] = np.ascontiguousarray(np.broadcast_to(f(hy_skip[li])[:, chs][None], (64, 2, 64))).astype(np.float32)
            m[p + "pw"] = np.ascontiguousarray(f(pool_w[li])[g]); m[p + "psc"] = np.ascontiguousarray(f(pool_scale[li])[chs].reshape(64, 1))
            wo = f(w_out[li])
            m[p + "wo_part"] = np.ascontiguousarray(np.stack([wo[mm * 256 + h * 64:mm * 256 + (h + 1) * 64] for mm in range(4)]))
            m[p + "w1"] = f(moe_w1[li]); m[p + "w3"] = f(moe_w3[li]); m[p + "w2"] = f(moe_w2[li])
        in_maps.append({k: v for k, v in m.items() if k in cx.ins})
    missing = [k for k in cx.ins if k not in in_maps[0]]
    assert not missing, missing
    res = run_bass_kernel_spmd(nc, in_maps, core_ids=list(range(NCORE)))
    out = np.stack([np.concatenate([res.results[b * 4 + j]["out"] for j in range(4)], 0) for b in range(2)])
    return out.astype(np.float32)
```

```python
import math
from contextlib import ExitStack
import numpy as np
import concourse.bass as bass
import concourse.mybir as mybir
from concourse.bass_utils import run_bass_kernel_spmd

F32 = mybir.dt.float32
BF16 = mybir.dt.bfloat16
AF = mybir.ActivationFunctionType
ALU = mybir.AluOpType
AX = mybir.AxisListType

D = 1024
SEQ = 16384
NB = 2
CTX = 256
DIN = 2560
NCORE = 8
TPC = SEQ // 4
NAGC = 65
RSR = 208
CPC = CTX // 4
EPS = 1e-6

COMPUTE = ("pe", "dve", "act", "pool")
QUEUES = ("sp", "actq", "poolq")
ISSUER = {"sp": "sp", "actq": "act", "poolq": "pool"}
NDMASEM = 6


class Sched:
    def __init__(self, nc, same_engine_sync=True):
        self.nc = nc
        self.same = same_engine_sync
        self.streams = {e: [] for e in ("pe", "dve", "act", "pool", "sp")}
        self.cnt = {e: 0 for e in COMPUTE}
        self.sem = {}
        self.dsem = {}
        self.dnext = {q: 0 for q in QUEUES}
        self.seen = {}
        self.writer = {}
        self.readers = {}

    def alloc_sems(self, stack):
        for e in COMPUTE:
            self.sem[e] = stack.enter_context(self.nc.semaphore("s_" + e))
        for q in QUEUES:
            for i in range(NDMASEM):
                self.dsem[(q, i)] = [stack.enter_context(self.nc.semaphore(f"d_{q}{i}")), 0]

    def _deps(self, reads, writes):
        deps = []
        for b in reads:
            if b in self.writer:
                deps.append(self.writer[b])
        for b in writes:
            if b in self.writer:
                deps.append(self.writer[b])
            deps.extend(self.readers.get(b, ()))
        return deps

    def _record(self, tok, reads, writes):
        for b in reads:
            self.readers.setdefault(b, []).append(tok)
        for b in writes:
            self.writer[b] = tok
            self.readers[b] = []

    def _waits(self, stream, deps, eng_name):
        ws = []
        for (sk, val, en) in deps:
            if en == eng_name and (eng_name == "pe" or not self.same):
                continue
            key = (stream, sk)
            if self.seen.get(key, 0) >= val:
                continue
            self.seen[key] = val
            ws.append((sk, val))
        return ws

    def _semobj(self, sk):
        return self.sem[sk] if sk in self.sem else self.dsem[sk][0]

    def op(self, eng, fn, reads=(), writes=()):
        deps = self._deps(reads, writes)
        ws = self._waits(eng, deps, eng)
        self.cnt[eng] += 1
        tok = (eng, self.cnt[eng], eng)
        self.streams[eng].append((ws, fn, (eng, 1)))
        self._record(tok, reads, writes)
        return tok

    def dma(self, q, out, in_, reads=(), writes=(), **kw):
        stream = ISSUER[q]
        deps = self._deps(reads, writes)
        i = self.dnext[q]
        self.dnext[q] = (i + 1) % NDMASEM
        ent = self.dsem[(q, i)]
        if ent[1] > 0:
            deps = deps + [((q, i), ent[1], "dma")]
        ws = self._waits(stream, deps, "dma?")
        ent[1] += 16
        tok = ((q, i), ent[1], "dma")

        def fn(e, out=out, in_=in_, kw=kw):
            return e.dma_start(out=out, in_=in_, **kw)
        self.streams[stream].append((ws, fn, ((q, i), 16)))
        self._record(tok, reads, writes)
        return tok

    def _all_tokens(self):
        toks = [(e, self.cnt[e], "x") for e in COMPUTE if self.cnt[e] > 0]
        for k, ent in self.dsem.items():
            if ent[1] > 0:
                toks.append((k, ent[1], "dma"))
        return toks

    def barrier(self):
        toks = self._all_tokens()
        for s in self.streams:
            ws = self._waits(s, toks, "none")
            if ws:
                self.streams[s].append((ws, None, None))
        self.writer.clear()
        self.readers.clear()

    def emit(self):
        ws = [(sk, val) for (sk, val, _) in self._all_tokens()]
        self.streams["sp"].append((ws, None, None))
        nc = self.nc
        with nc.Block() as block:
            def mk(sname):
                def body(e):
                    for (ws, fn, inc) in self.streams[sname]:
                        for (sk, val) in ws:
                            e.wait_ge(self._semobj(sk), val)
                        if fn is not None:
                            fn(e).then_inc(self._semobj(inc[0]), inc[1])
                return body
            block.tensor(mk("pe"))
            block.vector(mk("dve"))
            block.scalar(mk("act"))
            block.gpsimd(mk("pool"))
            block.sync(mk("sp"))


class Ctx:
    def __init__(self, name="k", nc=None):
        self.nc = nc or bass.Bass("TRN2", target_bir_lowering=False)
        self.root = ExitStack()
        self.st = ExitStack()
        self.S = Sched(self.nc)
        self.S.alloc_sems(self.root)
        self.ins = {}
        self.outs = {}
        self.over = {}
        self.prefix = ""
        self.shared = set()
        self.PSALL = self.root.enter_context(self.nc.psum_tensor("psall", [128, 4096], F32))
        self.PS = [self.PSALL[:, i * 512:(i + 1) * 512] for i in range(8)]
        self._n = 0
        self._ncc = 0

    def inp(self, name, shape, dt=F32):
        if name in self.over:
            return self.over[name]
        full = name if (name in self.shared or name.startswith("c_")) else self.prefix + name
        if full in self.ins:
            return self.ins[full]
        t = self.nc.dram_tensor(full, list(shape), dt, kind="ExternalInput").ap()
        self.ins[full] = t
        return t

    def out(self, name, shape, dt=F32):
        if name in self.over:
            return self.over[name]
        t = self.nc.dram_tensor(self.prefix + name, list(shape), dt, kind="ExternalOutput").ap()
        self.outs[self.prefix + name] = t
        return t

    def scratch(self, name, shape, dt=F32):
        self._n += 1
        return self.nc.dram_tensor(f"scr{self._n}_{name}", list(shape), dt, kind="Internal").ap()

    def sb(self, name, shape, dt=F32, stack=None):
        self._n += 1
        return (stack or self.st).enter_context(self.nc.sbuf_tensor(f"sb{self._n}_{name}", list(shape), dt))

    def end_phase(self):
        self.S.barrier()
        self.st.close()
        self.st = ExitStack()

    def collective(self, kind, op, groups, pairs):
        S = self.S
        S.barrier()
        sem = self.root.enter_context(self.nc.semaphore(f"cc{self._ncc}"))
        key = ("cc", self._ncc)
        self._ncc += 1
        S.dsem[key] = [sem, len(pairs)]
        for (src, dst) in pairs:
            S.streams["pool"].append(([], lambda e, src=src, dst=dst: e.collective_compute(kind, op, replica_groups=groups, ins=[src], outs=[dst]), (key, 1)))
        S.barrier()

    def coll_group(self):
        sem = self.root.enter_context(self.nc.semaphore(f"cc{self._ncc}"))
        key = ("cc", self._ncc)
        self._ncc += 1
        self.S.dsem[key] = [sem, 0]
        return key

    def coll(self, key, kind, op, groups, src, dst, reads):
        S = self.S
        ws = S._waits("pool", S._deps(reads, []), "pool-cc")
        S.dsem[key][1] += 1
        S.streams["pool"].append((ws, lambda e: e.collective_compute(kind, op, replica_groups=groups, ins=[src], outs=[dst]), (key, 1)))

    def finish(self):
        self.S.emit()
        self.st.close()
        self.root.close()
        return self.nc


def emit_mod_rows(cx, st, scT, ada_w, ada_b, modrow, tag):
    S = cx.S
    wbuf = [cx.sb(f"adaw{tag}{i}", [128, 8, 512], F32, st) for i in range(2)]
    adab = cx.sb(f"adab{tag}", [2, 6144], F32, st)
    S.dma("sp", adab[0:1, :], ada_b, writes=["adab"])
    S.dma("sp", adab[1:2, :], ada_b, writes=["adab"])
    awv = ada_w.rearrange("(kc p) n -> p kc n", p=128)
    for cb in range(12):
        wb = wbuf[cb % 2]
        q = ("sp", "actq")[cb % 2]
        S.dma(q, wb[:, 0:4, :], awv[:, 0:4, cb * 512:(cb + 1) * 512], writes=[f"adaw{cb%2}a"])
        S.dma(q, wb[:, 4:8, :], awv[:, 4:8, cb * 512:(cb + 1) * 512], writes=[f"adaw{cb%2}b"])
        ps = cx.PS[cb % 2]
        for kc in range(8):
            S.op("pe", lambda e, ps=ps, kc=kc, wb=wb: e.matmul(ps[0:2, :], scT[:, kc, :], wb[:, kc, :],
                                                                start=(kc == 0), stop=(kc == 7)),
                 reads=["scT", f"adaw{cb%2}a", f"adaw{cb%2}b"], writes=[f"ps{cb%2}"])
        S.op("dve", lambda e, ps=ps, cb=cb: e.tensor_tensor(modrow[:, cb * 512:(cb + 1) * 512], ps[0:2, :],
                                                            adab[:, cb * 512:(cb + 1) * 512], ALU.add),
             reads=[f"ps{cb%2}", "adab"], writes=["modrow"])


def emit_bcast_row(cx, dst, row2, sel, which, ps_ids, rkey, wkey):
    S = cx.S
    for hb in range(2):
        ps = cx.PS[ps_ids[hb]]
        S.op("pe", lambda e, ps=ps, hb=hb: e.matmul(ps[:, :], sel[:, which, :], row2[:, hb * 512:(hb + 1) * 512],
                                                    start=True, stop=True),
             reads=[rkey, "sel"], writes=[f"ps{ps_ids[hb]}"])
        S.op("act", lambda e, ps=ps, hb=hb: e.copy(dst[:, hb * 512:(hb + 1) * 512], ps[:, :]),
             reads=[f"ps{ps_ids[hb]}"], writes=[wkey])


def emit_rstd(cx, xt, P, ss, rstd, junk, xkey, slot):
    S = cx.S
    S.op("pool", lambda e: e.memset(ss[0:P, :], 0.0), writes=[f"ss{slot}"])
    S.op("act", lambda e: e.activation(junk[0:P, :], xt[0:P, :], AF.Square, accum_out=ss[0:P, :]),
         reads=[xkey], writes=[f"ss{slot}", "junk"])
    S.op("dve", lambda e: e.tensor_scalar(rstd[0:P, :], ss[0:P, :], 1.0 / D, EPS, ALU.mult, ALU.add),
         reads=[f"ss{slot}"], writes=[f"rstd{slot}"])
    S.op("act", lambda e: e.sqrt(rstd[0:P, :], rstd[0:P, :]), reads=[f"rstd{slot}"], writes=[f"rstd{slot}"])
    S.op("dve", lambda e: e.reciprocal(rstd[0:P, :], rstd[0:P, :]), reads=[f"rstd{slot}"], writes=[f"rstd{slot}"])


def emit_transpose8(cx, hb, P, ident, hT, psb, hkey, tkey, pskey):
    S = cx.S
    psv = psb[:].bitcast(BF16).rearrange("p (k t) -> p k t", t=128)
    for kc in range(8):
        S.op("pe", lambda e, kc=kc: e.transpose(psv[:, kc, 0:P], hb[0:P, kc * 128:(kc + 1) * 128], ident[0:P, 0:P]),
             reads=[hkey, "ident"], writes=[pskey])
    S.op("act", lambda e: e.copy(hT[:, :, 0:P], psv[:, :, 0:P]), reads=[pskey], writes=[tkey])


def emit_A(cx, has_rope=True):
    S = cx.S
    xl = cx.inp("xl", [TPC, D])
    xc = cx.inp("xc", [CPC, D])
    cT = cx.inp("cT", [128, 8, 2])
    ada_w = cx.inp("ada_w", [D, 6 * D])
    ada_b = cx.inp("ada_b", [1, 6 * D])
    norm_g = cx.inp("norm_g", [1, D])
    w_in = cx.inp("w_in", [D, DIN])
    sel_d = cx.inp("sel", [2, 2, 128])
    ident_d = cx.inp("ident", [128, 128], BF16)
    ropec = cx.inp("ropec", [TPC, 16])
    ropes = cx.inp("ropes", [TPC, 16])
    ag_in = cx.inp("ag_in", [NAGC, DIN, 64])
    ag_out = cx.inp("ag_out", [NAGC, 4 * DIN, 64])
    cckey = cx.coll_group()
    identF_d = cx.inp("identF", [128, 128])

    sel = cx.sb("sel", [2, 2, 128])
    ident = cx.sb("ident", [128, 128], BF16)
    identF = cx.sb("identF", [128, 128])
    utT = cx.sb("utT", [128, 20, 128])
    S.dma("sp", identF[:], identF_d, writes=["identF"])
    scT = cx.sb("scT", [128, 8, 2])
    modrow = cx.sb("modrow", [2, 6 * D])
    normg2 = cx.sb("normg2", [2, D])
    grow = cx.sb("grow", [2, D])
    GL = cx.sb("GL", [128, D]); SHL = cx.sb("SHL", [128, D])
    GC = cx.sb("GC", [128, D]); SHC = cx.sb("SHC", [128, D])
    wbf = cx.sb("wbf", [128, 8, DIN], BF16)
    S.dma("sp", sel[:], sel_d, writes=["sel"])
    S.dma("sp", ident[:], ident_d, writes=["ident"])
    S.dma("sp", scT[:], cT, writes=["scT"])
    S.dma("sp", normg2[0:1, :], norm_g, writes=["normg2"])
    S.dma("sp", normg2[1:2, :], norm_g, writes=["normg2"])
    S.op("act", lambda e: e.activation(scT[:], scT[:], AF.Silu), reads=["scT"], writes=["scT"])
    with ExitStack() as st:
        emit_mod_rows(cx, st, scT, ada_w, ada_b, modrow, "A")
        S.dma("sp", cx.over["modrow_s"], modrow[:], reads=["modrow"], writes=["modrow_s"])
        wst = [cx.sb(f"wst{i}", [128, DIN], F32, st) for i in range(2)]
        wv = w_in.rearrange("(kc p) n -> p kc n", p=128)
        for kc in range(8):
            S.dma(("sp", "actq")[kc % 2], wst[kc % 2][:], wv[:, kc, :], writes=[f"wst{kc%2}"])
            eng = ("dve", "pool")[kc % 2]
            S.op(eng, lambda e, kc=kc: e.tensor_copy(wbf[:, kc, :], wst[kc % 2][:]), reads=[f"wst{kc%2}"], writes=["wbf"])
        S.barrier()
    S.op("dve", lambda e: e.scalar_tensor_tensor(grow[:], modrow[:, D:2 * D], 1.0, normg2[:], ALU.add, ALU.mult),
         reads=["modrow", "normg2"], writes=["grow"])
    emit_bcast_row(cx, GL, grow, sel, 0, (0, 1), "grow", "GL")
    emit_bcast_row(cx, SHL, modrow[:, 0:D], sel, 0, (0, 1), "modrow", "SHL")
    emit_bcast_row(cx, GC, grow, sel, 1, (0, 1), "grow", "GC")
    emit_bcast_row(cx, SHC, modrow[:, 0:D], sel, 1, (0, 1), "modrow", "SHC")

    xt = [cx.sb(f"xt{i}", [128, D]) for i in range(2)]
    junk = cx.sb("junk", [128, D], BF16)
    tmp = cx.sb("tmp", [128, D])
    hb = [cx.sb(f"hb{i}", [128, D], BF16) for i in range(2)]
    hT = [cx.sb(f"hT{i}", [128, 8, 128], BF16) for i in range(2)]
    ut = [cx.sb(f"ut{i}", [128, DIN]) for i in range(2)]
    ss = [cx.sb(f"ss{i}", [128, 1]) for i in range(2)]
    rstd = [cx.sb(f"rstd{i}", [128, 1]) for i in range(2)]
    rc = [cx.sb(f"rc{i}", [128, 16]) for i in range(2)]
    rs = [cx.sb(f"rs{i}", [128, 16]) for i in range(2)]
    rt = [cx.sb(f"rt{i}", [128, 16, 16]) for i in range(4)]

    ntl = TPC // 128
    tiles = [("l", i) for i in range(ntl)] + [("c", 0)]

    def load(ti):
        kind, i = tiles[ti]
        s = ti % 2
        if kind == "l":
            S.dma("sp", xt[s][:], xl[i * 128:(i + 1) * 128, :], writes=[f"xt{s}"])
            if has_rope:
                S.dma("sp", rc[s][:], ropec[i * 128:(i + 1) * 128, :], writes=[f"rc{s}"])
                S.dma("sp", rs[s][:], ropes[i * 128:(i + 1) * 128, :], writes=[f"rs{s}"])
        else:
            S.dma("sp", xt[s][0:CPC, :], xc, writes=[f"xt{s}"])

    load(0)
    for ti, (kind, i) in enumerate(tiles):
        s = ti % 2
        P = 128 if kind == "l" else CPC
        G, SH = (GL, SHL) if kind == "l" else (GC, SHC)
        if ti + 1 < len(tiles):
            load(ti + 1)
        emit_rstd(cx, xt[s], P, ss[s], rstd[s], junk, f"xt{s}", s)
        S.op("dve", lambda e, s=s, P=P, G=G: e.scalar_tensor_tensor(tmp[0:P, :], xt[s][0:P, :], rstd[s][0:P, :], G[0:P, :],
                                                                     ALU.mult, ALU.mult),
             reads=[f"xt{s}", f"rstd{s}", "GL", "GC"], writes=["tmp"])
        S.op("dve", lambda e, s=s, P=P, SH=SH: e.tensor_tensor(hb[s][0:P, :], tmp[0:P, :], SH[0:P, :], ALU.add),
             reads=["tmp", "SHL", "SHC"], writes=[f"hb{s}"])
        emit_transpose8(cx, hb[s], P, ident, hT[s], cx.PS[2], f"hb{s}", f"hT{s}", "ps2")
        for cb in range(5):
            ps = cx.PS[3 + cb]
            for kc in range(8):
                S.op("pe", lambda e, ps=ps, kc=kc, cb=cb, s=s, P=P: e.matmul(
                    ps[0:P, :], hT[s][:, kc, 0:P], wbf[:, kc, cb * 512:(cb + 1) * 512], start=(kc == 0), stop=(kc == 7)),
                    reads=[f"hT{s}", "wbf"], writes=[f"ps{3+cb}"])
            if cb % 2 == 0:
                S.op("act", lambda e, ps=ps, cb=cb, s=s, P=P: e.copy(ut[s][0:P, cb * 512:(cb + 1) * 512], ps[0:P, :]),
                     reads=[f"ps{3+cb}"], writes=[f"ut{s}c{cb}"])
            else:
                S.op("dve", lambda e, ps=ps, cb=cb, s=s, P=P: e.tensor_copy(ut[s][0:P, cb * 512:(cb + 1) * 512], ps[0:P, :]),
                     reads=[f"ps{3+cb}"], writes=[f"ut{s}c{cb}"])
        if kind == "l" and has_rope:
            xv = ut[s][:, 0:512].rearrange("p (g d) -> p g d", d=32)
            x1 = xv[:, :, 0:16]
            x2 = xv[:, :, 16:32]
            cb_ = rc[s][:].unsqueeze(1).to_broadcast([128, 16, 16])
            sb_ = rs[s][:].unsqueeze(1).to_broadcast([128, 16, 16])
            S.op("dve", lambda e, x1=x1, cb_=cb_: e.tensor_tensor(rt[0][:], x1, cb_, ALU.mult), reads=[f"ut{s}c0", f"rc{s}"], writes=["rt0"])
            S.op("pool", lambda e, x2=x2, sb_=sb_: e.tensor_tensor(rt[1][:], x2, sb_, ALU.mult), reads=[f"ut{s}c0", f"rs{s}"], writes=["rt1"])
            S.op("dve", lambda e, x2=x2, cb_=cb_: e.tensor_tensor(rt[2][:], x2, cb_, ALU.mult), reads=[f"ut{s}c0", f"rc{s}"], writes=["rt2"])
            S.op("pool", lambda e, x1=x1, sb_=sb_: e.tensor_tensor(rt[3][:], x1, sb_, ALU.mult), reads=[f"ut{s}c0", f"rs{s}"], writes=["rt3"])
            S.op("dve", lambda e, x1=x1: e.tensor_tensor(x1, rt[0][:], rt[1][:], ALU.subtract), reads=["rt0", "rt1"], writes=[f"ut{s}c0"])
            S.op("pool", lambda e, x2=x2: e.tensor_tensor(x2, rt[2][:], rt[3][:], ALU.add), reads=["rt2", "rt3", f"ut{s}c0"], writes=[f"ut{s}c0"])
        for q4 in range(5):
            psq = cx.PS[q4 % 2]
            for j4 in range(4):
                cc = q4 * 4 + j4
                S.op("pe", lambda e, psq=psq, j4=j4, cc=cc, s=s, P=P: e.matmul(psq[:, j4 * 128:j4 * 128 + P], ut[s][0:P, cc * 128:(cc + 1) * 128],
                                                                            identF[0:P, 0:P], start=True, stop=True),
                     reads=[f"ut{s}c{cc // 4}", "identF"], writes=[f"ps{q4 % 2}"])
            pqv = psq[:].rearrange("p (j t) -> p j t", t=128)
            S.op(("act", "dve")[q4 % 2], lambda e, pqv=pqv, q4=q4, P=P: (e.copy if hasattr(e, "copy") else e.tensor_copy)(utT[:, q4 * 4:(q4 + 1) * 4, 0:P], pqv[:, :, 0:P]),
                 reads=[f"ps{q4 % 2}"], writes=["utT"])
        for hf in range(P // 64):
            ck = (2 * i + hf) if kind == "l" else NAGC - 1
            S.dma("poolq", ag_in[ck].rearrange("(cc p) t -> p cc t", p=128), utT[:, :, hf * 64:(hf + 1) * 64], reads=["utT"], writes=[f"ag_in{ck}"])
            cx.coll(cckey, "AllGather", ALU.bypass, GROUPS, ag_in[ck], ag_out[ck], [f"ag_in{ck}"])
    cx.end_phase()


def _bf16(a):
    import ml_dtypes
    return np.asarray(a, dtype=np.float32).astype(ml_dtypes.bfloat16)


def const_sel():
    s = np.zeros((2, 2, 128), np.float32)
    s[0, 0, :] = 1.0
    s[1, 1, :] = 1.0
    return s


def const_rope():
    inv = 10000.0 ** (-np.arange(8, dtype=np.float32) / 8)
    t = np.arange(SEQ)
    row = (t // 64).astype(np.float32)
    col = (t % 64).astype(np.float32)
    ang = np.concatenate([row[:, None] * inv, col[:, None] * inv], axis=-1).astype(np.float32)
    return np.cos(ang).astype(np.float32), np.sin(ang).astype(np.float32)


def cT_layout(c_b, c_ctx):
    a = np.stack([c_b, c_ctx], axis=-1)
    return np.ascontiguousarray(a.reshape(8, 128, 2).transpose(1, 0, 2))


WIDE_EXP = False


class Attn:
    GROUPS_ = ((0, 1, 2), (5, 6, 7))
    G = 3

    def __init__(self, cx, ident):
        self.cx = cx
        self.ident = ident
        self.PT = [cx.sb(f"PT{i}", [128, self.G * 512], BF16) for i in range(2)]
        self.it = 0

    def run(self, QT, N, chunks, pso, psokey, qkeys):
        cx, S = self.cx, self.cx.S
        n = len(chunks)
        G = self.G
        ngrp = (n + G - 1) // G

        def qk(p):
            slot = (self.it + p) % 2
            for h_ in range(G):
                i = G * p + h_
                if i >= n:
                    continue
                KT, V, bias, keys = chunks[i]
                bank = self.GROUPS_[slot][h_]
                ps = cx.PS[bank]
                S.op("pe", lambda e, ps=ps, KT=KT, bias=bias: e.matmul(ps[:, 0:N], KT, QT, start=True, stop=(bias is None)),
                     reads=list(keys) + list(qkeys), writes=[f"ps{bank}"])
                if bias is not None:
                    S.op("pe", lambda e, ps=ps, bias=bias: e.matmul(ps[:, 0:N], self.ident[:], bias, start=False, stop=True),
                         reads=["bias", "ident"], writes=[f"ps{bank}"])
        qk(0)
        for p in range(ngrp):
            if p + 1 < ngrp:
                qk(p + 1)
            slot = (self.it + p) % 2
            PT = self.PT[slot]
            m = min(G, n - G * p)
            for h_ in range(m):
                bank = self.GROUPS_[slot][h_]
                S.op("act", lambda e, bank=bank, PT=PT, h_=h_: e.activation(PT[:, h_ * 512:h_ * 512 + N], cx.PS[bank][:, 0:N], AF.Exp),
                     reads=[f"ps{bank}"], writes=[f"PT{slot}_{h_}"])
            for h_ in range(m):
                i = G * p + h_
                V = chunks[i][1]
                S.op("pe", lambda e, PT=PT, V=V, i=i, h_=h_: e.matmul(pso[0:65, 0:N], V, PT[:, h_ * 512:h_ * 512 + N], start=(i == 0), stop=(i == n - 1)),
                     reads=[f"PT{slot}_{h_}", "vaug"], writes=[psokey])
        self.it += ngrp


def emit_cast_rows(cx, stg, dst, src, rows, cols, key, scale=None, chunk=2048):
    S = cx.S
    nch = (cols + chunk - 1) // chunk
    for c in range(nch):
        w = min(chunk, cols - c * chunk)
        sl = slice(c * chunk, c * chunk + w)
        S.dma(("sp", "poolq")[c % 2], stg[c % 2][0:rows, 0:w], src[:, sl], writes=[f"stg{c%2}"])
        if scale is None:
            S.op("dve", lambda e, c=c, w=w, sl=sl: e.tensor_copy(dst[0:rows, sl], stg[c % 2][0:rows, 0:w]),
                 reads=[f"stg{c%2}"], writes=[key])
        else:
            S.op("dve", lambda e, c=c, w=w, sl=sl: e.tensor_scalar(dst[0:rows, sl], stg[c % 2][0:rows, 0:w], scale, None, ALU.mult),
                 reads=[f"stg{c%2}"], writes=[key])


def emit_load_v(cx, stg, Vaug, vsrc, nk, key):
    S = cx.S
    nch = nk // 128
    vv = vsrc.rearrange("(c p) d -> p c d", p=128)
    step = 26
    for i, c0 in enumerate(range(0, nch, step)):
        c1 = min(nch, c0 + step)
        sv = stg[i % 2][:, 0:(c1 - c0) * 65].rearrange("p (c d) -> p c d", d=65)
        S.dma(("sp", "poolq")[i % 2], sv, vv[:, c0:c1, :], writes=[f"stg{i%2}"])
        S.op("dve", lambda e, sv=sv, c0=c0, c1=c1: e.tensor_copy(Vaug[:, c0:c1, :], sv), reads=[f"stg{i%2}"], writes=[key])


def emit_qbound(cx, st, QT, d, N_total, kfac, ones_b, qsrc, key):
    S = cx.S
    sq = [cx.sb(f"sq_{key}{i}", [d, 512], BF16, st) for i in range(2)]
    for c in range(N_total // 512 if N_total >= 512 else 1):
        w = min(512, N_total)
        sl = slice(c * 512, c * 512 + w)
        S.op("dve", lambda e, c=c, sl=sl, w=w: e.tensor_tensor(sq[c % 2][:, 0:w], QT[0:d, sl], QT[0:d, sl], ALU.mult),
             reads=[key], writes=[f"sq{c%2}"])
        ps = cx.PS[6 + c % 2]
        S.op("pe", lambda e, ps=ps, c=c, w=w: e.matmul(ps[0:d + 1, 0:w], ones_b[0:d, 0:d + 1], sq[c % 2][:, 0:w], start=True, stop=True),
             reads=[f"sq{c%2}", "ones_b"], writes=[f"ps{6+c%2}"])
        S.op("act", lambda e, ps=ps, sl=sl, w=w: e.sqrt(QT[d:d + 1, sl], ps[d:d + 1, 0:w]),
             reads=[f"ps{6+c%2}"], writes=[key + "r"])
        S.op("dve", lambda e, sl=sl: e.tensor_scalar(QT[d:d + 1, sl], QT[d:d + 1, sl], kfac[d:d + 1, 0:1], -1.0, ALU.mult, ALU.mult),
             reads=[key + "r", "kfac"], writes=[key + "r"])


def emit_kmax(cx, st, KT, d, nk, kfac, ones_b, key):
    S = cx.S
    sq = [cx.sb(f"ksq_{key}{i}", [d, 512], BF16, st) for i in range(2)]
    kmx = cx.sb(f"kmx_{key}", [d + 1, 64], F32, st)
    S.op("pool", lambda e: e.memset(kmx[:], 0.0), writes=["kmx"])
    nch = (nk + 511) // 512
    for c in range(nch):
        w = min(512, nk - c * 512)
        sl = slice(c * 512, c * 512 + w)
        S.op("dve", lambda e, c=c, sl=sl, w=w: e.tensor_tensor(sq[c % 2][:, 0:w], KT[0:d, sl], KT[0:d, sl], ALU.mult),
             reads=[key], writes=[f"sq{c%2}"])
        ps = cx.PS[6 + c % 2]
        S.op("pe", lambda e, ps=ps, c=c, w=w: e.matmul(ps[0:d + 1, 0:w], ones_b[0:d, 0:d + 1], sq[c % 2][:, 0:w], start=True, stop=True),
             reads=[f"sq{c%2}", "ones_b"], writes=[f"ps{6+c%2}"])
        S.op("dve", lambda e, ps=ps, c=c, w=w: e.tensor_reduce(kmx[d:d + 1, c:c + 1], ps[d:d + 1, 0:w], AX.X, ALU.max),
             reads=[f"ps{6+c%2}"], writes=["kmx"])
    S.op("dve", lambda e: e.tensor_reduce(kfac[d:d + 1, 0:1], kmx[d:d + 1, 0:nch], AX.X, ALU.max), reads=["kmx"], writes=["kfac"])
    S.op("act", lambda e: e.sqrt(kfac[d:d + 1, 0:1], kfac[d:d + 1, 0:1]), reads=["kfac"], writes=["kfac"])


def emit_finalize(cx, pso, N, recrow, osb, E65, tdst, psokey, tkey):
    S = cx.S
    S.op("dve", lambda e: e.reciprocal(recrow[64:65, 0:N], pso[64:65, 0:N]), reads=[psokey], writes=["recrow"])
    S.op("pe", lambda e: e.matmul(cx.PS[5][0:64, 0:N], E65[0:65, 0:64], recrow[0:65, 0:N], start=True, stop=True),
         reads=["recrow", "E65"], writes=["ps5"])
    S.op("act", lambda e: e.copy(osb[0:64, 0:N], pso[0:64, 0:N]), reads=[psokey], writes=["osb"])
    S.op("dve", lambda e: e.tensor_tensor(tdst[0:64, 0:N], osb[0:64, 0:N], cx.PS[5][0:64, 0:N], ALU.mult),
         reads=["osb", "ps5"], writes=[tkey])


def emit_B1(cx, li):
    lam_init = 0.8 - 0.6 * math.exp(-0.3 * li)
    NK = SEQ + CTX
    S = cx.S
    aqT = cx.inp("aqT", [2, 32, SEQ]); akT = cx.inp("akT", [2, 33, NK]); av = cx.inp("av", [NK, 65])
    aqcT = cx.inp("aqcT", [2, 32, CTX])
    alam = cx.inp("alam", [1, 128]); subg = cx.inp("subg", [64, 1])
    bqT = cx.inp("bqT", [64, SEQ]); bkT = cx.inp("bkT", [65, NK]); bv = cx.inp("bv", [NK, 65])
    bqcT = cx.inp("bqcT", [64, CTX])
    bias_d = cx.inp("bias", [3, 8, 128, 512])
    ident_d = cx.inp("ident", [128, 128], BF16); onesb_d = cx.inp("onesb", [128, 128], BF16)
    E65_d = cx.inp("E65", [65, 64]); ones64_d = cx.inp("ones64", [64, 64])
    oa = cx.out("oa", [64, SEQ]); oac = cx.out("oac", [64, CTX])
    ob = cx.out("ob", [64, SEQ]); obc = cx.out("obc", [64, CTX])

    ident = cx.sb("ident", [128, 128], BF16); ones_b = cx.sb("onesb", [128, 128], BF16)
    E65 = cx.sb("E65", [65, 64]); ones64 = cx.sb("ones64", [64, 64])
    S.dma("sp", ident[:], ident_d, writes=["ident"]); S.dma("sp", ones_b[:], onesb_d, writes=["ones_b"])
    S.dma("sp", E65[:], E65_d, writes=["E65"]); S.dma("sp", ones64[:], ones64_d, writes=["ones64"])
    at = Attn(cx, ident)
    recrow = cx.sb("recrow", [65, 512]); osb = cx.sb("osb", [64, 512])
    t0 = cx.sb("t0", [64, 512]); t1 = cx.sb("t1", [64, 512]); t2 = cx.sb("t2", [64, 512])
    kfac = cx.sb("kfac", [65, 1])
    stg = [cx.sb(f"stg{i}", [128, 2048]) for i in range(2)]
    S.op("pool", lambda e: e.memset(recrow[:], 0.0), writes=["recrow"])
    lrow = cx.sb("lrow", [1, 128]); lsum = cx.sb("lsum", [1, 4]); neglam = cx.sb("neglam", [64, 1]); gsc = cx.sb("gsc", [64, 1])
    S.dma("sp", lrow[:], alam, writes=["lrow"]); S.dma("sp", gsc[:], subg, writes=["gsc"])
    S.op("pool", lambda e: e.memset(lsum[:], 0.0), writes=["lsum"])
    S.op("dve", lambda e: e.tensor_tensor(lrow[:, 0:32], lrow[:, 0:32], lrow[:, 32:64], ALU.mult), reads=["lrow"], writes=["lrow"])
    S.op("dve", lambda e: e.tensor_tensor(lrow[:, 64:96], lrow[:, 64:96], lrow[:, 96:128], ALU.mult), reads=["lrow"], writes=["lrow"])
    S.op("dve", lambda e: e.tensor_reduce(lsum[:, 0:1], lrow[:, 0:32], AX.X, ALU.add), reads=["lrow", "lsum"], writes=["lsum"])
    S.op("dve", lambda e: e.tensor_reduce(lsum[:, 1:2], lrow[:, 64:96], AX.X, ALU.add), reads=["lrow", "lsum"], writes=["lsum"])
    S.op("act", lambda e: e.activation(lsum[:, 0:2], lsum[:, 0:2], AF.Exp), reads=["lsum"], writes=["lsum"])
    S.op("dve", lambda e: e.tensor_tensor(lsum[:, 2:3], lsum[:, 1:2], lsum[:, 0:1], ALU.subtract), reads=["lsum"], writes=["lsum"])
    S.op("dve", lambda e: e.tensor_scalar(lsum[:, 2:3], lsum[:, 2:3], -lam_init, None, ALU.add), reads=["lsum"], writes=["lsum"])
    S.op("pe", lambda e: e.matmul(cx.PS[7][0:64, 0:1], ones64[0:1, 0:64], lsum[0:1, 2:3], start=True, stop=True),
         reads=["lsum", "ones64"], writes=["ps7"])
    S.op("act", lambda e: e.copy(neglam[:], cx.PS[7][0:64, 0:1]), reads=["ps7"], writes=["neglam"])
    S.op("dve", lambda e: e.tensor_scalar(gsc[:], gsc[:], 1.0 - lam_init, None, ALU.mult), reads=["gsc"], writes=["gsc"])

    with ExitStack() as st:
        QT = [cx.sb(f"aQT{c}", [33, SEQ], BF16, st) for c in range(2)]
        QTc = [cx.sb(f"aQTc{c}", [33, CTX], BF16, st) for c in range(2)]
        KT = [cx.sb(f"aKT{c}", [33, NK], BF16, st) for c in range(2)]
        Va = cx.sb("aV", [128, NK // 128, 65], BF16, st)
        sc = 32 ** -0.5
        for c in range(2):
            emit_cast_rows(cx, stg, KT[c], akT[c], 33, NK, f"akt{c}")
            emit_cast_rows(cx, stg, QT[c], aqT[c], 32, SEQ, f"aqt{c}", scale=sc)
            emit_cast_rows(cx, stg, QTc[c], aqcT[c], 32, CTX, f"aqtc{c}", scale=sc)
        emit_load_v(cx, stg, Va, av, NK, "vaug")
        for c in range(2):
            emit_kmax(cx, st, KT[c], 32, NK, kfac, ones_b, f"akt{c}")
            emit_qbound(cx, st, QT[c], 32, SEQ, kfac, ones_b, None, f"aqt{c}")
            emit_qbound(cx, st, QTc[c], 32, CTX, kfac, ones_b, None, f"aqtc{c}")

        def diff_block(qts, N, chunk_ids, odst, qkeys):
            for c in range(2):
                chunks = [(KT[c][:, k * 128:(k + 1) * 128], Va[:, k, :], None, (f"akt{c}",)) for k in chunk_ids]
                at.run(qts[c], N, chunks, cx.PS[3 + c], f"ps{3+c}", [qkeys[c], qkeys[c] + "r"])
            emit_finalize(cx, cx.PS[3], N, recrow, osb, E65, t0, "ps3", "t0")
            emit_finalize(cx, cx.PS[4], N, recrow, osb, E65, t1, "ps4", "t1")
            S.op("dve", lambda e: e.scalar_tensor_tensor(t0[:, 0:N], t1[:, 0:N], neglam[:, 0:1], t0[:, 0:N], ALU.mult, ALU.add),
                 reads=["t0", "t1", "neglam"], writes=["t0"])
            S.op("act", lambda e: e.activation(t1[:, 0:N], t0[:, 0:N], AF.Square), reads=["t0"], writes=["t1"])
            S.op("pe", lambda e: e.matmul(cx.PS[2][0:64, 0:N], ones64[:, :], t1[:, 0:N], start=True, stop=True),
                 reads=["t1", "ones64"], writes=["ps2"])
            S.op("dve", lambda e: e.tensor_scalar(t1[:, 0:N], cx.PS[2][0:64, 0:N], 1.0 / 64, EPS, ALU.mult, ALU.add),
                 reads=["ps2"], writes=["t1"])
            S.op("act", lambda e: e.sqrt(t1[:, 0:N], t1[:, 0:N]), reads=["t1"], writes=["t1"])
            S.op("dve", lambda e: e.reciprocal(t1[:, 0:N], t1[:, 0:N]), reads=["t1"], writes=["t1"])
            S.op("dve", lambda e: e.scalar_tensor_tensor(t2[:, 0:N], t0[:, 0:N], gsc[:, 0:1], t1[:, 0:N], ALU.mult, ALU.mult),
                 reads=["t0", "t1", "gsc"], writes=["t2"])
            S.dma("poolq", odst, t2[:, 0:N], reads=["t2"], writes=["oa"])

        for qb in range(SEQ // 512):
            sl = slice(qb * 512, (qb + 1) * 512)
            diff_block([QT[0][:, sl], QT[1][:, sl]], 512, range(NK // 128), oa[:, sl], ["aqt0", "aqt1"])
        diff_block([QTc[0][:, :], QTc[1][:, :]], CTX, range(SEQ // 128, NK // 128), oac[:, :], ["aqtc0", "aqtc1"])
        S.barrier()

    with ExitStack() as st:
        QT = cx.sb("bQT", [65, SEQ], BF16, st); QTc = cx.sb("bQTc", [65, CTX], BF16, st)
        KT = cx.sb("bKT", [65, NK], BF16, st); Vb = cx.sb("bV", [128, NK // 128, 65], BF16, st)
        bias = cx.sb("bias", [128, 3, 8, 512], BF16, st)
        sc = 64 ** -0.5
        emit_cast_rows(cx, stg, KT, bkT, 65, NK, "bkt")
        emit_cast_rows(cx, stg, QT, bqT, 64, SEQ, "bqt", scale=sc)
        emit_cast_rows(cx, stg, QTc, bqcT, 64, CTX, "bqtc", scale=sc)
        emit_load_v(cx, stg, Vb, bv, NK, "vaug")
        for s_ in range(3):
            for j in range(8):
                i = s_ * 8 + j
                S.dma(("sp", "poolq")[i % 2], stg[i % 2][:, 0:512], bias_d[s_, j], writes=[f"stg{i%2}"])
                S.op("dve", lambda e, i=i, s_=s_, j=j: e.tensor_copy(bias[:, s_, j, :], stg[i % 2][:, 0:512]),
                     reads=[f"stg{i%2}"], writes=["bias"])
        emit_kmax(cx, st, KT, 64, NK, kfac, ones_b, "bkt")
        emit_qbound(cx, st, QT, 64, SEQ, kfac, ones_b, None, "bqt")
        emit_qbound(cx, st, QTc, 64, CTX, kfac, ones_b, None, "bqtc")
        for qb in range(32):
            R0 = qb * 8
            if qb == 0:
                bset, kr0 = 0, 0
            elif qb == 31:
                bset, kr0 = 2, 240
            else:
                bset, kr0 = 1, R0 - 4
            sl = slice(qb * 512, (qb + 1) * 512)
            chunks = [(KT[:, (kr0 // 2 + j) * 128:(kr0 // 2 + j + 1) * 128], Vb[:, kr0 // 2 + j, :], bias[:, bset, j, :], ("bkt",))
                      for j in range(8)]
            chunks += [(KT[:, k * 128:(k + 1) * 128], Vb[:, k, :], None, ("bkt",)) for k in range(SEQ // 128, NK // 128)]
            at.run(QT[:, sl], 512, chunks, cx.PS[3], "ps3", ["bqt", "bqtr"])
            emit_finalize(cx, cx.PS[3], 512, recrow, osb, E65, t0, "ps3", "t0")
            S.dma("poolq", ob[:, sl], t0[:, 0:512], reads=["t0"], writes=["ob"])
        chunks = [(KT[:, k * 128:(k + 1) * 128], Vb[:, k, :], None, ("bkt",)) for k in range(SEQ // 128, NK // 128)]
        at.run(QTc[:, :], CTX, chunks, cx.PS[3], "ps3", ["bqtc", "bqtcr"])
        emit_finalize(cx, cx.PS[3], CTX, recrow, osb, E65, t0, "ps3", "t0")
        S.dma("poolq", obc[:, :], t0[:, 0:CTX], reads=["t0"], writes=["obc"])
        S.barrier()
    cx.end_phase()


def na_bias_sets(rpb_h):
    out = np.full((3, 8, 128, 512), -30000.0, np.float32)
    cq = np.arange(64); ck = np.arange(64)
    c0 = np.clip(cq - 8, 0, 48)
    colok = (ck[:, None] >= c0[None, :]) & (ck[:, None] < c0[None, :] + 16)
    dc = np.clip(ck[:, None] - cq[None, :], -15, 15) + 15
    for s_, (R0, kr0) in enumerate([(0, 0), (8, 4), (248, 240)]):
        for j in range(8):
            for krl in range(2):
                kr = kr0 + 2 * j + krl
                for qrl in range(8):
                    r = R0 + qrl
                    r0 = min(max(r - 4, 0), 248)
                    if not (r0 <= kr < r0 + 8):
                        continue
                    dr = kr - r + 7
                    blk = np.where(colok, rpb_h[dr][dc], np.float32(-30000.0))
                    out[s_, j, krl * 64:(krl + 1) * 64, qrl * 64:(qrl + 1) * 64] = blk
    return out


def emit_C(cx, with_ctx, final):
    S = cx.S
    xl = cx.inp("xl", [TPC, D]); xc = cx.inp("xc", [CPC, D])
    rs_out = cx.inp("rs_out", [TPC + CPC, D])
    cT = cx.inp("cT", [128, 8, 2])
    ada_w = cx.inp("ada_w", [D, 6 * D]); ada_b = cx.inp("ada_b", [1, 6 * D])
    norm_g = cx.inp("norm2_g", [1, D]); fin_g = cx.inp("fin_g", [1, D])
    router_w = cx.inp("router_w", [D, 16]); router_b = cx.inp("router_b", [1, 16])
    w1 = cx.inp("w1", [16, D, 512]); w3 = cx.inp("w3", [16, D, 512]); w2 = cx.inp("w2", [16, 512, D])
    sel_d = cx.inp("sel", [2, 2, 128]); identf_d = cx.inp("ident", [128, 128], BF16)
    ol = cx.out("ol", [TPC, D]); oc = cx.out("oc", [CPC, D])

    sel = cx.sb("sel", [2, 2, 128]); identf = cx.sb("identb", [128, 128], BF16)
    rwh = cx.sb("rwh", [128, 8, 16], BF16); rwl = cx.sb("rwl", [128, 8, 16], BF16)
    G1 = cx.sb("G1", [128, D]); G2 = cx.sb("G2", [128, D]); SH2 = cx.sb("SH2", [128, D]); GG2 = cx.sb("GG2", [128, D])
    FG = cx.sb("FG", [128, D])
    rw = cx.sb("rw", [128, 8, 16]); rb = cx.sb("rb", [128, 16])
    bcs = cx.scratch("bc_scr", [4, 128, D])
    S.dma("sp", sel[:], sel_d, writes=["sel"]); S.dma("sp", identf[:], identf_d, writes=["identf"])
    S.dma("sp", rw[:], router_w.rearrange("(kc p) n -> p kc n", p=128), writes=["rw"])
    S.dma("sp", rb[:], router_b.partition_broadcast(128), writes=["rb"])
    S.op("dve", lambda e: e.tensor_copy(rwh[:], rw[:]), reads=["rw"], writes=["rwh"])
    S.op("dve", lambda e: e.tensor_tensor(rwl[:], rw[:], rwh[:], ALU.subtract), reads=["rw", "rwh"], writes=["rwl"])
    with ExitStack() as st:
        scT = cx.sb("scT", [128, 8, 2], F32, st); modrow = cx.sb("modrow", [2, 6 * D], F32, st)
        normg2 = cx.sb("normg2", [2, D], F32, st); grow = cx.sb("grow", [2, D], F32, st); fing2 = cx.sb("fing2", [2, D], F32, st)
        S.dma("sp", scT[:], cT, writes=["scT"])
        for r in range(2):
            S.dma("sp", normg2[r:r + 1, :], norm_g, writes=["normg2"])
            S.dma("sp", fing2[r:r + 1, :], fin_g, writes=["fing2"])
        S.op("act", lambda e: e.activation(scT[:], scT[:], AF.Silu), reads=["scT"], writes=["scT"])
        with ExitStack() as st2:
            S.dma("sp", modrow[:], cx.over["modrow_s"], writes=["modrow"])
            S.barrier()
        S.op("dve", lambda e: e.scalar_tensor_tensor(grow[:], modrow[:, 4 * D:5 * D], 1.0, normg2[:], ALU.add, ALU.mult),
             reads=["modrow", "normg2"], writes=["grow"])

        def set_bcast(which):
            emit_bcast_row(cx, G1, modrow[:, 2 * D:3 * D], sel, which, (0, 1), "modrow", "G1")
            emit_bcast_row(cx, G2, grow, sel, which, (0, 1), "grow", "G2")
            emit_bcast_row(cx, SH2, modrow[:, 3 * D:4 * D], sel, which, (0, 1), "modrow", "SH2")
            emit_bcast_row(cx, GG2, modrow[:, 5 * D:6 * D], sel, which, (0, 1), "modrow", "GG2")
        set_bcast(1)
        for i_, (t_, k_) in enumerate(((G1, "G1"), (G2, "G2"), (SH2, "SH2"), (GG2, "GG2"))):
            S.dma("sp", bcs[i_], t_[:], reads=[k_], writes=["bcs"])
        set_bcast(0)
        emit_bcast_row(cx, FG, fing2, sel, 0, (0, 1), "fing2", "FG")
        S.barrier()

    def load_ctx_bcast():
        for i_, (t_, k_) in enumerate(((G1, "G1"), (G2, "G2"), (SH2, "SH2"), (GG2, "GG2"))):
            S.dma("sp", t_[:], bcs[i_], reads=["bcs"], writes=[k_])

    GT = 8
    x1 = cx.sb("x1", [128, GT, D]); yacc = cx.sb("yacc", [128, GT, D])
    hT = cx.sb("hT", [128, 8, GT * 128], BF16)
    gate = cx.sb("gate", [128, GT, 16])
    xt = [cx.sb(f"xt{i}", [128, D]) for i in range(2)]
    mpt = [cx.sb("mpt0", [128, D])] * 2
    junk = cx.sb("junk", [128, D], BF16); tmp = cx.sb("tmp", [128, D]); h2 = cx.sb("h2", [128, D])
    hTl = cx.sb("hTl", [128, 8, 128], BF16); h2hi = cx.sb("h2hi", [128, D], BF16); h2lo = cx.sb("h2lo", [128, D], BF16)
    ss = cx.sb("ss", [128, 1]); rstd = cx.sb("rstd", [128, 1])
    r_ = {k: cx.sb("r_" + k, [128, 16]) for k in ("ex", "sc", "sel", "eq", "s2", "selm", "k1", "sm2", "k2", "w")}
    q_ = {k: cx.sb("q_" + k, [128, 4]) for k in ("m1", "m2", "gs", "gmask", "pen")}
    c_ = {k: cx.sb("c_" + k, [128, 1]) for k in ("mx", "sm", "gm", "e1", "e2", "ws")}
    W13 = [cx.sb(f"W13_{i}", [128, 2, 8, 512], BF16) for i in range(2)]
    W2 = [cx.sb(f"W2_{i}", [128, 4, D], BF16) for i in range(2)]
    hs = cx.sb("hs", [128, 512]); hh = [cx.sb(f"hh{i}", [128, 4, 512], BF16) for i in range(2)]
    stage_n = [0]
    conv_st = ExitStack()
    wstg = [cx.sb(f"wstg{i}", [128, 4, 512], F32, conv_st) for i in range(2)]

    def router(P, g):
        lg = cx.PS[2]
        n_ = 0
        for (lhs, rhs) in (("hi", rwh), ("lo", rwh), ("hi", rwl)):
            for kc in range(8):
                lt = hT[:, kc, g * 128:g * 128 + P] if lhs == "hi" else hTl[:, kc, 0:P]
                S.op("pe", lambda e, lt=lt, rhs=rhs, kc=kc, n_=n_: e.matmul(lg[0:P, 0:16], lt, rhs[:, kc, :], start=(n_ == 0), stop=(n_ == 23)),
                     reads=["hT", "hTl", "rwh", "rwl"], writes=["ps2"])
                n_ += 1
        V = lambda k: r_[k][0:P, :]
        V4 = lambda k: r_[k][0:P, :].rearrange("p (g e) -> p g e", e=4)
        Q = lambda k: q_[k][0:P, :]
        Cc = lambda k: c_[k][0:P, :]
        dv = lambda fn, rd, wr: S.op("dve", fn, reads=rd, writes=wr)
        dv(lambda e: e.tensor_reduce(Cc("mx"), lg[0:P, 0:16], AX.X, ALU.max), ["ps2"], ["c_mx"])
        dv(lambda e: e.tensor_scalar(Cc("mx"), Cc("mx"), -1.0, None, ALU.mult), ["c_mx"], ["c_mx"])
        S.op("pool", lambda e: e.memset(Cc("sm"), 0.0), writes=["c_sm"])
        S.op("act", lambda e: e.activation(V("ex"), lg[0:P, 0:16], AF.Exp, bias=Cc("mx"), accum_out=Cc("sm")),
             reads=["ps2", "c_mx", "c_sm"], writes=["r_ex", "c_sm"])
        dv(lambda e: e.reciprocal(Cc("sm"), Cc("sm")), ["c_sm"], ["c_sm"])
        dv(lambda e: e.tensor_scalar(V("sc"), V("ex"), Cc("sm"), None, ALU.mult), ["r_ex", "c_sm"], ["r_sc"])
        dv(lambda e: e.tensor_tensor(V("sel"), V("sc"), rb[0:P, :], ALU.add), ["r_sc", "rb"], ["r_sel"])
        dv(lambda e: e.tensor_reduce(Q("m1"), V4("sel"), AX.X, ALU.max), ["r_sel"], ["q_m1"])
        dv(lambda e: e.tensor_tensor(V4("eq"), V4("sel"), Q("m1").unsqueeze(2).to_broadcast([P, 4, 4]), ALU.is_equal), ["r_sel", "q_m1"], ["r_eq"])
        dv(lambda e: e.scalar_tensor_tensor(V("s2"), V("eq"), -1e9, V("sel"), ALU.mult, ALU.add), ["r_eq", "r_sel"], ["r_s2"])
        dv(lambda e: e.tensor_reduce(Q("m2"), V4("s2"), AX.X, ALU.max), ["r_s2"], ["q_m2"])
        dv(lambda e: e.tensor_tensor(Q("gs"), Q("m1"), Q("m2"), ALU.add), ["q_m1", "q_m2"], ["q_gs"])
        dv(lambda e: e.tensor_reduce(Cc("gm"), Q("gs"), AX.X, ALU.max), ["q_gs"], ["c_gm"])
        dv(lambda e: e.tensor_scalar(Q("gmask"), Q("gs"), Cc("gm"), None, ALU.is_equal), ["q_gs", "c_gm"], ["q_gmask"])
        dv(lambda e: e.tensor_scalar(Q("pen"), Q("gmask"), -1.0, 1e9, ALU.add, ALU.mult), ["q_gmask"], ["q_pen"])
        dv(lambda e: e.tensor_tensor(V4("selm"), V4("sel"), Q("pen").unsqueeze(2).to_broadcast([P, 4, 4]), ALU.add), ["r_sel", "q_pen"], ["r_selm"])
        dv(lambda e: e.tensor_reduce(Cc("e1"), V("selm"), AX.X, ALU.max), ["r_selm"], ["c_e1"])
        dv(lambda e: e.tensor_scalar(V("k1"), V("selm"), Cc("e1"), None, ALU.is_equal), ["r_selm", "c_e1"], ["r_k1"])
        dv(lambda e: e.scalar_tensor_tensor(V("sm2"), V("k1"), -1e9, V("selm"), ALU.mult, ALU.add), ["r_k1", "r_selm"], ["r_sm2"])
        dv(lambda e: e.tensor_reduce(Cc("e2"), V("sm2"), AX.X, ALU.max), ["r_sm2"], ["c_e2"])
        dv(lambda e: e.tensor_scalar(V("k2"), V("sm2"), Cc("e2"), None, ALU.is_equal), ["r_sm2", "c_e2"], ["r_k2"])
        dv(lambda e: e.tensor_tensor(V("k1"), V("k1"), V("k2"), ALU.add), ["r_k1", "r_k2"], ["r_k1"])
        dv(lambda e: e.tensor_tensor(V("w"), V("sc"), V("k1"), ALU.mult), ["r_sc", "r_k1"], ["r_w"])
        dv(lambda e: e.tensor_reduce(Cc("ws"), V("w"), AX.X, ALU.add), ["r_w"], ["c_ws"])
        dv(lambda e: e.reciprocal(Cc("ws"), Cc("ws")), ["c_ws"], ["c_ws"])
        dv(lambda e: e.tensor_scalar(gate[0:P, g, :], V("w"), Cc("ws"), None, ALU.mult), ["r_w", "c_ws"], ["gate"])

    wb13 = cx.scratch("wb13", [16, 128, 2 * 8 * 512], BF16)
    wb2 = cx.scratch("wb2", [16, 128, 4 * D], BF16)

    def convert_expert(e_, slot):
        pieces = []
        for wi, wsrc in enumerate((w1, w3)):
            v = wsrc[e_].rearrange("(kc p) n -> p kc n", p=128)
            for hf in range(2):
                pieces.append((v[:, hf * 4:(hf + 1) * 4, :], W13[slot][:, wi, hf * 4:(hf + 1) * 4, :]))
        v2 = w2[e_].rearrange("(fc p) n -> p fc n", p=128)
        for hf in range(2):
            pieces.append((v2[:, :, hf * 512:(hf + 1) * 512], W2[slot][:, :, hf * 512:(hf + 1) * 512]))
        for src, dst in pieces:
            n = stage_n[0]; stage_n[0] += 1
            sg = wstg[n % 2]
            S.dma(("sp", "actq")[n % 2], sg[:], src, writes=[f"wstg{n%2}"])
            S.op(("pool", "dve")[n % 2], lambda e, sg=sg, dst=dst: e.tensor_copy(dst, sg[:]), reads=[f"wstg{n%2}"], writes=[f"W{slot}"])
        S.dma("poolq", wb13[e_], W13[slot][:].rearrange("p a b c -> p (a b c)"), reads=[f"W{slot}"], writes=["wb"])
        S.dma("poolq", wb2[e_], W2[slot][:].rearrange("p a b -> p (a b)"), reads=[f"W{slot}"], writes=["wb"])

    for e_ in range(16):
        convert_expert(e_, e_ % 2)
    S.barrier()
    conv_st.close()

    def load_expert(e_, slot):
        S.dma("sp", W13[slot][:].rearrange("p a b c -> p (a b c)"), wb13[e_], reads=["wb"], writes=[f"W{slot}"])
        S.dma("actq", W2[slot][:].rearrange("p a b -> p (a b)"), wb2[e_], reads=["wb"], writes=[f"W{slot}"])

    ntl = TPC // 128
    groups = [[("l", g * GT + t) for t in range(GT)] for g in range(ntl // GT)]
    if with_ctx:
        groups.append([("c", 0)])
    ti_glob = 0
    eload = 0
    for gi, grp in enumerate(groups):
        isctx = grp[0][0] == "c"
        if isctx:
            load_ctx_bcast()
        P = CPC if isctx else 128
        NT = len(grp) * 128 if not isctx else CPC
        for g, (kind, i) in enumerate(grp):
            s = ti_glob % 2; ti_glob += 1
            xsrc = xc if isctx else xl[i * 128:(i + 1) * 128, :]
            msrc = rs_out[TPC:TPC + CPC, :] if isctx else rs_out[i * 128:(i + 1) * 128, :]
            S.dma("sp", xt[s][0:P, :], xsrc, writes=[f"xt{s}"])
            S.dma("actq", mpt[s][0:P, :], msrc, writes=["mpt"])
            S.op("dve", lambda e, s=s, P=P: e.tensor_tensor(tmp[0:P, :], mpt[s][0:P, :], G1[0:P, :], ALU.mult),
                 reads=["mpt", "G1"], writes=["tmp"])
            S.op("dve", lambda e, s=s, g=g, P=P: e.tensor_tensor(x1[0:P, g, :], tmp[0:P, :], xt[s][0:P, :], ALU.add),
                 reads=["tmp", f"xt{s}"], writes=["x1"])
            S.op("pool", lambda e, P=P: e.memset(ss[0:P, :], 0.0), writes=["ss"])
            S.op("act", lambda e, g=g, P=P: e.activation(junk[0:P, :], x1[0:P, g, :], AF.Square, accum_out=ss[0:P, :]),
                 reads=["x1", "ss"], writes=["ss", "junk"])
            S.op("dve", lambda e, P=P: e.tensor_scalar(rstd[0:P, :], ss[0:P, :], 1.0 / D, EPS, ALU.mult, ALU.add), reads=["ss"], writes=["rstd"])
            S.op("act", lambda e, P=P: e.sqrt(rstd[0:P, :], rstd[0:P, :]), reads=["rstd"], writes=["rstd"])
            S.op("dve", lambda e, P=P: e.reciprocal(rstd[0:P, :], rstd[0:P, :]), reads=["rstd"], writes=["rstd"])
            S.op("dve", lambda e, g=g, P=P: e.scalar_tensor_tensor(tmp[0:P, :], x1[0:P, g, :], rstd[0:P, :], G2[0:P, :], ALU.mult, ALU.mult),
                 reads=["x1", "rstd", "G2"], writes=["tmp"])
            S.op("dve", lambda e, P=P: e.tensor_tensor(h2[0:P, :], tmp[0:P, :], SH2[0:P, :], ALU.add), reads=["tmp", "SH2"], writes=["h2"])
            S.op("dve", lambda e, P=P: e.tensor_copy(h2hi[0:P, :], h2[0:P, :]), reads=["h2"], writes=["h2hi"])
            S.op("dve", lambda e, P=P: e.tensor_tensor(h2lo[0:P, :], h2[0:P, :], h2hi[0:P, :], ALU.subtract), reads=["h2", "h2hi"], writes=["h2lo"])
            for (src, skey, dstv, dkey, bank) in ((h2hi, "h2hi", hT[:, :, g * 128:g * 128 + P], "hT", 3), (h2lo, "h2lo", hTl[:, :, 0:P], "hTl", 4)):
                psv = cx.PS[bank][:].bitcast(BF16).rearrange("p (k t) -> p k t", t=128)
                for kc in range(8):
                    S.op("pe", lambda e, psv=psv, kc=kc, src=src, P=P: e.transpose(psv[:, kc, 0:P], src[0:P, kc * 128:(kc + 1) * 128], identf[0:P, 0:P]),
                         reads=[skey, "identf"], writes=[f"ps{bank}"])
                S.op("act", lambda e, psv=psv, dstv=dstv, P=P: e.copy(dstv, psv[:, :, 0:P]), reads=[f"ps{bank}"], writes=[dkey])
            router(P, g)
            S.op("pool", lambda e, g=g, P=P: e.memset(yacc[0:P, g, :], 0.0), writes=["yacc"])
        for e_ in range(16):
            slot = eload % 2; eload += 1
            load_expert(e_, slot)
            for c0 in range(0, NT, 512):
                w = min(512, NT - c0)
                hsl = hh[(c0 // 512) % 2]
                for fc in range(4):
                    p1 = cx.PS[4 + (fc % 2) * 2]; p3 = cx.PS[5 + (fc % 2) * 2]
                    k1 = f"ps{4 + (fc % 2) * 2}"; k3 = f"ps{5 + (fc % 2) * 2}"
                    for wi, (pp, pk) in enumerate(((p1, k1), (p3, k3))):
                        for kc in range(8):
                            S.op("pe", lambda e, pp=pp, wi=wi, kc=kc, fc=fc, slot=slot, c0=c0, w=w: e.matmul(
                                pp[:, 0:w], W13[slot][:, wi, kc, fc * 128:(fc + 1) * 128], hT[:, kc, c0:c0 + w], start=(kc == 0), stop=(kc == 7)),
                                reads=[f"W{slot}", "hT"], writes=[pk])
                    S.op("act", lambda e, p1=p1, w=w: e.activation(hs[:, 0:w], p1[:, 0:w], AF.Silu), reads=[k1], writes=["hs"])
                    S.op("dve", lambda e, p3=p3, w=w, fc=fc, hsl=hsl: e.tensor_tensor(hsl[:, fc, 0:w], hs[:, 0:w], p3[:, 0:w], ALU.mult),
                         reads=["hs", k3], writes=[f"hh{(c0//512)%2}"])
                for t0_ in range(0, w, 128):
                    pw = min(128, w - t0_)
                    g = (c0 + t0_) // 128
                    for hb_ in range(2):
                        ps = cx.PS[hb_]
                        for fc in range(4):
                            S.op("pe", lambda e, ps=ps, fc=fc, hb_=hb_, slot=slot, t0_=t0_, pw=pw, hsl=hsl: e.matmul(
                                ps[0:pw, :], hsl[:, fc, t0_:t0_ + pw], W2[slot][:, fc, hb_ * 512:(hb_ + 1) * 512], start=(fc == 0), stop=(fc == 3)),
                                reads=[f"hh{(c0//512)%2}", f"W{slot}"], writes=[f"ps{hb_}"])
                        cs = slice(hb_ * 512, (hb_ + 1) * 512)
                        S.op("dve", lambda e, ps=ps, cs=cs, g=g, pw=pw, e_=e_: e.scalar_tensor_tensor(
                            yacc[0:pw, g, cs], ps[0:pw, :], gate[0:pw, g, e_:e_ + 1], yacc[0:pw, g, cs], ALU.mult, ALU.add),
                            reads=[f"ps{hb_}", "gate", "yacc"], writes=["yacc"])
        for g, (kind, i) in enumerate(grp):
            S.op("dve", lambda e, g=g, P=P: e.tensor_tensor(tmp[0:P, :], yacc[0:P, g, :], GG2[0:P, :], ALU.mult), reads=["yacc", "GG2"], writes=["tmp"])
            S.op("dve", lambda e, g=g, P=P: e.tensor_tensor(x1[0:P, g, :], x1[0:P, g, :], tmp[0:P, :], ALU.add), reads=["tmp", "x1"], writes=["x1"])
            dst = oc if isctx else ol[i * 128:(i + 1) * 128, :]
            if final:
                S.op("pool", lambda e, P=P: e.memset(ss[0:P, :], 0.0), writes=["ss"])
                S.op("act", lambda e, g=g, P=P: e.activation(junk[0:P, :], x1[0:P, g, :], AF.Square, accum_out=ss[0:P, :]),
                     reads=["x1", "ss"], writes=["ss", "junk"])
                S.op("dve", lambda e, P=P: e.tensor_scalar(rstd[0:P, :], ss[0:P, :], 1.0 / D, EPS, ALU.mult, ALU.add), reads=["ss"], writes=["rstd"])
                S.op("act", lambda e, P=P: e.sqrt(rstd[0:P, :], rstd[0:P, :]), reads=["rstd"], writes=["rstd"])
                S.op("dve", lambda e, P=P: e.reciprocal(rstd[0:P, :], rstd[0:P, :]), reads=["rstd"], writes=["rstd"])
                S.op("dve", lambda e, g=g, P=P: e.scalar_tensor_tensor(h2[0:P, :], x1[0:P, g, :], rstd[0:P, :], FG[0:P, :], ALU.mult, ALU.mult),
                     reads=["x1", "rstd", "FG"], writes=["h2"])
                S.dma("poolq", dst, h2[0:P, :], reads=["h2"], writes=["ol"])
            else:
                S.dma("poolq", dst, x1[0:P, g, :], reads=["x1"], writes=["ol"])
    if not with_ctx:
        S.dma("sp", xt[0][0:CPC, :], xc, writes=["xt0"])
        S.dma("sp", oc, xt[0][0:CPC, :], reads=["xt0"], writes=["oc"])
    cx.end_phase()


def fft_consts():
    N = 32768
    n1 = np.arange(64)[:, None]; k1 = np.arange(128)[None, :]
    a = 2 * np.pi * n1 * k1 / 128
    F1cat = np.concatenate([np.cos(a), -np.sin(a)], 1)
    n2 = np.arange(256)[:, None]
    t = 2 * np.pi * n2 * k1 / N
    Tr, Ti = np.cos(t), -np.sin(t)
    k2 = np.arange(256)[None, :]
    b = 2 * np.pi * n2 * k2 / 256
    F2r, F2i = np.cos(b), -np.sin(b)
    Er, Ei = np.cos(b.T), np.sin(b.T)
    IA = np.concatenate([Er, Ei], 1); IB = np.concatenate([-Ei, Er], 1)
    ITr, ITi = np.cos(t.T), np.sin(t.T)
    c = 2 * np.pi * np.arange(128)[:, None] * np.arange(64)[None, :] / 128
    G1r, G1i = np.cos(c) / N, -np.sin(c) / N
    ch2 = lambda m: np.ascontiguousarray(m.reshape(2, 128, m.shape[1]).transpose(1, 0, 2))
    psm = (np.arange(128)[:, None] % 64 == np.arange(128)[None, :] % 64).astype(np.float32)
    return {"F1cat": _bf16(F1cat), "Tr": ch2(Tr).astype(np.float32), "Ti": ch2(Ti).astype(np.float32),
            "F2r": _bf16(ch2(F2r)), "F2i": _bf16(ch2(F2i)), "F2in": _bf16(ch2(-F2i)),
            "IA": _bf16(ch2(IA)), "IB": _bf16(ch2(IB)), "ITr": ITr.astype(np.float32), "ITi": ITi.astype(np.float32),
            "G1r": _bf16(G1r), "G1i": _bf16(G1i), "PSM": psm}


def hy_pos_consts(L):
    t = np.arange(L, dtype=np.float32)
    t_norm = t / max(L - 1, 1)
    bands = np.linspace(1e-4, 15, 16, dtype=np.float32)
    ang = (np.float32(2.0 * math.pi / L) * t[:, None] * bands[None, :]).astype(np.float32)
    z = np.concatenate([t_norm[:, None], np.cos(ang), np.sin(ang)], axis=-1).astype(np.float32)
    return np.ascontiguousarray(z.T), np.ascontiguousarray(np.broadcast_to(t_norm[None, :], (128, L))).astype(np.float32)


def emit_B2(cx, with_ctx):
    L = SEQ
    PI = math.pi
    S = cx.S
    paths = [("l", SEQ)] + ([("c", CTX)] if with_ctx else [])
    I = {}
    for tag, Le in paths:
        I[tag] = dict(hy=cx.inp(f"hy_{tag}", [3, 64, Le + 2]), zT=cx.inp(f"zT_{tag}", [33, Le]), tn=cx.inp(f"tn_{tag}", [128, Le]),
                      pl=cx.inp(f"pl_{tag}", [64, Le + 24]), icnt=cx.inp(f"icnt_{tag}", [64, Le]),
                      oh=cx.out(f"oh_{tag}", [64, Le]), op=cx.out(f"op_{tag}", [64, Le]))
    shw_d = cx.inp("shw", [64, 3, 3]); shb_d = cx.inp("shb", [64, 3])
    fw1_d = cx.inp("fw1", [33, 64]); fb1_d = cx.inp("fb1", [64, 1]); fw2_d = cx.inp("fw2", [64, 64]); fb2_d = cx.inp("fb2", [64, 1])
    fw3_d = cx.inp("fw3", [64, 256]); fb3_d = cx.inp("fb3", [128, 2]); ndel_d = cx.inp("ndel", [128, 1])
    dsk_d = cx.inp("dsk", [64, 2, 64])
    psel_d = cx.inp("psel", [64, 4]); pw_d = cx.inp("pw", [64, 64]); psc_d = cx.inp("psc", [64, 1])
    FC = fft_consts()
    cd = {k: cx.inp("c_" + k, list(v.shape), BF16 if v.dtype != np.float32 else F32) for k, v in FC.items()}
    c = {k: cx.sb("c_" + k, list(v.shape), BF16 if v.dtype != np.float32 else F32) for k, v in FC.items()}
    for k in FC:
        S.dma("sp", c[k][:], cd[k], writes=["c_" + k])
    ck = ["c_" + k for k in FC]
    small = {}
    for nm, d_, shp in (("shw", shw_d, [64, 3, 3]), ("shb", shb_d, [64, 3]), ("fw1", fw1_d, [33, 64]), ("fb1", fb1_d, [64, 1]),
                        ("fw2", fw2_d, [64, 64]), ("fb2", fb2_d, [64, 1]), ("fw3", fw3_d, [64, 256]), ("fb3", fb3_d, [128, 2]),
                        ("ndel", ndel_d, [128, 1]), ("dsk", dsk_d, [64, 2, 64]), ("psel", psel_d, [64, 4]), ("pw", pw_d, [64, 64]),
                        ("psc", psc_d, [64, 1])):
        small[nm] = cx.sb("w_" + nm, shp)
        S.dma("sp", small[nm][:], d_, writes=["w_" + nm])
    pwb = cx.sb("pwb", [64, 64], BF16)
    S.op("dve", lambda e: e.tensor_copy(pwb[:], small["pw"][:]), reads=["w_pw"], writes=["pwb"])
    scx = cx.scratch("scx", [3, 64, L]); filt_s = cx.scratch("filt_s", [2, 128, L])
    zero = cx.sb("zero", [128, 2048])
    S.op("pool", lambda e: e.memset(zero[:], 0.0), writes=["zero"])

    def do_path(tag, Le):
        io = I[tag]
        CH = min(2048, Le)
        with ExitStack() as st:
            pin = cx.sb("pin", [64, CH + 24], F32, st); A2 = cx.sb("A2", [64, CH + 24], F32, st); A4 = cx.sb("A4", [64, CH + 24], F32, st)
            A8 = cx.sb("A8", [64, CH + 24], F32, st); A16 = cx.sb("A16", [64, CH + 24], F32, st)
            acc = cx.sb("pacc", [64, CH], F32, st); ic = cx.sb("pic", [64, CH], F32, st); pd = cx.sb("pd", [64, CH], BF16, st)
            po = cx.sb("po", [64, CH], F32, st)
            for c0 in range(0, Le, CH):
                S.dma("sp", pin[:], io["pl"][:, c0:c0 + CH + 24], writes=["pin"])
                S.dma("sp", ic[:], io["icnt"][:, c0:c0 + CH], writes=["pic"])
                W_ = CH + 24
                S.op("dve", lambda e: e.tensor_tensor(A2[:, 0:W_ - 1], pin[:, 0:W_ - 1], pin[:, 1:W_], ALU.add), reads=["pin"], writes=["A2"])
                S.op("dve", lambda e: e.tensor_tensor(A4[:, 0:W_ - 3], A2[:, 0:W_ - 3], A2[:, 2:W_ - 1], ALU.add), reads=["A2"], writes=["A4"])
                S.op("dve", lambda e: e.tensor_tensor(A8[:, 0:W_ - 7], A4[:, 0:W_ - 7], A4[:, 4:W_ - 3], ALU.add), reads=["A4"], writes=["A8"])
                S.op("dve", lambda e: e.tensor_tensor(A16[:, 0:W_ - 15], A8[:, 0:W_ - 15], A8[:, 8:W_ - 7], ALU.add), reads=["A8"], writes=["A16"])
                S.op("dve", lambda e: e.tensor_scalar(acc[:], A2[:, 7:7 + CH], small["psel"][:, 0:1], None, ALU.mult), reads=["A2", "w_psel"], writes=["pacc"])
                for k_, (Aw, off, key) in enumerate(((A4, 6, "A4"), (A8, 4, "A8"), (A16, 0, "A16"))):
                    S.op("dve", lambda e, Aw=Aw, off=off, k_=k_: e.scalar_tensor_tensor(acc[:], Aw[:, off:off + CH], small["psel"][:, k_ + 1:k_ + 2], acc[:],
                                                                                        ALU.mult, ALU.add), reads=[key, "w_psel", "pacc"], writes=["pacc"])
                S.op("dve", lambda e: e.tensor_tensor(acc[:], acc[:], ic[:], ALU.mult), reads=["pacc", "pic"], writes=["pacc"])
                S.op("dve", lambda e: e.tensor_tensor(pd[:], acc[:], pin[:, 8:8 + CH], ALU.subtract), reads=["pacc", "pin"], writes=["pd"])
                for s0 in range(0, CH, 512):
                    w = min(512, CH - s0)
                    S.op("pe", lambda e, s0=s0, w=w: e.matmul(cx.PS[7][0:64, 0:w], pwb[:, :], pd[:, s0:s0 + w], start=True, stop=True),
                         reads=["pd", "pwb"], writes=["ps7"])
                    S.op("dve", lambda e, s0=s0, w=w: e.tensor_scalar(po[:, s0:s0 + w], cx.PS[7][0:64, 0:w], small["psc"][:, 0:1], None, ALU.mult),
                         reads=["ps7", "w_psc"], writes=["po"])
                S.dma("poolq", io["op"][:, c0:c0 + CH], po[:], reads=["po"], writes=["op"])
            S.barrier()

        with ExitStack() as st:
            hin = cx.sb("hin", [64, CH + 2], F32, st); ho = cx.sb("ho", [64, CH], F32, st)
            if Le < L:
                for p in range(3):
                    for c0 in range(0, L, 2048):
                        S.dma("sp", scx[p][:, c0:c0 + 2048], zero[0:64, :], reads=["zero"], writes=["scx"])
                for oc in range(2):
                    for c0 in range(0, L, 2048):
                        S.dma("sp", filt_s[oc][:, c0:c0 + 2048], zero[:, :], reads=["zero"], writes=["filt_s"])
            for p in range(3):
                for c0 in range(0, Le, CH):
                    S.dma("sp", hin[:], io["hy"][p][:, c0:c0 + CH + 2], writes=["hin"])
                    S.op("dve", lambda e, p=p: e.tensor_scalar(ho[:], hin[:, 1:CH + 1], small["shw"][:, p, 1:2], small["shb"][:, p:p + 1], ALU.mult, ALU.add),
                         reads=["hin", "w_shw", "w_shb"], writes=["ho"])
                    S.op("dve", lambda e, p=p: e.scalar_tensor_tensor(ho[:], hin[:, 0:CH], small["shw"][:, p, 0:1], ho[:], ALU.mult, ALU.add),
                         reads=["hin", "w_shw", "ho"], writes=["ho"])
                    S.op("dve", lambda e, p=p: e.scalar_tensor_tensor(ho[:], hin[:, 2:CH + 2], small["shw"][:, p, 2:3], ho[:], ALU.mult, ALU.add),
                         reads=["hin", "w_shw", "ho"], writes=["ho"])
                    S.dma("poolq", scx[p][:, c0:c0 + CH], ho[:], reads=["ho"], writes=["scx"])
            S.barrier()

        with ExitStack() as st:
            FW = min(512, Le)
            nfc = Le // FW
            zt = cx.sb("zt", [33, FW], F32, st); tnt = cx.sb("tnt", [128, FW], F32, st)
            pre = cx.sb("pre", [64, FW], F32, st); msk = cx.sb("msk", [64, FW], F32, st); h1 = cx.sb("h1", [64, FW], F32, st); h2 = cx.sb("h2f", [64, FW], F32, st)
            dec = cx.sb("dec", [128, FW], F32, st); hf = [cx.sb(f"hf{i}", [128, FW], F32, st) for i in range(2)]
            junk = cx.sb("fjunk", [128, FW], F32, st)
            accsq = cx.sb("accsq", [128, 2, 32], F32, st); ssum = cx.sb("ssum", [128, 2], F32, st); rn = cx.sb("rn", [128, 2], F32, st)
            S.op("pool", lambda e: e.memset(accsq[:], 0.0), writes=["accsq"])

            def sin_layer(ps, bias, dst, key):
                S.op("dve", lambda e: e.tensor_scalar(pre[:], ps[0:64, 0:FW], bias[:, 0:1], None, ALU.add), reads=["ps0", "ps1", "w_fb1", "w_fb2"], writes=["pre"])
                for _ in range(2):
                    S.op("dve", lambda e: e.tensor_scalar(msk[:], pre[:], PI, -2 * PI, ALU.is_gt, ALU.mult), reads=["pre"], writes=["msk"])
                    S.op("dve", lambda e: e.tensor_tensor(pre[:], pre[:], msk[:], ALU.add), reads=["pre", "msk"], writes=["pre"])
                    S.op("dve", lambda e: e.tensor_scalar(msk[:], pre[:], -PI, 2 * PI, ALU.is_lt, ALU.mult), reads=["pre"], writes=["msk"])
                    S.op("dve", lambda e: e.tensor_tensor(pre[:], pre[:], msk[:], ALU.add), reads=["pre", "msk"], writes=["pre"])
                S.op("act", lambda e: e.activation(dst[:], pre[:], AF.Sin), reads=["pre"], writes=[key])

            for fc_ in range(nfc):
                sl = slice(fc_ * FW, (fc_ + 1) * FW)
                S.dma("sp", zt[:], io["zT"][:, sl], writes=["zt"]); S.dma("sp", tnt[:], io["tn"][:, sl], writes=["tnt"])
                S.op("pe", lambda e: e.matmul(cx.PS[0][0:64, 0:FW], small["fw1"][:, :], zt[:, :], start=True, stop=True), reads=["zt", "w_fw1"], writes=["ps0"])
                sin_layer(cx.PS[0], small["fb1"], h1, "h1")
                S.op("pe", lambda e: e.matmul(cx.PS[1][0:64, 0:FW], small["fw2"][:, :], h1[:, :], start=True, stop=True), reads=["h1", "w_fw2"], writes=["ps1"])
                sin_layer(cx.PS[1], small["fb2"], h2, "h2f")
                S.op("act", lambda e: e.activation(dec[:], tnt[:], AF.Exp, scale=small["ndel"][:, 0:1]), reads=["tnt", "w_ndel"], writes=["dec"])
                for oc in range(2):
                    S.op("pe", lambda e, oc=oc: e.matmul(cx.PS[2 + oc][:, 0:FW], small["fw3"][:, oc * 128:(oc + 1) * 128], h2[:, :], start=True, stop=True),
                         reads=["h2f", "w_fw3"], writes=[f"ps{2+oc}"])
                    S.op("dve", lambda e, oc=oc: e.scalar_tensor_tensor(hf[oc][:], cx.PS[2 + oc][:, 0:FW], small["fb3"][:, oc:oc + 1], dec[:], ALU.add, ALU.mult),
                         reads=[f"ps{2+oc}", "w_fb3", "dec"], writes=[f"hf{oc}"])
                    S.op("act", lambda e, oc=oc, fc_=fc_: e.activation(junk[:], hf[oc][:], AF.Square, accum_out=accsq[:, oc, fc_:fc_ + 1]),
                         reads=[f"hf{oc}", "accsq"], writes=["fjunk", "accsq"])
                    S.dma("poolq", filt_s[oc][:, sl], hf[oc][:], reads=[f"hf{oc}"], writes=["filt_s"])
            S.op("dve", lambda e: e.tensor_reduce(ssum[:], accsq[:], AX.X, ALU.add), reads=["accsq"], writes=["ssum"])
            S.op("pe", lambda e: e.matmul(cx.PS[4][:, 0:2], c["PSM"][:, :], ssum[:, :], start=True, stop=True), reads=["ssum", "c_PSM"], writes=["ps4"])
            S.op("dve", lambda e: e.tensor_scalar(rn[:], cx.PS[4][:, 0:2], EPS, None, ALU.add), reads=["ps4"], writes=["rn"])
            S.op("act", lambda e: e.sqrt(rn[:], rn[:]), reads=["rn"], writes=["rn"])
            S.op("dve", lambda e: e.reciprocal(rn[:], rn[:]), reads=["rn"], writes=["rn"])
            nb = cx.sb("nb", [128, 2048], F32, st)
            NW = min(2048, Le)
            for oc in range(2):
                for c0 in range(0, Le, NW):
                    S.dma("sp", nb[:, 0:NW], filt_s[oc][:, c0:c0 + NW], reads=["filt_s"], writes=["nb"])
                    S.op("dve", lambda e, oc=oc: e.tensor_scalar(nb[:, 0:NW], nb[:, 0:NW], rn[:, oc:oc + 1], None, ALU.mult), reads=["nb", "rn"], writes=["nb"])
                    if c0 == 0:
                        S.op("pool", lambda e: e.memset(nb[64:128, 0:1], 0.0), reads=["nb"], writes=["nb"])
                    S.dma("poolq", filt_s[oc][:, c0:c0 + NW], nb[:, 0:NW], reads=["nb"], writes=["filt_s"])
            S.barrier()

        with ExitStack() as st:
            Af = cx.sb("Af", [64, 2, 256], F32, st); Ab = cx.sb("Ab", [64, 2, 256], BF16, st)
            Bp = [cx.sb(f"Bp{i}", [128, 2, 2, 128], BF16, st) for i in range(2)]
            tw = [cx.sb(f"tw{i}", [128, 2, 128], F32, st) for i in range(4)]
            Xr = cx.sb("Xr", [128, 2, 2, 128], F32, st); Xi = cx.sb("Xi", [128, 2, 2, 128], F32, st)
            Kr = [cx.sb(f"Kr{o}", [128, 2, 2, 128], F32, st) for o in range(2)]; Ki = [cx.sb(f"Ki{o}", [128, 2, 2, 128], F32, st) for o in range(2)]
            yt_ = [cx.sb(f"yt{i}", [128, 2, 2, 128], F32, st) for i in range(4)]
            Yb = cx.sb("Yb", [128, 2, 2, 2, 128], BF16, st)
            Cp = cx.sb("Cp", [128, 2, 2, 256], BF16, st)
            it_ = [cx.sb(f"it{i}", [128, 256], F32, st) for i in range(4)]
            x1t = cx.sb("x1t", [64, 2, 256], F32, st); x2t = cx.sb("x2t", [64, 2, 256], F32, st); vt = cx.sb("vt", [64, 2, 256], F32, st)
            zt_ = cx.sb("zt_", [64, 2, 256], F32, st); e1 = cx.sb("e1", [64, 2, 256], F32, st); hout = cx.sb("hout", [64, 2, 256], F32, st)

            def tb(src2d):
                return src2d.rearrange("c (n1 n2) -> n1 c n2", n2=256)

            def fwd(Xr_, Xi_, xkey):
                for cc in range(2):
                    ps = cx.PS[cc]
                    for g in range(2):
                        S.op("pe", lambda e, ps=ps, g=g, cc=cc: e.matmul(ps[:, g * 256:(g + 1) * 256], Ab[:, g, cc * 128:(cc + 1) * 128], c["F1cat"][:, :],
                                                                      start=True, stop=True), reads=["Ab", "c_F1cat"], writes=[f"ps{cc}"])
                    psv = ps[:].rearrange("p (g r k) -> p g r k", g=2, r=2)
                    Trb = c["Tr"][:, cc, :].unsqueeze(1).to_broadcast([128, 2, 128]); Tib = c["Ti"][:, cc, :].unsqueeze(1).to_broadcast([128, 2, 128])
                    S.op("dve", lambda e, psv=psv, Trb=Trb: e.tensor_tensor(tw[0][:], psv[:, :, 0, :], Trb, ALU.mult), reads=[f"ps{cc}", "c_Tr"], writes=["tw0"])
                    S.op("dve", lambda e, psv=psv, Tib=Tib: e.tensor_tensor(tw[1][:], psv[:, :, 1, :], Tib, ALU.mult), reads=[f"ps{cc}", "c_Ti"], writes=["tw1"])
                    S.op("dve", lambda e, psv=psv, Tib=Tib: e.tensor_tensor(tw[2][:], psv[:, :, 0, :], Tib, ALU.mult), reads=[f"ps{cc}", "c_Ti"], writes=["tw2"])
                    S.op("dve", lambda e, psv=psv, Trb=Trb: e.tensor_tensor(tw[3][:], psv[:, :, 1, :], Trb, ALU.mult), reads=[f"ps{cc}", "c_Tr"], writes=["tw3"])
                    S.op("pool", lambda e, cc=cc: e.tensor_tensor(Bp[cc][:, 0, :, :], tw[0][:], tw[1][:], ALU.subtract), reads=["tw0", "tw1"], writes=[f"Bp{cc}"])
                    S.op("pool", lambda e, cc=cc: e.tensor_tensor(Bp[cc][:, 1, :, :], tw[2][:], tw[3][:], ALU.add), reads=["tw2", "tw3"], writes=[f"Bp{cc}"])
                for kc in range(2):
                    ks = slice(kc * 128, (kc + 1) * 128)
                    psr, psi = cx.PS[2 + 2 * kc], cx.PS[3 + 2 * kc]
                    seq_r = [(c["F2r"], 0), (c["F2in"], 1)]
                    seq_i = [(c["F2i"], 0), (c["F2r"], 1)]
                    for (pp, seq, pk) in ((psr, seq_r, f"ps{2+2*kc}"), (psi, seq_i, f"ps{3+2*kc}")):
                        n_ = 0
                        for cc in range(2):
                            for (M_, ri) in seq:
                                S.op("pe", lambda e, pp=pp, M_=M_, ri=ri, cc=cc, n_=n_, ks=ks: e.matmul(
                                    pp[:, 0:256], M_[:, cc, ks], Bp[cc][:, ri, :, :], start=(n_ == 0), stop=(n_ == 3)),
                                    reads=[f"Bp{cc}"] + ck, writes=[pk])
                                n_ += 1
                    S.op("act", lambda e, psr=psr, kc=kc: e.copy(Xr_[:, kc, :, :], psr[:, 0:256]), reads=[f"ps{2+2*kc}"], writes=[xkey + "r"])
                    S.op("act", lambda e, psi=psi, kc=kc: e.copy(Xi_[:, kc, :, :], psi[:, 0:256]), reads=[f"ps{3+2*kc}"], writes=[xkey + "i"])

            def conv(o, ydst_key):
                fwd(Xr, Xi, "X")
                fl = lambda t_: t_[:].rearrange("p a b c -> p (a b c)")
                S.op("dve", lambda e: e.tensor_tensor(fl(yt_[0]), fl(Xr), fl(Kr[o]), ALU.mult), reads=["Xr", f"K{o}r"], writes=["yt0"])
                S.op("pool", lambda e: e.tensor_tensor(fl(yt_[1]), fl(Xi), fl(Ki[o]), ALU.mult), reads=["Xi", f"K{o}i"], writes=["yt1"])
                S.op("dve", lambda e: e.tensor_tensor(fl(yt_[2]), fl(Xr), fl(Ki[o]), ALU.mult), reads=["Xr", f"K{o}i"], writes=["yt2"])
                S.op("pool", lambda e: e.tensor_tensor(fl(yt_[3]), fl(Xi), fl(Kr[o]), ALU.mult), reads=["Xi", f"K{o}r"], writes=["yt3"])
                S.op("dve", lambda e: e.tensor_tensor(Yb[:, :, 0, :, :], yt_[0][:], yt_[1][:], ALU.subtract), reads=["yt0", "yt1"], writes=["Yb"])
                S.op("pool", lambda e: e.tensor_tensor(Yb[:, :, 1, :, :], yt_[2][:], yt_[3][:], ALU.add), reads=["yt2", "yt3"], writes=["Yb"])
                for g in range(2):
                    ps = cx.PS[g]
                    n_ = 0
                    for kc in range(2):
                        for (ri, M_) in ((0, c["IA"]), (1, c["IB"])):
                            S.op("pe", lambda e, ps=ps, kc=kc, ri=ri, M_=M_, g=g, n_=n_: e.matmul(ps[:, :], Yb[:, kc, ri, g, :], M_[:, kc, :], start=(n_ == 0), stop=(n_ == 3)),
                                 reads=["Yb"] + ck, writes=[f"ps{g}"])
                            n_ += 1
                    S.op("dve", lambda e, ps=ps: e.tensor_tensor(it_[0][:], ps[:, 0:256], c["ITr"][:], ALU.mult), reads=[f"ps{g}", "c_ITr"], writes=["it0"])
                    S.op("dve", lambda e, ps=ps: e.tensor_tensor(it_[1][:], ps[:, 256:512], c["ITi"][:], ALU.mult), reads=[f"ps{g}", "c_ITi"], writes=["it1"])
                    S.op("dve", lambda e, ps=ps: e.tensor_tensor(it_[2][:], ps[:, 0:256], c["ITi"][:], ALU.mult), reads=[f"ps{g}", "c_ITi"], writes=["it2"])
                    S.op("dve", lambda e, ps=ps: e.tensor_tensor(it_[3][:], ps[:, 256:512], c["ITr"][:], ALU.mult), reads=[f"ps{g}", "c_ITr"], writes=["it3"])
                    S.op("pool", lambda e, g=g: e.tensor_tensor(Cp[:, 0, g, :], it_[0][:], it_[1][:], ALU.subtract), reads=["it0", "it1"], writes=["Cp"])
                    S.op("pool", lambda e, g=g: e.tensor_tensor(Cp[:, 1, g, :], it_[2][:], it_[3][:], ALU.add), reads=["it2", "it3"], writes=["Cp"])
                S.op("pe", lambda e: e.matmul(cx.PS[6][0:64, :], c["G1r"][:, :], Cp[:, 0, :, :], start=True, stop=False), reads=["Cp", "c_G1r"], writes=["ps6"])
                S.op("pe", lambda e: e.matmul(cx.PS[6][0:64, :], c["G1i"][:, :], Cp[:, 1, :, :], start=False, stop=True), reads=["Cp", "c_G1i"], writes=["ps6"])

            for pr in range(32):
                ch0 = pr * 2
                for o in range(2):
                    for d_ in range(2):
                        S.dma("sp", Af[:], tb(filt_s[o][d_ * 64 + ch0:d_ * 64 + ch0 + 2, :]), reads=["filt_s"], writes=["Af"])
                        S.op("dve", lambda e: e.tensor_copy(Ab[:], Af[:]), reads=["Af"], writes=["Ab"])
                        if d_ == 0:
                            fwd(Kr[o], Ki[o], f"K{o}")
                        else:
                            fwd(Xr, Xi, "X")
                            S.op("pool", lambda e, o=o: e.tensor_tensor(Kr[o][:], Kr[o][:], Xr[:], ALU.add), reads=[f"K{o}r", "Xr"], writes=[f"K{o}r"])
                            S.op("pool", lambda e, o=o: e.tensor_tensor(Ki[o][:], Ki[o][:], Xi[:], ALU.subtract), reads=[f"K{o}i", "Xi"], writes=[f"K{o}i"])
                S.dma("sp", x1t[:], tb(scx[0][ch0:ch0 + 2, :]), reads=["scx"], writes=["x1t"])
                S.dma("sp", x2t[:], tb(scx[1][ch0:ch0 + 2, :]), reads=["scx"], writes=["x2t"])
                S.dma("sp", vt[:], tb(scx[2][ch0:ch0 + 2, :]), reads=["scx"], writes=["vt"])
                S.op("dve", lambda e: e.tensor_copy(Ab[:], vt[:]), reads=["vt"], writes=["Ab"])
                conv(0, "y1")
                dk = lambda o: small["dsk"][:, o, ch0:ch0 + 2].unsqueeze(2).to_broadcast([64, 2, 256])
                psy = cx.PS[6][0:64, :].rearrange("p (g n) -> p g n", g=2)
                dk0 = dk(0); dk1 = dk(1)
                S.op("pool", lambda e, dk0=dk0: e.tensor_tensor(e1[:], vt[:], dk0, ALU.mult), reads=["vt", "w_dsk"], writes=["e1"])
                S.op("dve", lambda e: e.tensor_tensor(e1[:], e1[:], psy, ALU.add), reads=["e1", "ps6"], writes=["e1"])
                S.op("dve", lambda e: e.tensor_tensor(zt_[:], e1[:], x1t[:], ALU.mult), reads=["e1", "x1t"], writes=["zt_"])
                S.op("dve", lambda e: e.tensor_copy(Ab[:], zt_[:]), reads=["zt_"], writes=["Ab"])
                conv(1, "y2")
                S.op("pool", lambda e, dk1=dk1: e.tensor_tensor(e1[:], zt_[:], dk1, ALU.mult), reads=["zt_", "w_dsk"], writes=["e1"])
                S.op("dve", lambda e: e.tensor_tensor(e1[:], e1[:], psy, ALU.add), reads=["e1", "ps6"], writes=["e1"])
                S.op("dve", lambda e: e.tensor_tensor(hout[:], e1[:], x2t[:], ALU.mult), reads=["e1", "x2t"], writes=["hout"])
                if Le == L:
                    S.dma("poolq", tb(io["oh"][ch0:ch0 + 2, :]), hout[:], reads=["hout"], writes=["oh"])
                else:
                    S.dma("poolq", io["oh"][ch0:ch0 + 2, :].rearrange("(o c) n -> o c n", o=1), hout[0:1, :, 0:Le], reads=["hout"], writes=["oh"])
            S.barrier()

    for tag_, Le_ in paths:
        do_path(tag_, Le_)
    cx.end_phase()


POOL_SIZES = (2, 4, 8, 16)
NKEY = SEQ + CTX
NLOC = TPC + CPC
GROUPS = [[0, 1, 2, 3], [4, 5, 6, 7]]
SECS = {"aq": 0, "ak": 256, "av": 512, "bq": 768, "bk": 1024, "bv": 1280, "pool": 1536, "hy0": 1792, "hy1": 2048, "hy2": 2304}


def emit_R(cx, need_ctx, T):
    S = cx.S
    ag_out = T["ag_out"]
    selq_d = cx.inp("selq", [128, 2, 2, 32]); selg_d = cx.inp("selg", [128, 2, 64])
    selq = cx.sb("selq", [128, 2, 2, 32]); selg = cx.sb("selg", [128, 2, 64])
    S.dma("sp", selq[:], selq_d, writes=["selq"]); S.dma("sp", selg[:], selg_d, writes=["selg"])
    xs = [cx.sb(f"rx{i}", [128, 2, 512]) for i in range(3)]
    ev = [cx.sb(f"rev{i}", [64, 512]) for i in range(2)]
    k33 = [cx.sb(f"k33_{i}", [33, 512]) for i in range(2)]
    k65 = cx.sb("k65", [65, 512])
    v65 = [cx.sb(f"v65_{i}", [128, 65]) for i in range(2)]
    zero = cx.sb("rzero", [64, 16])
    S.op("pool", lambda e: e.memset(zero[:], 0.0), writes=["rzero"])
    for t_ in k33:
        S.op("pool", lambda e, t_=t_: e.memset(t_[32:33, :], 1.0), writes=["k33"])
    S.op("pool", lambda e: e.memset(k65[64:65, :], 1.0), writes=["k65"])
    for t_ in v65:
        S.op("pool", lambda e, t_=t_: e.memset(t_[:, 64:65], 1.0), writes=["v65"])
    for tag, Le in (("l", SEQ),) + ((("c", CTX),) if need_ctx else ()):
        for p in range(3):
            S.dma("sp", T[f"hy_{tag}"][p][:, 0:1], zero[:, 0:1], reads=["rzero"], writes=["hy"], allow_slow_non_contiguous=True)
            S.dma("sp", T[f"hy_{tag}"][p][:, Le + 1:Le + 2], zero[:, 0:1], reads=["rzero"], writes=["hy"], allow_slow_non_contiguous=True)
        S.dma("sp", T[f"pl_{tag}"][:, 0:8], zero[:, 0:8], reads=["rzero"], writes=["pl"])
        S.dma("sp", T[f"pl_{tag}"][:, Le + 8:Le + 24], zero[:, 0:16], reads=["rzero"], writes=["pl"])
    cnt = {"x": 0, "ps": 0, "ev": 0, "k": 0, "v": 0, "q": 0}

    def load(sec, pieces, w):
        i = cnt["x"] % 3; cnt["x"] += 1
        X = xs[i]
        o = 0
        for (r, c0, pw) in pieces:
            n = pw // 64
            for kc in range(2):
                r0 = r * DIN + SECS[sec] + kc * 128
                src = ag_out[c0 // 64:c0 // 64 + n, r0:r0 + 128, :].rearrange("ck p t -> p ck t")
                q = ("sp", "actq")[cnt["q"] % 2]; cnt["q"] += 1
                S.dma(q, X[:, kc, o:o + pw].rearrange("p (ck t) -> p ck t", t=64), src, writes=[f"rx{i}"])
            o += pw
        return X, f"rx{i}"

    def sel_fm(X, xk, w, SEL, M, dst, ones=None):
        pi = cnt["ps"] % 8; cnt["ps"] += 1
        ps = cx.PS[pi]
        for kc in range(2):
            S.op("pe", lambda e, ps=ps, kc=kc, SEL=SEL: e.matmul(ps[0:M, 0:w], SEL[:, kc, :], X[:, kc, 0:w], start=(kc == 0), stop=(kc == 1)),
                 reads=[xk, "selq", "selg"], writes=[f"ps{pi}"])
        if ones == "k33":
            i = cnt["k"] % 2; cnt["k"] += 1
            dt_, dk, rows = k33[i], f"k33_{i}", 33
        elif ones == "k65":
            dt_, dk, rows = k65, "k65", 65
        else:
            i = cnt["ev"] % 2; cnt["ev"] += 1
            dt_, dk, rows = ev[i], f"rev{i}", M
        S.op(("act", "dve")[cnt["ps"] % 2], lambda e, ps=ps, dt_=dt_: (e.copy if hasattr(e, "copy") else e.tensor_copy)(dt_[0:M, 0:w], ps[0:M, 0:w]),
             reads=[f"ps{pi}"], writes=[dk])
        S.dma("poolq", dst, dt_[0:rows, 0:w], reads=[dk], writes=["rdst"])

    def sel_tm(X, xk, w, dst_rows):
        for s0 in range(0, w, 128):
            pi = cnt["ps"] % 8; cnt["ps"] += 1
            ps = cx.PS[pi]
            for kc in range(2):
                S.op("pe", lambda e, ps=ps, kc=kc, s0=s0: e.matmul(ps[:, 0:64], X[:, kc, s0:s0 + 128], selg[:, kc, :], start=(kc == 0), stop=(kc == 1)),
                     reads=[xk, "selg"], writes=[f"ps{pi}"])
            i = cnt["v"] % 2; cnt["v"] += 1
            S.op(("act", "dve")[i], lambda e, ps=ps, i=i: (e.copy if hasattr(e, "copy") else e.tensor_copy)(v65[i][:, 0:64], ps[:, 0:64]),
                 reads=[f"ps{pi}"], writes=[f"v65_{i}"])
            S.dma("poolq", dst_rows[s0:s0 + 128, :], v65[i][:, :], reads=[f"v65_{i}"], writes=["rdst"])

    chunks = [("l", [(r, cp * 512, 512)], r * TPC + cp * 512, 512) for r in range(4) for cp in range(8)]
    chunks.append(("c", [(r, TPC, CPC) for r in range(4)], SEQ, CTX))
    for kind, pieces, t0, w in chunks:
        isl = kind == "l"
        if isl or need_ctx:
            X, xk = load("aq", pieces, w)
            for c_ in range(2):
                sel_fm(X, xk, w, selq[:, :, c_, :], 32, (T["aqT"][c_][:, t0:t0 + w] if isl else T["aqcT"][c_][:, :]))
            X, xk = load("bq", pieces, w)
            sel_fm(X, xk, w, selg, 64, (T["bqT"][:, t0:t0 + w] if isl else T["bqcT"][:, :]))
            tag = "l" if isl else "c"
            tt = t0 if isl else 0
            X, xk = load("pool", pieces, w)
            sel_fm(X, xk, w, selg, 64, T[f"pl_{tag}"][:, 8 + tt:8 + tt + w])
            for p in range(3):
                X, xk = load(f"hy{p}", pieces, w)
                sel_fm(X, xk, w, selg, 64, T[f"hy_{tag}"][p][:, 1 + tt:1 + tt + w])
        X, xk = load("ak", pieces, w)
        for c_ in range(2):
            sel_fm(X, xk, w, selq[:, :, c_, :], 32, T["akT"][c_][:, t0:t0 + w], ones="k33")
        X, xk = load("bk", pieces, w)
        sel_fm(X, xk, w, selg, 64, T["bkT"][:, t0:t0 + w], ones="k65")
        X, xk = load("av", pieces, w)
        sel_tm(X, xk, w, T["av"][t0:t0 + w, :])
        X, xk = load("bv", pieces, w)
        sel_tm(X, xk, w, T["bv"][t0:t0 + w, :])
    cx.end_phase()


def emit_W(cx, need_ctx, T):
    S = cx.S
    mixT = T["mixT"]; rs_in = T["rs_in"]
    wop_d = cx.inp("wo_part", [4, 64, D])
    wst = cx.sb("wwst", [64, 4, D]); wb = cx.sb("wwb", [64, 4, D], BF16)
    S.dma("sp", wst[:], wop_d.rearrange("m p n -> p m n"), writes=["wwst"])
    S.op("dve", lambda e: e.tensor_copy(wb[:], wst[:]), reads=["wwst"], writes=["wwb"])
    mt = [cx.sb(f"wmt{i}", [64, 4, 128]) for i in range(2)]
    mb = [cx.sb(f"wmb{i}", [64, 4, 128], BF16) for i in range(2)]
    ot = [cx.sb(f"wot{i}", [128, D]) for i in range(2)]
    rs_out = T["rs_out"]
    cckey = cx.coll_group()
    nlb = TPC // 128
    order = [j * nlb + lb for lb in range(nlb) for j in range(4)]
    if need_ctx:
        order += [SEQ // 128 + ci for ci in range(CTX // 128)]
    NCH = NLOC // RSR
    issued = [0]
    chunk_keys = {k: [] for k in range(NCH)}

    def issue_upto(local_done):
        while issued[0] < NCH and (issued[0] + 1) * RSR <= local_done:
            k = issued[0]
            cx.coll(cckey, "ReduceScatter", ALU.add, GROUPS, rs_in[k], rs_out[k * RSR:(k + 1) * RSR, :], sorted(set(chunk_keys[k])))
            issued[0] += 1

    for n_, i in enumerate(order):
        s = n_ % 2
        S.dma(("sp", "actq")[s], mt[s][:], mixT[:, :, i * 128:(i + 1) * 128].rearrange("m p t -> p m t"), writes=[f"wmt{s}"])
        S.op("pool", lambda e, s=s: e.tensor_copy(mb[s][:], mt[s][:]), reads=[f"wmt{s}"], writes=[f"wmb{s}"])
        for hb_ in range(2):
            pi = (2 * n_ + hb_) % 8
            ps = cx.PS[pi]
            for m in range(4):
                S.op("pe", lambda e, ps=ps, m=m, hb_=hb_, s=s: e.matmul(ps[:, :], mb[s][:, m, :], wb[:, m, hb_ * 512:(hb_ + 1) * 512], start=(m == 0), stop=(m == 3)),
                     reads=[f"wmb{s}", "wwb"], writes=[f"ps{pi}"])
            S.op(("act", "dve")[hb_], lambda e, ps=ps, hb_=hb_, s=s: (e.copy if hasattr(e, "copy") else e.tensor_copy)(ot[s][:, hb_ * 512:(hb_ + 1) * 512], ps[:, :]),
                 reads=[f"ps{pi}"], writes=[f"wot{s}h{hb_}"])

        def put(j, loc, p0, n):
            while n > 0:
                k, q0 = loc // RSR, loc % RSR
                m_ = min(n, RSR - q0)
                key = f"rs_in{k}_{j}_{q0}"
                S.dma("poolq", rs_in[k][j * RSR + q0:j * RSR + q0 + m_, :], ot[s][p0:p0 + m_, :], reads=[f"wot{s}h0", f"wot{s}h1"], writes=[key])
                chunk_keys[k].append(key)
                loc += m_; p0 += m_; n -= m_
        if i < SEQ // 128:
            j, lb = i // nlb, i % nlb
            put(j, lb * 128, 0, 128)
            if j == 3 and need_ctx:
                issue_upto((lb + 1) * 128)
            elif j == 3:
                issue_upto((lb + 1) * 128 if lb < nlb - 1 else NLOC)
        else:
            ci = i - SEQ // 128
            for hf in range(2):
                put(ci * 2 + hf, TPC, hf * 64, 64)
            if ci == CTX // 128 - 1:
                issue_upto(NLOC)
    cx.end_phase()


SHARED = {"sel", "ident", "identF", "ropec", "ropes", "onesb", "E65", "ones64", "cT", "fin_g", "router_w", "router_b",
          "zT_l", "tn_l", "zT_c", "tn_c", "icnt_l", "icnt_c", "psel", "ndel", "selq", "selg", "xl", "xc"}


def build_fused(stop=None):
    cx = Ctx("F")
    cx.shared = set(SHARED)
    sc = cx.scratch
    T = {"ag_in": sc("ag_in", [NAGC, DIN, 64]), "ag_out": sc("ag_out", [NAGC, 4 * DIN, 64]),
         "aqT": sc("aqT", [2, 32, SEQ]), "akT": sc("akT", [2, 33, NKEY]), "av": sc("av", [NKEY, 65]), "aqcT": sc("aqcT", [2, 32, CTX]),
         "bqT": sc("bqT", [64, SEQ]), "bkT": sc("bkT", [65, NKEY]), "bv": sc("bv", [NKEY, 65]), "bqcT": sc("bqcT", [64, CTX]),
         "hy_l": sc("hy_l", [3, 64, SEQ + 2]), "pl_l": sc("pl_l", [64, SEQ + 24]),
         "hy_c": sc("hy_c", [3, 64, CTX + 2]), "pl_c": sc("pl_c", [64, CTX + 24]),
         "mixT": sc("mixT", [4, 64, NKEY]), "rs_in": sc("rs_in", [NLOC // RSR, 4 * RSR, D]), "rs_out": sc("rs_out", [NLOC, D]),
         "modrow_s": sc("modrow_s", [2, 6 * D]), "xl_s": sc("xl_s", [TPC, D]), "xc_s": sc("xc_s", [CPC, D]), "dummy": sc("dummy_oc", [CPC, D])}
    x_l = cx.inp("xl", [TPC, D]); x_c = cx.inp("xc", [CPC, D])
    out = cx.nc.dram_tensor("out", [TPC, D], F32, kind="ExternalOutput").ap()
    mixT = T["mixT"]

    def dump(src2d):
        r, c_ = src2d.shape
        dst = out.rearrange("a d -> (a d)")[0:r * c_].rearrange("(r c) -> r c", c=c_)
        cx.S.dma("sp", dst, src2d, writes=["dbg"])
        return cx.finish(), cx

    for li in range(2):
        need_ctx = li < 1
        cx.prefix = f"L{li}_"
        cx.over = {"xl": x_l if li == 0 else T["xl_s"], "xc": x_c if li == 0 else T["xc_s"], "ag_in": T["ag_in"], "ag_out": T["ag_out"], "modrow_s": T["modrow_s"]}
        emit_A(cx)
        if stop == "A":
            return dump(T["ag_in"][0:25, 0:DIN, :].rearrange("a b c -> a (b c)"))
        if stop == "AG":
            return dump(T["ag_out"][0:25, DIN:2 * DIN, :].rearrange("a b c -> a (b c)"))
        cx.over = {}
        emit_R(cx, need_ctx, T)
        if stop == "R":
            return dump(T["bkT"][:, :])
        cx.over = {k: T[k] for k in ("aqT", "akT", "av", "aqcT", "bqT", "bkT", "bv", "bqcT")}
        cx.over.update({"oa": mixT[0][:, 0:SEQ], "oac": mixT[0][:, SEQ:NKEY], "ob": mixT[1][:, 0:SEQ], "obc": mixT[1][:, SEQ:NKEY]})
        emit_B1(cx, li)
        if stop == "B1":
            return dump(mixT[1][:, :])
        cx.over = {"hy_l": T["hy_l"], "pl_l": T["pl_l"], "hy_c": T["hy_c"], "pl_c": T["pl_c"],
                   "op_l": mixT[2][:, 0:SEQ], "op_c": mixT[2][:, SEQ:NKEY], "oh_l": mixT[3][:, 0:SEQ], "oh_c": mixT[3][:, SEQ:NKEY]}
        emit_B2(cx, need_ctx)
        cx.over = {}
        if stop == "B2":
            return dump(mixT[3][:, :])
        emit_W(cx, need_ctx, T)
        if stop == "W":
            return dump(T["rs_in"][0:4, :, :].rearrange("a b c -> (a b) c"))
        if stop == "RS":
            return dump(T["rs_out"][0:TPC, :])
        cx.over = {"xl": x_l if li == 0 else T["xl_s"], "xc": x_c if li == 0 else T["xc_s"], "rs_out": T["rs_out"], "modrow_s": T["modrow_s"],
                   "ol": T["xl_s"] if li == 0 else out, "oc": T["xc_s"] if li == 0 else T["dummy"]}
        emit_C(cx, need_ctx, li == 1)
    return cx.finish(), cx


_CACHE = {}
STOP = None


def kernel(x, c, ctx, c_ctx, norm1_g, norm2_g, ada_w, ada_b, w_in, w_out, a_lambda, a_subln_g,
           b_rpb, pool_w, pool_scale, hy_short_w, hy_short_b, hy_f_w1, hy_f_b1, hy_f_w2, hy_f_b2,
           hy_f_w3, hy_f_b3, hy_skip, router_w, router_b, moe_w1, moe_w3, moe_w2, final_g):
    f = lambda a: np.ascontiguousarray(np.asarray(a, dtype=np.float32))
    if "F" not in _CACHE:
        _CACHE["F"] = build_fused(STOP)
    nc, cx = _CACHE["F"]
    x, ctx, c, c_ctx = f(x), f(ctx), f(c), f(c_ctx)
    rc, rs = const_rope()
    FC = fft_consts()
    deltas = np.abs(np.linspace(math.log(1e-2) / 1.5, math.log(1e-2) / 0.3, 256, dtype=np.float32))
    pos = {"l": hy_pos_consts(SEQ), "c": hy_pos_consts(CTX)}
    E65 = np.zeros((65, 64), np.float32); E65[64] = 1.0
    shared_all = {"sel": const_sel(), "ident": _bf16(np.eye(128)), "identF": np.eye(128, dtype=np.float32), "onesb": _bf16(np.ones((128, 128))),
                  "E65": E65, "ones64": np.ones((64, 64), np.float32), "fin_g": f(final_g).reshape(1, -1),
                  "router_w": f(router_w), "router_b": f(router_b).reshape(1, -1),
                  "zT_l": pos["l"][0], "tn_l": pos["l"][1], "zT_c": pos["c"][0], "tn_c": pos["c"][1]}
    shared_all.update({"c_" + k: v for k, v in FC.items()})
    in_maps = []
    for core in range(NCORE):
        b, j = core // 4, core % 4
        h = g = j
        chs = slice(g * 64, (g + 1) * 64)
        m = dict(shared_all)
        m["xl"] = np.ascontiguousarray(x[b, j * TPC:(j + 1) * TPC]); m["xc"] = np.ascontiguousarray(ctx[b, j * CPC:(j + 1) * CPC])
        m["cT"] = cT_layout(c[b], c_ctx)
        m["ropec"] = np.ascontiguousarray(rc[j * TPC:(j + 1) * TPC]); m["ropes"] = np.ascontiguousarray(rs[j * TPC:(j + 1) * TPC])
        selq = np.zeros((256, 2, 32), np.float32); selg = np.zeros((256, 64), np.float32)
        for c_ in range(2):
            selq[h * 64 + c_ * 32 + np.arange(32), c_, np.arange(32)] = 1.0
        selg[h * 64 + np.arange(64), np.arange(64)] = 1.0
        m["selq"] = np.ascontiguousarray(selq.reshape(2, 128, 2, 32).transpose(1, 0, 2, 3))
        m["selg"] = np.ascontiguousarray(selg.reshape(2, 128, 64).transpose(1, 0, 2))
        for tag, Le in (("l", SEQ), ("c", CTX)):
            t = np.arange(Le); w = POOL_SIZES[g]
            cnt = (np.clip(t + w // 2, 0, Le) - np.clip(t - w // 2, 0, Le)).astype(np.float32)
            m[f"icnt_{tag}"] = np.ascontiguousarray(np.broadcast_to((1.0 / cnt)[None, :], (64, Le))).astype(np.float32)
        ps = np.zeros((64, 4), np.float32); ps[:, g] = 1.0
        m["psel"] = ps
        m["ndel"] = np.ascontiguousarray(np.tile(-deltas[chs], 2).reshape(128, 1))
        for li in range(2):
            p = f"L{li}_"
            m[p + "ada_w"] = f(ada_w[li]); m[p + "ada_b"] = f(ada_b[li]).reshape(1, -1)
            m[p + "norm_g"] = f(norm1_g[li]).reshape(1, -1); m[p + "norm2_g"] = f(norm2_g[li]).reshape(1, -1)
            m[p + "w_in"] = f(w_in[li])
            m[p + "alam"] = f(a_lambda[li]).reshape(1, 128); m[p + "subg"] = f(a_subln_g[li]).reshape(64, 1)
            m[p + "bias"] = na_bias_sets(f(b_rpb[li])[h])
            sw = f(hy_short_w[li]).reshape(3, 3, 256)[:, :, chs]
            m[p + "shw"] = np.ascontiguousarray(sw.transpose(2, 1, 0)); m[p + "shb"] = np.ascontiguousarray(f(hy_short_b[li]).reshape(3, 256)[:, chs].T)
            m[p + "fw1"] = f(hy_f_w1[li]); m[p + "fb1"] = f(hy_f_b1[li]).reshape(64, 1)
            m[p + "fw2"] = f(hy_f_w2[li]); m[p + "fb2"] = f(hy_f_b2[li]).reshape(64, 1)
            w3 = f(hy_f_w3[li]).reshape(64, 2, 2, 256)[:, :, :, chs]
            m[p + "fw3"] = np.ascontiguousarray(w3.reshape(64, 256))
            b3 = f(hy_f_b3[li]).reshape(2, 2, 256)[:, :, chs]
            m[p + "fb3"] = np.ascontiguousarray(b3.reshape(2, 128).T)
            m[p + "dsk"] = np.ascontiguousarray(np.broadcast_to(f(hy_skip[li])[:, chs][None], (64, 2, 64))).astype(np.float32)
            m[p + "pw"] = np.ascontiguousarray(f(pool_w[li])[g]); m[p + "psc"] = np.ascontiguousarray(f(pool_scale[li])[chs].reshape(64, 1))
            wo = f(w_out[li])
            m[p + "wo_part"] = np.ascontiguousarray(np.stack([wo[mm * 256 + h * 64:mm * 256 + (h + 1) * 64] for mm in range(4)]))
            m[p + "w1"] = f(moe_w1[li]); m[p + "w3"] = f(moe_w3[li]); m[p + "w2"] = f(moe_w2[li])
        in_maps.append({k: v for k, v in m.items() if k in cx.ins})
    missing = [k for k in cx.ins if k not in in_maps[0]]
    assert not missing, missing
    res = run_bass_kernel_spmd(nc, in_maps, core_ids=list(range(NCORE)))
    out = np.stack([np.concatenate([res.results[b * 4 + j]["out"] for j in range(4)], 0) for b in range(2)])
    return out.astype(np.float32)
```
